# Optimizing a Trainium2 kernel written in Bass

```python
import jax, jax.numpy as jnp
from jax import lax
import numpy as np

D_MODEL = 1024
BATCH = 2
SEQ = 8192
DEPTH = 4

CHUNK = 64
N_META = 16
N_PAD = CHUNK - N_META
N_MIXERS = 2
RMS_EPS = 1e-6
L2_EPS = 1e-6

GDN_QK_HEADS = 8
GDN_V_HEADS = 16
GDN_HEAD_DIM = 128
GDN_CONV = 4
GDN_KEY_DIM = GDN_QK_HEADS * GDN_HEAD_DIM
GDN_VAL_DIM = GDN_V_HEADS * GDN_HEAD_DIM
GDN_QKV_DIM = 2 * GDN_KEY_DIM + GDN_VAL_DIM
GDN_IN = GDN_QKV_DIM + GDN_VAL_DIM + 2 * GDN_V_HEADS

ML_HEADS = 4
ML_QK_DIM = 128
ML_V_DIM = 256
ML_GATE_CAP = 15.0
ML_QK_TOT = ML_HEADS * ML_QK_DIM
ML_V_TOT = ML_HEADS * ML_V_DIM
ML_IN = 2 * ML_QK_TOT + 2 * ML_V_TOT + 2 * ML_HEADS

FFN_DIM = 2816
FFN_CONV = 3

N_GDN_LAYERS = (DEPTH + 1) // 2
N_ML_LAYERS = DEPTH // 2

kernel_name = "hybrid_gdn_mlstm_convffn_trunk"


def rms_norm(x, w):
    xf = x.astype(jnp.float32)
    y = xf * lax.rsqrt(jnp.mean(xf * xf, -1, keepdims=True) + RMS_EPS)
    return (y * w.astype(jnp.float32)).astype(x.dtype)


def l2_normalize(x):
    return x * lax.rsqrt(jnp.sum(x * x, -1, keepdims=True) + L2_EPS)


def causal_depthwise_conv(x, w):
    K, C = w.shape
    return lax.conv_general_dilated(x, w[:, None, :].astype(x.dtype), window_strides=(1,),
                                    padding=[(K - 1, 0)], dimension_numbers=('NWC', 'WIO', 'NWC'),
                                    feature_group_count=C)


def to_chunks(t):
    B, L, H = t.shape[:3]
    t = t.reshape((B, L // CHUNK, CHUNK, H) + t.shape[3:])
    return jnp.moveaxis(t, 3, 1)


def from_chunks(t):
    B, H, nc, C, d = t.shape
    return jnp.moveaxis(t, 1, 3).reshape(B, nc * C, H, d)


def gated_deltanet(h, valid, w_in, conv_w, a_log, dt_bias, norm_w, w_out):
    f32 = jnp.float32
    B, L, _ = h.shape
    proj = h @ w_in
    o1 = GDN_QKV_DIM
    o2 = o1 + GDN_VAL_DIM
    o3 = o2 + GDN_V_HEADS
    qkv = jax.nn.silu(causal_depthwise_conv(proj[..., :o1], conv_w)).astype(f32)
    z = proj[..., o1:o2].astype(f32).reshape(B, L, GDN_V_HEADS, GDN_HEAD_DIM)
    beta = jax.nn.sigmoid(proj[..., o2:o3].astype(f32))
    g = -jnp.exp(a_log.astype(f32)) * jax.nn.softplus(proj[..., o3:].astype(f32) + dt_bias.astype(f32))

    rep = GDN_V_HEADS // GDN_QK_HEADS
    mask = valid[None, :, None, None]
    q = qkv[..., :GDN_KEY_DIM].reshape(B, L, GDN_QK_HEADS, GDN_HEAD_DIM)
    k = qkv[..., GDN_KEY_DIM:2 * GDN_KEY_DIM].reshape(B, L, GDN_QK_HEADS, GDN_HEAD_DIM)
    v = qkv[..., 2 * GDN_KEY_DIM:].reshape(B, L, GDN_V_HEADS, GDN_HEAD_DIM) * mask
    q = jnp.repeat(l2_normalize(q), rep, axis=2) * (GDN_HEAD_DIM ** -0.5)
    k = jnp.repeat(l2_normalize(k), rep, axis=2) * mask

    qc, kc, vc = to_chunks(q), to_chunks(k), to_chunks(v)
    bc, gc = to_chunks(beta), to_chunks(g)
    G = jnp.cumsum(gc, -1)
    incl = jnp.tri(CHUNK, dtype=bool)
    strict = jnp.tri(CHUNK, k=-1, dtype=f32)
    decay = jnp.exp(jnp.where(incl, G[..., :, None] - G[..., None, :], -jnp.inf))
    kb = kc * bc[..., None]
    a_mat = jnp.eye(CHUNK, dtype=f32) + jnp.einsum('bhnid,bhnjd->bhnij', kb, kc) * decay * strict
    rhs = jnp.concatenate([vc * bc[..., None], kb * jnp.exp(G)[..., None]], -1)
    sol = lax.linalg.triangular_solve(a_mat, rhs, left_side=True, lower=True)
    u, w = sol[..., :GDN_HEAD_DIM], sol[..., GDN_HEAD_DIM:]
    attn = jnp.einsum('bhnid,bhnjd->bhnij', qc, kc) * decay
    qd = qc * jnp.exp(G)[..., None]
    kt = kc * jnp.exp(G[..., -1:] - G)[..., None]
    gt = jnp.exp(G[..., -1])

    def step(S, xs):
        u_c, w_c, qd_c, att_c, kt_c, gt_c = xs
        v_new = u_c - jnp.einsum('bhck,bhkv->bhcv', w_c, S)
        o_c = jnp.einsum('bhck,bhkv->bhcv', qd_c, S) + jnp.einsum('bhij,bhjv->bhiv', att_c, v_new)
        S = S * gt_c[..., None, None] + jnp.einsum('bhck,bhcv->bhkv', kt_c, v_new)
        return S, o_c

    xs = tuple(jnp.moveaxis(t, 2, 0) for t in (u, w, qd, attn, kt, gt))
    S0 = jnp.zeros((B, GDN_V_HEADS, GDN_HEAD_DIM, GDN_HEAD_DIM), f32)
    _, o = lax.scan(step, S0, xs)
    o = from_chunks(jnp.moveaxis(o, 0, 2))
    o = rms_norm(o, norm_w) * jax.nn.silu(z)
    return o.reshape(B, L, GDN_VAL_DIM).astype(h.dtype) @ w_out


def mlstm(h, valid, w_in, gate_b, norm_w, w_out):
    f32 = jnp.float32
    B, L, _ = h.shape
    proj = h @ w_in
    o1 = ML_QK_TOT
    o2 = 2 * ML_QK_TOT
    o3 = o2 + ML_V_TOT
    o4 = o3 + ML_V_TOT
    mask = valid[None, :, None, None]
    q = proj[..., :o1].astype(f32).reshape(B, L, ML_HEADS, ML_QK_DIM) * (ML_QK_DIM ** -0.5)
    k = proj[..., o1:o2].astype(f32).reshape(B, L, ML_HEADS, ML_QK_DIM) * mask
    v = proj[..., o2:o3].astype(f32).reshape(B, L, ML_HEADS, ML_V_DIM) * mask
    o_gate = jax.nn.sigmoid(proj[..., o3:o4].astype(f32)).reshape(B, L, ML_HEADS, ML_V_DIM)
    gates = proj[..., o4:].astype(f32) + gate_b.astype(f32)
    gates = ML_GATE_CAP * jnp.tanh(gates / ML_GATE_CAP)
    log_i = gates[..., :ML_HEADS]
    log_f = jax.nn.log_sigmoid(gates[..., ML_HEADS:])

    qc, kc, vc = to_chunks(q), to_chunks(k), to_chunks(v)
    lic, lfc = to_chunks(log_i), to_chunks(log_f)
    b = jnp.cumsum(lfc, -1)
    b_last = b[..., -1]
    a = b_last[..., None] - b + lic

    def step(carry, xs):
        Cs, ns, ms = carry
        k_c, v_c, a_c, bl_c = xs
        m_new = jnp.maximum(bl_c + ms, jnp.max(a_c, -1))
        dec = jnp.exp(bl_c + ms - m_new)
        kw = k_c * jnp.exp(a_c - m_new[..., None])[..., None]
        C_new = dec[..., None, None] * Cs + jnp.einsum('bhck,bhcv->bhkv', kw, v_c)
        n_new = dec[..., None] * ns + jnp.sum(kw, -2)
        return (C_new, n_new, m_new), (Cs, ns, ms)

    xs = tuple(jnp.moveaxis(t, 2, 0) for t in (kc, vc, a, b_last))
    init = (jnp.zeros((B, ML_HEADS, ML_QK_DIM, ML_V_DIM), f32),
            jnp.zeros((B, ML_HEADS, ML_QK_DIM), f32),
            jnp.zeros((B, ML_HEADS), f32))
    _, (C_st, n_st, m_st) = lax.scan(step, init, xs)
    C_st = jnp.moveaxis(C_st, 0, 2)
    n_st = jnp.moveaxis(n_st, 0, 2)
    m_st = jnp.moveaxis(m_st, 0, 2)

    incl = jnp.tri(CHUNK, dtype=bool)
    D = jnp.where(incl, b[..., :, None] - b[..., None, :] + lic[..., None, :], -jnp.inf)
    inter = b + m_st[..., None]
    m_t = jnp.maximum(jnp.max(D, -1), inter)
    wD = jnp.exp(D - m_t[..., None]) * jnp.einsum('bhnid,bhnjd->bhnij', qc, kc)
    sc = jnp.exp(inter - m_t)
    num = sc[..., None] * jnp.einsum('bhnck,bhnkv->bhncv', qc, C_st) + jnp.einsum('bhnij,bhnjv->bhniv', wD, vc)
    den = sc * jnp.einsum('bhnck,bhnk->bhnc', qc, n_st) + jnp.sum(wD, -1)
    hh = num / jnp.maximum(jnp.abs(den), jnp.exp(-m_t))[..., None]
    hh = from_chunks(hh)
    hh = rms_norm(hh, norm_w.reshape(ML_HEADS, ML_V_DIM)) * o_gate
    return hh.reshape(B, L, ML_V_TOT).astype(h.dtype) @ w_out


def conv_ffn(h, w_up, conv_w, conv_b, w_down):
    u = causal_depthwise_conv(h @ w_up, conv_w) + conv_b
    gate, up = u[..., :FFN_DIM], u[..., FFN_DIM:]
    return (jax.nn.silu(gate) * up) @ w_down


def setup_inputs(seed: int = 0) -> dict:
    key = jax.random.key(seed)
    ks = jax.random.split(key, 20)
    f32 = jnp.float32
    nrm = lambda k, shape, s: jax.random.normal(k, shape, f32) * s
    x = nrm(ks[0], (BATCH, SEQ, D_MODEL), 1.0)
    meta_tokens = nrm(ks[1], (N_META, D_MODEL), 1.0)
    norm_w = 1.0 + nrm(ks[2], (DEPTH, 4, D_MODEL), 0.05)
    gdn_w_in = nrm(ks[3], (N_GDN_LAYERS, D_MODEL, GDN_IN), D_MODEL ** -0.5)
    gdn_conv_w = nrm(ks[4], (N_GDN_LAYERS, GDN_CONV, GDN_QKV_DIM), GDN_CONV ** -0.5)
    gdn_a_log = jnp.log(jax.random.uniform(ks[5], (N_GDN_LAYERS, GDN_V_HEADS), f32, 1.0, 16.0))
    dt = jnp.exp(jax.random.uniform(ks[6], (N_GDN_LAYERS, GDN_V_HEADS), f32, np.log(1e-3), np.log(1e-1)))
    gdn_dt_bias = dt + jnp.log(-jnp.expm1(-dt))
    gdn_norm_w = 1.0 + nrm(ks[7], (N_GDN_LAYERS, GDN_HEAD_DIM), 0.05)
    gdn_w_out = nrm(ks[8], (N_GDN_LAYERS, GDN_VAL_DIM, D_MODEL), GDN_VAL_DIM ** -0.5)
    ml_w_in = nrm(ks[9], (N_ML_LAYERS, D_MODEL, ML_IN), D_MODEL ** -0.5)
    ig_b = nrm(ks[10], (N_ML_LAYERS, ML_HEADS), 0.1)
    fg_b = jnp.linspace(3.0, 6.0, ML_HEADS, dtype=f32)[None] + nrm(ks[11], (N_ML_LAYERS, ML_HEADS), 0.1)
    ml_gate_b = jnp.concatenate([ig_b, fg_b], -1)
    ml_norm_w = 1.0 + nrm(ks[12], (N_ML_LAYERS, ML_V_TOT), 0.05)
    ml_w_out = nrm(ks[13], (N_ML_LAYERS, ML_V_TOT, D_MODEL), ML_V_TOT ** -0.5)
    ffn_w_up = nrm(ks[14], (DEPTH, D_MODEL, 2 * FFN_DIM), D_MODEL ** -0.5)
    ffn_conv_w = nrm(ks[15], (DEPTH, FFN_CONV, 2 * FFN_DIM), FFN_CONV ** -0.5)
    ffn_conv_b = nrm(ks[16], (DEPTH, 2 * FFN_DIM), 0.01)
    ffn_w_down = nrm(ks[17], (DEPTH, FFN_DIM, D_MODEL), FFN_DIM ** -0.5)
    return {"x": x, "meta_tokens": meta_tokens, "norm_w": norm_w,
            "gdn_w_in": gdn_w_in, "gdn_conv_w": gdn_conv_w, "gdn_a_log": gdn_a_log,
            "gdn_dt_bias": gdn_dt_bias, "gdn_norm_w": gdn_norm_w, "gdn_w_out": gdn_w_out,
            "ml_w_in": ml_w_in, "ml_gate_b": ml_gate_b, "ml_norm_w": ml_norm_w, "ml_w_out": ml_w_out,
            "ffn_w_up": ffn_w_up, "ffn_conv_w": ffn_conv_w, "ffn_conv_b": ffn_conv_b, "ffn_w_down": ffn_w_down}


def reference(x, meta_tokens, norm_w, gdn_w_in, gdn_conv_w, gdn_a_log, gdn_dt_bias, gdn_norm_w, gdn_w_out,
              ml_w_in, ml_gate_b, ml_norm_w, ml_w_out, ffn_w_up, ffn_conv_w, ffn_conv_b, ffn_w_down):
    B, S, D = x.shape
    pad = jnp.zeros((B, N_PAD, D), x.dtype)
    meta = jnp.broadcast_to(meta_tokens[None].astype(x.dtype), (B, N_META, D))
    hs = jnp.concatenate([pad, meta, x], 1)
    L = hs.shape[1]
    valid = (jnp.arange(L) >= N_PAD).astype(jnp.float32)
    keep = valid.astype(x.dtype)[None, :, None]
    for i in range(DEPTH):
        j = i // N_MIXERS
        a_in = rms_norm(hs, norm_w[i, 0])
        if i % N_MIXERS == 0:
            mix = gated_deltanet(a_in, valid, gdn_w_in[j], gdn_conv_w[j], gdn_a_log[j], gdn_dt_bias[j],
                                 gdn_norm_w[j], gdn_w_out[j])
        else:
            mix = mlstm(a_in, valid, ml_w_in[j], ml_gate_b[j], ml_norm_w[j], ml_w_out[j])
        hs = hs + keep * rms_norm(mix, norm_w[i, 1])
        f = conv_ffn(rms_norm(hs, norm_w[i, 2]), ffn_w_up[i], ffn_conv_w[i], ffn_conv_b[i], ffn_w_down[i])
        hs = hs + keep * rms_norm(f, norm_w[i, 3])
    return hs[:, N_PAD + N_META:]
```

```python
from contextlib import ExitStack
import numpy as np
import ml_dtypes
import concourse.bass as bass
import concourse.mybir as mybir
from concourse.bass_utils import run_bass_kernel_spmd

F32 = mybir.dt.float32
BF16 = mybir.dt.bfloat16
AF = mybir.ActivationFunctionType
ALU = mybir.AluOpType
AX = mybir.AxisListType

D = 1024
SEQ = 8192
NB = 2
LP = 8704
XPAD = LP - SEQ - 64
WIN = 64 + 2048
FFN = 2816
NG = FFN // 128
RMS_EPS = 1e-6
CH = 64
NCH = LP // CH
TS = 512
NTS = LP // TS
CPT = TS // CH


class Buf:
    __slots__ = ("t", "lw", "rd", "sem", "semv", "name")

    def __init__(self, t, name=""):
        self.t = t
        self.lw = None
        self.rd = {}
        self.sem = None
        self.semv = 0
        self.name = name

    def __getitem__(self, k):
        return self.t[k]


class Sched:
    def __init__(self, nc, es):
        self.nc = nc
        self.es = es
        self.eng = {"pe": nc.tensor, "act": nc.scalar, "dve": nc.vector, "pool": nc.gpsimd, "sp": nc.sync}
        self.sem = {k: es.enter_context(nc.semaphore("sem_" + k)) for k in self.eng}
        self.cnt = {k: 0 for k in self.eng}
        self.seen = {k: {} for k in self.eng}
        self.nsem = 0
        self.out_events = []
        self.ninst = 0
        self.scopes = []
        self.dsems = []
        self.free_dsems = []
        self.scope_bufs = []

    def push(self, tag):
        self.scopes.append((ExitStack(), tag))
        self.scope_bufs.append([])

    def _own_sem(self, own):
        if own.sem is None:
            if self.free_dsems:
                own.sem, own.semv = self.free_dsems.pop()
            else:
                own.sem = self.es.enter_context(self.nc.semaphore("dsem%d" % self.nsem))
                self.nsem += 1
            self.dsems.append(own)

    def pop(self):
        self.barrier()
        st, _ = self.scopes.pop()
        st.close()
        for b in self.scope_bufs.pop():
            if b.sem is not None:
                self.free_dsems.append((b.sem, b.semv))
                self.dsems.remove(b)
                b.sem = None

    def _scope(self):
        return self.scopes[-1] if self.scopes else (self.es, "g")

    def sb(self, name, shape, dt):
        st, tag = self._scope()
        name = tag + "_" + name
        b = Buf(st.enter_context(self.nc.sbuf_tensor(name, list(shape), dt)), name)
        if self.scope_bufs:
            self.scope_bufs[-1].append(b)
        return b

    def ps(self, name, shape, dt=F32):
        st, tag = self._scope()
        name = tag + "_" + name
        return Buf(st.enter_context(self.nc.psum_tensor(name, list(shape), dt)), name)

    def barrier(self):
        for e in self.eng:
            eng = self.eng[e]
            for k in ("pe", "act", "dve", "pool", "sp"):
                if k != e and self.cnt[k] and self.seen[e].get(k, 0) < self.cnt[k]:
                    eng.wait_ge(self.sem[k], self.cnt[k])
                    self.seen[e][k] = self.cnt[k]
            for b in self.dsems:
                key = "d_" + b.name
                if self.seen[e].get(key, 0) < b.semv:
                    eng.wait_ge(b.sem, b.semv)
                    self.seen[e][key] = b.semv

    def dram(self, name, shape, dt, kind="Internal"):
        t = self.nc.dram_tensor(name, list(shape), dt, kind=kind)
        return Buf(t.ap(), name)

    def _deps(self, reads, writes):
        deps = {}

        def add(ev):
            if ev is None:
                return
            sem, val, key = ev
            if key not in deps or deps[key][1] < val:
                deps[key] = (sem, val)

        for b in reads:
            add(b.lw)
        for b in writes:
            add(b.lw)
            for ev in b.rd.values():
                add(ev)
        return deps

    def _wait(self, e, deps):
        eng = self.eng[e]
        for key, (sem, val) in deps.items():
            if e == "pe" and key == "pe":
                continue
            if self.seen[e].get(key, 0) >= val:
                continue
            eng.wait_ge(sem, val)
            self.seen[e][key] = val

    def _record(self, ev, reads, writes):
        for b in writes:
            b.lw = ev
            b.rd = {}
        for b in reads:
            if b not in writes:
                b.rd[ev[2]] = ev

    def op(self, e, fn, reads=(), writes=()):
        self._wait(e, self._deps(reads, writes))
        ins = fn(self.eng[e])
        self.cnt[e] += 1
        ins.then_inc(self.sem[e], 1)
        self.ninst += 1
        self._record((self.sem[e], self.cnt[e], e), reads, writes)

    def dma(self, q, out, in_, reads=(), writes=(), owner=None):
        self._wait(q, self._deps(reads, writes))
        own = owner if owner is not None else (writes[0] if writes else reads[0])
        self._own_sem(own)
        own.semv += 16
        ins = self.eng[q].dma_start(out=out, in_=in_)
        ins.then_inc(own.sem, 16)
        self.ninst += 1
        ev = (own.sem, own.semv, "d_" + own.name)
        self._record(ev, reads, writes)
        return ev

    def gather(self, out_ap, table_ap, idx_ap, reads=(), writes=()):
        self._wait("pool", self._deps(reads, writes))
        own = writes[0]
        self._own_sem(own)
        own.semv += 16
        ins = self.nc.gpsimd.indirect_dma_start(out=out_ap, out_offset=None, in_=table_ap,
                                                in_offset=bass.IndirectOffsetOnAxis(ap=idx_ap, axis=0))
        ins.then_inc(own.sem, 16)
        self.ninst += 1
        self._record((own.sem, own.semv, "d_" + own.name), reads, writes)

    def coll(self, kind, out, in_, groups, extra=()):
        self._wait("pool", self._deps([in_] + list(extra), [out]))
        self._own_sem(out)
        out.semv += 1
        ins = self.nc.gpsimd.collective_compute(kind, ALU.bypass, replica_groups=groups, ins=[in_.t.opt()], outs=[out.t.opt()])
        ins.then_inc(out.sem, 1)
        self.ninst += 1
        self._record((out.sem, out.semv, "d_" + out.name), [in_], [out])

    def finish(self, bufs):
        for b in bufs:
            if b.sem is not None:
                self.eng["sp"].wait_ge(b.sem, b.semv)
        for k in ("pe", "act", "dve", "pool"):
            if self.cnt[k]:
                self.eng["sp"].wait_ge(self.sem[k], self.cnt[k])


class PsumPool:
    def __init__(self, S, n, prefix="pb"):
        self.banks = [S.ps("%s%d" % (prefix, i), [128, 512]) for i in range(n)]
        self.i = 0

    def get(self):
        b = self.banks[self.i % len(self.banks)]
        self.i += 1
        return b


def bcast_mid(ap2, n):
    return ap2.unsqueeze(1).broadcast_to([ap2.shape[0], n, ap2.shape[1]])


def bcast_last(ap2, n):
    return ap2.unsqueeze(2).broadcast_to([ap2.shape[0], ap2.shape[1], n])


def rstd_from_ss(S, ss_ps, out_sb, scale, eps, W):
    S.op("act", lambda e: e.activation(out=out_sb[:, :W], in_=ss_ps[:, :W], func=AF.Ln, scale=scale, bias=eps),
         reads=[ss_ps], writes=[out_sb])
    S.op("act", lambda e: e.activation(out=out_sb[:, :W], in_=out_sb[:, :W], func=AF.Exp, scale=-0.5),
         reads=[out_sb], writes=[out_sb])


T_TILES = [(0, 64), (64, 512), (576, 512), (1088, 512), (1600, 512)]


def emit_casts(S, io):
    for m in range(8):
        S.dma("pool", io["wout_b"][m], io["wout_d"][m], writes=[io["wout_b"]])
    for g in range(NG):
        S.dma("pool", io["wup_b"][g], io["wup_d"][g], writes=[io["wup_b"]])
    for m in range(8):
        S.dma("pool", io["wdn_b"][m], io["wdn_d"][m], writes=[io["wdn_b"]])


def emit_T(S, tag, KO, first, last, io):
    S.push(tag)
    KC = KO // 128
    c_og = KC // 4
    hs_in = io["hs_src"]
    nwT_d = io["nwT"]
    if not last:
        ainF, ainH = io["ainF"], io["ainH"]
    if not first:
        ogF_g, ogH_g = io["ogF_g"], io["ogH_g"]
        keep_d, cw_d, cb_d = io["keep"], io["cw"], io["cb"]
        wout_b, wup_b, wdn_b = io["wout_b"], io["wup_b"], io["wdn_b"]
        hs_out = io["hs_dst"]
        gidx = S.sb("gidx", [128, 20], mybir.dt.int32)
        S.dma("sp", gidx[:], io["gidx"][:, :], writes=[gidx])

    ones_f = S.sb("ones_f", [128, 128], F32)
    ones_b = S.sb("ones_b", [128, 128], BF16)
    S.op("pool", lambda e: e.memset(ones_f[:], 1.0), writes=[ones_f])
    S.op("act", lambda e: e.activation(out=ones_b[:], in_=ones_f[:], func=AF.Copy), reads=[ones_f], writes=[ones_b])
    nwT = S.sb("nwT_sb", [128, 4, 8], F32)
    S.dma("sp", nwT[:].rearrange("p a b -> p (a b)"), nwT_d[:, :], writes=[nwT])

    hs_sb = [S.sb("hs_sb%d" % i, [128, 8, 512], F32) for i in range(2)]
    sq_sb = [S.sb("sq_sb%d" % i, [128, 512], BF16) for i in range(2)]
    rstd = S.sb("rstd", [128, 512], F32)
    a_sb = S.sb("a_sb", [128, 8, 512], BF16)
    pp = PsumPool(S, 7)
    ss_ps = S.ps("ss_ps", [128, 512])
    if not first:
        og_sb = [S.sb("og_sb%d" % i, [128, KC, 512], BF16) for i in range(2)]
        ogh_sb = S.sb("ogh_sb", [128, KC, 64], BF16)
        keep_sb = S.sb("keep_sb", [128, WIN], F32)
        S.dma("sp", keep_sb[:], keep_d[0:1, :].partition_broadcast(128), writes=[keep_sb])
        cw = S.sb("cw_sb", [128, 44, 3], F32)
        cb = S.sb("cb_sb", [128, 44], F32)
        S.dma("sp", cw[:].rearrange("p a b -> p (a b)"), cw_d[:, :], writes=[cw])
        S.dma("sp", cb[:], cb_d[:, :], writes=[cb])
        mix_sb = S.sb("mix_sb", [128, 8, 512], F32)
        rk = S.sb("rk", [128, 512], F32)
        tmp_sb = [S.sb("tmp_sb%d" % i, [128, 512], F32) for i in range(2)]
        h_sb = S.sb("h_sb", [128, NG, 512], BF16)
        u_sb = [S.sb("u_sb%d" % i, [128, 2, 2 + 512], F32) for i in range(2)]
        y_sb = [S.sb("y_sb%d" % i, [128, 2, 512], F32) for i in range(2)]
        e_sb = [S.sb("e_sb%d" % i, [128, 512], F32) for i in range(2)]
        halo = S.sb("halo", [128, 44, 2], F32)
        S.op("pool", lambda e: e.memset(halo[:], 0.0), writes=[halo])
        wo_s = [S.sb("wo_s%d" % i, [128, KC, 128], BF16) for i in range(2)]
        wu_s = [S.sb("wu_s%d" % i, [128, 8, 256], BF16) for i in range(3)]
        wd_s = [S.sb("wd_s%d" % i, [128, NG, 128], BF16) for i in range(2)]

    def norm_ss(src, W, eng_sq="act"):
        for m in range(8):
            sq = sq_sb[m % 2]
            S.op(eng_sq, lambda e: e.activation(out=sq[:, :W], in_=src[:, m, :W], func=AF.Square),
                 reads=[src], writes=[sq])
            S.op("pe", lambda e: e.matmul(ss_ps[:, :W], ones_b[:], sq[:, :W], start=(m == 0), stop=(m == 7)),
                 reads=[ones_b, sq], writes=[ss_ps])

    def load_tile(i):
        t0, W = T_TILES[i]
        hb = hs_sb[i % 2]
        S.dma("sp", hb[:, :, :W], hs_in[:, t0:t0 + W].rearrange("(c p) t -> p c t", p=128), writes=[hb])
        if not first:
            ob = ogh_sb if i == 0 else og_sb[i % 2]
            tab = ogH_g if i == 0 else ogF_g
            for hg in range(4):
                S.gather(ob[:, hg * c_og:(hg + 1) * c_og, :].rearrange("p c w -> p (c w)"), tab[:, :],
                         gidx[:, hg * 5 + i:hg * 5 + i + 1], reads=[gidx], writes=[ob])

    load_tile(0)
    for ti, (t0, W) in enumerate(T_TILES):
        if ti + 1 < len(T_TILES):
            load_tile(ti + 1)
        hb = hs_sb[ti % 2]
        if not first:
            ob = ogh_sb if ti == 0 else og_sb[ti % 2]
            S.dma("sp", wo_s[0][:].rearrange("p a b -> p (a b)"), wout_b[0], reads=[wout_b], writes=[wo_s[0]])
            for m in range(8):
                if m + 1 < 8:
                    S.dma("sp", wo_s[(m + 1) % 2][:].rearrange("p a b -> p (a b)"), wout_b[m + 1],
                          reads=[wout_b], writes=[wo_s[(m + 1) % 2]])
                ws = wo_s[m % 2]
                pb = pp.get()
                for kc in range(KC):
                    S.op("pe", lambda e: e.matmul(pb[:, :W], ws[:, kc, :], ob[:, kc, :W], start=(kc == 0), stop=(kc == KC - 1)),
                         reads=[ws, ob], writes=[pb])
                S.op("act", lambda e: e.activation(out=mix_sb[:, m, :W], in_=pb[:, :W], func=AF.Copy), reads=[pb], writes=[mix_sb])
                sq = sq_sb[m % 2]
                S.op("act", lambda e: e.activation(out=sq[:, :W], in_=pb[:, :W], func=AF.Square), reads=[pb], writes=[sq])
                S.op("pe", lambda e: e.matmul(ss_ps[:, :W], ones_b[:], sq[:, :W], start=(m == 0), stop=(m == 7)),
                     reads=[ones_b, sq], writes=[ss_ps])
            rstd_from_ss(S, ss_ps, rstd, 1.0 / D, RMS_EPS, W)
            S.op("dve", lambda e: e.tensor_tensor(out=rk[:, :W], in0=rstd[:, :W], in1=keep_sb[:, t0:t0 + W], op=ALU.mult),
                 reads=[rstd, keep_sb], writes=[rk])
            for m in range(8):
                tb = tmp_sb[m % 2]
                S.op("dve", lambda e: e.scalar_tensor_tensor(out=tb[:, :W], in0=mix_sb[:, m, :W], scalar=nwT[:, 1, m:m + 1],
                                                             in1=rk[:, :W], op0=ALU.mult, op1=ALU.mult),
                     reads=[mix_sb, nwT, rk], writes=[tb])
                S.op("pool", lambda e: e.tensor_tensor(out=hb[:, m, :W], in0=hb[:, m, :W], in1=tb[:, :W], op=ALU.add),
                     reads=[hb, tb], writes=[hb])
            norm_ss(hb, W)
            rstd_from_ss(S, ss_ps, rstd, 1.0 / D, RMS_EPS, W)
            for m in range(8):
                S.op("dve", lambda e: e.scalar_tensor_tensor(out=a_sb[:, m, :W], in0=hb[:, m, :W], scalar=nwT[:, 2, m:m + 1],
                                                             in1=rstd[:, :W], op0=ALU.mult, op1=ALU.mult),
                     reads=[hb, nwT, rstd], writes=[a_sb])
            S.dma("sp", wu_s[0][:].rearrange("p a b -> p (a b)"), wup_b[0], reads=[wup_b], writes=[wu_s[0]])
            S.dma("sp", wu_s[1][:].rearrange("p a b -> p (a b)"), wup_b[1], reads=[wup_b], writes=[wu_s[1]])
            for g in range(NG):
                if g + 2 < NG:
                    S.dma("sp", wu_s[(g + 2) % 3][:].rearrange("p a b -> p (a b)"), wup_b[g + 2],
                          reads=[wup_b], writes=[wu_s[(g + 2) % 3]])
                ws = wu_s[g % 3]
                ub = u_sb[g % 2]
                yb = y_sb[g % 2]
                eb = e_sb[g % 2]
                for hf in range(2):
                    ci = g + hf * NG
                    pb = pp.get()
                    for kc in range(8):
                        S.op("pe", lambda e: e.matmul(pb[:, :W], ws[:, kc, hf * 128:(hf + 1) * 128], a_sb[:, kc, :W],
                                                      start=(kc == 0), stop=(kc == 7)),
                             reads=[ws, a_sb], writes=[pb])
                    S.op("pool", lambda e: e.tensor_copy(out=ub[:, hf, 0:2], in_=halo[:, ci, :]), reads=[halo], writes=[ub])
                    S.op("act", lambda e: e.activation(out=ub[:, hf, 2:2 + W], in_=pb[:, :W], func=AF.Copy), reads=[pb], writes=[ub])
                    S.op("pool", lambda e: e.tensor_copy(out=halo[:, ci, :], in_=ub[:, hf, W:W + 2]), reads=[ub], writes=[halo])
                    S.op("act", lambda e: e.activation(out=yb[:, hf, :W], in_=pb[:, :W], func=AF.Identity,
                                                       scale=cw[:, ci, 2:3], bias=cb[:, ci:ci + 1]),
                         reads=[pb, cw, cb], writes=[yb])
                    S.op("dve", lambda e: e.scalar_tensor_tensor(out=yb[:, hf, :W], in0=ub[:, hf, 1:1 + W], scalar=cw[:, ci, 1:2],
                                                                 in1=yb[:, hf, :W], op0=ALU.mult, op1=ALU.add),
                         reads=[ub, cw, yb], writes=[yb])
                    tb = tmp_sb[hf]
                    S.op("pool", lambda e: e.tensor_scalar(out=tb[:, :W], in0=ub[:, hf, 0:W], scalar1=cw[:, ci, 0:1], scalar2=None,
                                                           op0=ALU.mult),
                         reads=[ub, cw], writes=[tb])
                    S.op("pool", lambda e: e.tensor_tensor(out=yb[:, hf, :W], in0=yb[:, hf, :W], in1=tb[:, :W], op=ALU.add),
                         reads=[yb, tb], writes=[yb])
                S.op("act", lambda e: e.activation(out=eb[:, :W], in_=yb[:, 0, :W], func=AF.Exp, scale=-1.0), reads=[yb], writes=[eb])
                S.op("pool", lambda e: e.tensor_scalar_add(out=eb[:, :W], in0=eb[:, :W], scalar1=1.0), reads=[eb], writes=[eb])
                S.op("dve", lambda e: e.reciprocal(out=eb[:, :W], in_=eb[:, :W]), reads=[eb], writes=[eb])
                S.op("pool", lambda e: e.tensor_tensor(out=yb[:, 0, :W], in0=yb[:, 0, :W], in1=yb[:, 1, :W], op=ALU.mult),
                     reads=[yb], writes=[yb])
                S.op("dve", lambda e: e.tensor_tensor(out=h_sb[:, g, :W], in0=yb[:, 0, :W], in1=eb[:, :W], op=ALU.mult),
                     reads=[yb, eb], writes=[h_sb])
            S.dma("sp", wd_s[0][:].rearrange("p a b -> p (a b)"), wdn_b[0], reads=[wdn_b], writes=[wd_s[0]])
            for m in range(8):
                if m + 1 < 8:
                    S.dma("sp", wd_s[(m + 1) % 2][:].rearrange("p a b -> p (a b)"), wdn_b[m + 1],
                          reads=[wdn_b], writes=[wd_s[(m + 1) % 2]])
                ws = wd_s[m % 2]
                pb = pp.get()
                for kc in range(NG):
                    S.op("pe", lambda e: e.matmul(pb[:, :W], ws[:, kc, :], h_sb[:, kc, :W], start=(kc == 0), stop=(kc == NG - 1)),
                         reads=[ws, h_sb], writes=[pb])
                S.op("act", lambda e: e.activation(out=mix_sb[:, m, :W], in_=pb[:, :W], func=AF.Copy), reads=[pb], writes=[mix_sb])
                sq = sq_sb[m % 2]
                S.op("act", lambda e: e.activation(out=sq[:, :W], in_=pb[:, :W], func=AF.Square), reads=[pb], writes=[sq])
                S.op("pe", lambda e: e.matmul(ss_ps[:, :W], ones_b[:], sq[:, :W], start=(m == 0), stop=(m == 7)),
                     reads=[ones_b, sq], writes=[ss_ps])
            rstd_from_ss(S, ss_ps, rstd, 1.0 / D, RMS_EPS, W)
            S.op("dve", lambda e: e.tensor_tensor(out=rk[:, :W], in0=rstd[:, :W], in1=keep_sb[:, t0:t0 + W], op=ALU.mult),
                 reads=[rstd, keep_sb], writes=[rk])
            for m in range(8):
                tb = tmp_sb[m % 2]
                S.op("dve", lambda e: e.scalar_tensor_tensor(out=tb[:, :W], in0=mix_sb[:, m, :W], scalar=nwT[:, 3, m:m + 1],
                                                             in1=rk[:, :W], op0=ALU.mult, op1=ALU.mult),
                     reads=[mix_sb, nwT, rk], writes=[tb])
                S.op("pool", lambda e: e.tensor_tensor(out=hb[:, m, :W], in0=hb[:, m, :W], in1=tb[:, :W], op=ALU.add),
                     reads=[hb, tb], writes=[hb])
            S.dma("pool", hs_out[:, t0:t0 + W].rearrange("(c p) t -> p c t", p=128), hb[:, :, :W], reads=[hb], owner=hs_out)
        if not last:
            norm_ss(hb, W)
            rstd_from_ss(S, ss_ps, rstd, 1.0 / D, RMS_EPS, W)
            for m in range(8):
                S.op("dve", lambda e: e.scalar_tensor_tensor(out=a_sb[:, m, :W], in0=hb[:, m, :W], scalar=nwT[:, 0, m:m + 1],
                                                             in1=rstd[:, :W], op0=ALU.mult, op1=ALU.mult),
                     reads=[hb, nwT, rstd], writes=[a_sb])
            if ti == 0:
                S.dma("pool", ainH[:, :].rearrange("p (c t) -> p c t", c=8), a_sb[:, :, :W], reads=[a_sb], writes=[ainH])
            else:
                S.dma("pool", ainF[ti - 1][:, :].rearrange("p (c t) -> p c t", c=8), a_sb[:, :, :W], reads=[a_sb], writes=[ainF[ti - 1]])
            io["after_ain"](ti)
    S.pop()


def _cm(v):
    return np.ascontiguousarray(v.reshape(-1, 128).T)


def prep_T(inp, layer):
    j = layer // 2
    if layer % 2 == 0:
        w_out = inp["gdn_w_out"][j]
    else:
        w_out = inp["ml_w_out"][j]
    KO = w_out.shape[0]
    KC = KO // 128
    nw = inp["norm_w"]
    nxt = nw[layer + 1, 0] if layer + 1 < 4 else nw[layer, 0]
    nwT = np.stack([_cm(nxt), _cm(nw[layer, 1]), _cm(nw[layer, 2]), _cm(nw[layer, 3])], 1)
    wout = w_out.reshape(KC, 128, 8, 128).transpose(2, 1, 0, 3)
    wu = inp["ffn_w_up"][layer].reshape(8, 128, 2, NG, 128).transpose(3, 1, 0, 2, 4)
    wd = inp["ffn_w_down"][layer].reshape(NG, 128, 8, 128).transpose(2, 1, 0, 3)
    cw = inp["ffn_conv_w"][layer].reshape(3, 44, 128).transpose(2, 1, 0)
    cb = _cm(inp["ffn_conv_b"][layer])
    return {
        "nwT": np.ascontiguousarray(nwT.reshape(128, 32), np.float32),
        "wout": np.ascontiguousarray(wout.reshape(8, 128, KC * 128), np.float32),
        "wup": np.ascontiguousarray(wu.reshape(NG, 128, 8 * 256), np.float32),
        "wdn": np.ascontiguousarray(wd.reshape(8, 128, NG * 128), np.float32),
        "cw": np.ascontiguousarray(cw.reshape(128, 44 * 3), np.float32),
        "cb": np.ascontiguousarray(cb, np.float32),
    }


ML_DK = 128
ML_DV = 256
ML_WC = 128 + 128 + 128 + 256 + 256 + 2
BIGNEG = 30000.0


def emit_M_ml(S, tag, io):
    S.push(tag)
    ainF_g, ainH_g = io["ainF_g"], io["ainH_g"]
    w_d, gb_d, nwv_d, cst_d = io["w"], io["gb"], io["nwv"], io["cst"]
    ogF, ogH = io["ogF"], io["ogH"]
    c_og = 2

    w_sb = S.sb("w_sb", [128, 8, ML_WC], BF16)
    S.dma("pool", w_sb[:].rearrange("p a b -> p (a b)"), w_d[:, :], writes=[w_sb])
    wg_sb = S.sb("wg_sb", [128, 8, 2], BF16)
    cst = S.sb("cst_sb", [128, 64 + 512 + 128], F32)
    S.dma("sp", cst[:], cst_d[:, :], writes=[cst])
    identf = cst
    ident_b = S.sb("ident_b", [128, 128], BF16)
    S.op("act", lambda e: e.activation(out=ident_b[:], in_=cst[:, 576:704], func=AF.Copy), reads=[cst], writes=[ident_b])
    gb = S.sb("gb_sb", [1, 2], F32)
    S.dma("sp", gb[:], gb_d[:, :], writes=[gb])
    nwv = S.sb("nwv_sb", [64, ML_DV], F32)
    S.dma("sp", nwv[:], nwv_d[0:1, :].partition_broadcast(64), writes=[nwv])
    ones_row = S.sb("ones_row", [1, 128], F32)
    S.op("pool", lambda e: e.memset(ones_row[:], 1.0), writes=[ones_row])

    a_sb = [S.sb("a_sb%d" % i, [128, 8, 512], BF16) for i in range(2)]
    pp = PsumPool(S, 7)

    li_row = S.sb("li_row", [1, LP], F32)
    lf_row = S.sb("lf_row", [1, LP], F32)
    bb_row = S.sb("bb_row", [1, LP], F32)
    ones_bc = ones_row[0:1, 0:1].broadcast_to([1, LP])

    def load_a(i):
        ab = a_sb[i % 2]
        if i == 0:
            S.op("pool", lambda e: e.memset(ab[:, :, 0:XPAD], 0.0), writes=[ab])
            S.dma("sp", ab[:, :, XPAD:TS], ainH_g[0:128, :].rearrange("p (c t) -> p c t", c=8), reads=[ainH_g], writes=[ab])
        else:
            r0 = ((i - 1) % 4) * 512 + ((i - 1) // 4) * 128
            S.dma("sp", ab[:].rearrange("p c t -> p (c t)"), ainF_g[r0:r0 + 128, :], reads=[ainF_g], writes=[ab])

    load_a(0)
    for ti in range(NTS):
        if ti + 1 < NTS:
            load_a(ti + 1)
        ab = a_sb[ti % 2]
        for gi_, row in ((0, li_row), (1, lf_row)):
            pr = pp.get()
            for kc in range(8):
                S.op("pe", lambda e: e.matmul(pr[0:1, :], w_sb[:, kc, ML_WC - 2 + gi_:ML_WC - 1 + gi_], ab[:, kc, :],
                                              start=(kc == 0), stop=(kc == 7)), reads=[w_sb, ab], writes=[pr])
            S.op("act", lambda e: e.activation(out=row[:, ti * TS:(ti + 1) * TS], in_=pr[0:1, :], func=AF.Identity,
                                               bias=gb[:, gi_:gi_ + 1], scale=1.0), reads=[pr, gb], writes=[row])
    for row in (li_row, lf_row):
        S.op("act", lambda e: e.activation(out=row[:], in_=row[:], func=AF.Exp, scale=2.0 / 15.0), reads=[row], writes=[row])
        S.op("dve", lambda e: e.tensor_scalar_add(out=row[:], in0=row[:], scalar1=1.0), reads=[row], writes=[row])
        S.op("dve", lambda e: e.reciprocal(out=row[:], in_=row[:]), reads=[row], writes=[row])
        S.op("dve", lambda e: e.tensor_scalar(out=row[:], in0=row[:], scalar1=-30.0, scalar2=15.0, op0=ALU.mult, op1=ALU.add),
             reads=[row], writes=[row])
    S.op("act", lambda e: e.activation(out=lf_row[:], in_=lf_row[:], func=AF.Exp, scale=-1.0), reads=[lf_row], writes=[lf_row])
    S.op("act", lambda e: e.activation(out=lf_row[:], in_=lf_row[:], func=AF.Ln, bias=1.0, scale=1.0), reads=[lf_row], writes=[lf_row])
    S.op("dve", lambda e: e.tensor_scalar_mul(out=lf_row[:], in0=lf_row[:], scalar1=-1.0), reads=[lf_row], writes=[lf_row])
    S.op("pool", lambda e: e.memset(lf_row[:, 0:XPAD], 0.0), writes=[lf_row])
    S.op("pool", lambda e: e.memset(li_row[:, 0:XPAD], -BIGNEG), writes=[li_row])
    S.op("dve", lambda e: e.tensor_tensor_scan(out=bb_row[:], data0=ones_bc, data1=lf_row[:], initial=0.0,
                                               op0=ALU.mult, op1=ALU.add), reads=[ones_row, lf_row], writes=[bb_row])
    c_row = lf_row
    S.op("dve", lambda e: e.tensor_tensor(out=c_row[:], in0=li_row[:], in1=bb_row[:], op=ALU.subtract), reads=[li_row, bb_row], writes=[c_row])
    M_row = li_row
    S.op("dve", lambda e: e.tensor_tensor_scan(out=M_row[:], data0=ones_bc, data1=c_row[:], initial=0.0,
                                               op0=ALU.mult, op1=ALU.max), reads=[ones_row, c_row], writes=[M_row])
    en_row = bb_row
    S.op("dve", lambda e: e.tensor_tensor(out=en_row[:], in0=bb_row[:], in1=M_row[:], op=ALU.add), reads=[bb_row, M_row], writes=[en_row])
    S.op("act", lambda e: e.activation(out=en_row[:], in_=en_row[:], func=AF.Exp, scale=-1.0), reads=[en_row], writes=[en_row])

    c_tm = S.sb("c_tm", [64, NCH], F32)
    M_tm = S.sb("M_tm", [64, NCH], F32)
    en_tm = S.sb("en_tm", [64, NCH], F32)
    for row, tmb in ((c_row, c_tm), (M_row, M_tm), (en_row, en_tm)):
        pb = pp.get()
        for n in range(NCH):
            S.op("pe", lambda e: e.matmul(pb[0:64, n:n + 1], row[0:1, n * CH:(n + 1) * CH], ones_row[0:1, 0:1], start=True, stop=True),
                 reads=[row, ones_row], writes=[pb])
        S.op("act", lambda e: e.activation(out=tmb[:], in_=pb[0:64, 0:NCH], func=AF.Copy), reads=[pb], writes=[tmb])
    Mend_b = S.sb("Mend_b", [128, NCH], F32)
    Mprev_b = S.sb("Mprev_b", [128, NCH], F32)
    pb = pp.get()
    S.op("pe", lambda e: e.matmul(pb[:, 0:NCH], ones_row[0:1, :], M_row[0:1, CH - 1::CH], start=True, stop=True),
         reads=[ones_row, M_row], writes=[pb])
    S.op("act", lambda e: e.activation(out=Mend_b[:], in_=pb[:, 0:NCH], func=AF.Copy), reads=[pb], writes=[Mend_b])
    S.op("pool", lambda e: e.memset(Mprev_b[:, 0:1], 0.0), writes=[Mprev_b])
    S.op("pool", lambda e: e.tensor_copy(out=Mprev_b[:, 1:NCH], in_=Mend_b[:, 0:NCH - 1]), reads=[Mend_b], writes=[Mprev_b])
    sc_tm = S.sb("sc_tm", [64, NCH], F32)
    kws_tm = S.sb("kws_tm", [64, NCH], F32)
    dec_b = S.sb("dec_b", [128, NCH], F32)
    S.op("dve", lambda e: e.tensor_tensor(out=sc_tm[:], in0=Mprev_b[0:64, :], in1=M_tm[:], op=ALU.subtract), reads=[Mprev_b, M_tm], writes=[sc_tm])
    S.op("act", lambda e: e.activation(out=sc_tm[:], in_=sc_tm[:], func=AF.Exp), reads=[sc_tm], writes=[sc_tm])
    S.op("dve", lambda e: e.tensor_tensor(out=kws_tm[:], in0=c_tm[:], in1=Mend_b[0:64, :], op=ALU.subtract), reads=[c_tm, Mend_b], writes=[kws_tm])
    S.op("act", lambda e: e.activation(out=kws_tm[:], in_=kws_tm[:], func=AF.Exp), reads=[kws_tm], writes=[kws_tm])
    S.op("dve", lambda e: e.tensor_tensor(out=dec_b[:], in0=Mprev_b[:], in1=Mend_b[:], op=ALU.subtract), reads=[Mprev_b, Mend_b], writes=[dec_b])
    S.op("act", lambda e: e.activation(out=dec_b[:], in_=dec_b[:], func=AF.Exp), reads=[dec_b], writes=[dec_b])

    qT = [S.sb("qT%d" % i, [128, 512], BF16) for i in range(2)]
    kT = [S.sb("kT%d" % i, [128, 512], BF16) for i in range(2)]
    Wt = [S.sb("Wt%d" % i, [64, 512], F32) for i in range(2)]
    ogT = [S.sb("ogT%d" % i, [128, 2, 512], BF16) for i in range(2)]
    C_f = S.sb("C_f", [128, ML_DV + 1], F32)
    C_b = S.sb("C_b", [128, ML_DV + 1], BF16)
    S.op("pool", lambda e: e.memset(C_f[:], 0.0), writes=[C_f])
    S.op("pool", lambda e: e.memset(C_b[:], 0.0), writes=[C_b])
    NR = 3
    kw_sb = [S.sb("kw_sb%d" % i, [64, 128], BF16) for i in range(NR)]
    va_sb = [S.sb("va_sb%d" % i, [64, ML_DV + 1], BF16) for i in range(NR)]
    og_sb = [S.sb("ogs%d" % i, [64, ML_DV], F32) for i in range(NR)]
    St_sb = [S.sb("St%d" % i, [64, 64], BF16) for i in range(NR)]
    t1_sb = [S.sb("t1_%d" % i, [64, ML_DV + 1], F32) for i in range(NR)]
    sm_sb = [S.sb("sm%d" % i, [64, 8], F32) for i in range(NR)]
    junk = [S.sb("junk%d" % i, [64, ML_DV], F32) for i in range(NR)]
    hn_sb = [S.sb("hn%d" % i, [64, ML_DV], BF16) for i in range(NR)]
    for i in range(NR):
        S.op("pool", lambda e: e.memset(va_sb[i][:, ML_DV:ML_DV + 1], 1.0), writes=[va_sb[i]])
    pTall = S.ps("pTall", [128, 1024], BF16)

    load_a(0)
    for ti in range(NTS):
        if ti + 1 < NTS:
            load_a(ti + 1)
        ab = a_sb[ti % 2]
        t0 = ti * TS
        qTb, kTb, Wtb, ogTb = qT[ti % 2], kT[ti % 2], Wt[ti % 2], ogT[ti % 2]
        for wi, (dst, scl) in enumerate(((qTb, ML_DK ** -0.5), (kTb, 1.0))):
            pb = pp.get()
            for kc in range(8):
                S.op("pe", lambda e: e.matmul(pb[:, :], w_sb[:, kc, wi * 128:(wi + 1) * 128], ab[:, kc, :], start=(kc == 0), stop=(kc == 7)),
                     reads=[w_sb, ab], writes=[pb])
            S.op("act", lambda e: e.activation(out=dst[:], in_=pb[:, :], func=AF.Copy, scale=scl), reads=[pb], writes=[dst])
        pb = pp.get()
        S.op("pe", lambda e: e.matmul(pb[0:64, :], ones_row[0:1, 0:64], M_row[0:1, t0:t0 + TS], start=True, stop=False),
             reads=[ones_row, M_row], writes=[pb])
        S.op("pe", lambda e: e.matmul(pb[0:64, :], cst[0:64, 0:64], cst[0:64, 64:576], start=False, stop=True),
             reads=[cst], writes=[pb])
        S.op("dve", lambda e: e.tensor_tensor(out=Wtb[:].rearrange("p (n i) -> p n i", n=CPT), in0=pb[0:64, :].rearrange("p (n i) -> p n i", n=CPT),
                                              in1=bcast_last(c_tm[:, ti * CPT:(ti + 1) * CPT], CH), op=ALU.subtract),
             reads=[pb, c_tm], writes=[Wtb])
        S.op("act", lambda e: e.activation(out=Wtb[:], in_=Wtb[:], func=AF.Exp, scale=-1.0), reads=[Wtb], writes=[Wtb])
        for cj in range(CPT):
            n = ti * CPT + cj
            r = n % NR
            c0 = cj * CH
            pkv = pp.get()
            for kc in range(8):
                S.op("pe", lambda e: e.matmul(pkv[0:64, 0:384], ab[:, kc, c0:c0 + CH], w_sb[:, kc, 256:640], start=(kc == 0), stop=(kc == 7)),
                     reads=[w_sb, ab], writes=[pkv])
            pog = pp.get()
            for kc in range(8):
                S.op("pe", lambda e: e.matmul(pog[0:64, 0:256], ab[:, kc, c0:c0 + CH], w_sb[:, kc, 640:896], start=(kc == 0), stop=(kc == 7)),
                     reads=[w_sb, ab], writes=[pog])
            S.op("act", lambda e: e.activation(out=kw_sb[r][:], in_=pkv[0:64, 0:128], func=AF.Copy, scale=kws_tm[:, n:n + 1]),
                 reads=[pkv, kws_tm], writes=[kw_sb[r]])
            S.op("act", lambda e: e.activation(out=va_sb[r][:, 0:ML_DV], in_=pkv[0:64, 128:384], func=AF.Copy), reads=[pkv], writes=[va_sb[r]])
            S.op("act", lambda e: e.activation(out=og_sb[r][:], in_=pog[0:64, 0:256], func=AF.Exp, scale=-1.0), reads=[pog], writes=[og_sb[r]])
            S.op("pool", lambda e: e.tensor_scalar_add(out=og_sb[r][:], in0=og_sb[r][:], scalar1=1.0), reads=[og_sb[r]], writes=[og_sb[r]])
            S.op("dve", lambda e: e.reciprocal(out=og_sb[r][:], in_=og_sb[r][:]), reads=[og_sb[r]], writes=[og_sb[r]])
            S.op("pool", lambda e: e.tensor_tensor(out=og_sb[r][:], in0=og_sb[r][:], in1=nwv[:], op=ALU.mult), reads=[og_sb[r], nwv], writes=[og_sb[r]])
            pq = pp.get()
            S.op("pe", lambda e: e.matmul(pq[0:64, 0:64], kTb[:, c0:c0 + CH], qTb[:, c0:c0 + CH], start=True, stop=True),
                 reads=[kTb, qTb], writes=[pq])
            S.op("dve", lambda e: e.tensor_tensor(out=St_sb[r][:], in0=pq[0:64, 0:64], in1=Wtb[:, c0:c0 + CH], op=ALU.mult),
                 reads=[pq, Wtb], writes=[St_sb[r]])
            pint = pp.get()
            S.op("pe", lambda e: e.matmul(pint[0:64, 0:ML_DV + 1], qTb[:, c0:c0 + CH], C_b[:], start=True, stop=True),
                 reads=[qTb, C_b], writes=[pint])
            pia = pp.get()
            S.op("pe", lambda e: e.matmul(pia[0:64, 0:ML_DV + 1], St_sb[r][:], va_sb[r][:], start=True, stop=True),
                 reads=[St_sb[r], va_sb[r]], writes=[pia])
            pst = pp.get()
            S.op("pe", lambda e: e.matmul(pst[:, 0:ML_DV + 1], kw_sb[r][:], va_sb[r][:], start=True, stop=True),
                 reads=[kw_sb[r], va_sb[r]], writes=[pst])
            S.op("dve", lambda e: e.scalar_tensor_tensor(out=C_f[:], in0=C_f[:], scalar=dec_b[:, n:n + 1], in1=pst[:, 0:ML_DV + 1],
                                                         op0=ALU.mult, op1=ALU.add), reads=[C_f, dec_b, pst], writes=[C_f])
            S.op("act", lambda e: e.activation(out=C_b[:], in_=C_f[:], func=AF.Copy), reads=[C_f], writes=[C_b])
            t1 = t1_sb[r]
            S.op("act", lambda e: e.activation(out=t1[:], in_=pint[0:64, 0:ML_DV + 1], func=AF.Copy, scale=sc_tm[:, n:n + 1]),
                 reads=[pint, sc_tm], writes=[t1])
            S.op("dve", lambda e: e.tensor_tensor(out=t1[:], in0=t1[:], in1=pia[0:64, 0:ML_DV + 1], op=ALU.add), reads=[t1, pia], writes=[t1])
            sm = sm_sb[r]
            S.op("act", lambda e: e.activation(out=sm[:, 0:1], in_=t1[:, ML_DV:ML_DV + 1], func=AF.Abs), reads=[t1], writes=[sm])
            S.op("dve", lambda e: e.tensor_tensor(out=sm[:, 0:1], in0=sm[:, 0:1], in1=en_tm[:, n:n + 1], op=ALU.max),
                 reads=[sm, en_tm], writes=[sm])
            S.op("dve", lambda e: e.reciprocal(out=sm[:, 1:2], in_=sm[:, 0:1]), reads=[sm], writes=[sm])
            S.op("act", lambda e: e.activation(out=junk[r][:], in_=t1[:, 0:ML_DV], func=AF.Square, scale=sm[:, 1:2], accum_out=sm[:, 2:3]),
                 reads=[t1, sm], writes=[junk[r], sm])
            S.op("act", lambda e: e.activation(out=sm[:, 3:4], in_=sm[:, 2:3], func=AF.Ln, scale=1.0 / ML_DV, bias=RMS_EPS), reads=[sm], writes=[sm])
            S.op("act", lambda e: e.activation(out=sm[:, 3:4], in_=sm[:, 3:4], func=AF.Exp, scale=-0.5), reads=[sm], writes=[sm])
            S.op("dve", lambda e: e.tensor_tensor(out=sm[:, 4:5], in0=sm[:, 3:4], in1=sm[:, 1:2], op=ALU.mult), reads=[sm], writes=[sm])
            S.op("dve", lambda e: e.scalar_tensor_tensor(out=hn_sb[r][:], in0=t1[:, 0:ML_DV], scalar=sm[:, 4:5], in1=og_sb[r][:],
                                                         op0=ALU.mult, op1=ALU.mult), reads=[t1, sm, og_sb[r]], writes=[hn_sb[r]])
            ptv = pTall[:, (n % 4) * 128:(n % 4 + 1) * 128].rearrange("p (a b) -> p a b", a=2)
            for hh in range(2):
                S.op("pe", lambda e: e.transpose(ptv[:, hh, :], hn_sb[r][:, hh * 128:(hh + 1) * 128], ident_b[0:64, 0:64]),
                     reads=[hn_sb[r], ident_b], writes=[pTall])
            S.op("act", lambda e: e.activation(out=ogTb[:, :, c0:c0 + CH], in_=ptv, func=AF.Copy), reads=[pTall], writes=[ogTb])
        if ti >= 1:
            S.dma("pool", ogF[ti - 1][:, :].rearrange("p (c t) -> p c t", c=c_og), ogTb[:], reads=[ogTb], writes=[ogF[ti - 1]])
        if ti % 4 == 0 and ti <= 12:
            S.dma("pool", ogH[(ti // 4) * 128:(ti // 4 + 1) * 128, :].rearrange("p (c t) -> p c t", c=c_og), ogTb[:, :, TS - 64:TS],
                  reads=[ogTb], writes=[ogH])
        io["after_og"](ti)
    S.pop()


def prep_M_ml(inp, j, h):
    w = inp["ml_w_in"][j]
    q = w[:, h * 128:(h + 1) * 128]
    k = w[:, 512 + h * 128:512 + (h + 1) * 128]
    v = w[:, 1024 + h * 256:1024 + (h + 1) * 256]
    o = w[:, 2048 + h * 256:2048 + (h + 1) * 256]
    gi = w[:, 3072 + h:3073 + h]
    gf = w[:, 3076 + h:3077 + h]
    wc = np.concatenate([q, k, k, v, o, gi, gf], 1)
    wc = wc.reshape(8, 128, ML_WC).transpose(1, 0, 2)
    gb = inp["ml_gate_b"][j][[h, 4 + h]].reshape(1, 2)
    nwv = inp["ml_norm_w"][j][h * 256:(h + 1) * 256].reshape(1, 256)
    return {"w": np.ascontiguousarray(wc.reshape(128, 8 * ML_WC), np.float32),
            "gb": np.ascontiguousarray(gb, np.float32), "nwv": np.ascontiguousarray(nwv, np.float32),
            "cst": ml_consts()}


def ml_consts():
    c = np.zeros((128, 64 + 512 + 128), np.float32)
    c[0:64, 0:64] = np.eye(64, dtype=np.float32)
    jj = np.arange(64)[:, None]
    ii = np.arange(64)[None, :]
    mb = (jj > ii).astype(np.float32) * BIGNEG
    c[0:64, 64:576] = np.tile(mb, (1, 8))
    c[:, 576:704] = np.eye(128, dtype=np.float32)
    return c


G_WC = 12 * 128 + 8
L2_EPS = 1e-6
GDN_EPS = 1e-6
C_I64, C_MUI, C_MUS, C_MLS, C_NMLS, C_NMUS, C_I128, C_ONES = 0, 64, 128, 192, 256, 320, 384, 512
C_TOT = 640


def gdn_consts():
    c = np.zeros((128, C_TOT), np.float32)
    p = np.arange(64)[:, None]
    f = np.arange(64)[None, :]
    c[0:64, C_I64:C_I64 + 64] = (p == f)
    c[0:64, C_MUI:C_MUI + 64] = (p <= f)
    c[0:64, C_MUS:C_MUS + 64] = (p < f)
    c[0:64, C_MLS:C_MLS + 64] = (p > f)
    c[0:64, C_NMLS:C_NMLS + 64] = -1.0 * (p > f)
    c[0:64, C_NMUS:C_NMUS + 64] = -1.0 * (p < f)
    c[:, C_I128:C_I128 + 128] = np.eye(128, dtype=np.float32)
    c[:, C_ONES:C_ONES + 128] = 1.0
    return c


def emit_M_gdn(S, tag, io):
    S.push(tag)
    ainF_g, ainH_g = io["ainF_g"], io["ainH_g"]
    w_d, cw_d, hv_d, gnw_d, cst_d = io["w"], io["cw"], io["hv"], io["gnw"], io["cst"]
    ogF, ogH = io["ogF"], io["ogH"]
    c_og = 4

    w_sb = S.sb("w_sb", [128, 8, G_WC], BF16)
    S.dma("pool", w_sb[:].rearrange("p a b -> p (a b)"), w_d[:, :], writes=[w_sb])
    cst = S.sb("cst_sb", [128, C_TOT], F32)
    S.dma("sp", cst[:], cst_d[:, :], writes=[cst])
    cwg = S.sb("cwg", [128, 8, 4], F32)
    S.dma("sp", cwg[:].rearrange("p a b -> p (a b)"), cw_d[:, :], writes=[cwg])
    hv = S.sb("hv_sb", [64, 8], F32)
    S.dma("sp", hv[:], hv_d[0:1, :].partition_broadcast(64), writes=[hv])
    gnw = S.sb("gnw_sb", [128, 1], F32)
    S.dma("sp", gnw[:], gnw_d[:, :], writes=[gnw])
    ident_b = S.sb("ident_b", [128, 128], BF16)
    ones_b = S.sb("ones_b", [128, 128], BF16)
    S.op("act", lambda e: e.activation(out=ident_b[:], in_=cst[:, C_I128:C_I128 + 128], func=AF.Copy), reads=[cst], writes=[ident_b])
    S.op("act", lambda e: e.activation(out=ones_b[:], in_=cst[:, C_ONES:C_ONES + 128], func=AF.Copy), reads=[cst], writes=[ones_b])
    dg = S.sb("dg", [128, 32, 128], BF16)
    for mc in range(8):
        for k in range(4):
            S.op("dve", lambda e: e.tensor_scalar(out=dg[:, mc * 4 + k, :], in0=cst[:, C_I128:C_I128 + 128], scalar1=cwg[:, mc, k:k + 1],
                                                  scalar2=None, op0=ALU.mult), reads=[cst, cwg], writes=[dg])
    I64 = cst[0:64, C_I64:C_I64 + 64]
    MUI = cst[0:64, C_MUI:C_MUI + 64]
    MLS = cst[0:64, C_MLS:C_MLS + 64]
    NMLS = cst[0:64, C_NMLS:C_NMLS + 64]
    NMUS = cst[0:64, C_NMUS:C_NMUS + 64]
    ONES64x128 = cst[0:64, C_ONES:C_ONES + 128]

    a_sb = [S.sb("a_sb%d" % i, [128, 8, 512], BF16) for i in range(2)]
    pp = PsumPool(S, 7)
    pTall = S.ps("pTall", [128, 1024], BF16)

    def load_a(i):
        ab = a_sb[i % 2]
        if i == 0:
            S.op("pool", lambda e: e.memset(ab[:, :, 0:XPAD], 0.0), writes=[ab])
            S.dma("sp", ab[:, :, XPAD:TS], ainH_g[0:128, :].rearrange("p (c t) -> p c t", c=8), reads=[ainH_g], writes=[ab])
        else:
            r0 = ((i - 1) % 4) * 512 + ((i - 1) // 4) * 128
            S.dma("sp", ab[:].rearrange("p c t -> p (c t)"), ainF_g[r0:r0 + 128, :], reads=[ainF_g], writes=[ab])

    NH = NCH * 4
    bg = S.sb("bg", [64, NCH, 8], F32)
    load_a(0)
    for ti in range(NTS):
        if ti + 1 < NTS:
            load_a(ti + 1)
        ab = a_sb[ti % 2]
        pb = pp.get()
        for cj in range(CPT):
            for kc in range(8):
                S.op("pe", lambda e: e.matmul(pb[0:64, cj * 8:(cj + 1) * 8], ab[:, kc, cj * CH:(cj + 1) * CH], w_sb[:, kc, 1536:1544],
                                              start=(kc == 0), stop=(kc == 7)), reads=[ab, w_sb], writes=[pb])
        S.op("act", lambda e: e.activation(out=bg[:, ti * CPT:(ti + 1) * CPT, :], in_=pb[0:64, 0:64].rearrange("p (a b) -> p a b", a=CPT),
                                           func=AF.Copy), reads=[pb], writes=[bg])
    lnb = S.sb("lnb", [64, NCH, 4], F32)
    bt = S.sb("bt", [64, NCH, 4], F32)
    gt = S.sb("gt", [64, NCH, 4], F32)
    beG = S.sb("beG", [64, NCH, 4], F32)
    ekt = S.sb("ekt", [64, NCH, 4], F32)
    eGl = S.sb("eGl", [128, NCH, 4], F32)
    tmpg = S.sb("tmpg", [64, NCH, 4], F32)
    eal = S.sb("eal", [64, 4], F32)
    S.op("act", lambda e: e.activation(out=lnb[:], in_=bg[:, :, 0:4], func=AF.Exp, scale=-1.0), reads=[bg], writes=[lnb])
    S.op("act", lambda e: e.activation(out=lnb[:], in_=lnb[:], func=AF.Ln, bias=1.0, scale=1.0), reads=[lnb], writes=[lnb])
    S.op("dve", lambda e: e.tensor_scalar_mul(out=lnb[:], in0=lnb[:], scalar1=-1.0), reads=[lnb], writes=[lnb])
    S.op("act", lambda e: e.activation(out=bt[:], in_=lnb[:], func=AF.Exp), reads=[lnb], writes=[bt])
    S.op("dve", lambda e: e.tensor_tensor(out=gt[:], in0=bg[:, :, 4:8], in1=bcast_mid(hv[:, 4:8], NCH), op=ALU.add), reads=[bg, hv], writes=[gt])
    S.op("act", lambda e: e.activation(out=gt[:], in_=gt[:], func=AF.Exp), reads=[gt], writes=[gt])
    S.op("act", lambda e: e.activation(out=gt[:], in_=gt[:], func=AF.Ln, bias=1.0, scale=1.0), reads=[gt], writes=[gt])
    S.op("act", lambda e: e.activation(out=eal[:], in_=hv[:, 0:4], func=AF.Exp), reads=[hv], writes=[eal])
    S.op("dve", lambda e: e.tensor_scalar_mul(out=eal[:], in0=eal[:], scalar1=-1.0), reads=[eal], writes=[eal])
    S.op("dve", lambda e: e.tensor_tensor(out=gt[:], in0=gt[:], in1=bcast_mid(eal[:], NCH), op=ALU.mult), reads=[gt, eal], writes=[gt])
    gflat = gt[:].rearrange("p a b -> p (a b)")
    for (c0, c1) in ((0, 272), (272, NH)):
        pb = pp.get()
        S.op("pe", lambda e: e.matmul(pb[0:64, 0:c1 - c0], MUI, gflat[:, c0:c1], start=True, stop=True), reads=[cst, gt], writes=[pb])
        pl = pp.get()
        S.op("pe", lambda e: e.matmul(pl[:, 0:c1 - c0], ONES64x128, gflat[:, c0:c1], start=True, stop=True), reads=[cst, gt], writes=[pl])
        S.op("act", lambda e: e.activation(out=tmpg[:].rearrange("p a b -> p (a b)")[:, c0:c1], in_=pb[0:64, 0:c1 - c0], func=AF.Exp),
             reads=[pb], writes=[tmpg])
        S.op("act", lambda e: e.activation(out=eGl[:].rearrange("p a b -> p (a b)")[:, c0:c1], in_=pl[:, 0:c1 - c0], func=AF.Exp),
             reads=[pl], writes=[eGl])
        S.op("act", lambda e: e.activation(out=ekt[:].rearrange("p a b -> p (a b)")[:, c0:c1], in_=pb[0:64, 0:c1 - c0], func=AF.Copy),
             reads=[pb], writes=[ekt])
        S.op("dve", lambda e: e.tensor_tensor(out=ekt[:].rearrange("p a b -> p (a b)")[:, c0:c1], in0=pl[0:64, 0:c1 - c0],
                                              in1=ekt[:].rearrange("p a b -> p (a b)")[:, c0:c1], op=ALU.subtract), reads=[pl, ekt], writes=[ekt])
    S.op("act", lambda e: e.activation(out=ekt[:], in_=ekt[:], func=AF.Exp), reads=[ekt], writes=[ekt])
    S.op("dve", lambda e: e.tensor_tensor(out=beG[:], in0=bt[:], in1=tmpg[:], op=ALU.mult), reads=[bt, tmpg], writes=[beG])

    xb = [S.sb("xb%d" % i, [128, 8, 3 + TS], BF16) for i in range(2)]
    S.op("pool", lambda e: e.memset(xb[1][:, :, TS:TS + 3], 0.0), writes=[xb[1]])
    e_sb = [S.sb("e_sb%d" % i, [128, TS], F32) for i in range(2)]
    sx = [S.sb("sx%d" % i, [128, TS], F32) for i in range(2)]
    sqb = [S.sb("sqb%d" % i, [128, TS], BF16) for i in range(2)]
    rs = [S.sb("rs%d" % i, [128, TS], F32) for i in range(2)]
    qT = [S.sb("qT%d" % i, [128, 2, TS], BF16) for i in range(2)]
    kT = [S.sb("kT%d" % i, [128, 2, TS], BF16) for i in range(2)]
    svT = [S.sb("svT%d" % i, [128, 4, TS], BF16) for i in range(2)]
    zs = [S.sb("zs0", [128, 4, TS], F32)] * 2
    onT = [S.sb("onT%d" % i, [128, 4, TS], BF16) for i in range(2)]
    ogt = [S.sb("ogt0", [128, 4, TS], BF16)] * 2
    S_f = S.sb("S_f", [128, 4, 128], F32)
    S_b = S.sb("S_b", [128, 4, 128], BF16)
    S_t = S.sb("S_t", [128, 4, 128], F32)
    S.op("pool", lambda e: e.memset(S_f[:], 0.0), writes=[S_f])
    S.op("pool", lambda e: e.memset(S_b[:], 0.0), writes=[S_b])
    NR = 2
    mk = lambda nm, shp, dt: [S.sb("%s%d" % (nm, i), shp, dt) for i in range(NR)]
    kbg = mk("kbg", [64, 4, 128], BF16)
    ktm = mk("ktm", [64, 4, 128], BF16)
    vb = mk("vb", [64, 4, 128], BF16)
    rg1 = mk("rg1", [64, 4, 64], F32)
    rg2 = mk("rg2", [64, 4, 64], F32)
    rg3 = mk("rg3", [64, 4, 64], F32)
    Et = mk("Et", [64, 4, 64], F32)
    Wt_ = mk("Wt", [64, 4, 64], F32)
    W_ = mk("W", [64, 4, 64], F32)
    eGb = mk("eGb", [128, 4, 64], F32)
    KKlo = mk("KKlo", [64, 2, 64], F32)
    KKup = mk("KKup", [64, 2, 64], F32)
    KQm = mk("KQm", [64, 2, 64], F32)
    Qt = mk("Qt", [64, 4, 64], BF16)
    qdT = mk("qdT", [128, 4, 64], BF16)
    PP = [mk("PP%d" % k, [64, 8, 64], F32) for k in range(2)]
    Xt = [mk("Xt%d" % k, [64, 4, 64], F32) for k in range(2)]
    Tt = mk("Tt", [64, 4, 64], BF16)
    nwT = mk("nwT", [128, 4, 64], BF16)
    vn = mk("vn", [64, 4, 128], BF16)
    sqo = mk("sqo", [64, 4, 128], F32)
    sso = mk("sso", [64, 8], F32)
    on = mk("on", [64, 4, 128], BF16)

    def silu_from_psum(pb, W, out_ap, out_buf, idx):
        eb = e_sb[idx % 2]
        S.op("act", lambda e: e.activation(out=eb[:, :W], in_=pb[:, :W], func=AF.Exp, scale=-1.0), reads=[pb], writes=[eb])
        S.op("pool", lambda e: e.tensor_scalar_add(out=eb[:, :W], in0=eb[:, :W], scalar1=1.0), reads=[eb], writes=[eb])
        S.op("dve", lambda e: e.reciprocal(out=eb[:, :W], in_=eb[:, :W]), reads=[eb], writes=[eb])
        S.op("dve", lambda e: e.tensor_tensor(out=out_ap, in0=pb[:, :W], in1=eb[:, :W], op=ALU.mult), reads=[pb, eb], writes=[out_buf])

    load_a(0)
    for ti in range(NTS):
        if ti + 1 < NTS:
            load_a(ti + 1)
        ab = a_sb[ti % 2]
        t0 = ti * TS
        xcur, xprev = xb[ti % 2], xb[(ti + 1) % 2]
        qTb, kTb, svb, zsb, onTb, ogb = qT[ti % 2], kT[ti % 2], svT[ti % 2], zs[ti % 2], onT[ti % 2], ogt[ti % 2]
        S.op("pool", lambda e: e.tensor_copy(out=xcur[:, :, 0:3], in_=xprev[:, :, TS:TS + 3]), reads=[xprev], writes=[xcur])
        for mc in range(12):
            pb = pp.get()
            for kc in range(8):
                S.op("pe", lambda e: e.matmul(pb[:, :], w_sb[:, kc, mc * 128:(mc + 1) * 128], ab[:, kc, :], start=(kc == 0), stop=(kc == 7)),
                     reads=[w_sb, ab], writes=[pb])
            if mc < 8:
                S.op("act", lambda e: e.activation(out=xcur[:, mc, 3:3 + TS], in_=pb[:, :], func=AF.Copy), reads=[pb], writes=[xcur])
            else:
                silu_from_psum(pb, TS, zsb[:, mc - 8, :], zsb, mc)
        for mc in range(8):
            pb = pp.get()
            for k in range(4):
                S.op("pe", lambda e: e.matmul(pb[:, :], dg[:, mc * 4 + k, :], xcur[:, mc, k:k + TS], start=(k == 0), stop=(k == 3)),
                     reads=[dg, xcur], writes=[pb])
            if mc >= 4:
                silu_from_psum(pb, TS, svb[:, mc - 4, :], svb, mc)
            else:
                sxb, sq, rsb = sx[mc % 2], sqb[mc % 2], rs[mc % 2]
                silu_from_psum(pb, TS, sxb[:, :], sxb, mc)
                S.op("act", lambda e: e.activation(out=sq[:, :], in_=sxb[:, :], func=AF.Square), reads=[sxb], writes=[sq])
                ps2 = pp.get()
                S.op("pe", lambda e: e.matmul(ps2[:, :], ones_b[:], sq[:, :], start=True, stop=True), reads=[ones_b, sq], writes=[ps2])
                rstd_from_ss(S, ps2, rsb, 1.0, L2_EPS, TS)
                dst = qTb if mc < 2 else kTb
                scl = (128.0 ** -0.5) if mc < 2 else 1.0
                S.op("dve", lambda e: e.scalar_tensor_tensor(out=dst[:, mc % 2, :], in0=sxb[:, :], scalar=scl, in1=rsb[:, :],
                                                             op0=ALU.mult, op1=ALU.mult), reads=[sxb, rsb], writes=[dst])
        for cj in range(CPT):
            n = ti * CPT + cj
            r = n % NR
            c0 = cj * CH
            for qh in range(2):
                S.op("pe", lambda e: e.transpose(pTall[0:64, qh * 128:(qh + 1) * 128], kTb[:, qh, c0:c0 + CH], ident_b[:, :]),
                     reads=[kTb, ident_b], writes=[pTall])
            for h in range(4):
                S.op("pe", lambda e: e.transpose(pTall[0:64, 256 + h * 128:256 + (h + 1) * 128], svb[:, h, c0:c0 + CH], ident_b[:, :]),
                     reads=[svb, ident_b], writes=[pTall])
            ktm_ps = pTall[0:64, 0:256].rearrange("p (a b) -> p a b", a=2)
            ktm_rep = ktm_ps.unsqueeze(2).broadcast_to([64, 2, 2, 128])
            as4 = lambda ap: ap.rearrange("p (a r) d -> p a r d", r=2)
            S.op("dve", lambda e: e.tensor_tensor(out=as4(kbg[r][:]), in0=ktm_rep, in1=as4(bcast_last(beG[:, n, :], 128)), op=ALU.mult),
                 reads=[pTall, beG], writes=[kbg[r]])
            S.op("dve", lambda e: e.tensor_tensor(out=as4(ktm[r][:]), in0=ktm_rep, in1=as4(bcast_last(ekt[:, n, :], 128)), op=ALU.mult),
                 reads=[pTall, ekt], writes=[ktm[r]])
            S.op("dve", lambda e: e.tensor_tensor(out=vb[r][:], in0=pTall[0:64, 256:768].rearrange("p (a b) -> p a b", a=4),
                                                  in1=bcast_last(bt[:, n, :], 128), op=ALU.mult), reads=[pTall, bt], writes=[vb[r]])
            S.op("pool", lambda e: e.tensor_tensor(out=rg1[r][:], in0=bcast_mid(MUI, 4), in1=bcast_last(gt[:, n, :], 64), op=ALU.mult),
                 reads=[cst, gt], writes=[rg1[r]])
            S.op("pool", lambda e: e.tensor_tensor(out=rg2[r][:], in0=bcast_mid(I64, 4), in1=bcast_last(lnb[:, n, :], 64), op=ALU.mult),
                 reads=[cst, lnb], writes=[rg2[r]])
            S.op("pool", lambda e: e.tensor_tensor(out=rg2[r][:], in0=rg2[r][:], in1=rg1[r][:], op=ALU.add), reads=[rg1[r], rg2[r]], writes=[rg2[r]])
            S.op("pool", lambda e: e.tensor_tensor(out=rg3[r][:], in0=bcast_mid(MLS, 4), in1=bcast_last(gt[:, n, :], 64), op=ALU.mult),
                 reads=[cst, gt], writes=[rg3[r]])
            fl = lambda b_: b_[:].rearrange("p a b -> p (a b)")
            pd1 = pp.get()
            S.op("pe", lambda e: e.matmul(pd1[0:64, 0:256], MLS, fl(rg1[r]), start=True, stop=True), reads=[cst, rg1[r]], writes=[pd1])
            S.op("pe", lambda e: e.matmul(pd1[0:64, 256:512], MLS, fl(rg2[r]), start=True, stop=True), reads=[cst, rg2[r]], writes=[pd1])
            pd2 = pp.get()
            S.op("pe", lambda e: e.matmul(pd2[0:64, 0:256], MUI, fl(rg3[r]), start=True, stop=True), reads=[cst, rg3[r]], writes=[pd2])
            pd3 = pp.get()
            S.op("pe", lambda e: e.matmul(pd3[:, 0:256], ONES64x128, fl(rg1[r]), start=True, stop=True), reads=[cst, rg1[r]], writes=[pd3])
            S.op("act", lambda e: e.activation(out=fl(Et[r]), in_=pd1[0:64, 0:256], func=AF.Exp), reads=[pd1], writes=[Et[r]])
            S.op("act", lambda e: e.activation(out=fl(Wt_[r]), in_=pd1[0:64, 256:512], func=AF.Exp), reads=[pd1], writes=[Wt_[r]])
            for h in range(4):
                S.op("act", lambda e: e.activation(out=W_[r][:, h, :], in_=pd2[0:64, h * 64:(h + 1) * 64], func=AF.Exp,
                                                   bias=lnb[:, n, h:h + 1], scale=1.0), reads=[pd2, lnb], writes=[W_[r]])
            S.op("act", lambda e: e.activation(out=fl(eGb[r]), in_=pd3[:, 0:256], func=AF.Exp), reads=[pd3], writes=[eGb[r]])
            pg = pp.get()
            for qh in range(2):
                S.op("pe", lambda e: e.matmul(pg[0:64, qh * 64:(qh + 1) * 64], kTb[:, qh, c0:c0 + CH], kTb[:, qh, c0:c0 + CH], start=True, stop=True),
                     reads=[kTb], writes=[pg])
                S.op("pe", lambda e: e.matmul(pg[0:64, 128 + qh * 64:128 + (qh + 1) * 64], kTb[:, qh, c0:c0 + CH], qTb[:, qh, c0:c0 + CH],
                                              start=True, stop=True), reads=[kTb, qTb], writes=[pg])
            kkv = pg[0:64, 0:128].rearrange("p (a b) -> p a b", a=2)
            kqv = pg[0:64, 128:256].rearrange("p (a b) -> p a b", a=2)
            S.op("dve", lambda e: e.tensor_tensor(out=KKlo[r][:], in0=kkv, in1=bcast_mid(NMLS, 2), op=ALU.mult), reads=[pg, cst], writes=[KKlo[r]])
            S.op("dve", lambda e: e.tensor_tensor(out=KKup[r][:], in0=kkv, in1=bcast_mid(NMUS, 2), op=ALU.mult), reads=[pg, cst], writes=[KKup[r]])
            S.op("dve", lambda e: e.tensor_tensor(out=KQm[r][:], in0=kqv, in1=bcast_mid(MUI, 2), op=ALU.mult), reads=[pg, cst], writes=[KQm[r]])
            rep = lambda b_: b_[:].unsqueeze(2).broadcast_to([64, 2, 2, 64])
            P0 = PP[0][r]
            S.op("pool", lambda e: e.tensor_tensor(out=as4(P0[:, 0:4, :]), in0=rep(KKlo[r]), in1=as4(W_[r][:]), op=ALU.mult),
                 reads=[KKlo[r], W_[r]], writes=[P0])
            S.op("pool", lambda e: e.tensor_tensor(out=as4(P0[:, 4:8, :]), in0=rep(KKup[r]), in1=as4(Wt_[r][:]), op=ALU.mult),
                 reads=[KKup[r], Wt_[r]], writes=[P0])
            S.op("pool", lambda e: e.tensor_tensor(out=as4(Qt[r][:]), in0=rep(KQm[r]), in1=as4(Et[r][:]), op=ALU.mult),
                 reads=[KQm[r], Et[r]], writes=[Qt[r]])
            S.op("dve", lambda e: e.tensor_tensor(out=as4(qdT[r][:]), in0=qTb[:, :, c0:c0 + CH].unsqueeze(2).broadcast_to([128, 2, 2, 64]),
                                                  in1=as4(eGb[r][:]), op=ALU.mult), reads=[qTb, eGb[r]], writes=[qdT[r]])
            X = Xt[0][r]
            S.op("dve", lambda e: e.tensor_tensor(out=X[:], in0=P0[:, 4:8, :], in1=bcast_mid(I64, 4), op=ALU.add), reads=[P0, cst], writes=[X])
            for k in range(1, 6):
                Pp, Pn = PP[(k - 1) % 2][r], PP[k % 2][r]
                pq = pp.get()
                for h in range(4):
                    S.op("pe", lambda e: e.matmul(pq[0:64, h * 64:(h + 1) * 64], Pp[:, 4 + h, :], Pp[:, h, :], start=True, stop=True),
                         reads=[Pp], writes=[pq])
                if k < 5:
                    for h in range(4):
                        S.op("pe", lambda e: e.matmul(pq[0:64, 256 + h * 64:256 + (h + 1) * 64], Pp[:, h, :], Pp[:, 4 + h, :], start=True, stop=True),
                             reads=[Pp], writes=[pq])
                wdt = 512 if k < 5 else 256
                S.op("act", lambda e: e.activation(out=Pn[:].rearrange("p a b -> p (a b)")[:, 0:wdt], in_=pq[0:64, 0:wdt], func=AF.Copy),
                     reads=[pq], writes=[Pn])
                px = pp.get()
                Xo = Xt[(k - 1) % 2][r]
                Xn = Xt[k % 2][r]
                for h in range(4):
                    S.op("pe", lambda e: e.matmul(px[0:64, h * 64:(h + 1) * 64], Pn[:, h, :], Xo[:, h, :], start=True, stop=True),
                         reads=[Pn, Xo], writes=[px])
                if k < 5:
                    S.op("dve", lambda e: e.tensor_tensor(out=fl(Xn), in0=px[0:64, 0:256], in1=fl(Xo), op=ALU.add), reads=[px, Xo], writes=[Xn])
                else:
                    S.op("dve", lambda e: e.tensor_tensor(out=fl(Tt[r]), in0=px[0:64, 0:256], in1=fl(Xo), op=ALU.add), reads=[px, Xo], writes=[Tt[r]])
            pw = pp.get()
            for h in range(4):
                S.op("pe", lambda e: e.matmul(pw[:, h * 64:(h + 1) * 64], kbg[r][:, h, :], Tt[r][:, h, :], start=True, stop=True),
                     reads=[kbg[r], Tt[r]], writes=[pw])
            S.op("act", lambda e: e.activation(out=fl(nwT[r]), in_=pw[:, 0:256], func=AF.Copy, scale=-1.0), reads=[pw], writes=[nwT[r]])
            pu = pp.get()
            for h in range(4):
                S.op("pe", lambda e: e.matmul(pu[0:64, h * 128:(h + 1) * 128], Tt[r][:, h, :], vb[r][:, h, :], start=True, stop=False),
                     reads=[Tt[r], vb[r]], writes=[pu])
                S.op("pe", lambda e: e.matmul(pu[0:64, h * 128:(h + 1) * 128], nwT[r][:, h, :], S_b[:, h, :], start=False, stop=True),
                     reads=[nwT[r], S_b], writes=[pu])
            S.op("act", lambda e: e.activation(out=vn[r][:].rearrange("p a b -> p (a b)"), in_=pu[0:64, :], func=AF.Copy), reads=[pu], writes=[vn[r]])
            po = pp.get()
            for h in range(4):
                S.op("pe", lambda e: e.matmul(po[0:64, h * 128:(h + 1) * 128], qdT[r][:, h, :], S_b[:, h, :], start=True, stop=False),
                     reads=[qdT[r], S_b], writes=[po])
                S.op("pe", lambda e: e.matmul(po[0:64, h * 128:(h + 1) * 128], Qt[r][:, h, :], vn[r][:, h, :], start=False, stop=True),
                     reads=[Qt[r], vn[r]], writes=[po])
            pS = pp.get()
            for h in range(4):
                S.op("pe", lambda e: e.matmul(pS[:, h * 128:(h + 1) * 128], ktm[r][:, h, :], vn[r][:, h, :], start=True, stop=True),
                     reads=[ktm[r], vn[r]], writes=[pS])
            S.op("pool", lambda e: e.tensor_tensor(out=S_t[:], in0=S_f[:], in1=bcast_last(eGl[:, n, :], 128), op=ALU.mult),
                 reads=[S_f, eGl], writes=[S_t])
            S.op("dve", lambda e: e.tensor_tensor(out=S_f[:].rearrange("p a b -> p (a b)"), in0=pS[:, :], in1=S_t[:].rearrange("p a b -> p (a b)"),
                                                  op=ALU.add), reads=[pS, S_t], writes=[S_f])
            S.op("act", lambda e: e.activation(out=S_b[:], in_=S_f[:], func=AF.Copy), reads=[S_f], writes=[S_b])
            S.op("act", lambda e: e.activation(out=sqo[r][:].rearrange("p a b -> p (a b)"), in_=po[0:64, :], func=AF.Square), reads=[po], writes=[sqo[r]])
            S.op("dve", lambda e: e.tensor_reduce(out=sso[r][:, 0:4], in_=sqo[r][:], axis=AX.X, op=ALU.add), reads=[sqo[r]], writes=[sso[r]])
            S.op("act", lambda e: e.activation(out=sso[r][:, 4:8], in_=sso[r][:, 0:4], func=AF.Ln, scale=1.0 / 128.0, bias=GDN_EPS),
                 reads=[sso[r]], writes=[sso[r]])
            S.op("act", lambda e: e.activation(out=sso[r][:, 4:8], in_=sso[r][:, 4:8], func=AF.Exp, scale=-0.5), reads=[sso[r]], writes=[sso[r]])
            S.op("dve", lambda e: e.tensor_tensor(out=on[r][:], in0=po[0:64, :].rearrange("p (a b) -> p a b", a=4),
                                                  in1=bcast_last(sso[r][:, 4:8], 128), op=ALU.mult), reads=[po, sso[r]], writes=[on[r]])
            for h in range(4):
                S.op("pe", lambda e: e.transpose(pTall[:, 768 + h * 64:768 + (h + 1) * 64], on[r][:, h, :], ident_b[0:64, 0:64]),
                     reads=[on[r], ident_b], writes=[pTall])
            S.op("act", lambda e: e.activation(out=onTb[:, :, c0:c0 + CH], in_=pTall[:, 768:1024].rearrange("p (a b) -> p a b", a=4), func=AF.Copy),
                 reads=[pTall], writes=[onTb])
        S.op("dve", lambda e: e.scalar_tensor_tensor(out=ogb[:].rearrange("p a b -> p (a b)"), in0=onTb[:].rearrange("p a b -> p (a b)"),
                                                     scalar=gnw[:, 0:1], in1=zsb[:].rearrange("p a b -> p (a b)"), op0=ALU.mult, op1=ALU.mult),
             reads=[onTb, gnw, zsb], writes=[ogb])
        if ti >= 1:
            S.dma("pool", ogF[ti - 1][:, :].rearrange("p (c t) -> p c t", c=c_og), ogb[:], reads=[ogb], writes=[ogF[ti - 1]])
        if ti % 4 == 0 and ti <= 12:
            S.dma("pool", ogH[(ti // 4) * 128:(ti // 4 + 1) * 128, :].rearrange("p (c t) -> p c t", c=c_og), ogb[:, :, TS - 64:TS],
                  reads=[ogb], writes=[ogH])
        io["after_og"](ti)
    S.pop()


def prep_M_gdn(inp, j, hg):
    w = inp["gdn_w_in"][j]
    cols = []
    for qh in range(2):
        cols.append(np.arange(128) + 128 * (2 * hg + qh))
    for qh in range(2):
        cols.append(1024 + np.arange(128) + 128 * (2 * hg + qh))
    for h in range(4):
        cols.append(2048 + np.arange(128) + 128 * (4 * hg + h))
    conv_cols = np.concatenate(cols)
    for h in range(4):
        cols.append(4096 + np.arange(128) + 128 * (4 * hg + h))
    cols.append(6144 + 4 * hg + np.arange(4))
    cols.append(6160 + 4 * hg + np.arange(4))
    cols = np.concatenate(cols)
    wc = w[:, cols].reshape(8, 128, G_WC).transpose(1, 0, 2)
    cw = inp["gdn_conv_w"][j][:, conv_cols].reshape(4, 8, 128).transpose(2, 1, 0)
    hv = np.concatenate([inp["gdn_a_log"][j][4 * hg:4 * hg + 4], inp["gdn_dt_bias"][j][4 * hg:4 * hg + 4]]).reshape(1, 8)
    return {"w": np.ascontiguousarray(wc.reshape(128, 8 * G_WC), np.float32),
            "cw": np.ascontiguousarray(cw.reshape(128, 32), np.float32),
            "hv": np.ascontiguousarray(hv, np.float32),
            "gnw": np.ascontiguousarray(inp["gdn_norm_w"][j].reshape(128, 1), np.float32),
            "cst": gdn_consts()}


GROUPS = [[0, 1, 2, 3], [4, 5, 6, 7]]


def build_fused(nl=4):
    nc = bass.Bass("TRN2", target_bir_lowering=False)
    es = ExitStack()
    S = Sched(nc, es)
    ext = lambda n, shp, dt=F32: S.dram(n, shp, dt, kind="ExternalInput")
    hs0 = ext("hs0", [D, WIN])
    keep = ext("keep", [1, WIN])
    gidx4 = ext("gidx4", [128, 20], mybir.dt.int32)
    gidx2 = ext("gidx2", [128, 20], mybir.dt.int32) if nl > 1 else None
    nwT0 = ext("nwT_0", [128, 32])
    cst_g = ext("cst_g", [128, C_TOT])
    cst_m = ext("cst_m", [128, 64 + 512 + 128]) if nl > 1 else None
    hs_out = S.dram("hs_out", [D, WIN], F32, kind="ExternalOutput")
    hs_loc = S.dram("hs_loc", [D, WIN], F32)
    def slices(name, rows_total, cols, rows_per):
        big = S.dram(name, [rows_total, cols], BF16)
        return big, [Buf(big.t[k * rows_per:(k + 1) * rows_per, :], "%s_%d" % (name, k)) for k in range(rows_total // rows_per)]

    ainF_all, ainF = slices("ainF", 512, 4096, 128)
    ainF_g, ainF_gs = slices("ainF_g", 2048, 4096, 512)
    ainH = S.dram("ainH", [128, 512], BF16)
    ainH_g = S.dram("ainH_g", [512, 512], BF16)
    og = {}
    for c in (4, 2):
        tpc = 8 // c
        ogF_all, ogF = slices("ogF%d" % c, 2048, c * 512, 128)
        ogF_g, _ = slices("ogF%d_g" % c, 8192, c * 512, 8192)
        nq = 16 // tpc
        og[c] = dict(ogF=ogF, ogF_all=ogF_all, ogH=S.dram("ogH%d" % c, [512, c * 64], BF16), tpc=tpc,
                     ogF_g=ogF_g, ogH_g=S.dram("ogH%d_g" % c, [2048, c * 64], BF16),
                     src=[Buf(ogF_all.t[q * tpc * 128:(q + 1) * tpc * 128, :], "ogsrc%d_%d" % (c, q)) for q in range(nq)],
                     dst=[Buf(ogF_g.t[q * 4 * tpc * 128:(q + 1) * 4 * tpc * 128, :], "ogdst%d_%d" % (c, q)) for q in range(nq)])
    lay = []
    for l in range(nl):
        KO = 2048 if l % 2 == 0 else 1024
        KC = KO // 128
        d = dict(nwT=ext("nwT_l%d" % l, [128, 32]), cw=ext("cw_l%d" % l, [128, 44 * 3]), cb=ext("cb_l%d" % l, [128, 44]),
                 wout_d=ext("wout_l%d" % l, [8, 128, KC * 128]), wup_d=ext("wup_l%d" % l, [NG, 128, 8 * 256]),
                 wdn_d=ext("wdn_l%d" % l, [8, 128, NG * 128]),
                 wout_b=S.dram("wout_b%d" % l, [8, 128, KC * 128], BF16), wup_b=S.dram("wup_b%d" % l, [NG, 128, 8 * 256], BF16),
                 wdn_b=S.dram("wdn_b%d" % l, [8, 128, NG * 128], BF16))
        if l % 2 == 0:
            d["m"] = dict(w=ext("gw_l%d" % l, [128, 8 * G_WC]), cw=ext("gcw_l%d" % l, [128, 32]), hv=ext("ghv_l%d" % l, [1, 8]),
                          gnw=ext("ggnw_l%d" % l, [128, 1]), cst=cst_g)
        else:
            d["m"] = dict(w=ext("mw_l%d" % l, [128, 8 * ML_WC]), gb=ext("mgb_l%d" % l, [1, 2]), nwv=ext("mnwv_l%d" % l, [1, ML_DV]), cst=cst_m)
        lay.append(d)

    def after_ain(ti):
        if ti == 0:
            S.coll("AllGather", ainH_g, ainH, GROUPS)
        else:
            S.coll("AllGather", ainF_gs[ti - 1], ainF[ti - 1], GROUPS)

    def mk_after_og(c):
        o = og[c]
        tpc = o["tpc"]

        def after_og(ti):
            wt = ti - 1
            if ti >= 1 and (wt + 1) % tpc == 0:
                q = wt // tpc
                src = o["src"][q]
                S.coll("AllGather", o["dst"][q], src, GROUPS, extra=[o["ogF"][k] for k in range(q * tpc, (q + 1) * tpc)])
            if ti == 12:
                S.coll("AllGather", o["ogH_g"], o["ogH"], GROUPS)
        return after_og

    emit_T(S, "t0", 2048, True, False, dict(hs_src=hs0, nwT=nwT0, ainF=ainF, ainH=ainH, after_ain=after_ain))
    for l in range(nl):
        d = lay[l]
        c = 4 if l % 2 == 0 else 2
        emit_casts(S, d)
        mio = dict(d["m"], ainF_g=ainF_g, ainH_g=ainH_g, ogF=og[c]["ogF"], ogH=og[c]["ogH"], after_og=mk_after_og(c))
        if l % 2 == 0:
            emit_M_gdn(S, "g%d" % l, mio)
        else:
            emit_M_ml(S, "m%d" % l, mio)
        last = l == nl - 1
        tio = dict(d, hs_src=(hs0 if l == 0 else hs_loc), hs_dst=(hs_out if last else hs_loc), ogF_g=og[c]["ogF_g"], ogH_g=og[c]["ogH_g"],
                   keep=keep, gidx=(gidx4 if c == 4 else gidx2), ainF=ainF, ainH=ainH, after_ain=after_ain)
        emit_T(S, "t%d" % (l + 1), 512 * c, False, last, tio)
    S.finish([hs_out])
    return nc, es


def kernel(x, meta_tokens, norm_w, gdn_w_in, gdn_conv_w, gdn_a_log, gdn_dt_bias, gdn_norm_w, gdn_w_out,
           ml_w_in, ml_gate_b, ml_norm_w, ml_w_out, ffn_w_up, ffn_conv_w, ffn_conv_b, ffn_w_down, _nl=4):
    inp = dict(x=x, meta_tokens=meta_tokens, norm_w=norm_w, gdn_w_in=gdn_w_in, gdn_conv_w=gdn_conv_w, gdn_a_log=gdn_a_log,
               gdn_dt_bias=gdn_dt_bias, gdn_norm_w=gdn_norm_w, gdn_w_out=gdn_w_out, ml_w_in=ml_w_in, ml_gate_b=ml_gate_b,
               ml_norm_w=ml_norm_w, ml_w_out=ml_w_out, ffn_w_up=ffn_w_up, ffn_conv_w=ffn_conv_w, ffn_conv_b=ffn_conv_b,
               ffn_w_down=ffn_w_down)
    inp = {k: np.asarray(v, np.float32) for k, v in inp.items()}
    shared = {"cst_g": gdn_consts(), "cst_m": ml_consts(),
              "nwT_0": np.ascontiguousarray(np.stack([_cm(inp["norm_w"][0, 0])] * 4, 1).reshape(128, 32), np.float32)}
    for l in range(_nl):
        t = prep_T(inp, l)
        for k, v in t.items():
            shared["%s_l%d" % (k, l)] = v
    maps = []
    for c in range(8):
        b, r = c // 4, c % 4
        m = dict(shared)
        h = np.zeros((LP, D), np.float32)
        h[XPAD + 48:XPAD + 64] = inp["meta_tokens"]
        h[XPAD + 64:] = inp["x"][b]
        lo = XPAD + 2048 * r
        m["hs0"] = np.ascontiguousarray(h[lo:lo + WIN].T)
        k = np.ones((1, WIN), np.float32)
        if r == 0:
            k[0, :48] = 0.0
        m["keep"] = k
        p = np.arange(128)
        for cc in (4, 2):
            tpc = 8 // cc
            gi = np.zeros((128, 20), np.int32)
            for hg in range(4):
                gi[:, hg * 5] = hg * 512 + r * 128 + p
                for i in range(1, 5):
                    wt = 4 * r + i - 1
                    gi[:, hg * 5 + i] = (wt // tpc) * (4 * tpc * 128) + hg * (tpc * 128) + (wt % tpc) * 128 + p
            m["gidx%d" % cc] = gi
        for l in range(_nl):
            if l % 2 == 0:
                g = prep_M_gdn(inp, l // 2, r)
                m["gw_l%d" % l], m["gcw_l%d" % l], m["ghv_l%d" % l], m["ggnw_l%d" % l] = g["w"], g["cw"], g["hv"], g["gnw"]
            else:
                g = prep_M_ml(inp, l // 2, r)
                m["mw_l%d" % l], m["mgb_l%d" % l], m["mnwv_l%d" % l] = g["w"], g["gb"], g["nwv"]
        maps.append(m)
    if _nl == 1:
        shared.pop("cst_m")
        for m in maps:
            m.pop("cst_m", None)
            m.pop("gidx2", None)
    nc, es = build_fused(_nl)
    res = run_bass_kernel_spmd(nc, maps, core_ids=list(range(8))).results
    out = np.zeros((NB, SEQ, D), np.float32)
    for c in range(8):
        b, r = c // 4, c % 4
        out[b, 2048 * r:2048 * (r + 1)] = res[c]["hs_out"][:, 64:].T
    return out
```

```python
from contextlib import ExitStack
import numpy as np
import ml_dtypes
import concourse.bass as bass
import concourse.mybir as mybir
from concourse.bass_utils import run_bass_kernel_spmd

F32 = mybir.dt.float32
BF16 = mybir.dt.bfloat16
AF = mybir.ActivationFunctionType
ALU = mybir.AluOpType
AX = mybir.AxisListType

D = 1024
SEQ = 8192
NB = 2
LP = 8704
XPAD = LP - SEQ - 64
WIN = 64 + 2048
FFN = 2816
NG = FFN // 128
RMS_EPS = 1e-6
CH = 64
NCH = LP // CH
TS = 512
NTS = LP // TS
CPT = TS // CH


class Buf:
    __slots__ = ("t", "lw", "rd", "sem", "semv", "name")

    def __init__(self, t, name=""):
        self.t = t
        self.lw = None
        self.rd = {}
        self.sem = None
        self.semv = 0
        self.name = name

    def __getitem__(self, k):
        return self.t[k]


class Sched:
    def __init__(self, nc, es):
        self.nc = nc
        self.es = es
        self.eng = {"pe": nc.tensor, "act": nc.scalar, "dve": nc.vector, "pool": nc.gpsimd, "sp": nc.sync}
        self.sem = {k: es.enter_context(nc.semaphore("sem_" + k)) for k in self.eng}
        self.cnt = {k: 0 for k in self.eng}
        self.seen = {k: {} for k in self.eng}
        self.nsem = 0
        self.out_events = []
        self.ninst = 0
        self.scopes = []
        self.dsems = []
        self.free_dsems = []
        self.scope_bufs = []

    def push(self, tag):
        self.scopes.append((ExitStack(), tag))
        self.scope_bufs.append([])

    def _own_sem(self, own):
        if own.sem is None:
            if self.free_dsems:
                own.sem, own.semv = self.free_dsems.pop()
            else:
                own.sem = self.es.enter_context(self.nc.semaphore("dsem%d" % self.nsem))
                self.nsem += 1
            self.dsems.append(own)

    def pop(self):
        self.barrier()
        st, _ = self.scopes.pop()
        st.close()
        for b in self.scope_bufs.pop():
            if b.sem is not None:
                self.free_dsems.append((b.sem, b.semv))
                self.dsems.remove(b)
                b.sem = None

    def _scope(self):
        return self.scopes[-1] if self.scopes else (self.es, "g")

    def sb(self, name, shape, dt):
        st, tag = self._scope()
        name = tag + "_" + name
        b = Buf(st.enter_context(self.nc.sbuf_tensor(name, list(shape), dt)), name)
        if self.scope_bufs:
            self.scope_bufs[-1].append(b)
        return b

    def ps(self, name, shape, dt=F32):
        st, tag = self._scope()
        name = tag + "_" + name
        return Buf(st.enter_context(self.nc.psum_tensor(name, list(shape), dt)), name)

    def barrier(self):
        for e in self.eng:
            eng = self.eng[e]
            for k in ("pe", "act", "dve", "pool", "sp"):
                if k != e and self.cnt[k] and self.seen[e].get(k, 0) < self.cnt[k]:
                    eng.wait_ge(self.sem[k], self.cnt[k])
                    self.seen[e][k] = self.cnt[k]
            for b in self.dsems:
                key = "d_" + b.name
                if self.seen[e].get(key, 0) < b.semv:
                    eng.wait_ge(b.sem, b.semv)
                    self.seen[e][key] = b.semv

    def dram(self, name, shape, dt, kind="Internal"):
        t = self.nc.dram_tensor(name, list(shape), dt, kind=kind)
        return Buf(t.ap(), name)

    def _deps(self, reads, writes):
        deps = {}

        def add(ev):
            if ev is None:
                return
            sem, val, key = ev
            if key not in deps or deps[key][1] < val:
                deps[key] = (sem, val)

        for b in reads:
            add(b.lw)
        for b in writes:
            add(b.lw)
            for ev in b.rd.values():
                add(ev)
        return deps

    def _wait(self, e, deps):
        eng = self.eng[e]
        for key, (sem, val) in deps.items():
            if e == "pe" and key == "pe":
                continue
            if self.seen[e].get(key, 0) >= val:
                continue
            eng.wait_ge(sem, val)
            self.seen[e][key] = val

    def _record(self, ev, reads, writes):
        for b in writes:
            b.lw = ev
            b.rd = {}
        for b in reads:
            if b not in writes:
                b.rd[ev[2]] = ev

    def op(self, e, fn, reads=(), writes=()):
        self._wait(e, self._deps(reads, writes))
        ins = fn(self.eng[e])
        self.cnt[e] += 1
        ins.then_inc(self.sem[e], 1)
        self.ninst += 1
        self._record((self.sem[e], self.cnt[e], e), reads, writes)

    def dma(self, q, out, in_, reads=(), writes=(), owner=None):
        self._wait(q, self._deps(reads, writes))
        own = owner if owner is not None else (writes[0] if writes else reads[0])
        self._own_sem(own)
        own.semv += 16
        ins = self.eng[q].dma_start(out=out, in_=in_)
        ins.then_inc(own.sem, 16)
        self.ninst += 1
        ev = (own.sem, own.semv, "d_" + own.name)
        self._record(ev, reads, writes)
        return ev

    def gather(self, out_ap, table_ap, idx_ap, reads=(), writes=()):
        self._wait("pool", self._deps(reads, writes))
        own = writes[0]
        self._own_sem(own)
        own.semv += 16
        ins = self.nc.gpsimd.indirect_dma_start(out=out_ap, out_offset=None, in_=table_ap,
                                                in_offset=bass.IndirectOffsetOnAxis(ap=idx_ap, axis=0))
        ins.then_inc(own.sem, 16)
        self.ninst += 1
        self._record((own.sem, own.semv, "d_" + own.name), reads, writes)

    def coll(self, kind, out, in_, groups, extra=()):
        self._wait("pool", self._deps([in_] + list(extra), [out]))
        self._own_sem(out)
        out.semv += 1
        ins = self.nc.gpsimd.collective_compute(kind, ALU.bypass, replica_groups=groups, ins=[in_.t.opt()], outs=[out.t.opt()])
        ins.then_inc(out.sem, 1)
        self.ninst += 1
        self._record((out.sem, out.semv, "d_" + out.name), [in_], [out])

    def finish(self, bufs):
        for b in bufs:
            if b.sem is not None:
                self.eng["sp"].wait_ge(b.sem, b.semv)
        for k in ("pe", "act", "dve", "pool"):
            if self.cnt[k]:
                self.eng["sp"].wait_ge(self.sem[k], self.cnt[k])


class PsumPool:
    def __init__(self, S, n, prefix="pb"):
        self.banks = [S.ps("%s%d" % (prefix, i), [128, 512]) for i in range(n)]
        self.i = 0

    def get(self):
        b = self.banks[self.i % len(self.banks)]
        self.i += 1
        return b


def bcast_mid(ap2, n):
    return ap2.unsqueeze(1).broadcast_to([ap2.shape[0], n, ap2.shape[1]])


def bcast_last(ap2, n):
    return ap2.unsqueeze(2).broadcast_to([ap2.shape[0], ap2.shape[1], n])


def rstd_from_ss(S, ss_ps, out_sb, scale, eps, W):
    S.op("act", lambda e: e.activation(out=out_sb[:, :W], in_=ss_ps[:, :W], func=AF.Ln, scale=scale, bias=eps),
         reads=[ss_ps], writes=[out_sb])
    S.op("act", lambda e: e.activation(out=out_sb[:, :W], in_=out_sb[:, :W], func=AF.Exp, scale=-0.5),
         reads=[out_sb], writes=[out_sb])


T_TILES = [(0, 64), (64, 512), (576, 512), (1088, 512), (1600, 512)]


def emit_casts(S, io):
    for m in range(8):
        S.dma("pool", io["wout_b"][m], io["wout_d"][m], writes=[io["wout_b"]])
    for g in range(NG):
        S.dma("pool", io["wup_b"][g], io["wup_d"][g], writes=[io["wup_b"]])
    for m in range(8):
        S.dma("pool", io["wdn_b"][m], io["wdn_d"][m], writes=[io["wdn_b"]])


def emit_T(S, tag, KO, first, last, io):
    S.push(tag)
    KC = KO // 128
    c_og = KC // 4
    hs_in = io["hs_src"]
    nwT_d = io["nwT"]
    if not last:
        ainF, ainH = io["ainF"], io["ainH"]
    if not first:
        ogF_g, ogH_g = io["ogF_g"], io["ogH_g"]
        keep_d, cw_d, cb_d = io["keep"], io["cw"], io["cb"]
        wout_b, wup_b, wdn_b = io["wout_b"], io["wup_b"], io["wdn_b"]
        hs_out = io["hs_dst"]
        gidx = S.sb("gidx", [128, 20], mybir.dt.int32)
        S.dma("sp", gidx[:], io["gidx"][:, :], writes=[gidx])

    ones_f = S.sb("ones_f", [128, 128], F32)
    ones_b = S.sb("ones_b", [128, 128], BF16)
    S.op("pool", lambda e: e.memset(ones_f[:], 1.0), writes=[ones_f])
    S.op("act", lambda e: e.activation(out=ones_b[:], in_=ones_f[:], func=AF.Copy), reads=[ones_f], writes=[ones_b])
    nwT = S.sb("nwT_sb", [128, 4, 8], F32)
    S.dma("sp", nwT[:].rearrange("p a b -> p (a b)"), nwT_d[:, :], writes=[nwT])

    hs_sb = [S.sb("hs_sb%d" % i, [128, 8, 512], F32) for i in range(2)]
    sq_sb = [S.sb("sq_sb%d" % i, [128, 512], BF16) for i in range(2)]
    rstd = S.sb("rstd", [128, 512], F32)
    a_sb = S.sb("a_sb", [128, 8, 512], BF16)
    pp = PsumPool(S, 7)
    ss_ps = S.ps("ss_ps", [128, 512])
    if not first:
        og_sb = [S.sb("og_sb%d" % i, [128, KC, 512], BF16) for i in range(2)]
        ogh_sb = S.sb("ogh_sb", [128, KC, 64], BF16)
        keep_sb = S.sb("keep_sb", [128, WIN], F32)
        S.dma("sp", keep_sb[:], keep_d[0:1, :].partition_broadcast(128), writes=[keep_sb])
        cw = S.sb("cw_sb", [128, 44, 3], F32)
        cb = S.sb("cb_sb", [128, 44], F32)
        S.dma("sp", cw[:].rearrange("p a b -> p (a b)"), cw_d[:, :], writes=[cw])
        S.dma("sp", cb[:], cb_d[:, :], writes=[cb])
        mix_sb = S.sb("mix_sb", [128, 8, 512], F32)
        rk = S.sb("rk", [128, 512], F32)
        tmp_sb = [S.sb("tmp_sb%d" % i, [128, 512], F32) for i in range(2)]
        h_sb = S.sb("h_sb", [128, NG, 512], BF16)
        u_sb = [S.sb("u_sb%d" % i, [128, 2, 2 + 512], F32) for i in range(2)]
        y_sb = [S.sb("y_sb%d" % i, [128, 2, 512], F32) for i in range(2)]
        e_sb = [S.sb("e_sb%d" % i, [128, 512], F32) for i in range(2)]
        halo = S.sb("halo", [128, 44, 2], F32)
        S.op("pool", lambda e: e.memset(halo[:], 0.0), writes=[halo])
        wo_s = [S.sb("wo_s%d" % i, [128, KC, 128], BF16) for i in range(2)]
        wu_s = [S.sb("wu_s%d" % i, [128, 8, 256], BF16) for i in range(3)]
        wd_s = [S.sb("wd_s%d" % i, [128, NG, 128], BF16) for i in range(2)]

    def norm_ss(src, W, eng_sq="act"):
        for m in range(8):
            sq = sq_sb[m % 2]
            S.op(eng_sq, lambda e: e.activation(out=sq[:, :W], in_=src[:, m, :W], func=AF.Square),
                 reads=[src], writes=[sq])
            S.op("pe", lambda e: e.matmul(ss_ps[:, :W], ones_b[:], sq[:, :W], start=(m == 0), stop=(m == 7)),
                 reads=[ones_b, sq], writes=[ss_ps])

    def load_tile(i):
        t0, W = T_TILES[i]
        hb = hs_sb[i % 2]
        S.dma("sp", hb[:, :, :W], hs_in[:, t0:t0 + W].rearrange("(c p) t -> p c t", p=128), writes=[hb])
        if not first:
            ob = ogh_sb if i == 0 else og_sb[i % 2]
            tab = ogH_g if i == 0 else ogF_g
            for hg in range(4):
                S.gather(ob[:, hg * c_og:(hg + 1) * c_og, :].rearrange("p c w -> p (c w)"), tab[:, :],
                         gidx[:, hg * 5 + i:hg * 5 + i + 1], reads=[gidx], writes=[ob])

    load_tile(0)
    for ti, (t0, W) in enumerate(T_TILES):
        if ti + 1 < len(T_TILES):
            load_tile(ti + 1)
        hb = hs_sb[ti % 2]
        if not first:
            ob = ogh_sb if ti == 0 else og_sb[ti % 2]
            S.dma("sp", wo_s[0][:].rearrange("p a b -> p (a b)"), wout_b[0], reads=[wout_b], writes=[wo_s[0]])
            for m in range(8):
                if m + 1 < 8:
                    S.dma("sp", wo_s[(m + 1) % 2][:].rearrange("p a b -> p (a b)"), wout_b[m + 1],
                          reads=[wout_b], writes=[wo_s[(m + 1) % 2]])
                ws = wo_s[m % 2]
                pb = pp.get()
                for kc in range(KC):
                    S.op("pe", lambda e: e.matmul(pb[:, :W], ws[:, kc, :], ob[:, kc, :W], start=(kc == 0), stop=(kc == KC - 1)),
                         reads=[ws, ob], writes=[pb])
                S.op("act", lambda e: e.activation(out=mix_sb[:, m, :W], in_=pb[:, :W], func=AF.Copy), reads=[pb], writes=[mix_sb])
                sq = sq_sb[m % 2]
                S.op("act", lambda e: e.activation(out=sq[:, :W], in_=pb[:, :W], func=AF.Square), reads=[pb], writes=[sq])
                S.op("pe", lambda e: e.matmul(ss_ps[:, :W], ones_b[:], sq[:, :W], start=(m == 0), stop=(m == 7)),
                     reads=[ones_b, sq], writes=[ss_ps])
            rstd_from_ss(S, ss_ps, rstd, 1.0 / D, RMS_EPS, W)
            S.op("dve", lambda e: e.tensor_tensor(out=rk[:, :W], in0=rstd[:, :W], in1=keep_sb[:, t0:t0 + W], op=ALU.mult),
                 reads=[rstd, keep_sb], writes=[rk])
            for m in range(8):
                tb = tmp_sb[m % 2]
                S.op("dve", lambda e: e.tensor_tensor(out=tb[:, :W], in0=mix_sb[:, m, :W], in1=rk[:, :W], op=ALU.mult),
                     reads=[mix_sb, rk], writes=[tb])
                S.op("dve", lambda e: e.scalar_tensor_tensor(out=hb[:, m, :W], in0=tb[:, :W], scalar=nwT[:, 1, m:m + 1],
                                                             in1=hb[:, m, :W], op0=ALU.mult, op1=ALU.add),
                     reads=[tb, nwT, hb], writes=[hb])
            norm_ss(hb, W)
            rstd_from_ss(S, ss_ps, rstd, 1.0 / D, RMS_EPS, W)
            for m in range(8):
                S.op("dve", lambda e: e.scalar_tensor_tensor(out=a_sb[:, m, :W], in0=hb[:, m, :W], scalar=nwT[:, 2, m:m + 1],
                                                             in1=rstd[:, :W], op0=ALU.mult, op1=ALU.mult),
                     reads=[hb, nwT, rstd], writes=[a_sb])
            S.dma("sp", wu_s[0][:].rearrange("p a b -> p (a b)"), wup_b[0], reads=[wup_b], writes=[wu_s[0]])
            S.dma("sp", wu_s[1][:].rearrange("p a b -> p (a b)"), wup_b[1], reads=[wup_b], writes=[wu_s[1]])
            for g in range(NG):
                if g + 2 < NG:
                    S.dma("sp", wu_s[(g + 2) % 3][:].rearrange("p a b -> p (a b)"), wup_b[g + 2],
                          reads=[wup_b], writes=[wu_s[(g + 2) % 3]])
                ws = wu_s[g % 3]
                ub = u_sb[g % 2]
                yb = y_sb[g % 2]
                eb = e_sb[g % 2]
                for hf in range(2):
                    ci = g + hf * NG
                    pb = pp.get()
                    for kc in range(8):
                        S.op("pe", lambda e: e.matmul(pb[:, :W], ws[:, kc, hf * 128:(hf + 1) * 128], a_sb[:, kc, :W],
                                                      start=(kc == 0), stop=(kc == 7)),
                             reads=[ws, a_sb], writes=[pb])
                    S.op("pool", lambda e: e.tensor_copy(out=ub[:, hf, 0:2], in_=halo[:, ci, :]), reads=[halo], writes=[ub])
                    S.op("act", lambda e: e.activation(out=ub[:, hf, 2:2 + W], in_=pb[:, :W], func=AF.Copy), reads=[pb], writes=[ub])
                    S.op("pool", lambda e: e.tensor_copy(out=halo[:, ci, :], in_=ub[:, hf, W:W + 2]), reads=[ub], writes=[halo])
                    S.op("act", lambda e: e.activation(out=yb[:, hf, :W], in_=pb[:, :W], func=AF.Identity,
                                                       scale=cw[:, ci, 2:3], bias=cb[:, ci:ci + 1]),
                         reads=[pb, cw, cb], writes=[yb])
                    S.op("dve", lambda e: e.scalar_tensor_tensor(out=yb[:, hf, :W], in0=ub[:, hf, 1:1 + W], scalar=cw[:, ci, 1:2],
                                                                 in1=yb[:, hf, :W], op0=ALU.mult, op1=ALU.add),
                         reads=[ub, cw, yb], writes=[yb])
                    S.op("dve", lambda e: e.scalar_tensor_tensor(out=yb[:, hf, :W], in0=ub[:, hf, 0:W], scalar=cw[:, ci, 0:1],
                                                                 in1=yb[:, hf, :W], op0=ALU.mult, op1=ALU.add),
                         reads=[ub, cw, yb], writes=[yb])
                S.op("act", lambda e: e.activation(out=eb[:, :W], in_=yb[:, 0, :W], func=AF.Silu), reads=[yb], writes=[eb])
                S.op("dve", lambda e: e.tensor_tensor(out=h_sb[:, g, :W], in0=yb[:, 1, :W], in1=eb[:, :W], op=ALU.mult),
                     reads=[yb, eb], writes=[h_sb])
            S.dma("sp", wd_s[0][:].rearrange("p a b -> p (a b)"), wdn_b[0], reads=[wdn_b], writes=[wd_s[0]])
            for m in range(8):
                if m + 1 < 8:
                    S.dma("sp", wd_s[(m + 1) % 2][:].rearrange("p a b -> p (a b)"), wdn_b[m + 1],
                          reads=[wdn_b], writes=[wd_s[(m + 1) % 2]])
                ws = wd_s[m % 2]
                pb = pp.get()
                for kc in range(NG):
                    S.op("pe", lambda e: e.matmul(pb[:, :W], ws[:, kc, :], h_sb[:, kc, :W], start=(kc == 0), stop=(kc == NG - 1)),
                         reads=[ws, h_sb], writes=[pb])
                S.op("act", lambda e: e.activation(out=mix_sb[:, m, :W], in_=pb[:, :W], func=AF.Copy), reads=[pb], writes=[mix_sb])
                sq = sq_sb[m % 2]
                S.op("act", lambda e: e.activation(out=sq[:, :W], in_=pb[:, :W], func=AF.Square), reads=[pb], writes=[sq])
                S.op("pe", lambda e: e.matmul(ss_ps[:, :W], ones_b[:], sq[:, :W], start=(m == 0), stop=(m == 7)),
                     reads=[ones_b, sq], writes=[ss_ps])
            rstd_from_ss(S, ss_ps, rstd, 1.0 / D, RMS_EPS, W)
            S.op("dve", lambda e: e.tensor_tensor(out=rk[:, :W], in0=rstd[:, :W], in1=keep_sb[:, t0:t0 + W], op=ALU.mult),
                 reads=[rstd, keep_sb], writes=[rk])
            for m in range(8):
                tb = tmp_sb[m % 2]
                S.op("dve", lambda e: e.tensor_tensor(out=tb[:, :W], in0=mix_sb[:, m, :W], in1=rk[:, :W], op=ALU.mult),
                     reads=[mix_sb, rk], writes=[tb])
                S.op("dve", lambda e: e.scalar_tensor_tensor(out=hb[:, m, :W], in0=tb[:, :W], scalar=nwT[:, 3, m:m + 1],
                                                             in1=hb[:, m, :W], op0=ALU.mult, op1=ALU.add),
                     reads=[tb, nwT, hb], writes=[hb])
            S.dma("pool", hs_out[:, t0:t0 + W].rearrange("(c p) t -> p c t", p=128), hb[:, :, :W], reads=[hb], owner=hs_out)
        if not last:
            norm_ss(hb, W)
            rstd_from_ss(S, ss_ps, rstd, 1.0 / D, RMS_EPS, W)
            for m in range(8):
                S.op("dve", lambda e: e.scalar_tensor_tensor(out=a_sb[:, m, :W], in0=hb[:, m, :W], scalar=nwT[:, 0, m:m + 1],
                                                             in1=rstd[:, :W], op0=ALU.mult, op1=ALU.mult),
                     reads=[hb, nwT, rstd], writes=[a_sb])
            if ti == 0:
                S.dma("pool", ainH[:, :].rearrange("p (c t) -> p c t", c=8), a_sb[:, :, :W], reads=[a_sb], writes=[ainH])
            else:
                S.dma("pool", ainF[ti - 1][:, :].rearrange("p (c t) -> p c t", c=8), a_sb[:, :, :W], reads=[a_sb], writes=[ainF[ti - 1]])
            io["after_ain"](ti)
    S.pop()


def _cm(v):
    return np.ascontiguousarray(v.reshape(-1, 128).T)


def prep_T(inp, layer):
    j = layer // 2
    if layer % 2 == 0:
        w_out = inp["gdn_w_out"][j]
    else:
        w_out = inp["ml_w_out"][j]
    KO = w_out.shape[0]
    KC = KO // 128
    nw = inp["norm_w"]
    nxt = nw[layer + 1, 0] if layer + 1 < 4 else nw[layer, 0]
    nwT = np.stack([_cm(nxt), _cm(nw[layer, 1]), _cm(nw[layer, 2]), _cm(nw[layer, 3])], 1)
    wout = w_out.reshape(KC, 128, 8, 128).transpose(2, 1, 0, 3)
    wu = inp["ffn_w_up"][layer].reshape(8, 128, 2, NG, 128).transpose(3, 1, 0, 2, 4)
    wd = inp["ffn_w_down"][layer].reshape(NG, 128, 8, 128).transpose(2, 1, 0, 3)
    cw = inp["ffn_conv_w"][layer].reshape(3, 44, 128).transpose(2, 1, 0)
    cb = _cm(inp["ffn_conv_b"][layer])
    return {
        "nwT": np.ascontiguousarray(nwT.reshape(128, 32), np.float32),
        "wout": np.ascontiguousarray(wout.reshape(8, 128, KC * 128), np.float32),
        "wup": np.ascontiguousarray(wu.reshape(NG, 128, 8 * 256), np.float32),
        "wdn": np.ascontiguousarray(wd.reshape(8, 128, NG * 128), np.float32),
        "cw": np.ascontiguousarray(cw.reshape(128, 44 * 3), np.float32),
        "cb": np.ascontiguousarray(cb, np.float32),
    }


ML_DK = 128
ML_DV = 256
ML_WC = 128 + 128 + 128 + 256 + 256 + 2
BIGNEG = 30000.0


def emit_M_ml(S, tag, io):
    S.push(tag)
    ainF_g, ainH_g = io["ainF_g"], io["ainH_g"]
    w_d, gb_d, nwv_d, cst_d = io["w"], io["gb"], io["nwv"], io["cst"]
    ogF, ogH = io["ogF"], io["ogH"]
    c_og = 2

    w_sb = S.sb("w_sb", [128, 8, ML_WC], BF16)
    S.dma("pool", w_sb[:].rearrange("p a b -> p (a b)"), w_d[:, :], writes=[w_sb])
    wg_sb = S.sb("wg_sb", [128, 8, 2], BF16)
    cst = S.sb("cst_sb", [128, 64 + 512 + 128], F32)
    S.dma("sp", cst[:], cst_d[:, :], writes=[cst])
    identf = cst
    ident_b = S.sb("ident_b", [128, 128], BF16)
    S.op("act", lambda e: e.activation(out=ident_b[:], in_=cst[:, 576:704], func=AF.Copy), reads=[cst], writes=[ident_b])
    gb = S.sb("gb_sb", [1, 2], F32)
    S.dma("sp", gb[:], gb_d[:, :], writes=[gb])
    nwv = S.sb("nwv_sb", [64, ML_DV], F32)
    S.dma("sp", nwv[:], nwv_d[0:1, :].partition_broadcast(64), writes=[nwv])
    ones_row = S.sb("ones_row", [1, 128], F32)
    S.op("pool", lambda e: e.memset(ones_row[:], 1.0), writes=[ones_row])

    a_sb = [S.sb("a_sb%d" % i, [128, 8, 512], BF16) for i in range(2)]
    pp = PsumPool(S, 7)

    li_row = S.sb("li_row", [1, LP], F32)
    lf_row = S.sb("lf_row", [1, LP], F32)
    bb_row = S.sb("bb_row", [1, LP], F32)
    ones_bc = ones_row[0:1, 0:1].broadcast_to([1, LP])

    def load_a(i):
        ab = a_sb[i % 2]
        if i == 0:
            S.op("pool", lambda e: e.memset(ab[:, :, 0:XPAD], 0.0), writes=[ab])
            S.dma("sp", ab[:, :, XPAD:TS], ainH_g[0:128, :].rearrange("p (c t) -> p c t", c=8), reads=[ainH_g], writes=[ab])
        else:
            r0 = ((i - 1) % 4) * 512 + ((i - 1) // 4) * 128
            S.dma("sp", ab[:].rearrange("p c t -> p (c t)"), ainF_g[r0:r0 + 128, :], reads=[ainF_g], writes=[ab])

    load_a(0)
    for ti in range(NTS):
        if ti + 1 < NTS:
            load_a(ti + 1)
        ab = a_sb[ti % 2]
        for gi_, row in ((0, li_row), (1, lf_row)):
            pr = pp.get()
            for kc in range(8):
                S.op("pe", lambda e: e.matmul(pr[0:1, :], w_sb[:, kc, ML_WC - 2 + gi_:ML_WC - 1 + gi_], ab[:, kc, :],
                                              start=(kc == 0), stop=(kc == 7)), reads=[w_sb, ab], writes=[pr])
            S.op("act", lambda e: e.activation(out=row[:, ti * TS:(ti + 1) * TS], in_=pr[0:1, :], func=AF.Identity,
                                               bias=gb[:, gi_:gi_ + 1], scale=1.0), reads=[pr, gb], writes=[row])
    for row in (li_row, lf_row):
        S.op("act", lambda e: e.activation(out=row[:], in_=row[:], func=AF.Exp, scale=2.0 / 15.0), reads=[row], writes=[row])
        S.op("dve", lambda e: e.tensor_scalar_add(out=row[:], in0=row[:], scalar1=1.0), reads=[row], writes=[row])
        S.op("dve", lambda e: e.reciprocal(out=row[:], in_=row[:]), reads=[row], writes=[row])
        S.op("dve", lambda e: e.tensor_scalar(out=row[:], in0=row[:], scalar1=-30.0, scalar2=15.0, op0=ALU.mult, op1=ALU.add),
             reads=[row], writes=[row])
    S.op("act", lambda e: e.activation(out=lf_row[:], in_=lf_row[:], func=AF.Exp, scale=-1.0), reads=[lf_row], writes=[lf_row])
    S.op("act", lambda e: e.activation(out=lf_row[:], in_=lf_row[:], func=AF.Ln, bias=1.0, scale=1.0), reads=[lf_row], writes=[lf_row])
    S.op("dve", lambda e: e.tensor_scalar_mul(out=lf_row[:], in0=lf_row[:], scalar1=-1.0), reads=[lf_row], writes=[lf_row])
    S.op("pool", lambda e: e.memset(lf_row[:, 0:XPAD], 0.0), writes=[lf_row])
    S.op("pool", lambda e: e.memset(li_row[:, 0:XPAD], -BIGNEG), writes=[li_row])
    S.op("dve", lambda e: e.tensor_tensor_scan(out=bb_row[:], data0=ones_bc, data1=lf_row[:], initial=0.0,
                                               op0=ALU.mult, op1=ALU.add), reads=[ones_row, lf_row], writes=[bb_row])
    c_row = lf_row
    S.op("dve", lambda e: e.tensor_tensor(out=c_row[:], in0=li_row[:], in1=bb_row[:], op=ALU.subtract), reads=[li_row, bb_row], writes=[c_row])
    M_row = li_row
    S.op("dve", lambda e: e.tensor_tensor_scan(out=M_row[:], data0=ones_bc, data1=c_row[:], initial=0.0,
                                               op0=ALU.mult, op1=ALU.max), reads=[ones_row, c_row], writes=[M_row])
    en_row = bb_row
    S.op("dve", lambda e: e.tensor_tensor(out=en_row[:], in0=bb_row[:], in1=M_row[:], op=ALU.add), reads=[bb_row, M_row], writes=[en_row])
    S.op("act", lambda e: e.activation(out=en_row[:], in_=en_row[:], func=AF.Exp, scale=-1.0), reads=[en_row], writes=[en_row])

    c_tm = S.sb("c_tm", [64, NCH], F32)
    M_tm = S.sb("M_tm", [64, NCH], F32)
    en_tm = S.sb("en_tm", [64, NCH], F32)
    for row, tmb in ((c_row, c_tm), (M_row, M_tm), (en_row, en_tm)):
        pb = pp.get()
        for n in range(NCH):
            S.op("pe", lambda e: e.matmul(pb[0:64, n:n + 1], row[0:1, n * CH:(n + 1) * CH], ones_row[0:1, 0:1], start=True, stop=True),
                 reads=[row, ones_row], writes=[pb])
        S.op("act", lambda e: e.activation(out=tmb[:], in_=pb[0:64, 0:NCH], func=AF.Copy), reads=[pb], writes=[tmb])
    Mend_b = S.sb("Mend_b", [128, NCH], F32)
    Mprev_b = S.sb("Mprev_b", [128, NCH], F32)
    pb = pp.get()
    S.op("pe", lambda e: e.matmul(pb[:, 0:NCH], ones_row[0:1, :], M_row[0:1, CH - 1::CH], start=True, stop=True),
         reads=[ones_row, M_row], writes=[pb])
    S.op("act", lambda e: e.activation(out=Mend_b[:], in_=pb[:, 0:NCH], func=AF.Copy), reads=[pb], writes=[Mend_b])
    S.op("pool", lambda e: e.memset(Mprev_b[:, 0:1], 0.0), writes=[Mprev_b])
    S.op("pool", lambda e: e.tensor_copy(out=Mprev_b[:, 1:NCH], in_=Mend_b[:, 0:NCH - 1]), reads=[Mend_b], writes=[Mprev_b])
    sc_tm = S.sb("sc_tm", [64, NCH], F32)
    kws_tm = S.sb("kws_tm", [64, NCH], F32)
    dec_b = S.sb("dec_b", [128, NCH], F32)
    S.op("dve", lambda e: e.tensor_tensor(out=sc_tm[:], in0=Mprev_b[0:64, :], in1=M_tm[:], op=ALU.subtract), reads=[Mprev_b, M_tm], writes=[sc_tm])
    S.op("act", lambda e: e.activation(out=sc_tm[:], in_=sc_tm[:], func=AF.Exp), reads=[sc_tm], writes=[sc_tm])
    S.op("dve", lambda e: e.tensor_tensor(out=kws_tm[:], in0=c_tm[:], in1=Mend_b[0:64, :], op=ALU.subtract), reads=[c_tm, Mend_b], writes=[kws_tm])
    S.op("act", lambda e: e.activation(out=kws_tm[:], in_=kws_tm[:], func=AF.Exp), reads=[kws_tm], writes=[kws_tm])
    S.op("dve", lambda e: e.tensor_tensor(out=dec_b[:], in0=Mprev_b[:], in1=Mend_b[:], op=ALU.subtract), reads=[Mprev_b, Mend_b], writes=[dec_b])
    S.op("act", lambda e: e.activation(out=dec_b[:], in_=dec_b[:], func=AF.Exp), reads=[dec_b], writes=[dec_b])

    qT = [S.sb("qT%d" % i, [128, 512], BF16) for i in range(2)]
    kT = [S.sb("kT%d" % i, [128, 512], BF16) for i in range(2)]
    Wt = [S.sb("Wt%d" % i, [64, 512], F32) for i in range(2)]
    ogT = [S.sb("ogT%d" % i, [128, 2, 512], BF16) for i in range(2)]
    C_f = S.sb("C_f", [128, ML_DV + 1], F32)
    C_b = S.sb("C_b", [128, ML_DV + 1], BF16)
    S.op("pool", lambda e: e.memset(C_f[:], 0.0), writes=[C_f])
    S.op("pool", lambda e: e.memset(C_b[:], 0.0), writes=[C_b])
    NR = 3
    kw_sb = [S.sb("kw_sb%d" % i, [64, 128], BF16) for i in range(NR)]
    va_sb = [S.sb("va_sb%d" % i, [64, ML_DV + 1], BF16) for i in range(NR)]
    og_sb = [S.sb("ogs%d" % i, [64, ML_DV], F32) for i in range(NR)]
    St_sb = [S.sb("St%d" % i, [64, 64], BF16) for i in range(NR)]
    t1_sb = [S.sb("t1_%d" % i, [64, ML_DV + 1], F32) for i in range(NR)]
    sm_sb = [S.sb("sm%d" % i, [64, 8], F32) for i in range(NR)]
    junk = [S.sb("junk%d" % i, [64, ML_DV], F32) for i in range(NR)]
    hn_sb = [S.sb("hn%d" % i, [64, ML_DV], BF16) for i in range(NR)]
    for i in range(NR):
        S.op("pool", lambda e: e.memset(va_sb[i][:, ML_DV:ML_DV + 1], 1.0), writes=[va_sb[i]])
    pTall = S.ps("pTall", [128, 1024], BF16)

    load_a(0)
    for ti in range(NTS):
        if ti + 1 < NTS:
            load_a(ti + 1)
        ab = a_sb[ti % 2]
        t0 = ti * TS
        qTb, kTb, Wtb, ogTb = qT[ti % 2], kT[ti % 2], Wt[ti % 2], ogT[ti % 2]
        for wi, (dst, scl) in enumerate(((qTb, ML_DK ** -0.5), (kTb, 1.0))):
            pb = pp.get()
            for kc in range(8):
                S.op("pe", lambda e: e.matmul(pb[:, :], w_sb[:, kc, wi * 128:(wi + 1) * 128], ab[:, kc, :], start=(kc == 0), stop=(kc == 7)),
                     reads=[w_sb, ab], writes=[pb])
            S.op("act", lambda e: e.activation(out=dst[:], in_=pb[:, :], func=AF.Copy, scale=scl), reads=[pb], writes=[dst])
        pb = pp.get()
        S.op("pe", lambda e: e.matmul(pb[0:64, :], ones_row[0:1, 0:64], M_row[0:1, t0:t0 + TS], start=True, stop=False),
             reads=[ones_row, M_row], writes=[pb])
        S.op("pe", lambda e: e.matmul(pb[0:64, :], cst[0:64, 0:64], cst[0:64, 64:576], start=False, stop=True),
             reads=[cst], writes=[pb])
        S.op("dve", lambda e: e.tensor_tensor(out=Wtb[:].rearrange("p (n i) -> p n i", n=CPT), in0=pb[0:64, :].rearrange("p (n i) -> p n i", n=CPT),
                                              in1=bcast_last(c_tm[:, ti * CPT:(ti + 1) * CPT], CH), op=ALU.subtract),
             reads=[pb, c_tm], writes=[Wtb])
        S.op("act", lambda e: e.activation(out=Wtb[:], in_=Wtb[:], func=AF.Exp, scale=-1.0), reads=[Wtb], writes=[Wtb])
        for cj in range(CPT):
            n = ti * CPT + cj
            r = n % NR
            c0 = cj * CH
            pkv = pp.get()
            for kc in range(8):
                S.op("pe", lambda e: e.matmul(pkv[0:64, 0:384], ab[:, kc, c0:c0 + CH], w_sb[:, kc, 256:640], start=(kc == 0), stop=(kc == 7)),
                     reads=[w_sb, ab], writes=[pkv])
            pog = pp.get()
            for kc in range(8):
                S.op("pe", lambda e: e.matmul(pog[0:64, 0:256], ab[:, kc, c0:c0 + CH], w_sb[:, kc, 640:896], start=(kc == 0), stop=(kc == 7)),
                     reads=[w_sb, ab], writes=[pog])
            S.op("act", lambda e: e.activation(out=kw_sb[r][:], in_=pkv[0:64, 0:128], func=AF.Copy, scale=kws_tm[:, n:n + 1]),
                 reads=[pkv, kws_tm], writes=[kw_sb[r]])
            S.op("act", lambda e: e.activation(out=va_sb[r][:, 0:ML_DV], in_=pkv[0:64, 128:384], func=AF.Copy), reads=[pkv], writes=[va_sb[r]])
            S.op("act", lambda e: e.activation(out=og_sb[r][:], in_=pog[0:64, 0:256], func=AF.Exp, scale=-1.0), reads=[pog], writes=[og_sb[r]])
            S.op("pool", lambda e: e.tensor_scalar_add(out=og_sb[r][:], in0=og_sb[r][:], scalar1=1.0), reads=[og_sb[r]], writes=[og_sb[r]])
            S.op("dve", lambda e: e.reciprocal(out=og_sb[r][:], in_=og_sb[r][:]), reads=[og_sb[r]], writes=[og_sb[r]])
            S.op("dve", lambda e: e.tensor_tensor(out=og_sb[r][:], in0=og_sb[r][:], in1=nwv[:], op=ALU.mult), reads=[og_sb[r], nwv], writes=[og_sb[r]])
            pq = pp.get()
            S.op("pe", lambda e: e.matmul(pq[0:64, 0:64], kTb[:, c0:c0 + CH], qTb[:, c0:c0 + CH], start=True, stop=True),
                 reads=[kTb, qTb], writes=[pq])
            S.op("dve", lambda e: e.tensor_tensor(out=St_sb[r][:], in0=pq[0:64, 0:64], in1=Wtb[:, c0:c0 + CH], op=ALU.mult),
                 reads=[pq, Wtb], writes=[St_sb[r]])
            pint = pp.get()
            S.op("pe", lambda e: e.matmul(pint[0:64, 0:ML_DV + 1], qTb[:, c0:c0 + CH], C_b[:], start=True, stop=True),
                 reads=[qTb, C_b], writes=[pint])
            pia = pp.get()
            S.op("pe", lambda e: e.matmul(pia[0:64, 0:ML_DV + 1], St_sb[r][:], va_sb[r][:], start=True, stop=True),
                 reads=[St_sb[r], va_sb[r]], writes=[pia])
            pst = pp.get()
            S.op("pe", lambda e: e.matmul(pst[:, 0:ML_DV + 1], kw_sb[r][:], va_sb[r][:], start=True, stop=True),
                 reads=[kw_sb[r], va_sb[r]], writes=[pst])
            S.op("dve", lambda e: e.scalar_tensor_tensor(out=C_f[:], in0=C_f[:], scalar=dec_b[:, n:n + 1], in1=pst[:, 0:ML_DV + 1],
                                                         op0=ALU.mult, op1=ALU.add), reads=[C_f, dec_b, pst], writes=[C_f])
            S.op("act", lambda e: e.activation(out=C_b[:], in_=C_f[:], func=AF.Copy), reads=[C_f], writes=[C_b])
            t1 = t1_sb[r]
            S.op("act", lambda e: e.activation(out=t1[:], in_=pint[0:64, 0:ML_DV + 1], func=AF.Copy, scale=sc_tm[:, n:n + 1]),
                 reads=[pint, sc_tm], writes=[t1])
            S.op("dve", lambda e: e.tensor_tensor(out=t1[:], in0=t1[:], in1=pia[0:64, 0:ML_DV + 1], op=ALU.add), reads=[t1, pia], writes=[t1])
            sm = sm_sb[r]
            S.op("act", lambda e: e.activation(out=sm[:, 0:1], in_=t1[:, ML_DV:ML_DV + 1], func=AF.Abs), reads=[t1], writes=[sm])
            S.op("dve", lambda e: e.tensor_tensor(out=sm[:, 0:1], in0=sm[:, 0:1], in1=en_tm[:, n:n + 1], op=ALU.max),
                 reads=[sm, en_tm], writes=[sm])
            S.op("dve", lambda e: e.reciprocal(out=sm[:, 1:2], in_=sm[:, 0:1]), reads=[sm], writes=[sm])
            S.op("act", lambda e: e.activation(out=junk[r][:], in_=t1[:, 0:ML_DV], func=AF.Square, scale=sm[:, 1:2], accum_out=sm[:, 2:3]),
                 reads=[t1, sm], writes=[junk[r], sm])
            S.op("act", lambda e: e.activation(out=sm[:, 3:4], in_=sm[:, 2:3], func=AF.Ln, scale=1.0 / ML_DV, bias=RMS_EPS), reads=[sm], writes=[sm])
            S.op("act", lambda e: e.activation(out=sm[:, 3:4], in_=sm[:, 3:4], func=AF.Exp, scale=-0.5), reads=[sm], writes=[sm])
            S.op("dve", lambda e: e.tensor_tensor(out=sm[:, 4:5], in0=sm[:, 3:4], in1=sm[:, 1:2], op=ALU.mult), reads=[sm], writes=[sm])
            S.op("dve", lambda e: e.scalar_tensor_tensor(out=hn_sb[r][:], in0=t1[:, 0:ML_DV], scalar=sm[:, 4:5], in1=og_sb[r][:],
                                                         op0=ALU.mult, op1=ALU.mult), reads=[t1, sm, og_sb[r]], writes=[hn_sb[r]])
            ptv = pTall[:, (n % 4) * 128:(n % 4 + 1) * 128].rearrange("p (a b) -> p a b", a=2)
            for hh in range(2):
                S.op("pe", lambda e: e.transpose(ptv[:, hh, :], hn_sb[r][:, hh * 128:(hh + 1) * 128], ident_b[0:64, 0:64]),
                     reads=[hn_sb[r], ident_b], writes=[pTall])
            S.op("act", lambda e: e.activation(out=ogTb[:, :, c0:c0 + CH], in_=ptv, func=AF.Copy), reads=[pTall], writes=[ogTb])
        if ti >= 1:
            S.dma("pool", ogF[ti - 1][:, :].rearrange("p (c t) -> p c t", c=c_og), ogTb[:], reads=[ogTb], writes=[ogF[ti - 1]])
        if ti % 4 == 0 and ti <= 12:
            S.dma("pool", ogH[(ti // 4) * 128:(ti // 4 + 1) * 128, :].rearrange("p (c t) -> p c t", c=c_og), ogTb[:, :, TS - 64:TS],
                  reads=[ogTb], writes=[ogH])
        io["after_og"](ti)
    S.pop()


def prep_M_ml(inp, j, h):
    w = inp["ml_w_in"][j]
    q = w[:, h * 128:(h + 1) * 128]
    k = w[:, 512 + h * 128:512 + (h + 1) * 128]
    v = w[:, 1024 + h * 256:1024 + (h + 1) * 256]
    o = w[:, 2048 + h * 256:2048 + (h + 1) * 256]
    gi = w[:, 3072 + h:3073 + h]
    gf = w[:, 3076 + h:3077 + h]
    wc = np.concatenate([q, k, k, v, o, gi, gf], 1)
    wc = wc.reshape(8, 128, ML_WC).transpose(1, 0, 2)
    gb = inp["ml_gate_b"][j][[h, 4 + h]].reshape(1, 2)
    nwv = inp["ml_norm_w"][j][h * 256:(h + 1) * 256].reshape(1, 256)
    return {"w": np.ascontiguousarray(wc.reshape(128, 8 * ML_WC), np.float32),
            "gb": np.ascontiguousarray(gb, np.float32), "nwv": np.ascontiguousarray(nwv, np.float32),
            "cst": ml_consts()}


def ml_consts():
    c = np.zeros((128, 64 + 512 + 128), np.float32)
    c[0:64, 0:64] = np.eye(64, dtype=np.float32)
    jj = np.arange(64)[:, None]
    ii = np.arange(64)[None, :]
    mb = (jj > ii).astype(np.float32) * BIGNEG
    c[0:64, 64:576] = np.tile(mb, (1, 8))
    c[:, 576:704] = np.eye(128, dtype=np.float32)
    return c


G_WC = 12 * 128 + 8
L2_EPS = 1e-6
GDN_EPS = 1e-6
C_I64, C_MUI, C_MUS, C_MLS, C_NMLS, C_NMUS, C_I128, C_ONES = 0, 64, 128, 192, 256, 320, 384, 512
C_TOT = 640


def gdn_consts():
    c = np.zeros((128, C_TOT), np.float32)
    p = np.arange(64)[:, None]
    f = np.arange(64)[None, :]
    c[0:64, C_I64:C_I64 + 64] = (p == f)
    c[0:64, C_MUI:C_MUI + 64] = (p <= f)
    c[0:64, C_MUS:C_MUS + 64] = (p < f)
    c[0:64, C_MLS:C_MLS + 64] = (p > f)
    c[0:64, C_NMLS:C_NMLS + 64] = -1.0 * (p > f)
    c[0:64, C_NMUS:C_NMUS + 64] = -1.0 * (p < f)
    c[:, C_I128:C_I128 + 128] = np.eye(128, dtype=np.float32)
    c[:, C_ONES:C_ONES + 128] = 1.0
    return c


def emit_M_gdn(S, tag, io):
    S.push(tag)
    ainF_g, ainH_g = io["ainF_g"], io["ainH_g"]
    w_d, cw_d, hv_d, gnw_d, cst_d = io["w"], io["cw"], io["hv"], io["gnw"], io["cst"]
    ogF, ogH = io["ogF"], io["ogH"]
    c_og = 4

    w_sb = S.sb("w_sb", [128, 8, G_WC], BF16)
    S.dma("pool", w_sb[:].rearrange("p a b -> p (a b)"), w_d[:, :], writes=[w_sb])
    cst = S.sb("cst_sb", [128, C_TOT], F32)
    S.dma("sp", cst[:], cst_d[:, :], writes=[cst])
    cwg = S.sb("cwg", [128, 8, 4], F32)
    S.dma("sp", cwg[:].rearrange("p a b -> p (a b)"), cw_d[:, :], writes=[cwg])
    hv = S.sb("hv_sb", [64, 8], F32)
    S.dma("sp", hv[:], hv_d[0:1, :].partition_broadcast(64), writes=[hv])
    gnw = S.sb("gnw_sb", [128, 1], F32)
    S.dma("sp", gnw[:], gnw_d[:, :], writes=[gnw])
    ident_b = S.sb("ident_b", [128, 128], BF16)
    ones_b = S.sb("ones_b", [128, 128], BF16)
    S.op("act", lambda e: e.activation(out=ident_b[:], in_=cst[:, C_I128:C_I128 + 128], func=AF.Copy), reads=[cst], writes=[ident_b])
    S.op("act", lambda e: e.activation(out=ones_b[:], in_=cst[:, C_ONES:C_ONES + 128], func=AF.Copy), reads=[cst], writes=[ones_b])
    dg = S.sb("dg", [128, 32, 128], BF16)
    for mc in range(8):
        for k in range(4):
            S.op("dve", lambda e: e.tensor_scalar(out=dg[:, mc * 4 + k, :], in0=cst[:, C_I128:C_I128 + 128], scalar1=cwg[:, mc, k:k + 1],
                                                  scalar2=None, op0=ALU.mult), reads=[cst, cwg], writes=[dg])
    I64 = cst[0:64, C_I64:C_I64 + 64]
    MUI = cst[0:64, C_MUI:C_MUI + 64]
    MLS = cst[0:64, C_MLS:C_MLS + 64]
    NMLS = cst[0:64, C_NMLS:C_NMLS + 64]
    NMUS = cst[0:64, C_NMUS:C_NMUS + 64]
    ONES64x128 = cst[0:64, C_ONES:C_ONES + 128]

    a_sb = [S.sb("a_sb%d" % i, [128, 8, 512], BF16) for i in range(2)]
    pp = PsumPool(S, 7)
    pTall = S.ps("pTall", [128, 1024], BF16)

    def load_a(i):
        ab = a_sb[i % 2]
        if i == 0:
            S.op("pool", lambda e: e.memset(ab[:, :, 0:XPAD], 0.0), writes=[ab])
            S.dma("sp", ab[:, :, XPAD:TS], ainH_g[0:128, :].rearrange("p (c t) -> p c t", c=8), reads=[ainH_g], writes=[ab])
        else:
            r0 = ((i - 1) % 4) * 512 + ((i - 1) // 4) * 128
            S.dma("sp", ab[:].rearrange("p c t -> p (c t)"), ainF_g[r0:r0 + 128, :], reads=[ainF_g], writes=[ab])

    NH = NCH * 4
    bg = S.sb("bg", [64, NCH, 8], F32)
    load_a(0)
    for ti in range(NTS):
        if ti + 1 < NTS:
            load_a(ti + 1)
        ab = a_sb[ti % 2]
        pb = pp.get()
        for cj in range(CPT):
            for kc in range(8):
                S.op("pe", lambda e: e.matmul(pb[0:64, cj * 8:(cj + 1) * 8], ab[:, kc, cj * CH:(cj + 1) * CH], w_sb[:, kc, 1536:1544],
                                              start=(kc == 0), stop=(kc == 7)), reads=[ab, w_sb], writes=[pb])
        S.op("act", lambda e: e.activation(out=bg[:, ti * CPT:(ti + 1) * CPT, :], in_=pb[0:64, 0:64].rearrange("p (a b) -> p a b", a=CPT),
                                           func=AF.Copy), reads=[pb], writes=[bg])
    lnb = S.sb("lnb", [64, NCH, 4], F32)
    bt = S.sb("bt", [64, NCH, 4], F32)
    gt = S.sb("gt", [64, NCH, 4], F32)
    beG = S.sb("beG", [64, NCH, 4], F32)
    ekt = S.sb("ekt", [64, NCH, 4], F32)
    eGl = S.sb("eGl", [128, NCH, 4], F32)
    tmpg = S.sb("tmpg", [64, NCH, 4], F32)
    eal = S.sb("eal", [64, 4], F32)
    S.op("act", lambda e: e.activation(out=lnb[:], in_=bg[:, :, 0:4], func=AF.Exp, scale=-1.0), reads=[bg], writes=[lnb])
    S.op("act", lambda e: e.activation(out=lnb[:], in_=lnb[:], func=AF.Ln, bias=1.0, scale=1.0), reads=[lnb], writes=[lnb])
    S.op("dve", lambda e: e.tensor_scalar_mul(out=lnb[:], in0=lnb[:], scalar1=-1.0), reads=[lnb], writes=[lnb])
    S.op("act", lambda e: e.activation(out=bt[:], in_=lnb[:], func=AF.Exp), reads=[lnb], writes=[bt])
    S.op("dve", lambda e: e.tensor_tensor(out=gt[:], in0=bg[:, :, 4:8], in1=bcast_mid(hv[:, 4:8], NCH), op=ALU.add), reads=[bg, hv], writes=[gt])
    S.op("act", lambda e: e.activation(out=gt[:], in_=gt[:], func=AF.Exp), reads=[gt], writes=[gt])
    S.op("act", lambda e: e.activation(out=gt[:], in_=gt[:], func=AF.Ln, bias=1.0, scale=1.0), reads=[gt], writes=[gt])
    S.op("act", lambda e: e.activation(out=eal[:], in_=hv[:, 0:4], func=AF.Exp), reads=[hv], writes=[eal])
    S.op("dve", lambda e: e.tensor_scalar_mul(out=eal[:], in0=eal[:], scalar1=-1.0), reads=[eal], writes=[eal])
    S.op("dve", lambda e: e.tensor_tensor(out=gt[:], in0=gt[:], in1=bcast_mid(eal[:], NCH), op=ALU.mult), reads=[gt, eal], writes=[gt])
    gflat = gt[:].rearrange("p a b -> p (a b)")
    for (c0, c1) in ((0, 272), (272, NH)):
        pb = pp.get()
        S.op("pe", lambda e: e.matmul(pb[0:64, 0:c1 - c0], MUI, gflat[:, c0:c1], start=True, stop=True), reads=[cst, gt], writes=[pb])
        pl = pp.get()
        S.op("pe", lambda e: e.matmul(pl[:, 0:c1 - c0], ONES64x128, gflat[:, c0:c1], start=True, stop=True), reads=[cst, gt], writes=[pl])
        S.op("act", lambda e: e.activation(out=tmpg[:].rearrange("p a b -> p (a b)")[:, c0:c1], in_=pb[0:64, 0:c1 - c0], func=AF.Exp),
             reads=[pb], writes=[tmpg])
        S.op("act", lambda e: e.activation(out=eGl[:].rearrange("p a b -> p (a b)")[:, c0:c1], in_=pl[:, 0:c1 - c0], func=AF.Exp),
             reads=[pl], writes=[eGl])
        S.op("act", lambda e: e.activation(out=ekt[:].rearrange("p a b -> p (a b)")[:, c0:c1], in_=pb[0:64, 0:c1 - c0], func=AF.Copy),
             reads=[pb], writes=[ekt])
        S.op("dve", lambda e: e.tensor_tensor(out=ekt[:].rearrange("p a b -> p (a b)")[:, c0:c1], in0=pl[0:64, 0:c1 - c0],
                                              in1=ekt[:].rearrange("p a b -> p (a b)")[:, c0:c1], op=ALU.subtract), reads=[pl, ekt], writes=[ekt])
    S.op("act", lambda e: e.activation(out=ekt[:], in_=ekt[:], func=AF.Exp), reads=[ekt], writes=[ekt])
    S.op("dve", lambda e: e.tensor_tensor(out=beG[:], in0=bt[:], in1=tmpg[:], op=ALU.mult), reads=[bt, tmpg], writes=[beG])

    xb = [S.sb("xb%d" % i, [128, 8, 3 + TS], BF16) for i in range(2)]
    S.op("pool", lambda e: e.memset(xb[1][:, :, TS:TS + 3], 0.0), writes=[xb[1]])
    e_sb = [S.sb("e_sb%d" % i, [128, TS], F32) for i in range(2)]
    sx = [S.sb("sx%d" % i, [128, TS], F32) for i in range(2)]
    sqb = [S.sb("sqb%d" % i, [128, TS], BF16) for i in range(2)]
    rs = [S.sb("rs%d" % i, [128, TS], F32) for i in range(2)]
    qT = [S.sb("qT%d" % i, [128, 2, TS], BF16) for i in range(2)]
    kT = [S.sb("kT%d" % i, [128, 2, TS], BF16) for i in range(2)]
    svT = [S.sb("svT%d" % i, [128, 4, TS], BF16) for i in range(2)]
    zs = [S.sb("zs0", [128, 4, TS], F32)] * 2
    onT = [S.sb("onT%d" % i, [128, 4, TS], BF16) for i in range(2)]
    ogt = [S.sb("ogt0", [128, 4, TS], BF16)] * 2
    S_f = S.sb("S_f", [128, 4, 128], F32)
    S_b = S.sb("S_b", [128, 4, 128], BF16)
    S_t = S.sb("S_t", [128, 4, 128], F32)
    S.op("pool", lambda e: e.memset(S_f[:], 0.0), writes=[S_f])
    S.op("pool", lambda e: e.memset(S_b[:], 0.0), writes=[S_b])
    NR = 2
    mk = lambda nm, shp, dt: [S.sb("%s%d" % (nm, i), shp, dt) for i in range(NR)]
    kbg = mk("kbg", [64, 4, 128], BF16)
    ktm = mk("ktm", [64, 4, 128], BF16)
    vb = mk("vb", [64, 4, 128], BF16)
    rg1 = mk("rg1", [64, 4, 64], F32)
    rg2 = mk("rg2", [64, 4, 64], F32)
    rg3 = mk("rg3", [64, 4, 64], F32)
    Et = mk("Et", [64, 4, 64], F32)
    Wt_ = mk("Wt", [64, 4, 64], F32)
    W_ = mk("W", [64, 4, 64], F32)
    eGb = mk("eGb", [128, 4, 64], F32)
    KKlo = mk("KKlo", [64, 2, 64], F32)
    KKup = mk("KKup", [64, 2, 64], F32)
    KQm = mk("KQm", [64, 2, 64], F32)
    Qt = mk("Qt", [64, 4, 64], BF16)
    qdT = mk("qdT", [128, 4, 64], BF16)
    PP = [mk("PP%d" % k, [64, 8, 64], F32) for k in range(2)]
    Xt = [mk("Xt%d" % k, [64, 4, 64], F32) for k in range(2)]
    Tt = mk("Tt", [64, 4, 64], BF16)
    nwT = mk("nwT", [128, 4, 64], BF16)
    vn = mk("vn", [64, 4, 128], BF16)
    sqo = mk("sqo", [64, 4, 128], F32)
    sso = mk("sso", [64, 8], F32)
    on = mk("on", [64, 4, 128], BF16)

    def silu_from_psum(pb, W, out_ap, out_buf, idx):
        eb = e_sb[idx % 2]
        S.op("act", lambda e: e.activation(out=eb[:, :W], in_=pb[:, :W], func=AF.Exp, scale=-1.0), reads=[pb], writes=[eb])
        S.op("pool", lambda e: e.tensor_scalar_add(out=eb[:, :W], in0=eb[:, :W], scalar1=1.0), reads=[eb], writes=[eb])
        S.op("dve", lambda e: e.reciprocal(out=eb[:, :W], in_=eb[:, :W]), reads=[eb], writes=[eb])
        S.op("dve", lambda e: e.tensor_tensor(out=out_ap, in0=pb[:, :W], in1=eb[:, :W], op=ALU.mult), reads=[pb, eb], writes=[out_buf])

    load_a(0)
    for ti in range(NTS):
        if ti + 1 < NTS:
            load_a(ti + 1)
        ab = a_sb[ti % 2]
        t0 = ti * TS
        xcur, xprev = xb[ti % 2], xb[(ti + 1) % 2]
        qTb, kTb, svb, zsb, onTb, ogb = qT[ti % 2], kT[ti % 2], svT[ti % 2], zs[ti % 2], onT[ti % 2], ogt[ti % 2]
        S.op("pool", lambda e: e.tensor_copy(out=xcur[:, :, 0:3], in_=xprev[:, :, TS:TS + 3]), reads=[xprev], writes=[xcur])
        for mc in range(12):
            pb = pp.get()
            for kc in range(8):
                S.op("pe", lambda e: e.matmul(pb[:, :], w_sb[:, kc, mc * 128:(mc + 1) * 128], ab[:, kc, :], start=(kc == 0), stop=(kc == 7)),
                     reads=[w_sb, ab], writes=[pb])
            if mc < 8:
                S.op("act", lambda e: e.activation(out=xcur[:, mc, 3:3 + TS], in_=pb[:, :], func=AF.Copy), reads=[pb], writes=[xcur])
            else:
                silu_from_psum(pb, TS, zsb[:, mc - 8, :], zsb, mc)
        for mc in range(8):
            pb = pp.get()
            for k in range(4):
                S.op("pe", lambda e: e.matmul(pb[:, :], dg[:, mc * 4 + k, :], xcur[:, mc, k:k + TS], start=(k == 0), stop=(k == 3)),
                     reads=[dg, xcur], writes=[pb])
            if mc >= 4:
                silu_from_psum(pb, TS, svb[:, mc - 4, :], svb, mc)
            else:
                sxb, sq, rsb = sx[mc % 2], sqb[mc % 2], rs[mc % 2]
                silu_from_psum(pb, TS, sxb[:, :], sxb, mc)
                S.op("act", lambda e: e.activation(out=sq[:, :], in_=sxb[:, :], func=AF.Square), reads=[sxb], writes=[sq])
                ps2 = pp.get()
                S.op("pe", lambda e: e.matmul(ps2[:, :], ones_b[:], sq[:, :], start=True, stop=True), reads=[ones_b, sq], writes=[ps2])
                rstd_from_ss(S, ps2, rsb, 1.0, L2_EPS, TS)
                dst = qTb if mc < 2 else kTb
                scl = (128.0 ** -0.5) if mc < 2 else 1.0
                S.op("dve", lambda e: e.scalar_tensor_tensor(out=dst[:, mc % 2, :], in0=sxb[:, :], scalar=scl, in1=rsb[:, :],
                                                             op0=ALU.mult, op1=ALU.mult), reads=[sxb, rsb], writes=[dst])
        for cj in range(CPT):
            n = ti * CPT + cj
            r = n % NR
            c0 = cj * CH
            for qh in range(2):
                S.op("pe", lambda e: e.transpose(pTall[0:64, qh * 128:(qh + 1) * 128], kTb[:, qh, c0:c0 + CH], ident_b[:, :]),
                     reads=[kTb, ident_b], writes=[pTall])
            for h in range(4):
                S.op("pe", lambda e: e.transpose(pTall[0:64, 256 + h * 128:256 + (h + 1) * 128], svb[:, h, c0:c0 + CH], ident_b[:, :]),
                     reads=[svb, ident_b], writes=[pTall])
            ktm_ps = pTall[0:64, 0:256].rearrange("p (a b) -> p a b", a=2)
            ktm_rep = ktm_ps.unsqueeze(2).broadcast_to([64, 2, 2, 128])
            as4 = lambda ap: ap.rearrange("p (a r) d -> p a r d", r=2)
            S.op("dve", lambda e: e.tensor_tensor(out=as4(kbg[r][:]), in0=ktm_rep, in1=as4(bcast_last(beG[:, n, :], 128)), op=ALU.mult),
                 reads=[pTall, beG], writes=[kbg[r]])
            S.op("dve", lambda e: e.tensor_tensor(out=as4(ktm[r][:]), in0=ktm_rep, in1=as4(bcast_last(ekt[:, n, :], 128)), op=ALU.mult),
                 reads=[pTall, ekt], writes=[ktm[r]])
            S.op("dve", lambda e: e.tensor_tensor(out=vb[r][:], in0=pTall[0:64, 256:768].rearrange("p (a b) -> p a b", a=4),
                                                  in1=bcast_last(bt[:, n, :], 128), op=ALU.mult), reads=[pTall, bt], writes=[vb[r]])
            S.op("dve", lambda e: e.tensor_tensor(out=rg1[r][:], in0=bcast_mid(MUI, 4), in1=bcast_last(gt[:, n, :], 64), op=ALU.mult),
                 reads=[cst, gt], writes=[rg1[r]])
            S.op("dve", lambda e: e.tensor_tensor(out=rg2[r][:], in0=bcast_mid(I64, 4), in1=bcast_last(lnb[:, n, :], 64), op=ALU.mult),
                 reads=[cst, lnb], writes=[rg2[r]])
            S.op("dve", lambda e: e.tensor_tensor(out=rg2[r][:], in0=rg2[r][:], in1=rg1[r][:], op=ALU.add), reads=[rg1[r], rg2[r]], writes=[rg2[r]])
            S.op("dve", lambda e: e.tensor_tensor(out=rg3[r][:], in0=bcast_mid(MLS, 4), in1=bcast_last(gt[:, n, :], 64), op=ALU.mult),
                 reads=[cst, gt], writes=[rg3[r]])
            fl = lambda b_: b_[:].rearrange("p a b -> p (a b)")
            pd1 = pp.get()
            S.op("pe", lambda e: e.matmul(pd1[0:64, 0:256], MLS, fl(rg1[r]), start=True, stop=True), reads=[cst, rg1[r]], writes=[pd1])
            S.op("pe", lambda e: e.matmul(pd1[0:64, 256:512], MLS, fl(rg2[r]), start=True, stop=True), reads=[cst, rg2[r]], writes=[pd1])
            pd2 = pp.get()
            S.op("pe", lambda e: e.matmul(pd2[0:64, 0:256], MUI, fl(rg3[r]), start=True, stop=True), reads=[cst, rg3[r]], writes=[pd2])
            pd3 = pp.get()
            S.op("pe", lambda e: e.matmul(pd3[:, 0:256], ONES64x128, fl(rg1[r]), start=True, stop=True), reads=[cst, rg1[r]], writes=[pd3])
            S.op("act", lambda e: e.activation(out=fl(Et[r]), in_=pd1[0:64, 0:256], func=AF.Exp), reads=[pd1], writes=[Et[r]])
            S.op("act", lambda e: e.activation(out=fl(Wt_[r]), in_=pd1[0:64, 256:512], func=AF.Exp), reads=[pd1], writes=[Wt_[r]])
            for h in range(4):
                S.op("act", lambda e: e.activation(out=W_[r][:, h, :], in_=pd2[0:64, h * 64:(h + 1) * 64], func=AF.Exp,
                                                   bias=lnb[:, n, h:h + 1], scale=1.0), reads=[pd2, lnb], writes=[W_[r]])
            S.op("act", lambda e: e.activation(out=fl(eGb[r]), in_=pd3[:, 0:256], func=AF.Exp), reads=[pd3], writes=[eGb[r]])
            pg = pp.get()
            for qh in range(2):
                S.op("pe", lambda e: e.matmul(pg[0:64, qh * 64:(qh + 1) * 64], kTb[:, qh, c0:c0 + CH], kTb[:, qh, c0:c0 + CH], start=True, stop=True),
                     reads=[kTb], writes=[pg])
                S.op("pe", lambda e: e.matmul(pg[0:64, 128 + qh * 64:128 + (qh + 1) * 64], kTb[:, qh, c0:c0 + CH], qTb[:, qh, c0:c0 + CH],
                                              start=True, stop=True), reads=[kTb, qTb], writes=[pg])
            kkv = pg[0:64, 0:128].rearrange("p (a b) -> p a b", a=2)
            kqv = pg[0:64, 128:256].rearrange("p (a b) -> p a b", a=2)
            S.op("dve", lambda e: e.tensor_tensor(out=KKlo[r][:], in0=kkv, in1=bcast_mid(NMLS, 2), op=ALU.mult), reads=[pg, cst], writes=[KKlo[r]])
            S.op("dve", lambda e: e.tensor_tensor(out=KKup[r][:], in0=kkv, in1=bcast_mid(NMUS, 2), op=ALU.mult), reads=[pg, cst], writes=[KKup[r]])
            S.op("dve", lambda e: e.tensor_tensor(out=KQm[r][:], in0=kqv, in1=bcast_mid(MUI, 2), op=ALU.mult), reads=[pg, cst], writes=[KQm[r]])
            rep = lambda b_: b_[:].unsqueeze(2).broadcast_to([64, 2, 2, 64])
            P0 = PP[0][r]
            S.op("dve", lambda e: e.tensor_tensor(out=as4(P0[:, 0:4, :]), in0=rep(KKlo[r]), in1=as4(W_[r][:]), op=ALU.mult),
                 reads=[KKlo[r], W_[r]], writes=[P0])
            S.op("dve", lambda e: e.tensor_tensor(out=as4(P0[:, 4:8, :]), in0=rep(KKup[r]), in1=as4(Wt_[r][:]), op=ALU.mult),
                 reads=[KKup[r], Wt_[r]], writes=[P0])
            S.op("dve", lambda e: e.tensor_tensor(out=as4(Qt[r][:]), in0=rep(KQm[r]), in1=as4(Et[r][:]), op=ALU.mult),
                 reads=[KQm[r], Et[r]], writes=[Qt[r]])
            S.op("dve", lambda e: e.tensor_tensor(out=as4(qdT[r][:]), in0=qTb[:, :, c0:c0 + CH].unsqueeze(2).broadcast_to([128, 2, 2, 64]),
                                                  in1=as4(eGb[r][:]), op=ALU.mult), reads=[qTb, eGb[r]], writes=[qdT[r]])
            X = Xt[0][r]
            S.op("dve", lambda e: e.tensor_tensor(out=X[:], in0=P0[:, 4:8, :], in1=bcast_mid(I64, 4), op=ALU.add), reads=[P0, cst], writes=[X])
            for k in range(1, 6):
                Pp, Pn = PP[(k - 1) % 2][r], PP[k % 2][r]
                pq = pp.get()
                for h in range(4):
                    S.op("pe", lambda e: e.matmul(pq[0:64, h * 64:(h + 1) * 64], Pp[:, 4 + h, :], Pp[:, h, :], start=True, stop=True),
                         reads=[Pp], writes=[pq])
                if k < 5:
                    for h in range(4):
                        S.op("pe", lambda e: e.matmul(pq[0:64, 256 + h * 64:256 + (h + 1) * 64], Pp[:, h, :], Pp[:, 4 + h, :], start=True, stop=True),
                             reads=[Pp], writes=[pq])
                wdt = 512 if k < 5 else 256
                S.op("act", lambda e: e.activation(out=Pn[:].rearrange("p a b -> p (a b)")[:, 0:wdt], in_=pq[0:64, 0:wdt], func=AF.Copy),
                     reads=[pq], writes=[Pn])
                px = pp.get()
                Xo = Xt[(k - 1) % 2][r]
                Xn = Xt[k % 2][r]
                for h in range(4):
                    S.op("pe", lambda e: e.matmul(px[0:64, h * 64:(h + 1) * 64], Pn[:, h, :], Xo[:, h, :], start=True, stop=True),
                         reads=[Pn, Xo], writes=[px])
                if k < 5:
                    S.op("dve", lambda e: e.tensor_tensor(out=fl(Xn), in0=px[0:64, 0:256], in1=fl(Xo), op=ALU.add), reads=[px, Xo], writes=[Xn])
                else:
                    S.op("dve", lambda e: e.tensor_tensor(out=fl(Tt[r]), in0=px[0:64, 0:256], in1=fl(Xo), op=ALU.add), reads=[px, Xo], writes=[Tt[r]])
            pw = pp.get()
            for h in range(4):
                S.op("pe", lambda e: e.matmul(pw[:, h * 64:(h + 1) * 64], kbg[r][:, h, :], Tt[r][:, h, :], start=True, stop=True),
                     reads=[kbg[r], Tt[r]], writes=[pw])
            S.op("act", lambda e: e.activation(out=fl(nwT[r]), in_=pw[:, 0:256], func=AF.Copy, scale=-1.0), reads=[pw], writes=[nwT[r]])
            pu = pp.get()
            for h in range(4):
                S.op("pe", lambda e: e.matmul(pu[0:64, h * 128:(h + 1) * 128], Tt[r][:, h, :], vb[r][:, h, :], start=True, stop=False),
                     reads=[Tt[r], vb[r]], writes=[pu])
                S.op("pe", lambda e: e.matmul(pu[0:64, h * 128:(h + 1) * 128], nwT[r][:, h, :], S_b[:, h, :], start=False, stop=True),
                     reads=[nwT[r], S_b], writes=[pu])
            S.op("act", lambda e: e.activation(out=vn[r][:].rearrange("p a b -> p (a b)"), in_=pu[0:64, :], func=AF.Copy), reads=[pu], writes=[vn[r]])
            po = pp.get()
            for h in range(4):
                S.op("pe", lambda e: e.matmul(po[0:64, h * 128:(h + 1) * 128], qdT[r][:, h, :], S_b[:, h, :], start=True, stop=False),
                     reads=[qdT[r], S_b], writes=[po])
                S.op("pe", lambda e: e.matmul(po[0:64, h * 128:(h + 1) * 128], Qt[r][:, h, :], vn[r][:, h, :], start=False, stop=True),
                     reads=[Qt[r], vn[r]], writes=[po])
            pS = pp.get()
            for h in range(4):
                S.op("pe", lambda e: e.matmul(pS[:, h * 128:(h + 1) * 128], ktm[r][:, h, :], vn[r][:, h, :], start=True, stop=True),
                     reads=[ktm[r], vn[r]], writes=[pS])
            for h in range(4):
                S.op("dve", lambda e: e.scalar_tensor_tensor(out=S_f[:, h, :], in0=S_f[:, h, :], scalar=eGl[:, n, h:h + 1],
                                                             in1=pS[:, h * 128:(h + 1) * 128], op0=ALU.mult, op1=ALU.add),
                     reads=[S_f, eGl, pS], writes=[S_f])
            S.op("act", lambda e: e.activation(out=S_b[:], in_=S_f[:], func=AF.Copy), reads=[S_f], writes=[S_b])
            S.op("act", lambda e: e.activation(out=sqo[r][:].rearrange("p a b -> p (a b)"), in_=po[0:64, :], func=AF.Square), reads=[po], writes=[sqo[r]])
            S.op("dve", lambda e: e.tensor_reduce(out=sso[r][:, 0:4], in_=sqo[r][:], axis=AX.X, op=ALU.add), reads=[sqo[r]], writes=[sso[r]])
            S.op("act", lambda e: e.activation(out=sso[r][:, 4:8], in_=sso[r][:, 0:4], func=AF.Ln, scale=1.0 / 128.0, bias=GDN_EPS),
                 reads=[sso[r]], writes=[sso[r]])
            S.op("act", lambda e: e.activation(out=sso[r][:, 4:8], in_=sso[r][:, 4:8], func=AF.Exp, scale=-0.5), reads=[sso[r]], writes=[sso[r]])
            S.op("dve", lambda e: e.tensor_tensor(out=on[r][:], in0=po[0:64, :].rearrange("p (a b) -> p a b", a=4),
                                                  in1=bcast_last(sso[r][:, 4:8], 128), op=ALU.mult), reads=[po, sso[r]], writes=[on[r]])
            for h in range(4):
                S.op("pe", lambda e: e.transpose(pTall[:, 768 + h * 64:768 + (h + 1) * 64], on[r][:, h, :], ident_b[0:64, 0:64]),
                     reads=[on[r], ident_b], writes=[pTall])
            S.op("act", lambda e: e.activation(out=onTb[:, :, c0:c0 + CH], in_=pTall[:, 768:1024].rearrange("p (a b) -> p a b", a=4), func=AF.Copy),
                 reads=[pTall], writes=[onTb])
        S.op("dve", lambda e: e.scalar_tensor_tensor(out=ogb[:].rearrange("p a b -> p (a b)"), in0=onTb[:].rearrange("p a b -> p (a b)"),
                                                     scalar=gnw[:, 0:1], in1=zsb[:].rearrange("p a b -> p (a b)"), op0=ALU.mult, op1=ALU.mult),
             reads=[onTb, gnw, zsb], writes=[ogb])
        if ti >= 1:
            S.dma("pool", ogF[ti - 1][:, :].rearrange("p (c t) -> p c t", c=c_og), ogb[:], reads=[ogb], writes=[ogF[ti - 1]])
        if ti % 4 == 0 and ti <= 12:
            S.dma("pool", ogH[(ti // 4) * 128:(ti // 4 + 1) * 128, :].rearrange("p (c t) -> p c t", c=c_og), ogb[:, :, TS - 64:TS],
                  reads=[ogb], writes=[ogH])
        io["after_og"](ti)
    S.pop()


def prep_M_gdn(inp, j, hg):
    w = inp["gdn_w_in"][j]
    cols = []
    for qh in range(2):
        cols.append(np.arange(128) + 128 * (2 * hg + qh))
    for qh in range(2):
        cols.append(1024 + np.arange(128) + 128 * (2 * hg + qh))
    for h in range(4):
        cols.append(2048 + np.arange(128) + 128 * (4 * hg + h))
    conv_cols = np.concatenate(cols)
    for h in range(4):
        cols.append(4096 + np.arange(128) + 128 * (4 * hg + h))
    cols.append(6144 + 4 * hg + np.arange(4))
    cols.append(6160 + 4 * hg + np.arange(4))
    cols = np.concatenate(cols)
    wc = w[:, cols].reshape(8, 128, G_WC).transpose(1, 0, 2)
    cw = inp["gdn_conv_w"][j][:, conv_cols].reshape(4, 8, 128).transpose(2, 1, 0)
    hv = np.concatenate([inp["gdn_a_log"][j][4 * hg:4 * hg + 4], inp["gdn_dt_bias"][j][4 * hg:4 * hg + 4]]).reshape(1, 8)
    return {"w": np.ascontiguousarray(wc.reshape(128, 8 * G_WC), np.float32),
            "cw": np.ascontiguousarray(cw.reshape(128, 32), np.float32),
            "hv": np.ascontiguousarray(hv, np.float32),
            "gnw": np.ascontiguousarray(inp["gdn_norm_w"][j].reshape(128, 1), np.float32),
            "cst": gdn_consts()}


GROUPS = [[0, 1, 2, 3], [4, 5, 6, 7]]


def build_fused(nl=4):
    nc = bass.Bass("TRN2", target_bir_lowering=False)
    es = ExitStack()
    S = Sched(nc, es)
    ext = lambda n, shp, dt=F32: S.dram(n, shp, dt, kind="ExternalInput")
    hs0 = ext("hs0", [D, WIN])
    keep = ext("keep", [1, WIN])
    gidx4 = ext("gidx4", [128, 20], mybir.dt.int32)
    gidx2 = ext("gidx2", [128, 20], mybir.dt.int32) if nl > 1 else None
    nwT0 = ext("nwT_0", [128, 32])
    cst_g = ext("cst_g", [128, C_TOT])
    cst_m = ext("cst_m", [128, 64 + 512 + 128]) if nl > 1 else None
    hs_out = S.dram("hs_out", [D, WIN], F32, kind="ExternalOutput")
    hs_loc = S.dram("hs_loc", [D, WIN], F32)
    def slices(name, rows_total, cols, rows_per):
        big = S.dram(name, [rows_total, cols], BF16)
        return big, [Buf(big.t[k * rows_per:(k + 1) * rows_per, :], "%s_%d" % (name, k)) for k in range(rows_total // rows_per)]

    ainF_all, ainF = slices("ainF", 512, 4096, 128)
    ainF_g, ainF_gs = slices("ainF_g", 2048, 4096, 512)
    ainH = S.dram("ainH", [128, 512], BF16)
    ainH_g = S.dram("ainH_g", [512, 512], BF16)
    og = {}
    for c in (4, 2):
        tpc = 8 // c
        ogF_all, ogF = slices("ogF%d" % c, 2048, c * 512, 128)
        ogF_g, _ = slices("ogF%d_g" % c, 8192, c * 512, 8192)
        nq = 16 // tpc
        og[c] = dict(ogF=ogF, ogF_all=ogF_all, ogH=S.dram("ogH%d" % c, [512, c * 64], BF16), tpc=tpc,
                     ogF_g=ogF_g, ogH_g=S.dram("ogH%d_g" % c, [2048, c * 64], BF16),
                     src=[Buf(ogF_all.t[q * tpc * 128:(q + 1) * tpc * 128, :], "ogsrc%d_%d" % (c, q)) for q in range(nq)],
                     dst=[Buf(ogF_g.t[q * 4 * tpc * 128:(q + 1) * 4 * tpc * 128, :], "ogdst%d_%d" % (c, q)) for q in range(nq)])
    lay = []
    for l in range(nl):
        KO = 2048 if l % 2 == 0 else 1024
        KC = KO // 128
        d = dict(nwT=ext("nwT_l%d" % l, [128, 32]), cw=ext("cw_l%d" % l, [128, 44 * 3]), cb=ext("cb_l%d" % l, [128, 44]),
                 wout_d=ext("wout_l%d" % l, [8, 128, KC * 128]), wup_d=ext("wup_l%d" % l, [NG, 128, 8 * 256]),
                 wdn_d=ext("wdn_l%d" % l, [8, 128, NG * 128]),
                 wout_b=S.dram("wout_b%d" % l, [8, 128, KC * 128], BF16), wup_b=S.dram("wup_b%d" % l, [NG, 128, 8 * 256], BF16),
                 wdn_b=S.dram("wdn_b%d" % l, [8, 128, NG * 128], BF16))
        if l % 2 == 0:
            d["m"] = dict(w=ext("gw_l%d" % l, [128, 8 * G_WC]), cw=ext("gcw_l%d" % l, [128, 32]), hv=ext("ghv_l%d" % l, [1, 8]),
                          gnw=ext("ggnw_l%d" % l, [128, 1]), cst=cst_g)
        else:
            d["m"] = dict(w=ext("mw_l%d" % l, [128, 8 * ML_WC]), gb=ext("mgb_l%d" % l, [1, 2]), nwv=ext("mnwv_l%d" % l, [1, ML_DV]), cst=cst_m)
        lay.append(d)

    def after_ain(ti):
        if ti == 0:
            S.coll("AllGather", ainH_g, ainH, GROUPS)
        else:
            S.coll("AllGather", ainF_gs[ti - 1], ainF[ti - 1], GROUPS)

    def mk_after_og(c):
        o = og[c]
        tpc = o["tpc"]

        def after_og(ti):
            wt = ti - 1
            if ti >= 1 and (wt + 1) % tpc == 0:
                q = wt // tpc
                src = o["src"][q]
                S.coll("AllGather", o["dst"][q], src, GROUPS, extra=[o["ogF"][k] for k in range(q * tpc, (q + 1) * tpc)])
            if ti == 12:
                S.coll("AllGather", o["ogH_g"], o["ogH"], GROUPS)
        return after_og

    emit_T(S, "t0", 2048, True, False, dict(hs_src=hs0, nwT=nwT0, ainF=ainF, ainH=ainH, after_ain=after_ain))
    for l in range(nl):
        d = lay[l]
        c = 4 if l % 2 == 0 else 2
        emit_casts(S, d)
        mio = dict(d["m"], ainF_g=ainF_g, ainH_g=ainH_g, ogF=og[c]["ogF"], ogH=og[c]["ogH"], after_og=mk_after_og(c))
        if l % 2 == 0:
            emit_M_gdn(S, "g%d" % l, mio)
        else:
            emit_M_ml(S, "m%d" % l, mio)
        last = l == nl - 1
        tio = dict(d, hs_src=(hs0 if l == 0 else hs_loc), hs_dst=(hs_out if last else hs_loc), ogF_g=og[c]["ogF_g"], ogH_g=og[c]["ogH_g"],
                   keep=keep, gidx=(gidx4 if c == 4 else gidx2), ainF=ainF, ainH=ainH, after_ain=after_ain)
        emit_T(S, "t%d" % (l + 1), 512 * c, False, last, tio)
    S.finish([hs_out])
    return nc, es


def kernel(x, meta_tokens, norm_w, gdn_w_in, gdn_conv_w, gdn_a_log, gdn_dt_bias, gdn_norm_w, gdn_w_out,
           ml_w_in, ml_gate_b, ml_norm_w, ml_w_out, ffn_w_up, ffn_conv_w, ffn_conv_b, ffn_w_down, _nl=4):
    inp = dict(x=x, meta_tokens=meta_tokens, norm_w=norm_w, gdn_w_in=gdn_w_in, gdn_conv_w=gdn_conv_w, gdn_a_log=gdn_a_log,
               gdn_dt_bias=gdn_dt_bias, gdn_norm_w=gdn_norm_w, gdn_w_out=gdn_w_out, ml_w_in=ml_w_in, ml_gate_b=ml_gate_b,
               ml_norm_w=ml_norm_w, ml_w_out=ml_w_out, ffn_w_up=ffn_w_up, ffn_conv_w=ffn_conv_w, ffn_conv_b=ffn_conv_b,
               ffn_w_down=ffn_w_down)
    inp = {k: np.asarray(v, np.float32) for k, v in inp.items()}
    shared = {"cst_g": gdn_consts(), "cst_m": ml_consts(),
              "nwT_0": np.ascontiguousarray(np.stack([_cm(inp["norm_w"][0, 0])] * 4, 1).reshape(128, 32), np.float32)}
    for l in range(_nl):
        t = prep_T(inp, l)
        for k, v in t.items():
            shared["%s_l%d" % (k, l)] = v
    maps = []
    for c in range(8):
        b, r = c // 4, c % 4
        m = dict(shared)
        h = np.zeros((LP, D), np.float32)
        h[XPAD + 48:XPAD + 64] = inp["meta_tokens"]
        h[XPAD + 64:] = inp["x"][b]
        lo = XPAD + 2048 * r
        m["hs0"] = np.ascontiguousarray(h[lo:lo + WIN].T)
        k = np.ones((1, WIN), np.float32)
        if r == 0:
            k[0, :48] = 0.0
        m["keep"] = k
        p = np.arange(128)
        for cc in (4, 2):
            tpc = 8 // cc
            gi = np.zeros((128, 20), np.int32)
            for hg in range(4):
                gi[:, hg * 5] = hg * 512 + r * 128 + p
                for i in range(1, 5):
                    wt = 4 * r + i - 1
                    gi[:, hg * 5 + i] = (wt // tpc) * (4 * tpc * 128) + hg * (tpc * 128) + (wt % tpc) * 128 + p
            m["gidx%d" % cc] = gi
        for l in range(_nl):
            if l % 2 == 0:
                g = prep_M_gdn(inp, l // 2, r)
                m["gw_l%d" % l], m["gcw_l%d" % l], m["ghv_l%d" % l], m["ggnw_l%d" % l] = g["w"], g["cw"], g["hv"], g["gnw"]
            else:
                g = prep_M_ml(inp, l // 2, r)
                m["mw_l%d" % l], m["mgb_l%d" % l], m["mnwv_l%d" % l] = g["w"], g["gb"], g["nwv"]
        maps.append(m)
    if _nl == 1:
        shared.pop("cst_m")
        for m in maps:
            m.pop("cst_m", None)
            m.pop("gidx2", None)
    nc, es = build_fused(_nl)
    res = run_bass_kernel_spmd(nc, maps, core_ids=list(range(8))).results
    out = np.zeros((NB, SEQ, D), np.float32)
    for c in range(8):
        b, r = c // 4, c % 4
        out[b, 2048 * r:2048 * (r + 1)] = res[c]["hs_out"][:, 64:].T
    return out
```

```python
from contextlib import ExitStack
import numpy as np
import ml_dtypes
import concourse.bass as bass
import concourse.mybir as mybir
from concourse.bass_utils import run_bass_kernel_spmd

F32 = mybir.dt.float32
BF16 = mybir.dt.bfloat16
AF = mybir.ActivationFunctionType
ALU = mybir.AluOpType
AX = mybir.AxisListType

D = 1024
SEQ = 8192
NB = 2
LP = 8704
XPAD = LP - SEQ - 64
WIN = 64 + 2048
FFN = 2816
NG = FFN // 128
RMS_EPS = 1e-6
CH = 64
NCH = LP // CH
TS = 512
NTS = LP // TS
CPT = TS // CH


class Buf:
    __slots__ = ("t", "lw", "rd", "sem", "semv", "name")

    def __init__(self, t, name=""):
        self.t = t
        self.lw = None
        self.rd = {}
        self.sem = None
        self.semv = 0
        self.name = name

    def __getitem__(self, k):
        return self.t[k]


class Sched:
    def __init__(self, nc, es):
        self.nc = nc
        self.es = es
        self.eng = {"pe": nc.tensor, "act": nc.scalar, "dve": nc.vector, "pool": nc.gpsimd, "sp": nc.sync}
        self.sem = {k: es.enter_context(nc.semaphore("sem_" + k)) for k in self.eng}
        self.cnt = {k: 0 for k in self.eng}
        self.seen = {k: {} for k in self.eng}
        self.nsem = 0
        self.out_events = []
        self.ninst = 0
        self.scopes = []
        self.dsems = []
        self.free_dsems = []
        self.scope_bufs = []

    def push(self, tag):
        self.scopes.append((ExitStack(), tag))
        self.scope_bufs.append([])

    def _own_sem(self, own):
        if own.sem is None:
            if self.free_dsems:
                own.sem, own.semv = self.free_dsems.pop()
            else:
                own.sem = self.es.enter_context(self.nc.semaphore("dsem%d" % self.nsem))
                self.nsem += 1
            self.dsems.append(own)

    def pop(self):
        self.barrier()
        st, _ = self.scopes.pop()
        st.close()
        for b in self.scope_bufs.pop():
            if b.sem is not None:
                self.free_dsems.append((b.sem, b.semv))
                self.dsems.remove(b)
                b.sem = None

    def _scope(self):
        return self.scopes[-1] if self.scopes else (self.es, "g")

    def sb(self, name, shape, dt):
        st, tag = self._scope()
        name = tag + "_" + name
        b = Buf(st.enter_context(self.nc.sbuf_tensor(name, list(shape), dt)), name)
        if self.scope_bufs:
            self.scope_bufs[-1].append(b)
        return b

    def ps(self, name, shape, dt=F32):
        st, tag = self._scope()
        name = tag + "_" + name
        return Buf(st.enter_context(self.nc.psum_tensor(name, list(shape), dt)), name)

    def barrier(self):
        for e in self.eng:
            eng = self.eng[e]
            for k in ("pe", "act", "dve", "pool", "sp"):
                if k != e and self.cnt[k] and self.seen[e].get(k, 0) < self.cnt[k]:
                    eng.wait_ge(self.sem[k], self.cnt[k])
                    self.seen[e][k] = self.cnt[k]
            for b in self.dsems:
                key = "d_" + b.name
                if self.seen[e].get(key, 0) < b.semv:
                    eng.wait_ge(b.sem, b.semv)
                    self.seen[e][key] = b.semv

    def dram(self, name, shape, dt, kind="Internal"):
        t = self.nc.dram_tensor(name, list(shape), dt, kind=kind)
        return Buf(t.ap(), name)

    def _deps(self, reads, writes):
        deps = {}

        def add(ev):
            if ev is None:
                return
            sem, val, key = ev
            if key not in deps or deps[key][1] < val:
                deps[key] = (sem, val)

        for b in reads:
            add(b.lw)
        for b in writes:
            add(b.lw)
            for ev in b.rd.values():
                add(ev)
        return deps

    def _wait(self, e, deps):
        eng = self.eng[e]
        for key, (sem, val) in deps.items():
            if e == "pe" and key == "pe":
                continue
            if self.seen[e].get(key, 0) >= val:
                continue
            eng.wait_ge(sem, val)
            self.seen[e][key] = val

    def _record(self, ev, reads, writes):
        for b in writes:
            b.lw = ev
            b.rd = {}
        for b in reads:
            if b not in writes:
                b.rd[ev[2]] = ev

    def op(self, e, fn, reads=(), writes=()):
        self._wait(e, self._deps(reads, writes))
        ins = fn(self.eng[e])
        self.cnt[e] += 1
        ins.then_inc(self.sem[e], 1)
        self.ninst += 1
        self._record((self.sem[e], self.cnt[e], e), reads, writes)

    def dma(self, q, out, in_, reads=(), writes=(), owner=None):
        self._wait(q, self._deps(reads, writes))
        own = owner if owner is not None else (writes[0] if writes else reads[0])
        self._own_sem(own)
        own.semv += 16
        ins = self.eng[q].dma_start(out=out, in_=in_)
        ins.then_inc(own.sem, 16)
        self.ninst += 1
        ev = (own.sem, own.semv, "d_" + own.name)
        self._record(ev, reads, writes)
        return ev

    def gather(self, out_ap, table_ap, idx_ap, reads=(), writes=()):
        self._wait("pool", self._deps(reads, writes))
        own = writes[0]
        self._own_sem(own)
        own.semv += 16
        ins = self.nc.gpsimd.indirect_dma_start(out=out_ap, out_offset=None, in_=table_ap,
                                                in_offset=bass.IndirectOffsetOnAxis(ap=idx_ap, axis=0))
        ins.then_inc(own.sem, 16)
        self.ninst += 1
        self._record((own.sem, own.semv, "d_" + own.name), reads, writes)

    def coll(self, kind, out, in_, groups, extra=()):
        self._wait("pool", self._deps([in_] + list(extra), [out]))
        self._own_sem(out)
        out.semv += 1
        ins = self.nc.gpsimd.collective_compute(kind, ALU.bypass, replica_groups=groups, ins=[in_.t.opt()], outs=[out.t.opt()])
        ins.then_inc(out.sem, 1)
        self.ninst += 1
        self._record((out.sem, out.semv, "d_" + out.name), [in_], [out])

    def finish(self, bufs):
        for b in bufs:
            if b.sem is not None:
                self.eng["sp"].wait_ge(b.sem, b.semv)
        for k in ("pe", "act", "dve", "pool"):
            if self.cnt[k]:
                self.eng["sp"].wait_ge(self.sem[k], self.cnt[k])


class PsumPool:
    def __init__(self, S, n, prefix="pb"):
        self.banks = [S.ps("%s%d" % (prefix, i), [128, 512]) for i in range(n)]
        self.i = 0

    def get(self):
        b = self.banks[self.i % len(self.banks)]
        self.i += 1
        return b


def bcast_mid(ap2, n):
    return ap2.unsqueeze(1).broadcast_to([ap2.shape[0], n, ap2.shape[1]])


def bcast_last(ap2, n):
    return ap2.unsqueeze(2).broadcast_to([ap2.shape[0], ap2.shape[1], n])


def rstd_from_ss(S, ss_ps, out_sb, scale, eps, W):
    S.op("act", lambda e: e.activation(out=out_sb[:, :W], in_=ss_ps[:, :W], func=AF.Ln, scale=scale, bias=eps),
         reads=[ss_ps], writes=[out_sb])
    S.op("act", lambda e: e.activation(out=out_sb[:, :W], in_=out_sb[:, :W], func=AF.Exp, scale=-0.5),
         reads=[out_sb], writes=[out_sb])


T_TILES = [(0, 64), (64, 512), (576, 512), (1088, 512), (1600, 512)]


def emit_casts(S, io):
    for m in range(8):
        S.dma("pool", io["wout_b"][m], io["wout_d"][m], writes=[io["wout_b"]])
    for g in range(NG):
        S.dma("pool", io["wup_b"][g], io["wup_d"][g], writes=[io["wup_b"]])
    for m in range(8):
        S.dma("pool", io["wdn_b"][m], io["wdn_d"][m], writes=[io["wdn_b"]])


def emit_T(S, tag, KO, first, last, io):
    S.push(tag)
    KC = KO // 128
    c_og = KC // 4
    hs_in = io["hs_src"]
    nwT_d = io["nwT"]
    if not last:
        ainF, ainH = io["ainF"], io["ainH"]
    if not first:
        ogF_g, ogH_g = io["ogF_g"], io["ogH_g"]
        keep_d, cw_d, cb_d = io["keep"], io["cw"], io["cb"]
        wout_b, wup_b, wdn_b = io["wout_b"], io["wup_b"], io["wdn_b"]
        hs_out = io["hs_dst"]
        gidx = S.sb("gidx", [128, 20], mybir.dt.int32)
        S.dma("sp", gidx[:], io["gidx"][:, :], writes=[gidx])

    ones_f = S.sb("ones_f", [128, 128], F32)
    ones_b = S.sb("ones_b", [128, 128], BF16)
    S.op("pool", lambda e: e.memset(ones_f[:], 1.0), writes=[ones_f])
    S.op("act", lambda e: e.activation(out=ones_b[:], in_=ones_f[:], func=AF.Copy), reads=[ones_f], writes=[ones_b])
    nwT = S.sb("nwT_sb", [128, 4, 8], F32)
    S.dma("sp", nwT[:].rearrange("p a b -> p (a b)"), nwT_d[:, :], writes=[nwT])

    hs_sb = [S.sb("hs_sb%d" % i, [128, 8, 512], F32) for i in range(2)]
    sq_sb = [S.sb("sq_sb%d" % i, [128, 512], BF16) for i in range(2)]
    rstd = S.sb("rstd", [128, 512], F32)
    a_sb = S.sb("a_sb", [128, 8, 512], BF16)
    pp = PsumPool(S, 7)
    ss_ps = S.ps("ss_ps", [128, 512])
    if not first:
        og_sb = [S.sb("og_sb%d" % i, [128, KC, 512], BF16) for i in range(2)]
        ogh_sb = S.sb("ogh_sb", [128, KC, 64], BF16)
        keep_sb = S.sb("keep_sb", [128, WIN], F32)
        S.dma("sp", keep_sb[:], keep_d[0:1, :].partition_broadcast(128), writes=[keep_sb])
        cw = S.sb("cw_sb", [128, 44, 3], F32)
        cb = S.sb("cb_sb", [128, 44], F32)
        S.dma("sp", cw[:].rearrange("p a b -> p (a b)"), cw_d[:, :], writes=[cw])
        S.dma("sp", cb[:], cb_d[:, :], writes=[cb])
        mix_sb = S.sb("mix_sb", [128, 8, 512], F32)
        rk = S.sb("rk", [128, 512], F32)
        tmp_sb = [S.sb("tmp_sb%d" % i, [128, 512], F32) for i in range(2)]
        h_sb = S.sb("h_sb", [128, NG, 512], BF16)
        u_sb = [S.sb("u_sb%d" % i, [128, 2, 2 + 512], F32) for i in range(2)]
        y_sb = [S.sb("y_sb%d" % i, [128, 2, 512], F32) for i in range(2)]
        e_sb = [S.sb("e_sb%d" % i, [128, 512], F32) for i in range(2)]
        halo = S.sb("halo", [128, 44, 2], F32)
        S.op("pool", lambda e: e.memset(halo[:], 0.0), writes=[halo])
        wo_s = [S.sb("wo_s%d" % i, [128, KC, 128], BF16) for i in range(2)]
        wu_s = [S.sb("wu_s%d" % i, [128, 8, 256], BF16) for i in range(3)]
        wd_s = [S.sb("wd_s%d" % i, [128, NG, 128], BF16) for i in range(2)]

    def norm_ss(src, W, eng_sq="act"):
        for m in range(8):
            sq = sq_sb[m % 2]
            S.op(eng_sq, lambda e: e.activation(out=sq[:, :W], in_=src[:, m, :W], func=AF.Square),
                 reads=[src], writes=[sq])
            S.op("pe", lambda e: e.matmul(ss_ps[:, :W], ones_b[:], sq[:, :W], start=(m == 0), stop=(m == 7)),
                 reads=[ones_b, sq], writes=[ss_ps])

    def load_tile(i):
        t0, W = T_TILES[i]
        hb = hs_sb[i % 2]
        S.dma("sp", hb[:, :, :W], hs_in[:, t0:t0 + W].rearrange("(c p) t -> p c t", p=128), writes=[hb])
        if not first:
            ob = ogh_sb if i == 0 else og_sb[i % 2]
            tab = ogH_g if i == 0 else ogF_g
            for hg in range(4):
                S.gather(ob[:, hg * c_og:(hg + 1) * c_og, :].rearrange("p c w -> p (c w)"), tab[:, :],
                         gidx[:, hg * 5 + i:hg * 5 + i + 1], reads=[gidx], writes=[ob])

    load_tile(0)
    for ti, (t0, W) in enumerate(T_TILES):
        if ti + 1 < len(T_TILES):
            load_tile(ti + 1)
        hb = hs_sb[ti % 2]
        if not first:
            ob = ogh_sb if ti == 0 else og_sb[ti % 2]
            S.dma("sp", wo_s[0][:].rearrange("p a b -> p (a b)"), wout_b[0], reads=[wout_b], writes=[wo_s[0]])
            for m in range(8):
                if m + 1 < 8:
                    S.dma("sp", wo_s[(m + 1) % 2][:].rearrange("p a b -> p (a b)"), wout_b[m + 1],
                          reads=[wout_b], writes=[wo_s[(m + 1) % 2]])
                ws = wo_s[m % 2]
                pb = pp.get()
                for kc in range(KC):
                    S.op("pe", lambda e: e.matmul(pb[:, :W], ws[:, kc, :], ob[:, kc, :W], start=(kc == 0), stop=(kc == KC - 1)),
                         reads=[ws, ob], writes=[pb])
                S.op("act", lambda e: e.activation(out=mix_sb[:, m, :W], in_=pb[:, :W], func=AF.Copy), reads=[pb], writes=[mix_sb])
                sq = sq_sb[m % 2]
                S.op("act", lambda e: e.activation(out=sq[:, :W], in_=pb[:, :W], func=AF.Square), reads=[pb], writes=[sq])
                S.op("pe", lambda e: e.matmul(ss_ps[:, :W], ones_b[:], sq[:, :W], start=(m == 0), stop=(m == 7)),
                     reads=[ones_b, sq], writes=[ss_ps])
            rstd_from_ss(S, ss_ps, rstd, 1.0 / D, RMS_EPS, W)
            S.op("dve", lambda e: e.tensor_tensor(out=rk[:, :W], in0=rstd[:, :W], in1=keep_sb[:, t0:t0 + W], op=ALU.mult),
                 reads=[rstd, keep_sb], writes=[rk])
            for m in range(8):
                tb = tmp_sb[m % 2]
                S.op("dve", lambda e: e.tensor_tensor(out=tb[:, :W], in0=mix_sb[:, m, :W], in1=rk[:, :W], op=ALU.mult),
                     reads=[mix_sb, rk], writes=[tb])
                S.op("dve", lambda e: e.scalar_tensor_tensor(out=hb[:, m, :W], in0=tb[:, :W], scalar=nwT[:, 1, m:m + 1],
                                                             in1=hb[:, m, :W], op0=ALU.mult, op1=ALU.add),
                     reads=[tb, nwT, hb], writes=[hb])
            norm_ss(hb, W)
            rstd_from_ss(S, ss_ps, rstd, 1.0 / D, RMS_EPS, W)
            for m in range(8):
                S.op("dve", lambda e: e.scalar_tensor_tensor(out=a_sb[:, m, :W], in0=hb[:, m, :W], scalar=nwT[:, 2, m:m + 1],
                                                             in1=rstd[:, :W], op0=ALU.mult, op1=ALU.mult),
                     reads=[hb, nwT, rstd], writes=[a_sb])
            S.dma("sp", wu_s[0][:].rearrange("p a b -> p (a b)"), wup_b[0], reads=[wup_b], writes=[wu_s[0]])
            S.dma("sp", wu_s[1][:].rearrange("p a b -> p (a b)"), wup_b[1], reads=[wup_b], writes=[wu_s[1]])
            for g in range(NG):
                if g + 2 < NG:
                    S.dma("sp", wu_s[(g + 2) % 3][:].rearrange("p a b -> p (a b)"), wup_b[g + 2],
                          reads=[wup_b], writes=[wu_s[(g + 2) % 3]])
                ws = wu_s[g % 3]
                ub = u_sb[g % 2]
                yb = y_sb[g % 2]
                eb = e_sb[g % 2]
                for hf in range(2):
                    ci = g + hf * NG
                    pb = pp.get()
                    for kc in range(8):
                        S.op("pe", lambda e: e.matmul(pb[:, :W], ws[:, kc, hf * 128:(hf + 1) * 128], a_sb[:, kc, :W],
                                                      start=(kc == 0), stop=(kc == 7)),
                             reads=[ws, a_sb], writes=[pb])
                    S.op("pool", lambda e: e.tensor_copy(out=ub[:, hf, 0:2], in_=halo[:, ci, :]), reads=[halo], writes=[ub])
                    S.op("act", lambda e: e.activation(out=ub[:, hf, 2:2 + W], in_=pb[:, :W], func=AF.Copy), reads=[pb], writes=[ub])
                    S.op("pool", lambda e: e.tensor_copy(out=halo[:, ci, :], in_=ub[:, hf, W:W + 2]), reads=[ub], writes=[halo])
                    S.op("act", lambda e: e.activation(out=yb[:, hf, :W], in_=pb[:, :W], func=AF.Identity,
                                                       scale=cw[:, ci, 2:3], bias=cb[:, ci:ci + 1]),
                         reads=[pb, cw, cb], writes=[yb])
                    S.op("dve", lambda e: e.scalar_tensor_tensor(out=yb[:, hf, :W], in0=ub[:, hf, 1:1 + W], scalar=cw[:, ci, 1:2],
                                                                 in1=yb[:, hf, :W], op0=ALU.mult, op1=ALU.add),
                         reads=[ub, cw, yb], writes=[yb])
                    S.op("dve", lambda e: e.scalar_tensor_tensor(out=yb[:, hf, :W], in0=ub[:, hf, 0:W], scalar=cw[:, ci, 0:1],
                                                                 in1=yb[:, hf, :W], op0=ALU.mult, op1=ALU.add),
                         reads=[ub, cw, yb], writes=[yb])
                S.op("act", lambda e: e.activation(out=eb[:, :W], in_=yb[:, 0, :W], func=AF.Silu), reads=[yb], writes=[eb])
                S.op("dve", lambda e: e.tensor_tensor(out=h_sb[:, g, :W], in0=yb[:, 1, :W], in1=eb[:, :W], op=ALU.mult),
                     reads=[yb, eb], writes=[h_sb])
            S.dma("sp", wd_s[0][:].rearrange("p a b -> p (a b)"), wdn_b[0], reads=[wdn_b], writes=[wd_s[0]])
            for m in range(8):
                if m + 1 < 8:
                    S.dma("sp", wd_s[(m + 1) % 2][:].rearrange("p a b -> p (a b)"), wdn_b[m + 1],
                          reads=[wdn_b], writes=[wd_s[(m + 1) % 2]])
                ws = wd_s[m % 2]
                pb = pp.get()
                for kc in range(NG):
                    S.op("pe", lambda e: e.matmul(pb[:, :W], ws[:, kc, :], h_sb[:, kc, :W], start=(kc == 0), stop=(kc == NG - 1)),
                         reads=[ws, h_sb], writes=[pb])
                S.op("act", lambda e: e.activation(out=mix_sb[:, m, :W], in_=pb[:, :W], func=AF.Copy), reads=[pb], writes=[mix_sb])
                sq = sq_sb[m % 2]
                S.op("act", lambda e: e.activation(out=sq[:, :W], in_=pb[:, :W], func=AF.Square), reads=[pb], writes=[sq])
                S.op("pe", lambda e: e.matmul(ss_ps[:, :W], ones_b[:], sq[:, :W], start=(m == 0), stop=(m == 7)),
                     reads=[ones_b, sq], writes=[ss_ps])
            rstd_from_ss(S, ss_ps, rstd, 1.0 / D, RMS_EPS, W)
            S.op("dve", lambda e: e.tensor_tensor(out=rk[:, :W], in0=rstd[:, :W], in1=keep_sb[:, t0:t0 + W], op=ALU.mult),
                 reads=[rstd, keep_sb], writes=[rk])
            for m in range(8):
                tb = tmp_sb[m % 2]
                S.op("dve", lambda e: e.tensor_tensor(out=tb[:, :W], in0=mix_sb[:, m, :W], in1=rk[:, :W], op=ALU.mult),
                     reads=[mix_sb, rk], writes=[tb])
                S.op("dve", lambda e: e.scalar_tensor_tensor(out=hb[:, m, :W], in0=tb[:, :W], scalar=nwT[:, 3, m:m + 1],
                                                             in1=hb[:, m, :W], op0=ALU.mult, op1=ALU.add),
                     reads=[tb, nwT, hb], writes=[hb])
            S.dma("pool", hs_out[:, t0:t0 + W].rearrange("(c p) t -> p c t", p=128), hb[:, :, :W], reads=[hb], owner=hs_out)
        if not last:
            norm_ss(hb, W)
            rstd_from_ss(S, ss_ps, rstd, 1.0 / D, RMS_EPS, W)
            for m in range(8):
                S.op("dve", lambda e: e.scalar_tensor_tensor(out=a_sb[:, m, :W], in0=hb[:, m, :W], scalar=nwT[:, 0, m:m + 1],
                                                             in1=rstd[:, :W], op0=ALU.mult, op1=ALU.mult),
                     reads=[hb, nwT, rstd], writes=[a_sb])
            if ti == 0:
                S.dma("pool", ainH[:, :].rearrange("p (c t) -> p c t", c=8), a_sb[:, :, :W], reads=[a_sb], writes=[ainH])
            else:
                S.dma("pool", ainF[ti - 1][:, :].rearrange("p (c t) -> p c t", c=8), a_sb[:, :, :W], reads=[a_sb], writes=[ainF[ti - 1]])
            io["after_ain"](ti)
    S.pop()


def _cm(v):
    return np.ascontiguousarray(v.reshape(-1, 128).T)


def prep_T(inp, layer):
    j = layer // 2
    if layer % 2 == 0:
        w_out = inp["gdn_w_out"][j]
    else:
        w_out = inp["ml_w_out"][j]
    KO = w_out.shape[0]
    KC = KO // 128
    nw = inp["norm_w"]
    nxt = nw[layer + 1, 0] if layer + 1 < 4 else nw[layer, 0]
    nwT = np.stack([_cm(nxt), _cm(nw[layer, 1]), _cm(nw[layer, 2]), _cm(nw[layer, 3])], 1)
    wout = w_out.reshape(KC, 128, 8, 128).transpose(2, 1, 0, 3)
    wu = inp["ffn_w_up"][layer].reshape(8, 128, 2, NG, 128).transpose(3, 1, 0, 2, 4)
    wd = inp["ffn_w_down"][layer].reshape(NG, 128, 8, 128).transpose(2, 1, 0, 3)
    cw = inp["ffn_conv_w"][layer].reshape(3, 44, 128).transpose(2, 1, 0)
    cb = _cm(inp["ffn_conv_b"][layer])
    return {
        "nwT": np.ascontiguousarray(nwT.reshape(128, 32), np.float32),
        "wout": np.ascontiguousarray(wout.reshape(8, 128, KC * 128), np.float32),
        "wup": np.ascontiguousarray(wu.reshape(NG, 128, 8 * 256), np.float32),
        "wdn": np.ascontiguousarray(wd.reshape(8, 128, NG * 128), np.float32),
        "cw": np.ascontiguousarray(cw.reshape(128, 44 * 3), np.float32),
        "cb": np.ascontiguousarray(cb, np.float32),
    }


ML_DK = 128
ML_DV = 256
ML_WC = 128 + 128 + 128 + 256 + 256 + 2
BIGNEG = 30000.0


def emit_M_ml(S, tag, io):
    S.push(tag)
    ainF_g, ainH_g = io["ainF_g"], io["ainH_g"]
    w_d, gb_d, nwv_d, cst_d = io["w"], io["gb"], io["nwv"], io["cst"]
    ogF, ogH = io["ogF"], io["ogH"]
    c_og = 2

    w_sb = S.sb("w_sb", [128, 8, ML_WC], BF16)
    S.dma("pool", w_sb[:].rearrange("p a b -> p (a b)"), w_d[:, :], writes=[w_sb])
    wg_sb = S.sb("wg_sb", [128, 8, 2], BF16)
    cst = S.sb("cst_sb", [128, 64 + 512 + 128], F32)
    S.dma("sp", cst[:], cst_d[:, :], writes=[cst])
    identf = cst
    ident_b = S.sb("ident_b", [128, 128], BF16)
    S.op("act", lambda e: e.activation(out=ident_b[:], in_=cst[:, 576:704], func=AF.Copy), reads=[cst], writes=[ident_b])
    gb = S.sb("gb_sb", [1, 2], F32)
    S.dma("sp", gb[:], gb_d[:, :], writes=[gb])
    nwv = S.sb("nwv_sb", [64, ML_DV], F32)
    S.dma("sp", nwv[:], nwv_d[0:1, :].partition_broadcast(64), writes=[nwv])
    ones_row = S.sb("ones_row", [1, 128], F32)
    S.op("pool", lambda e: e.memset(ones_row[:], 1.0), writes=[ones_row])

    a_sb = [S.sb("a_sb%d" % i, [128, 8, 512], BF16) for i in range(2)]
    pp = PsumPool(S, 7)

    li_row = S.sb("li_row", [1, LP], F32)
    lf_row = S.sb("lf_row", [1, LP], F32)
    bb_row = S.sb("bb_row", [1, LP], F32)
    ones_bc = ones_row[0:1, 0:1].broadcast_to([1, LP])

    def load_a(i):
        ab = a_sb[i % 2]
        if i == 0:
            S.op("pool", lambda e: e.memset(ab[:, :, 0:XPAD], 0.0), writes=[ab])
            S.dma("sp", ab[:, :, XPAD:TS], ainH_g[0:128, :].rearrange("p (c t) -> p c t", c=8), reads=[ainH_g], writes=[ab])
        else:
            r0 = ((i - 1) % 4) * 512 + ((i - 1) // 4) * 128
            S.dma("sp", ab[:].rearrange("p c t -> p (c t)"), ainF_g[r0:r0 + 128, :], reads=[ainF_g], writes=[ab])

    load_a(0)
    for ti in range(NTS):
        if ti + 1 < NTS:
            load_a(ti + 1)
        ab = a_sb[ti % 2]
        for gi_, row in ((0, li_row), (1, lf_row)):
            pr = pp.get()
            for kc in range(8):
                S.op("pe", lambda e: e.matmul(pr[0:1, :], w_sb[:, kc, ML_WC - 2 + gi_:ML_WC - 1 + gi_], ab[:, kc, :],
                                              start=(kc == 0), stop=(kc == 7)), reads=[w_sb, ab], writes=[pr])
            S.op("act", lambda e: e.activation(out=row[:, ti * TS:(ti + 1) * TS], in_=pr[0:1, :], func=AF.Identity,
                                               bias=gb[:, gi_:gi_ + 1], scale=1.0), reads=[pr, gb], writes=[row])
    for row in (li_row, lf_row):
        S.op("act", lambda e: e.activation(out=row[:], in_=row[:], func=AF.Exp, scale=2.0 / 15.0), reads=[row], writes=[row])
        S.op("dve", lambda e: e.tensor_scalar_add(out=row[:], in0=row[:], scalar1=1.0), reads=[row], writes=[row])
        S.op("dve", lambda e: e.reciprocal(out=row[:], in_=row[:]), reads=[row], writes=[row])
        S.op("dve", lambda e: e.tensor_scalar(out=row[:], in0=row[:], scalar1=-30.0, scalar2=15.0, op0=ALU.mult, op1=ALU.add),
             reads=[row], writes=[row])
    S.op("act", lambda e: e.activation(out=lf_row[:], in_=lf_row[:], func=AF.Exp, scale=-1.0), reads=[lf_row], writes=[lf_row])
    S.op("act", lambda e: e.activation(out=lf_row[:], in_=lf_row[:], func=AF.Ln, bias=1.0, scale=1.0), reads=[lf_row], writes=[lf_row])
    S.op("dve", lambda e: e.tensor_scalar_mul(out=lf_row[:], in0=lf_row[:], scalar1=-1.0), reads=[lf_row], writes=[lf_row])
    S.op("pool", lambda e: e.memset(lf_row[:, 0:XPAD], 0.0), writes=[lf_row])
    S.op("pool", lambda e: e.memset(li_row[:, 0:XPAD], -BIGNEG), writes=[li_row])
    S.op("dve", lambda e: e.tensor_tensor_scan(out=bb_row[:], data0=ones_bc, data1=lf_row[:], initial=0.0,
                                               op0=ALU.mult, op1=ALU.add), reads=[ones_row, lf_row], writes=[bb_row])
    c_row = lf_row
    S.op("dve", lambda e: e.tensor_tensor(out=c_row[:], in0=li_row[:], in1=bb_row[:], op=ALU.subtract), reads=[li_row, bb_row], writes=[c_row])
    M_row = li_row
    S.op("dve", lambda e: e.tensor_tensor_scan(out=M_row[:], data0=ones_bc, data1=c_row[:], initial=0.0,
                                               op0=ALU.mult, op1=ALU.max), reads=[ones_row, c_row], writes=[M_row])
    en_row = bb_row
    S.op("dve", lambda e: e.tensor_tensor(out=en_row[:], in0=bb_row[:], in1=M_row[:], op=ALU.add), reads=[bb_row, M_row], writes=[en_row])
    S.op("act", lambda e: e.activation(out=en_row[:], in_=en_row[:], func=AF.Exp, scale=-1.0), reads=[en_row], writes=[en_row])

    c_tm = S.sb("c_tm", [64, NCH], F32)
    M_tm = S.sb("M_tm", [64, NCH], F32)
    en_tm = S.sb("en_tm", [64, NCH], F32)
    for row, tmb in ((c_row, c_tm), (M_row, M_tm), (en_row, en_tm)):
        pb = pp.get()
        for n in range(NCH):
            S.op("pe", lambda e: e.matmul(pb[0:64, n:n + 1], row[0:1, n * CH:(n + 1) * CH], ones_row[0:1, 0:1], start=True, stop=True),
                 reads=[row, ones_row], writes=[pb])
        S.op("act", lambda e: e.activation(out=tmb[:], in_=pb[0:64, 0:NCH], func=AF.Copy), reads=[pb], writes=[tmb])
    Mend_b = S.sb("Mend_b", [128, NCH], F32)
    Mprev_b = S.sb("Mprev_b", [128, NCH], F32)
    pb = pp.get()
    S.op("pe", lambda e: e.matmul(pb[:, 0:NCH], ones_row[0:1, :], M_row[0:1, CH - 1::CH], start=True, stop=True),
         reads=[ones_row, M_row], writes=[pb])
    S.op("act", lambda e: e.activation(out=Mend_b[:], in_=pb[:, 0:NCH], func=AF.Copy), reads=[pb], writes=[Mend_b])
    S.op("pool", lambda e: e.memset(Mprev_b[:, 0:1], 0.0), writes=[Mprev_b])
    S.op("pool", lambda e: e.tensor_copy(out=Mprev_b[:, 1:NCH], in_=Mend_b[:, 0:NCH - 1]), reads=[Mend_b], writes=[Mprev_b])
    sc_tm = S.sb("sc_tm", [64, NCH], F32)
    kws_tm = S.sb("kws_tm", [64, NCH], F32)
    dec_b = S.sb("dec_b", [128, NCH], F32)
    S.op("dve", lambda e: e.tensor_tensor(out=sc_tm[:], in0=Mprev_b[0:64, :], in1=M_tm[:], op=ALU.subtract), reads=[Mprev_b, M_tm], writes=[sc_tm])
    S.op("act", lambda e: e.activation(out=sc_tm[:], in_=sc_tm[:], func=AF.Exp), reads=[sc_tm], writes=[sc_tm])
    S.op("dve", lambda e: e.tensor_tensor(out=kws_tm[:], in0=c_tm[:], in1=Mend_b[0:64, :], op=ALU.subtract), reads=[c_tm, Mend_b], writes=[kws_tm])
    S.op("act", lambda e: e.activation(out=kws_tm[:], in_=kws_tm[:], func=AF.Exp), reads=[kws_tm], writes=[kws_tm])
    S.op("dve", lambda e: e.tensor_tensor(out=dec_b[:], in0=Mprev_b[:], in1=Mend_b[:], op=ALU.subtract), reads=[Mprev_b, Mend_b], writes=[dec_b])
    S.op("act", lambda e: e.activation(out=dec_b[:], in_=dec_b[:], func=AF.Exp), reads=[dec_b], writes=[dec_b])

    qT = [S.sb("qT%d" % i, [128, 512], BF16) for i in range(2)]
    kT = [S.sb("kT%d" % i, [128, 512], BF16) for i in range(2)]
    Wt = [S.sb("Wt%d" % i, [64, 512], F32) for i in range(2)]
    ogT = [S.sb("ogT%d" % i, [128, 2, 512], BF16) for i in range(2)]
    C_f = S.sb("C_f", [128, ML_DV + 1], F32)
    C_b = S.sb("C_b", [128, ML_DV + 1], BF16)
    S.op("pool", lambda e: e.memset(C_f[:], 0.0), writes=[C_f])
    S.op("pool", lambda e: e.memset(C_b[:], 0.0), writes=[C_b])
    NR = 3
    kw_sb = [S.sb("kw_sb%d" % i, [64, 128], BF16) for i in range(NR)]
    va_sb = [S.sb("va_sb%d" % i, [64, ML_DV + 1], BF16) for i in range(NR)]
    og_sb = [S.sb("ogs%d" % i, [64, ML_DV], F32) for i in range(NR)]
    St_sb = [S.sb("St%d" % i, [64, 64], BF16) for i in range(NR)]
    t1_sb = [S.sb("t1_%d" % i, [64, ML_DV + 1], F32) for i in range(NR)]
    sm_sb = [S.sb("sm%d" % i, [64, 8], F32) for i in range(NR)]
    junk = [S.sb("junk%d" % i, [64, ML_DV], F32) for i in range(NR)]
    hn_sb = [S.sb("hn%d" % i, [64, ML_DV], BF16) for i in range(NR)]
    for i in range(NR):
        S.op("pool", lambda e: e.memset(va_sb[i][:, ML_DV:ML_DV + 1], 1.0), writes=[va_sb[i]])
    pTall = S.ps("pTall", [128, 1024], BF16)

    load_a(0)
    for ti in range(NTS):
        if ti + 1 < NTS:
            load_a(ti + 1)
        ab = a_sb[ti % 2]
        t0 = ti * TS
        qTb, kTb, Wtb, ogTb = qT[ti % 2], kT[ti % 2], Wt[ti % 2], ogT[ti % 2]
        for wi, (dst, scl) in enumerate(((qTb, ML_DK ** -0.5), (kTb, 1.0))):
            pb = pp.get()
            for kc in range(8):
                S.op("pe", lambda e: e.matmul(pb[:, :], w_sb[:, kc, wi * 128:(wi + 1) * 128], ab[:, kc, :], start=(kc == 0), stop=(kc == 7)),
                     reads=[w_sb, ab], writes=[pb])
            S.op("act", lambda e: e.activation(out=dst[:], in_=pb[:, :], func=AF.Copy, scale=scl), reads=[pb], writes=[dst])
        pb = pp.get()
        S.op("pe", lambda e: e.matmul(pb[0:64, :], ones_row[0:1, 0:64], M_row[0:1, t0:t0 + TS], start=True, stop=False),
             reads=[ones_row, M_row], writes=[pb])
        S.op("pe", lambda e: e.matmul(pb[0:64, :], cst[0:64, 0:64], cst[0:64, 64:576], start=False, stop=True),
             reads=[cst], writes=[pb])
        S.op("dve", lambda e: e.tensor_tensor(out=Wtb[:].rearrange("p (n i) -> p n i", n=CPT), in0=pb[0:64, :].rearrange("p (n i) -> p n i", n=CPT),
                                              in1=bcast_last(c_tm[:, ti * CPT:(ti + 1) * CPT], CH), op=ALU.subtract),
             reads=[pb, c_tm], writes=[Wtb])
        S.op("act", lambda e: e.activation(out=Wtb[:], in_=Wtb[:], func=AF.Exp, scale=-1.0), reads=[Wtb], writes=[Wtb])
        for cj in range(CPT):
            n = ti * CPT + cj
            r = n % NR
            c0 = cj * CH
            pkv = pp.get()
            for kc in range(8):
                S.op("pe", lambda e: e.matmul(pkv[0:64, 0:384], ab[:, kc, c0:c0 + CH], w_sb[:, kc, 256:640], start=(kc == 0), stop=(kc == 7)),
                     reads=[w_sb, ab], writes=[pkv])
            pog = pp.get()
            for kc in range(8):
                S.op("pe", lambda e: e.matmul(pog[0:64, 0:256], ab[:, kc, c0:c0 + CH], w_sb[:, kc, 640:896], start=(kc == 0), stop=(kc == 7)),
                     reads=[w_sb, ab], writes=[pog])
            S.op("act", lambda e: e.activation(out=kw_sb[r][:], in_=pkv[0:64, 0:128], func=AF.Copy, scale=kws_tm[:, n:n + 1]),
                 reads=[pkv, kws_tm], writes=[kw_sb[r]])
            S.op("act", lambda e: e.activation(out=va_sb[r][:, 0:ML_DV], in_=pkv[0:64, 128:384], func=AF.Copy), reads=[pkv], writes=[va_sb[r]])
            S.op("act", lambda e: e.activation(out=og_sb[r][:], in_=pog[0:64, 0:256], func=AF.Exp, scale=-1.0), reads=[pog], writes=[og_sb[r]])
            S.op("dve", lambda e: e.tensor_scalar_add(out=og_sb[r][:], in0=og_sb[r][:], scalar1=1.0), reads=[og_sb[r]], writes=[og_sb[r]])
            S.op("dve", lambda e: e.reciprocal(out=og_sb[r][:], in_=og_sb[r][:]), reads=[og_sb[r]], writes=[og_sb[r]])
            S.op("dve", lambda e: e.tensor_tensor(out=og_sb[r][:], in0=og_sb[r][:], in1=nwv[:], op=ALU.mult), reads=[og_sb[r], nwv], writes=[og_sb[r]])
            pq = pp.get()
            S.op("pe", lambda e: e.matmul(pq[0:64, 0:64], kTb[:, c0:c0 + CH], qTb[:, c0:c0 + CH], start=True, stop=True),
                 reads=[kTb, qTb], writes=[pq])
            S.op("dve", lambda e: e.tensor_tensor(out=St_sb[r][:], in0=pq[0:64, 0:64], in1=Wtb[:, c0:c0 + CH], op=ALU.mult),
                 reads=[pq, Wtb], writes=[St_sb[r]])
            pint = pp.get()
            S.op("pe", lambda e: e.matmul(pint[0:64, 0:ML_DV + 1], qTb[:, c0:c0 + CH], C_b[:], start=True, stop=True),
                 reads=[qTb, C_b], writes=[pint])
            pia = pp.get()
            S.op("pe", lambda e: e.matmul(pia[0:64, 0:ML_DV + 1], St_sb[r][:], va_sb[r][:], start=True, stop=True),
                 reads=[St_sb[r], va_sb[r]], writes=[pia])
            pst = pp.get()
            S.op("pe", lambda e: e.matmul(pst[:, 0:ML_DV + 1], kw_sb[r][:], va_sb[r][:], start=True, stop=True),
                 reads=[kw_sb[r], va_sb[r]], writes=[pst])
            S.op("dve", lambda e: e.scalar_tensor_tensor(out=C_f[:], in0=C_f[:], scalar=dec_b[:, n:n + 1], in1=pst[:, 0:ML_DV + 1],
                                                         op0=ALU.mult, op1=ALU.add), reads=[C_f, dec_b, pst], writes=[C_f])
            S.op("act", lambda e: e.activation(out=C_b[:], in_=C_f[:], func=AF.Copy), reads=[C_f], writes=[C_b])
            t1 = t1_sb[r]
            S.op("act", lambda e: e.activation(out=t1[:], in_=pint[0:64, 0:ML_DV + 1], func=AF.Copy, scale=sc_tm[:, n:n + 1]),
                 reads=[pint, sc_tm], writes=[t1])
            S.op("dve", lambda e: e.tensor_tensor(out=t1[:], in0=t1[:], in1=pia[0:64, 0:ML_DV + 1], op=ALU.add), reads=[t1, pia], writes=[t1])
            sm = sm_sb[r]
            S.op("act", lambda e: e.activation(out=sm[:, 0:1], in_=t1[:, ML_DV:ML_DV + 1], func=AF.Abs), reads=[t1], writes=[sm])
            S.op("dve", lambda e: e.tensor_tensor(out=sm[:, 0:1], in0=sm[:, 0:1], in1=en_tm[:, n:n + 1], op=ALU.max),
                 reads=[sm, en_tm], writes=[sm])
            S.op("dve", lambda e: e.reciprocal(out=sm[:, 1:2], in_=sm[:, 0:1]), reads=[sm], writes=[sm])
            S.op("act", lambda e: e.activation(out=junk[r][:], in_=t1[:, 0:ML_DV], func=AF.Square, scale=sm[:, 1:2], accum_out=sm[:, 2:3]),
                 reads=[t1, sm], writes=[junk[r], sm])
            S.op("act", lambda e: e.activation(out=sm[:, 3:4], in_=sm[:, 2:3], func=AF.Ln, scale=1.0 / ML_DV, bias=RMS_EPS), reads=[sm], writes=[sm])
            S.op("act", lambda e: e.activation(out=sm[:, 3:4], in_=sm[:, 3:4], func=AF.Exp, scale=-0.5), reads=[sm], writes=[sm])
            S.op("dve", lambda e: e.tensor_tensor(out=sm[:, 4:5], in0=sm[:, 3:4], in1=sm[:, 1:2], op=ALU.mult), reads=[sm], writes=[sm])
            S.op("dve", lambda e: e.scalar_tensor_tensor(out=hn_sb[r][:], in0=t1[:, 0:ML_DV], scalar=sm[:, 4:5], in1=og_sb[r][:],
                                                         op0=ALU.mult, op1=ALU.mult), reads=[t1, sm, og_sb[r]], writes=[hn_sb[r]])
            ptv = pTall[:, (n % 4) * 128:(n % 4 + 1) * 128].rearrange("p (a b) -> p a b", a=2)
            for hh in range(2):
                S.op("pe", lambda e: e.transpose(ptv[:, hh, :], hn_sb[r][:, hh * 128:(hh + 1) * 128], ident_b[0:64, 0:64]),
                     reads=[hn_sb[r], ident_b], writes=[pTall])
            S.op("act", lambda e: e.activation(out=ogTb[:, :, c0:c0 + CH], in_=ptv, func=AF.Copy), reads=[pTall], writes=[ogTb])
        if ti >= 1:
            S.dma("pool", ogF[ti - 1][:, :].rearrange("p (c t) -> p c t", c=c_og), ogTb[:], reads=[ogTb], writes=[ogF[ti - 1]])
        if ti % 4 == 0 and ti <= 12:
            S.dma("pool", ogH[(ti // 4) * 128:(ti // 4 + 1) * 128, :].rearrange("p (c t) -> p c t", c=c_og), ogTb[:, :, TS - 64:TS],
                  reads=[ogTb], writes=[ogH])
        io["after_og"](ti)
    S.pop()


def prep_M_ml(inp, j, h):
    w = inp["ml_w_in"][j]
    q = w[:, h * 128:(h + 1) * 128]
    k = w[:, 512 + h * 128:512 + (h + 1) * 128]
    v = w[:, 1024 + h * 256:1024 + (h + 1) * 256]
    o = w[:, 2048 + h * 256:2048 + (h + 1) * 256]
    gi = w[:, 3072 + h:3073 + h]
    gf = w[:, 3076 + h:3077 + h]
    wc = np.concatenate([q, k, k, v, o, gi, gf], 1)
    wc = wc.reshape(8, 128, ML_WC).transpose(1, 0, 2)
    gb = inp["ml_gate_b"][j][[h, 4 + h]].reshape(1, 2)
    nwv = inp["ml_norm_w"][j][h * 256:(h + 1) * 256].reshape(1, 256)
    return {"w": np.ascontiguousarray(wc.reshape(128, 8 * ML_WC), np.float32),
            "gb": np.ascontiguousarray(gb, np.float32), "nwv": np.ascontiguousarray(nwv, np.float32),
            "cst": ml_consts()}


def ml_consts():
    c = np.zeros((128, 64 + 512 + 128), np.float32)
    c[0:64, 0:64] = np.eye(64, dtype=np.float32)
    jj = np.arange(64)[:, None]
    ii = np.arange(64)[None, :]
    mb = (jj > ii).astype(np.float32) * BIGNEG
    c[0:64, 64:576] = np.tile(mb, (1, 8))
    c[:, 576:704] = np.eye(128, dtype=np.float32)
    return c


G_WC = 12 * 128 + 8
L2_EPS = 1e-6
GDN_EPS = 1e-6
C_I64, C_MUI, C_MUS, C_MLS, C_NMLS, C_NMUS, C_I128, C_ONES = 0, 64, 128, 192, 256, 320, 384, 512
C_TOT = 640


def gdn_consts():
    c = np.zeros((128, C_TOT), np.float32)
    p = np.arange(64)[:, None]
    f = np.arange(64)[None, :]
    c[0:64, C_I64:C_I64 + 64] = (p == f)
    c[0:64, C_MUI:C_MUI + 64] = (p <= f)
    c[0:64, C_MUS:C_MUS + 64] = (p < f)
    c[0:64, C_MLS:C_MLS + 64] = (p > f)
    c[0:64, C_NMLS:C_NMLS + 64] = -1.0 * (p > f)
    c[0:64, C_NMUS:C_NMUS + 64] = -1.0 * (p < f)
    c[:, C_I128:C_I128 + 128] = np.eye(128, dtype=np.float32)
    c[:, C_ONES:C_ONES + 128] = 1.0
    return c


def emit_M_gdn(S, tag, io):
    S.push(tag)
    ainF_g, ainH_g = io["ainF_g"], io["ainH_g"]
    w_d, cw_d, hv_d, gnw_d, cst_d = io["w"], io["cw"], io["hv"], io["gnw"], io["cst"]
    ogF, ogH = io["ogF"], io["ogH"]
    c_og = 4

    w_sb = S.sb("w_sb", [128, 8, G_WC], BF16)
    S.dma("pool", w_sb[:].rearrange("p a b -> p (a b)"), w_d[:, :], writes=[w_sb])
    cst = S.sb("cst_sb", [128, C_TOT], F32)
    S.dma("sp", cst[:], cst_d[:, :], writes=[cst])
    cwg = S.sb("cwg", [128, 8, 4], F32)
    S.dma("sp", cwg[:].rearrange("p a b -> p (a b)"), cw_d[:, :], writes=[cwg])
    hv = S.sb("hv_sb", [64, 8], F32)
    S.dma("sp", hv[:], hv_d[0:1, :].partition_broadcast(64), writes=[hv])
    gnw = S.sb("gnw_sb", [128, 1], F32)
    S.dma("sp", gnw[:], gnw_d[:, :], writes=[gnw])
    ident_b = S.sb("ident_b", [128, 128], BF16)
    ones_b = S.sb("ones_b", [128, 128], BF16)
    S.op("act", lambda e: e.activation(out=ident_b[:], in_=cst[:, C_I128:C_I128 + 128], func=AF.Copy), reads=[cst], writes=[ident_b])
    S.op("act", lambda e: e.activation(out=ones_b[:], in_=cst[:, C_ONES:C_ONES + 128], func=AF.Copy), reads=[cst], writes=[ones_b])
    dg = S.sb("dg", [128, 32, 128], BF16)
    for mc in range(8):
        for k in range(4):
            S.op("dve", lambda e: e.tensor_scalar(out=dg[:, mc * 4 + k, :], in0=cst[:, C_I128:C_I128 + 128], scalar1=cwg[:, mc, k:k + 1],
                                                  scalar2=None, op0=ALU.mult), reads=[cst, cwg], writes=[dg])
    I64 = cst[0:64, C_I64:C_I64 + 64]
    MUI = cst[0:64, C_MUI:C_MUI + 64]
    MLS = cst[0:64, C_MLS:C_MLS + 64]
    NMLS = cst[0:64, C_NMLS:C_NMLS + 64]
    NMUS = cst[0:64, C_NMUS:C_NMUS + 64]
    ONES64x128 = cst[0:64, C_ONES:C_ONES + 128]

    a_sb = [S.sb("a_sb%d" % i, [128, 8, 512], BF16) for i in range(2)]
    pp = PsumPool(S, 7)
    pTall = S.ps("pTall", [128, 1024], BF16)

    def load_a(i):
        ab = a_sb[i % 2]
        if i == 0:
            S.op("pool", lambda e: e.memset(ab[:, :, 0:XPAD], 0.0), writes=[ab])
            S.dma("sp", ab[:, :, XPAD:TS], ainH_g[0:128, :].rearrange("p (c t) -> p c t", c=8), reads=[ainH_g], writes=[ab])
        else:
            r0 = ((i - 1) % 4) * 512 + ((i - 1) // 4) * 128
            S.dma("sp", ab[:].rearrange("p c t -> p (c t)"), ainF_g[r0:r0 + 128, :], reads=[ainF_g], writes=[ab])

    NH = NCH * 4
    bg = S.sb("bg", [64, NCH, 8], F32)
    load_a(0)
    for ti in range(NTS):
        if ti + 1 < NTS:
            load_a(ti + 1)
        ab = a_sb[ti % 2]
        pb = pp.get()
        for cj in range(CPT):
            for kc in range(8):
                S.op("pe", lambda e: e.matmul(pb[0:64, cj * 8:(cj + 1) * 8], ab[:, kc, cj * CH:(cj + 1) * CH], w_sb[:, kc, 1536:1544],
                                              start=(kc == 0), stop=(kc == 7)), reads=[ab, w_sb], writes=[pb])
        S.op("act", lambda e: e.activation(out=bg[:, ti * CPT:(ti + 1) * CPT, :], in_=pb[0:64, 0:64].rearrange("p (a b) -> p a b", a=CPT),
                                           func=AF.Copy), reads=[pb], writes=[bg])
    lnb = S.sb("lnb", [64, NCH, 4], F32)
    bt = S.sb("bt", [64, NCH, 4], F32)
    gt = S.sb("gt", [64, NCH, 4], F32)
    beG = S.sb("beG", [64, NCH, 4], F32)
    ekt = S.sb("ekt", [64, NCH, 4], F32)
    eGl = S.sb("eGl", [128, NCH, 4], F32)
    tmpg = S.sb("tmpg", [64, NCH, 4], F32)
    eal = S.sb("eal", [64, 4], F32)
    S.op("act", lambda e: e.activation(out=lnb[:], in_=bg[:, :, 0:4], func=AF.Exp, scale=-1.0), reads=[bg], writes=[lnb])
    S.op("act", lambda e: e.activation(out=lnb[:], in_=lnb[:], func=AF.Ln, bias=1.0, scale=1.0), reads=[lnb], writes=[lnb])
    S.op("dve", lambda e: e.tensor_scalar_mul(out=lnb[:], in0=lnb[:], scalar1=-1.0), reads=[lnb], writes=[lnb])
    S.op("act", lambda e: e.activation(out=bt[:], in_=lnb[:], func=AF.Exp), reads=[lnb], writes=[bt])
    S.op("dve", lambda e: e.tensor_tensor(out=gt[:], in0=bg[:, :, 4:8], in1=bcast_mid(hv[:, 4:8], NCH), op=ALU.add), reads=[bg, hv], writes=[gt])
    S.op("act", lambda e: e.activation(out=gt[:], in_=gt[:], func=AF.Exp), reads=[gt], writes=[gt])
    S.op("act", lambda e: e.activation(out=gt[:], in_=gt[:], func=AF.Ln, bias=1.0, scale=1.0), reads=[gt], writes=[gt])
    S.op("act", lambda e: e.activation(out=eal[:], in_=hv[:, 0:4], func=AF.Exp), reads=[hv], writes=[eal])
    S.op("dve", lambda e: e.tensor_scalar_mul(out=eal[:], in0=eal[:], scalar1=-1.0), reads=[eal], writes=[eal])
    S.op("dve", lambda e: e.tensor_tensor(out=gt[:], in0=gt[:], in1=bcast_mid(eal[:], NCH), op=ALU.mult), reads=[gt, eal], writes=[gt])
    gflat = gt[:].rearrange("p a b -> p (a b)")
    for (c0, c1) in ((0, 272), (272, NH)):
        pb = pp.get()
        S.op("pe", lambda e: e.matmul(pb[0:64, 0:c1 - c0], MUI, gflat[:, c0:c1], start=True, stop=True), reads=[cst, gt], writes=[pb])
        pl = pp.get()
        S.op("pe", lambda e: e.matmul(pl[:, 0:c1 - c0], ONES64x128, gflat[:, c0:c1], start=True, stop=True), reads=[cst, gt], writes=[pl])
        S.op("act", lambda e: e.activation(out=tmpg[:].rearrange("p a b -> p (a b)")[:, c0:c1], in_=pb[0:64, 0:c1 - c0], func=AF.Exp),
             reads=[pb], writes=[tmpg])
        S.op("act", lambda e: e.activation(out=eGl[:].rearrange("p a b -> p (a b)")[:, c0:c1], in_=pl[:, 0:c1 - c0], func=AF.Exp),
             reads=[pl], writes=[eGl])
        S.op("act", lambda e: e.activation(out=ekt[:].rearrange("p a b -> p (a b)")[:, c0:c1], in_=pb[0:64, 0:c1 - c0], func=AF.Copy),
             reads=[pb], writes=[ekt])
        S.op("dve", lambda e: e.tensor_tensor(out=ekt[:].rearrange("p a b -> p (a b)")[:, c0:c1], in0=pl[0:64, 0:c1 - c0],
                                              in1=ekt[:].rearrange("p a b -> p (a b)")[:, c0:c1], op=ALU.subtract), reads=[pl, ekt], writes=[ekt])
    S.op("act", lambda e: e.activation(out=ekt[:], in_=ekt[:], func=AF.Exp), reads=[ekt], writes=[ekt])
    S.op("dve", lambda e: e.tensor_tensor(out=beG[:], in0=bt[:], in1=tmpg[:], op=ALU.mult), reads=[bt, tmpg], writes=[beG])

    xb = [S.sb("xb%d" % i, [128, 8, 3 + TS], BF16) for i in range(2)]
    S.op("pool", lambda e: e.memset(xb[1][:, :, TS:TS + 3], 0.0), writes=[xb[1]])
    sx = [S.sb("sx%d" % i, [128, TS], F32) for i in range(2)]
    sqb = [S.sb("sqb%d" % i, [128, TS], BF16) for i in range(2)]
    rs = [S.sb("rs%d" % i, [128, TS], F32) for i in range(2)]
    qT = [S.sb("qT%d" % i, [128, 2, TS], BF16) for i in range(2)]
    kT = [S.sb("kT%d" % i, [128, 2, TS], BF16) for i in range(2)]
    svT = [S.sb("svT%d" % i, [128, 4, TS], BF16) for i in range(2)]
    zs = [S.sb("zs0", [128, 4, TS], F32)] * 2
    onT = [S.sb("onT%d" % i, [128, 4, TS], BF16) for i in range(2)]
    ogt = [S.sb("ogt0", [128, 4, TS], BF16)] * 2
    S_f = S.sb("S_f", [128, 4, 128], F32)
    S_b = S.sb("S_b", [128, 4, 128], BF16)
    S_t = S.sb("S_t", [128, 4, 128], F32)
    S.op("pool", lambda e: e.memset(S_f[:], 0.0), writes=[S_f])
    S.op("pool", lambda e: e.memset(S_b[:], 0.0), writes=[S_b])
    NR = 2
    mk = lambda nm, shp, dt: [S.sb("%s%d" % (nm, i), shp, dt) for i in range(NR)]
    kbg = mk("kbg", [64, 4, 128], BF16)
    ktm = mk("ktm", [64, 4, 128], BF16)
    vb = mk("vb", [64, 4, 128], BF16)
    rg1 = mk("rg1", [64, 4, 64], F32)
    rg2 = mk("rg2", [64, 4, 64], F32)
    rg3 = mk("rg3", [64, 4, 64], F32)
    Et = mk("Et", [64, 4, 64], F32)
    Wt_ = mk("Wt", [64, 4, 64], F32)
    W_ = mk("W", [64, 4, 64], F32)
    eGb = mk("eGb", [128, 4, 64], F32)
    KKlo = mk("KKlo", [64, 2, 64], F32)
    KKup = mk("KKup", [64, 2, 64], F32)
    KQm = mk("KQm", [64, 2, 64], F32)
    Qt = mk("Qt", [64, 4, 64], BF16)
    qdT = mk("qdT", [128, 4, 64], BF16)
    PP = [mk("PP%d" % k, [64, 8, 64], F32) for k in range(2)]
    Xt = [mk("Xt%d" % k, [64, 4, 64], F32) for k in range(2)]
    Tt = mk("Tt", [64, 4, 64], BF16)
    nwT = mk("nwT", [128, 4, 64], BF16)
    vn = mk("vn", [64, 4, 128], BF16)
    sqo = mk("sqo", [64, 4, 128], F32)
    sso = mk("sso", [64, 8], F32)
    on = mk("on", [64, 4, 128], BF16)

    def silu_from_psum(pb, W, out_ap, out_buf, idx):
        S.op("act", lambda e: e.activation(out=out_ap, in_=pb[:, :W], func=AF.Silu), reads=[pb], writes=[out_buf])

    load_a(0)
    for ti in range(NTS):
        if ti + 1 < NTS:
            load_a(ti + 1)
        ab = a_sb[ti % 2]
        t0 = ti * TS
        xcur, xprev = xb[ti % 2], xb[(ti + 1) % 2]
        qTb, kTb, svb, zsb, onTb, ogb = qT[ti % 2], kT[ti % 2], svT[ti % 2], zs[ti % 2], onT[ti % 2], ogt[ti % 2]
        S.op("pool", lambda e: e.tensor_copy(out=xcur[:, :, 0:3], in_=xprev[:, :, TS:TS + 3]), reads=[xprev], writes=[xcur])
        for mc in range(12):
            pb = pp.get()
            for kc in range(8):
                S.op("pe", lambda e: e.matmul(pb[:, :], w_sb[:, kc, mc * 128:(mc + 1) * 128], ab[:, kc, :], start=(kc == 0), stop=(kc == 7)),
                     reads=[w_sb, ab], writes=[pb])
            if mc < 8:
                S.op("act", lambda e: e.activation(out=xcur[:, mc, 3:3 + TS], in_=pb[:, :], func=AF.Copy), reads=[pb], writes=[xcur])
            else:
                silu_from_psum(pb, TS, zsb[:, mc - 8, :], zsb, mc)
        for mc in range(8):
            pb = pp.get()
            for k in range(4):
                S.op("pe", lambda e: e.matmul(pb[:, :], dg[:, mc * 4 + k, :], xcur[:, mc, k:k + TS], start=(k == 0), stop=(k == 3)),
                     reads=[dg, xcur], writes=[pb])
            if mc >= 4:
                silu_from_psum(pb, TS, svb[:, mc - 4, :], svb, mc)
            else:
                sxb, sq, rsb = sx[mc % 2], sqb[mc % 2], rs[mc % 2]
                silu_from_psum(pb, TS, sxb[:, :], sxb, mc)
                S.op("act", lambda e: e.activation(out=sq[:, :], in_=sxb[:, :], func=AF.Square), reads=[sxb], writes=[sq])
                ps2 = pp.get()
                S.op("pe", lambda e: e.matmul(ps2[:, :], ones_b[:], sq[:, :], start=True, stop=True), reads=[ones_b, sq], writes=[ps2])
                rstd_from_ss(S, ps2, rsb, 1.0, L2_EPS, TS)
                dst = qTb if mc < 2 else kTb
                scl = (128.0 ** -0.5) if mc < 2 else 1.0
                S.op("dve", lambda e: e.scalar_tensor_tensor(out=dst[:, mc % 2, :], in0=sxb[:, :], scalar=scl, in1=rsb[:, :],
                                                             op0=ALU.mult, op1=ALU.mult), reads=[sxb, rsb], writes=[dst])
        for cj in range(CPT):
            n = ti * CPT + cj
            r = n % NR
            c0 = cj * CH
            for qh in range(2):
                S.op("pe", lambda e: e.transpose(pTall[0:64, qh * 128:(qh + 1) * 128], kTb[:, qh, c0:c0 + CH], ident_b[:, :]),
                     reads=[kTb, ident_b], writes=[pTall])
            for h in range(4):
                S.op("pe", lambda e: e.transpose(pTall[0:64, 256 + h * 128:256 + (h + 1) * 128], svb[:, h, c0:c0 + CH], ident_b[:, :]),
                     reads=[svb, ident_b], writes=[pTall])
            ktm_ps = pTall[0:64, 0:256].rearrange("p (a b) -> p a b", a=2)
            ktm_rep = ktm_ps.unsqueeze(2).broadcast_to([64, 2, 2, 128])
            as4 = lambda ap: ap.rearrange("p (a r) d -> p a r d", r=2)
            S.op("dve", lambda e: e.tensor_tensor(out=as4(kbg[r][:]), in0=ktm_rep, in1=as4(bcast_last(beG[:, n, :], 128)), op=ALU.mult),
                 reads=[pTall, beG], writes=[kbg[r]])
            S.op("dve", lambda e: e.tensor_tensor(out=as4(ktm[r][:]), in0=ktm_rep, in1=as4(bcast_last(ekt[:, n, :], 128)), op=ALU.mult),
                 reads=[pTall, ekt], writes=[ktm[r]])
            S.op("dve", lambda e: e.tensor_tensor(out=vb[r][:], in0=pTall[0:64, 256:768].rearrange("p (a b) -> p a b", a=4),
                                                  in1=bcast_last(bt[:, n, :], 128), op=ALU.mult), reads=[pTall, bt], writes=[vb[r]])
            S.op("dve", lambda e: e.tensor_tensor(out=rg1[r][:], in0=bcast_mid(MUI, 4), in1=bcast_last(gt[:, n, :], 64), op=ALU.mult),
                 reads=[cst, gt], writes=[rg1[r]])
            S.op("dve", lambda e: e.tensor_tensor(out=rg2[r][:], in0=bcast_mid(I64, 4), in1=bcast_last(lnb[:, n, :], 64), op=ALU.mult),
                 reads=[cst, lnb], writes=[rg2[r]])
            S.op("dve", lambda e: e.tensor_tensor(out=rg2[r][:], in0=rg2[r][:], in1=rg1[r][:], op=ALU.add), reads=[rg1[r], rg2[r]], writes=[rg2[r]])
            S.op("dve", lambda e: e.tensor_tensor(out=rg3[r][:], in0=bcast_mid(MLS, 4), in1=bcast_last(gt[:, n, :], 64), op=ALU.mult),
                 reads=[cst, gt], writes=[rg3[r]])
            fl = lambda b_: b_[:].rearrange("p a b -> p (a b)")
            pd1 = pp.get()
            S.op("pe", lambda e: e.matmul(pd1[0:64, 0:256], MLS, fl(rg1[r]), start=True, stop=True), reads=[cst, rg1[r]], writes=[pd1])
            S.op("pe", lambda e: e.matmul(pd1[0:64, 256:512], MLS, fl(rg2[r]), start=True, stop=True), reads=[cst, rg2[r]], writes=[pd1])
            pd2 = pp.get()
            S.op("pe", lambda e: e.matmul(pd2[0:64, 0:256], MUI, fl(rg3[r]), start=True, stop=True), reads=[cst, rg3[r]], writes=[pd2])
            pd3 = pp.get()
            S.op("pe", lambda e: e.matmul(pd3[:, 0:256], ONES64x128, fl(rg1[r]), start=True, stop=True), reads=[cst, rg1[r]], writes=[pd3])
            S.op("act", lambda e: e.activation(out=fl(Et[r]), in_=pd1[0:64, 0:256], func=AF.Exp), reads=[pd1], writes=[Et[r]])
            S.op("act", lambda e: e.activation(out=fl(Wt_[r]), in_=pd1[0:64, 256:512], func=AF.Exp), reads=[pd1], writes=[Wt_[r]])
            for h in range(4):
                S.op("act", lambda e: e.activation(out=W_[r][:, h, :], in_=pd2[0:64, h * 64:(h + 1) * 64], func=AF.Exp,
                                                   bias=lnb[:, n, h:h + 1], scale=1.0), reads=[pd2, lnb], writes=[W_[r]])
            S.op("act", lambda e: e.activation(out=fl(eGb[r]), in_=pd3[:, 0:256], func=AF.Exp), reads=[pd3], writes=[eGb[r]])
            pg = pp.get()
            for qh in range(2):
                S.op("pe", lambda e: e.matmul(pg[0:64, qh * 64:(qh + 1) * 64], kTb[:, qh, c0:c0 + CH], kTb[:, qh, c0:c0 + CH], start=True, stop=True),
                     reads=[kTb], writes=[pg])
                S.op("pe", lambda e: e.matmul(pg[0:64, 128 + qh * 64:128 + (qh + 1) * 64], kTb[:, qh, c0:c0 + CH], qTb[:, qh, c0:c0 + CH],
                                              start=True, stop=True), reads=[kTb, qTb], writes=[pg])
            kkv = pg[0:64, 0:128].rearrange("p (a b) -> p a b", a=2)
            kqv = pg[0:64, 128:256].rearrange("p (a b) -> p a b", a=2)
            S.op("dve", lambda e: e.tensor_tensor(out=KKlo[r][:], in0=kkv, in1=bcast_mid(NMLS, 2), op=ALU.mult), reads=[pg, cst], writes=[KKlo[r]])
            S.op("dve", lambda e: e.tensor_tensor(out=KKup[r][:], in0=kkv, in1=bcast_mid(NMUS, 2), op=ALU.mult), reads=[pg, cst], writes=[KKup[r]])
            S.op("dve", lambda e: e.tensor_tensor(out=KQm[r][:], in0=kqv, in1=bcast_mid(MUI, 2), op=ALU.mult), reads=[pg, cst], writes=[KQm[r]])
            rep = lambda b_: b_[:].unsqueeze(2).broadcast_to([64, 2, 2, 64])
            P0 = PP[0][r]
            S.op("dve", lambda e: e.tensor_tensor(out=as4(P0[:, 0:4, :]), in0=rep(KKlo[r]), in1=as4(W_[r][:]), op=ALU.mult),
                 reads=[KKlo[r], W_[r]], writes=[P0])
            S.op("dve", lambda e: e.tensor_tensor(out=as4(P0[:, 4:8, :]), in0=rep(KKup[r]), in1=as4(Wt_[r][:]), op=ALU.mult),
                 reads=[KKup[r], Wt_[r]], writes=[P0])
            S.op("dve", lambda e: e.tensor_tensor(out=as4(Qt[r][:]), in0=rep(KQm[r]), in1=as4(Et[r][:]), op=ALU.mult),
                 reads=[KQm[r], Et[r]], writes=[Qt[r]])
            S.op("dve", lambda e: e.tensor_tensor(out=as4(qdT[r][:]), in0=qTb[:, :, c0:c0 + CH].unsqueeze(2).broadcast_to([128, 2, 2, 64]),
                                                  in1=as4(eGb[r][:]), op=ALU.mult), reads=[qTb, eGb[r]], writes=[qdT[r]])
            X = Xt[0][r]
            S.op("dve", lambda e: e.tensor_tensor(out=X[:], in0=P0[:, 4:8, :], in1=bcast_mid(I64, 4), op=ALU.add), reads=[P0, cst], writes=[X])
            for k in range(1, 6):
                Pp, Pn = PP[(k - 1) % 2][r], PP[k % 2][r]
                pq = pp.get()
                for h in range(4):
                    S.op("pe", lambda e: e.matmul(pq[0:64, h * 64:(h + 1) * 64], Pp[:, 4 + h, :], Pp[:, h, :], start=True, stop=True),
                         reads=[Pp], writes=[pq])
                if k < 5:
                    for h in range(4):
                        S.op("pe", lambda e: e.matmul(pq[0:64, 256 + h * 64:256 + (h + 1) * 64], Pp[:, h, :], Pp[:, 4 + h, :], start=True, stop=True),
                             reads=[Pp], writes=[pq])
                wdt = 512 if k < 5 else 256
                S.op("act", lambda e: e.activation(out=Pn[:].rearrange("p a b -> p (a b)")[:, 0:wdt], in_=pq[0:64, 0:wdt], func=AF.Copy),
                     reads=[pq], writes=[Pn])
                px = pp.get()
                Xo = Xt[(k - 1) % 2][r]
                Xn = Xt[k % 2][r]
                for h in range(4):
                    S.op("pe", lambda e: e.matmul(px[0:64, h * 64:(h + 1) * 64], Pn[:, h, :], Xo[:, h, :], start=True, stop=True),
                         reads=[Pn, Xo], writes=[px])
                if k < 5:
                    S.op("dve", lambda e: e.tensor_tensor(out=fl(Xn), in0=px[0:64, 0:256], in1=fl(Xo), op=ALU.add), reads=[px, Xo], writes=[Xn])
                else:
                    S.op("dve", lambda e: e.tensor_tensor(out=fl(Tt[r]), in0=px[0:64, 0:256], in1=fl(Xo), op=ALU.add), reads=[px, Xo], writes=[Tt[r]])
            pw = pp.get()
            for h in range(4):
                S.op("pe", lambda e: e.matmul(pw[:, h * 64:(h + 1) * 64], kbg[r][:, h, :], Tt[r][:, h, :], start=True, stop=True),
                     reads=[kbg[r], Tt[r]], writes=[pw])
            S.op("act", lambda e: e.activation(out=fl(nwT[r]), in_=pw[:, 0:256], func=AF.Copy, scale=-1.0), reads=[pw], writes=[nwT[r]])
            pu = pp.get()
            for h in range(4):
                S.op("pe", lambda e: e.matmul(pu[0:64, h * 128:(h + 1) * 128], Tt[r][:, h, :], vb[r][:, h, :], start=True, stop=False),
                     reads=[Tt[r], vb[r]], writes=[pu])
                S.op("pe", lambda e: e.matmul(pu[0:64, h * 128:(h + 1) * 128], nwT[r][:, h, :], S_b[:, h, :], start=False, stop=True),
                     reads=[nwT[r], S_b], writes=[pu])
            S.op("act", lambda e: e.activation(out=vn[r][:].rearrange("p a b -> p (a b)"), in_=pu[0:64, :], func=AF.Copy), reads=[pu], writes=[vn[r]])
            po = pp.get()
            for h in range(4):
                S.op("pe", lambda e: e.matmul(po[0:64, h * 128:(h + 1) * 128], qdT[r][:, h, :], S_b[:, h, :], start=True, stop=False),
                     reads=[qdT[r], S_b], writes=[po])
                S.op("pe", lambda e: e.matmul(po[0:64, h * 128:(h + 1) * 128], Qt[r][:, h, :], vn[r][:, h, :], start=False, stop=True),
                     reads=[Qt[r], vn[r]], writes=[po])
            pS = pp.get()
            for h in range(4):
                S.op("pe", lambda e: e.matmul(pS[:, h * 128:(h + 1) * 128], ktm[r][:, h, :], vn[r][:, h, :], start=True, stop=True),
                     reads=[ktm[r], vn[r]], writes=[pS])
            for h in range(4):
                S.op("dve", lambda e: e.scalar_tensor_tensor(out=S_f[:, h, :], in0=S_f[:, h, :], scalar=eGl[:, n, h:h + 1],
                                                             in1=pS[:, h * 128:(h + 1) * 128], op0=ALU.mult, op1=ALU.add),
                     reads=[S_f, eGl, pS], writes=[S_f])
            S.op("act", lambda e: e.activation(out=S_b[:], in_=S_f[:], func=AF.Copy), reads=[S_f], writes=[S_b])
            S.op("act", lambda e: e.activation(out=sqo[r][:].rearrange("p a b -> p (a b)"), in_=po[0:64, :], func=AF.Square), reads=[po], writes=[sqo[r]])
            S.op("dve", lambda e: e.tensor_reduce(out=sso[r][:, 0:4], in_=sqo[r][:], axis=AX.X, op=ALU.add), reads=[sqo[r]], writes=[sso[r]])
            S.op("act", lambda e: e.activation(out=sso[r][:, 4:8], in_=sso[r][:, 0:4], func=AF.Ln, scale=1.0 / 128.0, bias=GDN_EPS),
                 reads=[sso[r]], writes=[sso[r]])
            S.op("act", lambda e: e.activation(out=sso[r][:, 4:8], in_=sso[r][:, 4:8], func=AF.Exp, scale=-0.5), reads=[sso[r]], writes=[sso[r]])
            S.op("dve", lambda e: e.tensor_tensor(out=on[r][:], in0=po[0:64, :].rearrange("p (a b) -> p a b", a=4),
                                                  in1=bcast_last(sso[r][:, 4:8], 128), op=ALU.mult), reads=[po, sso[r]], writes=[on[r]])
            for h in range(4):
                S.op("pe", lambda e: e.transpose(pTall[:, 768 + h * 64:768 + (h + 1) * 64], on[r][:, h, :], ident_b[0:64, 0:64]),
                     reads=[on[r], ident_b], writes=[pTall])
            S.op("act", lambda e: e.activation(out=onTb[:, :, c0:c0 + CH], in_=pTall[:, 768:1024].rearrange("p (a b) -> p a b", a=4), func=AF.Copy),
                 reads=[pTall], writes=[onTb])
        S.op("dve", lambda e: e.scalar_tensor_tensor(out=ogb[:].rearrange("p a b -> p (a b)"), in0=onTb[:].rearrange("p a b -> p (a b)"),
                                                     scalar=gnw[:, 0:1], in1=zsb[:].rearrange("p a b -> p (a b)"), op0=ALU.mult, op1=ALU.mult),
             reads=[onTb, gnw, zsb], writes=[ogb])
        if ti >= 1:
            S.dma("pool", ogF[ti - 1][:, :].rearrange("p (c t) -> p c t", c=c_og), ogb[:], reads=[ogb], writes=[ogF[ti - 1]])
        if ti % 4 == 0 and ti <= 12:
            S.dma("pool", ogH[(ti // 4) * 128:(ti // 4 + 1) * 128, :].rearrange("p (c t) -> p c t", c=c_og), ogb[:, :, TS - 64:TS],
                  reads=[ogb], writes=[ogH])
        io["after_og"](ti)
    S.pop()


def prep_M_gdn(inp, j, hg):
    w = inp["gdn_w_in"][j]
    cols = []
    for qh in range(2):
        cols.append(np.arange(128) + 128 * (2 * hg + qh))
    for qh in range(2):
        cols.append(1024 + np.arange(128) + 128 * (2 * hg + qh))
    for h in range(4):
        cols.append(2048 + np.arange(128) + 128 * (4 * hg + h))
    conv_cols = np.concatenate(cols)
    for h in range(4):
        cols.append(4096 + np.arange(128) + 128 * (4 * hg + h))
    cols.append(6144 + 4 * hg + np.arange(4))
    cols.append(6160 + 4 * hg + np.arange(4))
    cols = np.concatenate(cols)
    wc = w[:, cols].reshape(8, 128, G_WC).transpose(1, 0, 2)
    cw = inp["gdn_conv_w"][j][:, conv_cols].reshape(4, 8, 128).transpose(2, 1, 0)
    hv = np.concatenate([inp["gdn_a_log"][j][4 * hg:4 * hg + 4], inp["gdn_dt_bias"][j][4 * hg:4 * hg + 4]]).reshape(1, 8)
    return {"w": np.ascontiguousarray(wc.reshape(128, 8 * G_WC), np.float32),
            "cw": np.ascontiguousarray(cw.reshape(128, 32), np.float32),
            "hv": np.ascontiguousarray(hv, np.float32),
            "gnw": np.ascontiguousarray(inp["gdn_norm_w"][j].reshape(128, 1), np.float32),
            "cst": gdn_consts()}


GROUPS = [[0, 1, 2, 3], [4, 5, 6, 7]]


def build_fused(nl=4):
    nc = bass.Bass("TRN2", target_bir_lowering=False)
    es = ExitStack()
    S = Sched(nc, es)
    ext = lambda n, shp, dt=F32: S.dram(n, shp, dt, kind="ExternalInput")
    hs0 = ext("hs0", [D, WIN])
    keep = ext("keep", [1, WIN])
    gidx4 = ext("gidx4", [128, 20], mybir.dt.int32)
    gidx2 = ext("gidx2", [128, 20], mybir.dt.int32) if nl > 1 else None
    nwT0 = ext("nwT_0", [128, 32])
    cst_g = ext("cst_g", [128, C_TOT])
    cst_m = ext("cst_m", [128, 64 + 512 + 128]) if nl > 1 else None
    hs_out = S.dram("hs_out", [D, WIN], F32, kind="ExternalOutput")
    hs_loc = S.dram("hs_loc", [D, WIN], F32)
    def slices(name, rows_total, cols, rows_per):
        big = S.dram(name, [rows_total, cols], BF16)
        return big, [Buf(big.t[k * rows_per:(k + 1) * rows_per, :], "%s_%d" % (name, k)) for k in range(rows_total // rows_per)]

    ainF_all, ainF = slices("ainF", 512, 4096, 128)
    ainF_g, ainF_gs = slices("ainF_g", 2048, 4096, 512)
    ainH = S.dram("ainH", [128, 512], BF16)
    ainH_g = S.dram("ainH_g", [512, 512], BF16)
    og = {}
    for c in (4, 2):
        tpc = 8 // c
        ogF_all, ogF = slices("ogF%d" % c, 2048, c * 512, 128)
        ogF_g, _ = slices("ogF%d_g" % c, 8192, c * 512, 8192)
        nq = 16 // tpc
        og[c] = dict(ogF=ogF, ogF_all=ogF_all, ogH=S.dram("ogH%d" % c, [512, c * 64], BF16), tpc=tpc,
                     ogF_g=ogF_g, ogH_g=S.dram("ogH%d_g" % c, [2048, c * 64], BF16),
                     src=[Buf(ogF_all.t[q * tpc * 128:(q + 1) * tpc * 128, :], "ogsrc%d_%d" % (c, q)) for q in range(nq)],
                     dst=[Buf(ogF_g.t[q * 4 * tpc * 128:(q + 1) * 4 * tpc * 128, :], "ogdst%d_%d" % (c, q)) for q in range(nq)])
    lay = []
    for l in range(nl):
        KO = 2048 if l % 2 == 0 else 1024
        KC = KO // 128
        d = dict(nwT=ext("nwT_l%d" % l, [128, 32]), cw=ext("cw_l%d" % l, [128, 44 * 3]), cb=ext("cb_l%d" % l, [128, 44]),
                 wout_d=ext("wout_l%d" % l, [8, 128, KC * 128]), wup_d=ext("wup_l%d" % l, [NG, 128, 8 * 256]),
                 wdn_d=ext("wdn_l%d" % l, [8, 128, NG * 128]),
                 wout_b=S.dram("wout_b%d" % l, [8, 128, KC * 128], BF16), wup_b=S.dram("wup_b%d" % l, [NG, 128, 8 * 256], BF16),
                 wdn_b=S.dram("wdn_b%d" % l, [8, 128, NG * 128], BF16))
        if l % 2 == 0:
            d["m"] = dict(w=ext("gw_l%d" % l, [128, 8 * G_WC]), cw=ext("gcw_l%d" % l, [128, 32]), hv=ext("ghv_l%d" % l, [1, 8]),
                          gnw=ext("ggnw_l%d" % l, [128, 1]), cst=cst_g)
        else:
            d["m"] = dict(w=ext("mw_l%d" % l, [128, 8 * ML_WC]), gb=ext("mgb_l%d" % l, [1, 2]), nwv=ext("mnwv_l%d" % l, [1, ML_DV]), cst=cst_m)
        lay.append(d)

    def after_ain(ti):
        if ti == 0:
            S.coll("AllGather", ainH_g, ainH, GROUPS)
        else:
            S.coll("AllGather", ainF_gs[ti - 1], ainF[ti - 1], GROUPS)

    def mk_after_og(c):
        o = og[c]
        tpc = o["tpc"]

        def after_og(ti):
            wt = ti - 1
            if ti >= 1 and (wt + 1) % tpc == 0:
                q = wt // tpc
                src = o["src"][q]
                S.coll("AllGather", o["dst"][q], src, GROUPS, extra=[o["ogF"][k] for k in range(q * tpc, (q + 1) * tpc)])
            if ti == 12:
                S.coll("AllGather", o["ogH_g"], o["ogH"], GROUPS)
        return after_og

    emit_T(S, "t0", 2048, True, False, dict(hs_src=hs0, nwT=nwT0, ainF=ainF, ainH=ainH, after_ain=after_ain))
    for l in range(nl):
        d = lay[l]
        c = 4 if l % 2 == 0 else 2
        emit_casts(S, d)
        mio = dict(d["m"], ainF_g=ainF_g, ainH_g=ainH_g, ogF=og[c]["ogF"], ogH=og[c]["ogH"], after_og=mk_after_og(c))
        if l % 2 == 0:
            emit_M_gdn(S, "g%d" % l, mio)
        else:
            emit_M_ml(S, "m%d" % l, mio)
        last = l == nl - 1
        tio = dict(d, hs_src=(hs0 if l == 0 else hs_loc), hs_dst=(hs_out if last else hs_loc), ogF_g=og[c]["ogF_g"], ogH_g=og[c]["ogH_g"],
                   keep=keep, gidx=(gidx4 if c == 4 else gidx2), ainF=ainF, ainH=ainH, after_ain=after_ain)
        emit_T(S, "t%d" % (l + 1), 512 * c, False, last, tio)
    S.finish([hs_out])
    return nc, es


def kernel(x, meta_tokens, norm_w, gdn_w_in, gdn_conv_w, gdn_a_log, gdn_dt_bias, gdn_norm_w, gdn_w_out,
           ml_w_in, ml_gate_b, ml_norm_w, ml_w_out, ffn_w_up, ffn_conv_w, ffn_conv_b, ffn_w_down, _nl=4):
    inp = dict(x=x, meta_tokens=meta_tokens, norm_w=norm_w, gdn_w_in=gdn_w_in, gdn_conv_w=gdn_conv_w, gdn_a_log=gdn_a_log,
               gdn_dt_bias=gdn_dt_bias, gdn_norm_w=gdn_norm_w, gdn_w_out=gdn_w_out, ml_w_in=ml_w_in, ml_gate_b=ml_gate_b,
               ml_norm_w=ml_norm_w, ml_w_out=ml_w_out, ffn_w_up=ffn_w_up, ffn_conv_w=ffn_conv_w, ffn_conv_b=ffn_conv_b,
               ffn_w_down=ffn_w_down)
    inp = {k: np.asarray(v, np.float32) for k, v in inp.items()}
    shared = {"cst_g": gdn_consts(), "cst_m": ml_consts(),
              "nwT_0": np.ascontiguousarray(np.stack([_cm(inp["norm_w"][0, 0])] * 4, 1).reshape(128, 32), np.float32)}
    for l in range(_nl):
        t = prep_T(inp, l)
        for k, v in t.items():
            shared["%s_l%d" % (k, l)] = v
    maps = []
    for c in range(8):
        b, r = c // 4, c % 4
        m = dict(shared)
        h = np.zeros((LP, D), np.float32)
        h[XPAD + 48:XPAD + 64] = inp["meta_tokens"]
        h[XPAD + 64:] = inp["x"][b]
        lo = XPAD + 2048 * r
        m["hs0"] = np.ascontiguousarray(h[lo:lo + WIN].T)
        k = np.ones((1, WIN), np.float32)
        if r == 0:
            k[0, :48] = 0.0
        m["keep"] = k
        p = np.arange(128)
        for cc in (4, 2):
            tpc = 8 // cc
            gi = np.zeros((128, 20), np.int32)
            for hg in range(4):
                gi[:, hg * 5] = hg * 512 + r * 128 + p
                for i in range(1, 5):
                    wt = 4 * r + i - 1
                    gi[:, hg * 5 + i] = (wt // tpc) * (4 * tpc * 128) + hg * (tpc * 128) + (wt % tpc) * 128 + p
            m["gidx%d" % cc] = gi
        for l in range(_nl):
            if l % 2 == 0:
                g = prep_M_gdn(inp, l // 2, r)
                m["gw_l%d" % l], m["gcw_l%d" % l], m["ghv_l%d" % l], m["ggnw_l%d" % l] = g["w"], g["cw"], g["hv"], g["gnw"]
            else:
                g = prep_M_ml(inp, l // 2, r)
                m["mw_l%d" % l], m["mgb_l%d" % l], m["mnwv_l%d" % l] = g["w"], g["gb"], g["nwv"]
        maps.append(m)
    if _nl == 1:
        shared.pop("cst_m")
        for m in maps:
            m.pop("cst_m", None)
            m.pop("gidx2", None)
    nc, es = build_fused(_nl)
    res = run_bass_kernel_spmd(nc, maps, core_ids=list(range(8))).results
    out = np.zeros((NB, SEQ, D), np.float32)
    for c in range(8):
        b, r = c // 4, c % 4
        out[b, 2048 * r:2048 * (r + 1)] = res[c]["hs_out"][:, 64:].T
    return out
```

```python
from contextlib import ExitStack
import numpy as np
import ml_dtypes
import concourse.bass as bass
import concourse.mybir as mybir
from concourse.bass_utils import run_bass_kernel_spmd

F32 = mybir.dt.float32
BF16 = mybir.dt.bfloat16
AF = mybir.ActivationFunctionType
ALU = mybir.AluOpType
AX = mybir.AxisListType

D = 1024
SEQ = 8192
NB = 2
LP = 8704
XPAD = LP - SEQ - 64
WIN = 64 + 2048
FFN = 2816
NG = FFN // 128
RMS_EPS = 1e-6
CH = 64
NCH = LP // CH
TS = 512
NTS = LP // TS
CPT = TS // CH


class Buf:
    __slots__ = ("t", "lw", "rd", "sem", "semv", "name")

    def __init__(self, t, name=""):
        self.t = t
        self.lw = None
        self.rd = {}
        self.sem = None
        self.semv = 0
        self.name = name

    def __getitem__(self, k):
        return self.t[k]


class Sched:
    def __init__(self, nc, es):
        self.nc = nc
        self.es = es
        self.eng = {"pe": nc.tensor, "act": nc.scalar, "dve": nc.vector, "pool": nc.gpsimd, "sp": nc.sync}
        self.sem = {k: es.enter_context(nc.semaphore("sem_" + k)) for k in self.eng}
        self.cnt = {k: 0 for k in self.eng}
        self.seen = {k: {} for k in self.eng}
        self.nsem = 0
        self.out_events = []
        self.ninst = 0
        self.scopes = []
        self.dsems = []
        self.free_dsems = []
        self.scope_bufs = []

    def push(self, tag):
        self.scopes.append((ExitStack(), tag))
        self.scope_bufs.append([])

    def _own_sem(self, own):
        if own.sem is None:
            if self.free_dsems:
                own.sem, own.semv = self.free_dsems.pop()
            else:
                own.sem = self.es.enter_context(self.nc.semaphore("dsem%d" % self.nsem))
                self.nsem += 1
            self.dsems.append(own)

    def pop(self):
        self.barrier()
        st, _ = self.scopes.pop()
        st.close()
        for b in self.scope_bufs.pop():
            if b.sem is not None:
                self.free_dsems.append((b.sem, b.semv))
                self.dsems.remove(b)
                b.sem = None

    def _scope(self):
        return self.scopes[-1] if self.scopes else (self.es, "g")

    def sb(self, name, shape, dt):
        st, tag = self._scope()
        name = tag + "_" + name
        b = Buf(st.enter_context(self.nc.sbuf_tensor(name, list(shape), dt)), name)
        if self.scope_bufs:
            self.scope_bufs[-1].append(b)
        return b

    def ps(self, name, shape, dt=F32):
        st, tag = self._scope()
        name = tag + "_" + name
        return Buf(st.enter_context(self.nc.psum_tensor(name, list(shape), dt)), name)

    def barrier(self):
        for e in self.eng:
            eng = self.eng[e]
            for k in ("pe", "act", "dve", "pool", "sp"):
                if k != e and self.cnt[k] and self.seen[e].get(k, 0) < self.cnt[k]:
                    eng.wait_ge(self.sem[k], self.cnt[k])
                    self.seen[e][k] = self.cnt[k]
            for b in self.dsems:
                key = "d_" + b.name
                if self.seen[e].get(key, 0) < b.semv:
                    eng.wait_ge(b.sem, b.semv)
                    self.seen[e][key] = b.semv

    def dram(self, name, shape, dt, kind="Internal"):
        t = self.nc.dram_tensor(name, list(shape), dt, kind=kind)
        return Buf(t.ap(), name)

    def _deps(self, reads, writes):
        deps = {}

        def add(ev):
            if ev is None:
                return
            sem, val, key = ev
            if key not in deps or deps[key][1] < val:
                deps[key] = (sem, val)

        for b in reads:
            add(b.lw)
        for b in writes:
            add(b.lw)
            for ev in b.rd.values():
                add(ev)
        return deps

    def _wait(self, e, deps):
        eng = self.eng[e]
        for key, (sem, val) in deps.items():
            if e == "pe" and key == "pe":
                continue
            if self.seen[e].get(key, 0) >= val:
                continue
            eng.wait_ge(sem, val)
            self.seen[e][key] = val

    def _record(self, ev, reads, writes):
        for b in writes:
            b.lw = ev
            b.rd = {}
        for b in reads:
            if b not in writes:
                b.rd[ev[2]] = ev

    def op(self, e, fn, reads=(), writes=()):
        self._wait(e, self._deps(reads, writes))
        ins = fn(self.eng[e])
        self.cnt[e] += 1
        ins.then_inc(self.sem[e], 1)
        self.ninst += 1
        self._record((self.sem[e], self.cnt[e], e), reads, writes)

    def dma(self, q, out, in_, reads=(), writes=(), owner=None):
        self._wait(q, self._deps(reads, writes))
        own = owner if owner is not None else (writes[0] if writes else reads[0])
        self._own_sem(own)
        own.semv += 16
        ins = self.eng[q].dma_start(out=out, in_=in_)
        ins.then_inc(own.sem, 16)
        self.ninst += 1
        ev = (own.sem, own.semv, "d_" + own.name)
        self._record(ev, reads, writes)
        return ev

    def gather(self, out_ap, table_ap, idx_ap, reads=(), writes=()):
        self._wait("pool", self._deps(reads, writes))
        own = writes[0]
        self._own_sem(own)
        own.semv += 16
        ins = self.nc.gpsimd.indirect_dma_start(out=out_ap, out_offset=None, in_=table_ap,
                                                in_offset=bass.IndirectOffsetOnAxis(ap=idx_ap, axis=0))
        ins.then_inc(own.sem, 16)
        self.ninst += 1
        self._record((own.sem, own.semv, "d_" + own.name), reads, writes)

    def coll(self, kind, out, in_, groups, extra=()):
        self._wait("pool", self._deps([in_] + list(extra), [out]))
        self._own_sem(out)
        out.semv += 1
        ins = self.nc.gpsimd.collective_compute(kind, ALU.bypass, replica_groups=groups, ins=[in_.t.opt()], outs=[out.t.opt()])
        ins.then_inc(out.sem, 1)
        self.ninst += 1
        self._record((out.sem, out.semv, "d_" + out.name), [in_], [out])

    def finish(self, bufs):
        for b in bufs:
            if b.sem is not None:
                self.eng["sp"].wait_ge(b.sem, b.semv)
        for k in ("pe", "act", "dve", "pool"):
            if self.cnt[k]:
                self.eng["sp"].wait_ge(self.sem[k], self.cnt[k])


class PsumPool:
    def __init__(self, S, n, prefix="pb"):
        self.banks = [S.ps("%s%d" % (prefix, i), [128, 512]) for i in range(n)]
        self.i = 0

    def get(self):
        b = self.banks[self.i % len(self.banks)]
        self.i += 1
        return b


def bcast_mid(ap2, n):
    return ap2.unsqueeze(1).broadcast_to([ap2.shape[0], n, ap2.shape[1]])


def bcast_last(ap2, n):
    return ap2.unsqueeze(2).broadcast_to([ap2.shape[0], ap2.shape[1], n])


def rstd_from_ss(S, ss_ps, out_sb, scale, eps, W):
    S.op("act", lambda e: e.activation(out=out_sb[:, :W], in_=ss_ps[:, :W], func=AF.Ln, scale=scale, bias=eps),
         reads=[ss_ps], writes=[out_sb])
    S.op("act", lambda e: e.activation(out=out_sb[:, :W], in_=out_sb[:, :W], func=AF.Exp, scale=-0.5),
         reads=[out_sb], writes=[out_sb])


T_TILES = [(0, 64), (64, 512), (576, 512), (1088, 512), (1600, 512)]


def emit_casts(S, io):
    for m in range(8):
        S.dma("pool", io["wout_b"][m], io["wout_d"][m], writes=[io["wout_b"]])
    for g in range(NG):
        S.dma("pool", io["wup_b"][g], io["wup_d"][g], writes=[io["wup_b"]])
    for m in range(8):
        S.dma("pool", io["wdn_b"][m], io["wdn_d"][m], writes=[io["wdn_b"]])


def emit_T(S, tag, KO, first, last, io):
    S.push(tag)
    KC = KO // 128
    c_og = KC // 4
    hs_in = io["hs_src"]
    nwT_d = io["nwT"]
    if not last:
        ainF, ainH = io["ainF"], io["ainH"]
    if not first:
        ogF_g, ogH_g = io["ogF_g"], io["ogH_g"]
        keep_d, cw_d, cb_d = io["keep"], io["cw"], io["cb"]
        wout_b, wup_b, wdn_b = io["wout_b"], io["wup_b"], io["wdn_b"]
        hs_out = io["hs_dst"]
        gidx = S.sb("gidx", [128, 20], mybir.dt.int32)
        S.dma("sp", gidx[:], io["gidx"][:, :], writes=[gidx])

    ones_f = S.sb("ones_f", [128, 128], F32)
    ones_b = S.sb("ones_b", [128, 128], BF16)
    S.op("pool", lambda e: e.memset(ones_f[:], 1.0), writes=[ones_f])
    S.op("act", lambda e: e.activation(out=ones_b[:], in_=ones_f[:], func=AF.Copy), reads=[ones_f], writes=[ones_b])
    nwT = S.sb("nwT_sb", [128, 4, 8], F32)
    S.dma("sp", nwT[:].rearrange("p a b -> p (a b)"), nwT_d[:, :], writes=[nwT])

    hs_sb = [S.sb("hs_sb%d" % i, [128, 8, 512], F32) for i in range(2)]
    sq_sb = [S.sb("sq_sb%d" % i, [128, 512], BF16) for i in range(2)]
    rstd = S.sb("rstd", [128, 512], F32)
    a_sb = S.sb("a_sb", [128, 8, 512], BF16)
    pp = PsumPool(S, 7)
    ss_ps = S.ps("ss_ps", [128, 512])
    if not first:
        og_sb = [S.sb("og_sb%d" % i, [128, KC, 512], BF16) for i in range(2)]
        ogh_sb = S.sb("ogh_sb", [128, KC, 64], BF16)
        keep_sb = S.sb("keep_sb", [128, WIN], F32)
        S.dma("sp", keep_sb[:], keep_d[0:1, :].partition_broadcast(128), writes=[keep_sb])
        cw = S.sb("cw_sb", [128, 44, 3], F32)
        cb = S.sb("cb_sb", [128, 44], F32)
        S.dma("sp", cw[:].rearrange("p a b -> p (a b)"), cw_d[:, :], writes=[cw])
        S.dma("sp", cb[:], cb_d[:, :], writes=[cb])
        mix_sb = S.sb("mix_sb", [128, 8, 512], F32)
        rk = S.sb("rk", [128, 512], F32)
        tmp_sb = [S.sb("tmp_sb%d" % i, [128, 512], F32) for i in range(2)]
        h_sb = S.sb("h_sb", [128, NG, 512], BF16)
        u_sb = [S.sb("u_sb%d" % i, [128, 2, 2 + 512], F32) for i in range(2)]
        y_sb = [S.sb("y_sb%d" % i, [128, 2, 512], F32) for i in range(2)]
        e_sb = [S.sb("e_sb%d" % i, [128, 512], F32) for i in range(2)]
        halo = S.sb("halo", [128, 44, 2], F32)
        S.op("pool", lambda e: e.memset(halo[:], 0.0), writes=[halo])
        wo_s = [S.sb("wo_s%d" % i, [128, KC, 128], BF16) for i in range(2)]
        wu_s = [S.sb("wu_s%d" % i, [128, 8, 256], BF16) for i in range(3)]
        wd_s = [S.sb("wd_s%d" % i, [128, NG, 128], BF16) for i in range(2)]

    def norm_ss(src, W, eng_sq="act"):
        for m in range(8):
            sq = sq_sb[m % 2]
            S.op(eng_sq, lambda e: e.activation(out=sq[:, :W], in_=src[:, m, :W], func=AF.Square),
                 reads=[src], writes=[sq])
            S.op("pe", lambda e: e.matmul(ss_ps[:, :W], ones_b[:], sq[:, :W], start=(m == 0), stop=(m == 7)),
                 reads=[ones_b, sq], writes=[ss_ps])

    def load_tile(i):
        t0, W = T_TILES[i]
        hb = hs_sb[i % 2]
        S.dma("sp", hb[:, :, :W], hs_in[:, t0:t0 + W].rearrange("(c p) t -> p c t", p=128), writes=[hb])
        if not first:
            ob = ogh_sb if i == 0 else og_sb[i % 2]
            tab = ogH_g if i == 0 else ogF_g
            for hg in range(4):
                S.gather(ob[:, hg * c_og:(hg + 1) * c_og, :].rearrange("p c w -> p (c w)"), tab[:, :],
                         gidx[:, hg * 5 + i:hg * 5 + i + 1], reads=[gidx], writes=[ob])

    load_tile(0)
    for ti, (t0, W) in enumerate(T_TILES):
        if ti + 1 < len(T_TILES):
            load_tile(ti + 1)
        hb = hs_sb[ti % 2]
        if not first:
            ob = ogh_sb if ti == 0 else og_sb[ti % 2]
            S.dma("sp", wo_s[0][:].rearrange("p a b -> p (a b)"), wout_b[0], reads=[wout_b], writes=[wo_s[0]])
            for m in range(8):
                if m + 1 < 8:
                    S.dma("sp", wo_s[(m + 1) % 2][:].rearrange("p a b -> p (a b)"), wout_b[m + 1],
                          reads=[wout_b], writes=[wo_s[(m + 1) % 2]])
                ws = wo_s[m % 2]
                pb = pp.get()
                for kc in range(KC):
                    S.op("pe", lambda e: e.matmul(pb[:, :W], ws[:, kc, :], ob[:, kc, :W], start=(kc == 0), stop=(kc == KC - 1)),
                         reads=[ws, ob], writes=[pb])
                S.op("act", lambda e: e.activation(out=mix_sb[:, m, :W], in_=pb[:, :W], func=AF.Copy), reads=[pb], writes=[mix_sb])
                sq = sq_sb[m % 2]
                S.op("act", lambda e: e.activation(out=sq[:, :W], in_=pb[:, :W], func=AF.Square), reads=[pb], writes=[sq])
                S.op("pe", lambda e: e.matmul(ss_ps[:, :W], ones_b[:], sq[:, :W], start=(m == 0), stop=(m == 7)),
                     reads=[ones_b, sq], writes=[ss_ps])
            rstd_from_ss(S, ss_ps, rstd, 1.0 / D, RMS_EPS, W)
            S.op("dve", lambda e: e.tensor_tensor(out=rk[:, :W], in0=rstd[:, :W], in1=keep_sb[:, t0:t0 + W], op=ALU.mult),
                 reads=[rstd, keep_sb], writes=[rk])
            for m in range(8):
                tb = tmp_sb[m % 2]
                S.op("dve", lambda e: e.tensor_tensor(out=tb[:, :W], in0=mix_sb[:, m, :W], in1=rk[:, :W], op=ALU.mult),
                     reads=[mix_sb, rk], writes=[tb])
                S.op("dve", lambda e: e.scalar_tensor_tensor(out=hb[:, m, :W], in0=tb[:, :W], scalar=nwT[:, 1, m:m + 1],
                                                             in1=hb[:, m, :W], op0=ALU.mult, op1=ALU.add),
                     reads=[tb, nwT, hb], writes=[hb])
            norm_ss(hb, W)
            rstd_from_ss(S, ss_ps, rstd, 1.0 / D, RMS_EPS, W)
            for m in range(8):
                S.op("dve", lambda e: e.scalar_tensor_tensor(out=a_sb[:, m, :W], in0=hb[:, m, :W], scalar=nwT[:, 2, m:m + 1],
                                                             in1=rstd[:, :W], op0=ALU.mult, op1=ALU.mult),
                     reads=[hb, nwT, rstd], writes=[a_sb])
            S.dma("sp", wu_s[0][:].rearrange("p a b -> p (a b)"), wup_b[0], reads=[wup_b], writes=[wu_s[0]])
            S.dma("sp", wu_s[1][:].rearrange("p a b -> p (a b)"), wup_b[1], reads=[wup_b], writes=[wu_s[1]])
            for g in range(NG):
                if g + 2 < NG:
                    S.dma("sp", wu_s[(g + 2) % 3][:].rearrange("p a b -> p (a b)"), wup_b[g + 2],
                          reads=[wup_b], writes=[wu_s[(g + 2) % 3]])
                ws = wu_s[g % 3]
                ub = u_sb[g % 2]
                yb = y_sb[g % 2]
                eb = e_sb[g % 2]
                for hf in range(2):
                    ci = g + hf * NG
                    pb = pp.get()
                    for kc in range(8):
                        S.op("pe", lambda e: e.matmul(pb[:, :W], ws[:, kc, hf * 128:(hf + 1) * 128], a_sb[:, kc, :W],
                                                      start=(kc == 0), stop=(kc == 7)),
                             reads=[ws, a_sb], writes=[pb])
                    S.op("pool", lambda e: e.tensor_copy(out=ub[:, hf, 0:2], in_=halo[:, ci, :]), reads=[halo], writes=[ub])
                    S.op("act", lambda e: e.activation(out=ub[:, hf, 2:2 + W], in_=pb[:, :W], func=AF.Copy), reads=[pb], writes=[ub])
                    S.op("pool", lambda e: e.tensor_copy(out=halo[:, ci, :], in_=ub[:, hf, W:W + 2]), reads=[ub], writes=[halo])
                    S.op("act", lambda e: e.activation(out=yb[:, hf, :W], in_=pb[:, :W], func=AF.Identity,
                                                       scale=cw[:, ci, 2:3], bias=cb[:, ci:ci + 1]),
                         reads=[pb, cw, cb], writes=[yb])
                    S.op("dve", lambda e: e.scalar_tensor_tensor(out=yb[:, hf, :W], in0=ub[:, hf, 1:1 + W], scalar=cw[:, ci, 1:2],
                                                                 in1=yb[:, hf, :W], op0=ALU.mult, op1=ALU.add),
                         reads=[ub, cw, yb], writes=[yb])
                    S.op("dve", lambda e: e.scalar_tensor_tensor(out=yb[:, hf, :W], in0=ub[:, hf, 0:W], scalar=cw[:, ci, 0:1],
                                                                 in1=yb[:, hf, :W], op0=ALU.mult, op1=ALU.add),
                         reads=[ub, cw, yb], writes=[yb])
                S.op("act", lambda e: e.activation(out=eb[:, :W], in_=yb[:, 0, :W], func=AF.Silu), reads=[yb], writes=[eb])
                S.op("dve", lambda e: e.tensor_tensor(out=h_sb[:, g, :W], in0=yb[:, 1, :W], in1=eb[:, :W], op=ALU.mult),
                     reads=[yb, eb], writes=[h_sb])
            S.dma("sp", wd_s[0][:].rearrange("p a b -> p (a b)"), wdn_b[0], reads=[wdn_b], writes=[wd_s[0]])
            for m in range(8):
                if m + 1 < 8:
                    S.dma("sp", wd_s[(m + 1) % 2][:].rearrange("p a b -> p (a b)"), wdn_b[m + 1],
                          reads=[wdn_b], writes=[wd_s[(m + 1) % 2]])
                ws = wd_s[m % 2]
                pb = pp.get()
                for kc in range(NG):
                    S.op("pe", lambda e: e.matmul(pb[:, :W], ws[:, kc, :], h_sb[:, kc, :W], start=(kc == 0), stop=(kc == NG - 1)),
                         reads=[ws, h_sb], writes=[pb])
                S.op("act", lambda e: e.activation(out=mix_sb[:, m, :W], in_=pb[:, :W], func=AF.Copy), reads=[pb], writes=[mix_sb])
                sq = sq_sb[m % 2]
                S.op("act", lambda e: e.activation(out=sq[:, :W], in_=pb[:, :W], func=AF.Square), reads=[pb], writes=[sq])
                S.op("pe", lambda e: e.matmul(ss_ps[:, :W], ones_b[:], sq[:, :W], start=(m == 0), stop=(m == 7)),
                     reads=[ones_b, sq], writes=[ss_ps])
            rstd_from_ss(S, ss_ps, rstd, 1.0 / D, RMS_EPS, W)
            S.op("dve", lambda e: e.tensor_tensor(out=rk[:, :W], in0=rstd[:, :W], in1=keep_sb[:, t0:t0 + W], op=ALU.mult),
                 reads=[rstd, keep_sb], writes=[rk])
            for m in range(8):
                tb = tmp_sb[m % 2]
                S.op("dve", lambda e: e.tensor_tensor(out=tb[:, :W], in0=mix_sb[:, m, :W], in1=rk[:, :W], op=ALU.mult),
                     reads=[mix_sb, rk], writes=[tb])
                S.op("dve", lambda e: e.scalar_tensor_tensor(out=hb[:, m, :W], in0=tb[:, :W], scalar=nwT[:, 3, m:m + 1],
                                                             in1=hb[:, m, :W], op0=ALU.mult, op1=ALU.add),
                     reads=[tb, nwT, hb], writes=[hb])
            S.dma("pool", hs_out[:, t0:t0 + W].rearrange("(c p) t -> p c t", p=128), hb[:, :, :W], reads=[hb], owner=hs_out)
        if not last:
            norm_ss(hb, W)
            rstd_from_ss(S, ss_ps, rstd, 1.0 / D, RMS_EPS, W)
            for m in range(8):
                S.op("dve", lambda e: e.scalar_tensor_tensor(out=a_sb[:, m, :W], in0=hb[:, m, :W], scalar=nwT[:, 0, m:m + 1],
                                                             in1=rstd[:, :W], op0=ALU.mult, op1=ALU.mult),
                     reads=[hb, nwT, rstd], writes=[a_sb])
            if ti == 0:
                S.dma("pool", ainH[:, :].rearrange("p (c t) -> p c t", c=8), a_sb[:, :, :W], reads=[a_sb], writes=[ainH])
            else:
                S.dma("pool", ainF[ti - 1][:, :].rearrange("p (c t) -> p c t", c=8), a_sb[:, :, :W], reads=[a_sb], writes=[ainF[ti - 1]])
            io["after_ain"](ti)
    S.pop()


def _cm(v):
    return np.ascontiguousarray(v.reshape(-1, 128).T)


def prep_T(inp, layer):
    j = layer // 2
    if layer % 2 == 0:
        w_out = inp["gdn_w_out"][j]
    else:
        w_out = inp["ml_w_out"][j]
    KO = w_out.shape[0]
    KC = KO // 128
    nw = inp["norm_w"]
    nxt = nw[layer + 1, 0] if layer + 1 < 4 else nw[layer, 0]
    nwT = np.stack([_cm(nxt), _cm(nw[layer, 1]), _cm(nw[layer, 2]), _cm(nw[layer, 3])], 1)
    wout = w_out.reshape(KC, 128, 8, 128).transpose(2, 1, 0, 3)
    wu = inp["ffn_w_up"][layer].reshape(8, 128, 2, NG, 128).transpose(3, 1, 0, 2, 4)
    wd = inp["ffn_w_down"][layer].reshape(NG, 128, 8, 128).transpose(2, 1, 0, 3)
    cw = inp["ffn_conv_w"][layer].reshape(3, 44, 128).transpose(2, 1, 0)
    cb = _cm(inp["ffn_conv_b"][layer])
    return {
        "nwT": np.ascontiguousarray(nwT.reshape(128, 32), np.float32),
        "wout": np.ascontiguousarray(wout.reshape(8, 128, KC * 128), np.float32),
        "wup": np.ascontiguousarray(wu.reshape(NG, 128, 8 * 256), np.float32),
        "wdn": np.ascontiguousarray(wd.reshape(8, 128, NG * 128), np.float32),
        "cw": np.ascontiguousarray(cw.reshape(128, 44 * 3), np.float32),
        "cb": np.ascontiguousarray(cb, np.float32),
    }


ML_DK = 128
ML_DV = 256
ML_WC = 128 + 128 + 128 + 256 + 256 + 2
BIGNEG = 30000.0


def emit_M_ml(S, tag, io):
    S.push(tag)
    ainF_g, ainH_g = io["ainF_g"], io["ainH_g"]
    w_d, gb_d, nwv_d, cst_d = io["w"], io["gb"], io["nwv"], io["cst"]
    ogF, ogH = io["ogF"], io["ogH"]
    c_og = 2

    w_sb = S.sb("w_sb", [128, 8, ML_WC], BF16)
    S.dma("pool", w_sb[:].rearrange("p a b -> p (a b)"), w_d[:, :], writes=[w_sb])
    wg_sb = S.sb("wg_sb", [128, 8, 2], BF16)
    cst = S.sb("cst_sb", [128, 64 + 512 + 128], F32)
    S.dma("sp", cst[:], cst_d[:, :], writes=[cst])
    identf = cst
    ident_b = S.sb("ident_b", [128, 128], BF16)
    S.op("act", lambda e: e.activation(out=ident_b[:], in_=cst[:, 576:704], func=AF.Copy), reads=[cst], writes=[ident_b])
    gb = S.sb("gb_sb", [1, 2], F32)
    S.dma("sp", gb[:], gb_d[:, :], writes=[gb])
    nwv = S.sb("nwv_sb", [64, ML_DV], F32)
    S.dma("sp", nwv[:], nwv_d[0:1, :].partition_broadcast(64), writes=[nwv])
    ones_row = S.sb("ones_row", [1, 128], F32)
    S.op("pool", lambda e: e.memset(ones_row[:], 1.0), writes=[ones_row])

    a_sb = [S.sb("a_sb%d" % i, [128, 8, 512], BF16) for i in range(2)]
    pp = PsumPool(S, 7)

    li_row = S.sb("li_row", [1, LP], F32)
    lf_row = S.sb("lf_row", [1, LP], F32)
    bb_row = S.sb("bb_row", [1, LP], F32)
    ones_bc = ones_row[0:1, 0:1].broadcast_to([1, LP])

    def load_a(i):
        ab = a_sb[i % 2]
        if i == 0:
            S.op("pool", lambda e: e.memset(ab[:, :, 0:XPAD], 0.0), writes=[ab])
            S.dma("sp", ab[:, :, XPAD:TS], ainH_g[0:128, :].rearrange("p (c t) -> p c t", c=8), reads=[ainH_g], writes=[ab])
        else:
            r0 = ((i - 1) % 4) * 512 + ((i - 1) // 4) * 128
            S.dma("sp", ab[:].rearrange("p c t -> p (c t)"), ainF_g[r0:r0 + 128, :], reads=[ainF_g], writes=[ab])

    load_a(0)
    for ti in range(NTS):
        if ti + 1 < NTS:
            load_a(ti + 1)
        ab = a_sb[ti % 2]
        for gi_, row in ((0, li_row), (1, lf_row)):
            pr = pp.get()
            for kc in range(8):
                S.op("pe", lambda e: e.matmul(pr[0:1, :], w_sb[:, kc, ML_WC - 2 + gi_:ML_WC - 1 + gi_], ab[:, kc, :],
                                              start=(kc == 0), stop=(kc == 7)), reads=[w_sb, ab], writes=[pr])
            S.op("act", lambda e: e.activation(out=row[:, ti * TS:(ti + 1) * TS], in_=pr[0:1, :], func=AF.Identity,
                                               bias=gb[:, gi_:gi_ + 1], scale=1.0), reads=[pr, gb], writes=[row])
    for row in (li_row, lf_row):
        S.op("act", lambda e: e.activation(out=row[:], in_=row[:], func=AF.Exp, scale=2.0 / 15.0), reads=[row], writes=[row])
        S.op("dve", lambda e: e.tensor_scalar_add(out=row[:], in0=row[:], scalar1=1.0), reads=[row], writes=[row])
        S.op("dve", lambda e: e.reciprocal(out=row[:], in_=row[:]), reads=[row], writes=[row])
        S.op("dve", lambda e: e.tensor_scalar(out=row[:], in0=row[:], scalar1=-30.0, scalar2=15.0, op0=ALU.mult, op1=ALU.add),
             reads=[row], writes=[row])
    S.op("act", lambda e: e.activation(out=lf_row[:], in_=lf_row[:], func=AF.Exp, scale=-1.0), reads=[lf_row], writes=[lf_row])
    S.op("act", lambda e: e.activation(out=lf_row[:], in_=lf_row[:], func=AF.Ln, bias=1.0, scale=1.0), reads=[lf_row], writes=[lf_row])
    S.op("dve", lambda e: e.tensor_scalar_mul(out=lf_row[:], in0=lf_row[:], scalar1=-1.0), reads=[lf_row], writes=[lf_row])
    S.op("pool", lambda e: e.memset(lf_row[:, 0:XPAD], 0.0), writes=[lf_row])
    S.op("pool", lambda e: e.memset(li_row[:, 0:XPAD], -BIGNEG), writes=[li_row])
    S.op("dve", lambda e: e.tensor_tensor_scan(out=bb_row[:], data0=ones_bc, data1=lf_row[:], initial=0.0,
                                               op0=ALU.mult, op1=ALU.add), reads=[ones_row, lf_row], writes=[bb_row])
    c_row = lf_row
    S.op("dve", lambda e: e.tensor_tensor(out=c_row[:], in0=li_row[:], in1=bb_row[:], op=ALU.subtract), reads=[li_row, bb_row], writes=[c_row])
    M_row = li_row
    S.op("dve", lambda e: e.tensor_tensor_scan(out=M_row[:], data0=ones_bc, data1=c_row[:], initial=0.0,
                                               op0=ALU.mult, op1=ALU.max), reads=[ones_row, c_row], writes=[M_row])
    en_row = bb_row
    S.op("dve", lambda e: e.tensor_tensor(out=en_row[:], in0=bb_row[:], in1=M_row[:], op=ALU.add), reads=[bb_row, M_row], writes=[en_row])
    S.op("act", lambda e: e.activation(out=en_row[:], in_=en_row[:], func=AF.Exp, scale=-1.0), reads=[en_row], writes=[en_row])

    c_tm = S.sb("c_tm", [64, NCH], F32)
    M_tm = S.sb("M_tm", [64, NCH], F32)
    en_tm = S.sb("en_tm", [64, NCH], F32)
    for row, tmb in ((c_row, c_tm), (M_row, M_tm), (en_row, en_tm)):
        pb = pp.get()
        for n in range(NCH):
            S.op("pe", lambda e: e.matmul(pb[0:64, n:n + 1], row[0:1, n * CH:(n + 1) * CH], ones_row[0:1, 0:1], start=True, stop=True),
                 reads=[row, ones_row], writes=[pb])
        S.op("act", lambda e: e.activation(out=tmb[:], in_=pb[0:64, 0:NCH], func=AF.Copy), reads=[pb], writes=[tmb])
    Mend_b = S.sb("Mend_b", [128, NCH], F32)
    Mprev_b = S.sb("Mprev_b", [128, NCH], F32)
    pb = pp.get()
    S.op("pe", lambda e: e.matmul(pb[:, 0:NCH], ones_row[0:1, :], M_row[0:1, CH - 1::CH], start=True, stop=True),
         reads=[ones_row, M_row], writes=[pb])
    S.op("act", lambda e: e.activation(out=Mend_b[:], in_=pb[:, 0:NCH], func=AF.Copy), reads=[pb], writes=[Mend_b])
    S.op("pool", lambda e: e.memset(Mprev_b[:, 0:1], 0.0), writes=[Mprev_b])
    S.op("pool", lambda e: e.tensor_copy(out=Mprev_b[:, 1:NCH], in_=Mend_b[:, 0:NCH - 1]), reads=[Mend_b], writes=[Mprev_b])
    sc_tm = S.sb("sc_tm", [64, NCH], F32)
    kws_tm = S.sb("kws_tm", [64, NCH], F32)
    dec_b = S.sb("dec_b", [128, NCH], F32)
    S.op("dve", lambda e: e.tensor_tensor(out=sc_tm[:], in0=Mprev_b[0:64, :], in1=M_tm[:], op=ALU.subtract), reads=[Mprev_b, M_tm], writes=[sc_tm])
    S.op("act", lambda e: e.activation(out=sc_tm[:], in_=sc_tm[:], func=AF.Exp), reads=[sc_tm], writes=[sc_tm])
    S.op("dve", lambda e: e.tensor_tensor(out=kws_tm[:], in0=c_tm[:], in1=Mend_b[0:64, :], op=ALU.subtract), reads=[c_tm, Mend_b], writes=[kws_tm])
    S.op("act", lambda e: e.activation(out=kws_tm[:], in_=kws_tm[:], func=AF.Exp), reads=[kws_tm], writes=[kws_tm])
    S.op("dve", lambda e: e.tensor_tensor(out=dec_b[:], in0=Mprev_b[:], in1=Mend_b[:], op=ALU.subtract), reads=[Mprev_b, Mend_b], writes=[dec_b])
    S.op("act", lambda e: e.activation(out=dec_b[:], in_=dec_b[:], func=AF.Exp), reads=[dec_b], writes=[dec_b])

    qT = [S.sb("qT%d" % i, [128, 512], BF16) for i in range(2)]
    kT = [S.sb("kT%d" % i, [128, 512], BF16) for i in range(2)]
    Wt = [S.sb("Wt%d" % i, [64, 512], F32) for i in range(2)]
    ogT = [S.sb("ogT%d" % i, [128, 2, 512], BF16) for i in range(2)]
    C_f = S.sb("C_f", [128, ML_DV + 1], F32)
    C_b = S.sb("C_b", [128, ML_DV + 1], BF16)
    S.op("pool", lambda e: e.memset(C_f[:], 0.0), writes=[C_f])
    S.op("pool", lambda e: e.memset(C_b[:], 0.0), writes=[C_b])
    NR = 3
    kw_sb = [S.sb("kw_sb%d" % i, [64, 128], BF16) for i in range(NR)]
    va_sb = [S.sb("va_sb%d" % i, [64, ML_DV + 1], BF16) for i in range(NR)]
    og_sb = [S.sb("ogs%d" % i, [64, ML_DV], F32) for i in range(NR)]
    St_sb = [S.sb("St%d" % i, [64, 64], BF16) for i in range(NR)]
    t1_sb = [S.sb("t1_%d" % i, [64, ML_DV + 1], F32) for i in range(NR)]
    sm_sb = [S.sb("sm%d" % i, [64, 8], F32) for i in range(NR)]
    junk = [S.sb("junk%d" % i, [64, ML_DV], F32) for i in range(NR)]
    hn_sb = [S.sb("hn%d" % i, [64, ML_DV], BF16) for i in range(NR)]
    for i in range(NR):
        S.op("pool", lambda e: e.memset(va_sb[i][:, ML_DV:ML_DV + 1], 1.0), writes=[va_sb[i]])
    pTall = S.ps("pTall", [128, 1024], BF16)

    load_a(0)
    for ti in range(NTS):
        if ti + 1 < NTS:
            load_a(ti + 1)
        ab = a_sb[ti % 2]
        t0 = ti * TS
        qTb, kTb, Wtb, ogTb = qT[ti % 2], kT[ti % 2], Wt[ti % 2], ogT[ti % 2]
        for wi, (dst, scl) in enumerate(((qTb, ML_DK ** -0.5), (kTb, 1.0))):
            pb = pp.get()
            for kc in range(8):
                S.op("pe", lambda e: e.matmul(pb[:, :], w_sb[:, kc, wi * 128:(wi + 1) * 128], ab[:, kc, :], start=(kc == 0), stop=(kc == 7)),
                     reads=[w_sb, ab], writes=[pb])
            S.op("act", lambda e: e.activation(out=dst[:], in_=pb[:, :], func=AF.Copy, scale=scl), reads=[pb], writes=[dst])
        pb = pp.get()
        S.op("pe", lambda e: e.matmul(pb[0:64, :], ones_row[0:1, 0:64], M_row[0:1, t0:t0 + TS], start=True, stop=False),
             reads=[ones_row, M_row], writes=[pb])
        S.op("pe", lambda e: e.matmul(pb[0:64, :], cst[0:64, 0:64], cst[0:64, 64:576], start=False, stop=True),
             reads=[cst], writes=[pb])
        S.op("dve", lambda e: e.tensor_tensor(out=Wtb[:].rearrange("p (n i) -> p n i", n=CPT), in0=pb[0:64, :].rearrange("p (n i) -> p n i", n=CPT),
                                              in1=bcast_last(c_tm[:, ti * CPT:(ti + 1) * CPT], CH), op=ALU.subtract),
             reads=[pb, c_tm], writes=[Wtb])
        S.op("act", lambda e: e.activation(out=Wtb[:], in_=Wtb[:], func=AF.Exp, scale=-1.0), reads=[Wtb], writes=[Wtb])
        for cj in range(CPT):
            n = ti * CPT + cj
            r = n % NR
            c0 = cj * CH
            pkv = pp.get()
            for kc in range(8):
                S.op("pe", lambda e: e.matmul(pkv[0:64, 0:384], ab[:, kc, c0:c0 + CH], w_sb[:, kc, 256:640], start=(kc == 0), stop=(kc == 7)),
                     reads=[w_sb, ab], writes=[pkv])
            pog = pp.get()
            for kc in range(8):
                S.op("pe", lambda e: e.matmul(pog[0:64, 0:256], ab[:, kc, c0:c0 + CH], w_sb[:, kc, 640:896], start=(kc == 0), stop=(kc == 7)),
                     reads=[w_sb, ab], writes=[pog])
            S.op("act", lambda e: e.activation(out=kw_sb[r][:], in_=pkv[0:64, 0:128], func=AF.Copy, scale=kws_tm[:, n:n + 1]),
                 reads=[pkv, kws_tm], writes=[kw_sb[r]])
            S.op("act", lambda e: e.activation(out=va_sb[r][:, 0:ML_DV], in_=pkv[0:64, 128:384], func=AF.Copy), reads=[pkv], writes=[va_sb[r]])
            S.op("act", lambda e: e.activation(out=og_sb[r][:], in_=pog[0:64, 0:256], func=AF.Exp, scale=-1.0), reads=[pog], writes=[og_sb[r]])
            S.op("dve", lambda e: e.tensor_scalar_add(out=og_sb[r][:], in0=og_sb[r][:], scalar1=1.0), reads=[og_sb[r]], writes=[og_sb[r]])
            S.op("dve", lambda e: e.reciprocal(out=og_sb[r][:], in_=og_sb[r][:]), reads=[og_sb[r]], writes=[og_sb[r]])
            S.op("dve", lambda e: e.tensor_tensor(out=og_sb[r][:], in0=og_sb[r][:], in1=nwv[:], op=ALU.mult), reads=[og_sb[r], nwv], writes=[og_sb[r]])
            pq = pp.get()
            S.op("pe", lambda e: e.matmul(pq[0:64, 0:64], kTb[:, c0:c0 + CH], qTb[:, c0:c0 + CH], start=True, stop=True),
                 reads=[kTb, qTb], writes=[pq])
            S.op("dve", lambda e: e.tensor_tensor(out=St_sb[r][:], in0=pq[0:64, 0:64], in1=Wtb[:, c0:c0 + CH], op=ALU.mult),
                 reads=[pq, Wtb], writes=[St_sb[r]])
            pint = pp.get()
            S.op("pe", lambda e: e.matmul(pint[0:64, 0:ML_DV + 1], qTb[:, c0:c0 + CH], C_b[:], start=True, stop=True),
                 reads=[qTb, C_b], writes=[pint])
            pia = pp.get()
            S.op("pe", lambda e: e.matmul(pia[0:64, 0:ML_DV + 1], St_sb[r][:], va_sb[r][:], start=True, stop=True),
                 reads=[St_sb[r], va_sb[r]], writes=[pia])
            pst = pp.get()
            S.op("pe", lambda e: e.matmul(pst[:, 0:ML_DV + 1], kw_sb[r][:], va_sb[r][:], start=True, stop=True),
                 reads=[kw_sb[r], va_sb[r]], writes=[pst])
            S.op("dve", lambda e: e.scalar_tensor_tensor(out=C_f[:], in0=C_f[:], scalar=dec_b[:, n:n + 1], in1=pst[:, 0:ML_DV + 1],
                                                         op0=ALU.mult, op1=ALU.add), reads=[C_f, dec_b, pst], writes=[C_f])
            S.op("act", lambda e: e.activation(out=C_b[:], in_=C_f[:], func=AF.Copy), reads=[C_f], writes=[C_b])
            t1 = t1_sb[r]
            S.op("act", lambda e: e.activation(out=t1[:], in_=pint[0:64, 0:ML_DV + 1], func=AF.Copy, scale=sc_tm[:, n:n + 1]),
                 reads=[pint, sc_tm], writes=[t1])
            S.op("dve", lambda e: e.tensor_tensor(out=t1[:], in0=t1[:], in1=pia[0:64, 0:ML_DV + 1], op=ALU.add), reads=[t1, pia], writes=[t1])
            sm = sm_sb[r]
            S.op("act", lambda e: e.activation(out=sm[:, 0:1], in_=t1[:, ML_DV:ML_DV + 1], func=AF.Abs), reads=[t1], writes=[sm])
            S.op("dve", lambda e: e.tensor_tensor(out=sm[:, 0:1], in0=sm[:, 0:1], in1=en_tm[:, n:n + 1], op=ALU.max),
                 reads=[sm, en_tm], writes=[sm])
            S.op("dve", lambda e: e.reciprocal(out=sm[:, 1:2], in_=sm[:, 0:1]), reads=[sm], writes=[sm])
            S.op("act", lambda e: e.activation(out=junk[r][:], in_=t1[:, 0:ML_DV], func=AF.Square, scale=sm[:, 1:2], accum_out=sm[:, 2:3]),
                 reads=[t1, sm], writes=[junk[r], sm])
            S.op("act", lambda e: e.activation(out=sm[:, 3:4], in_=sm[:, 2:3], func=AF.Ln, scale=1.0 / ML_DV, bias=RMS_EPS), reads=[sm], writes=[sm])
            S.op("act", lambda e: e.activation(out=sm[:, 3:4], in_=sm[:, 3:4], func=AF.Exp, scale=-0.5), reads=[sm], writes=[sm])
            S.op("dve", lambda e: e.tensor_tensor(out=sm[:, 4:5], in0=sm[:, 3:4], in1=sm[:, 1:2], op=ALU.mult), reads=[sm], writes=[sm])
            S.op("dve", lambda e: e.scalar_tensor_tensor(out=hn_sb[r][:], in0=t1[:, 0:ML_DV], scalar=sm[:, 4:5], in1=og_sb[r][:],
                                                         op0=ALU.mult, op1=ALU.mult), reads=[t1, sm, og_sb[r]], writes=[hn_sb[r]])
            ptv = pTall[:, (n % 4) * 128:(n % 4 + 1) * 128].rearrange("p (a b) -> p a b", a=2)
            for hh in range(2):
                S.op("pe", lambda e: e.transpose(ptv[:, hh, :], hn_sb[r][:, hh * 128:(hh + 1) * 128], ident_b[0:64, 0:64]),
                     reads=[hn_sb[r], ident_b], writes=[pTall])
            S.op("act", lambda e: e.activation(out=ogTb[:, :, c0:c0 + CH], in_=ptv, func=AF.Copy), reads=[pTall], writes=[ogTb])
        if ti >= 1:
            S.dma("pool", ogF[ti - 1][:, :].rearrange("p (c t) -> p c t", c=c_og), ogTb[:], reads=[ogTb], writes=[ogF[ti - 1]])
        if ti % 4 == 0 and ti <= 12:
            S.dma("pool", ogH[(ti // 4) * 128:(ti // 4 + 1) * 128, :].rearrange("p (c t) -> p c t", c=c_og), ogTb[:, :, TS - 64:TS],
                  reads=[ogTb], writes=[ogH])
        io["after_og"](ti)
    S.pop()


def prep_M_ml(inp, j, h):
    w = inp["ml_w_in"][j]
    q = w[:, h * 128:(h + 1) * 128]
    k = w[:, 512 + h * 128:512 + (h + 1) * 128]
    v = w[:, 1024 + h * 256:1024 + (h + 1) * 256]
    o = w[:, 2048 + h * 256:2048 + (h + 1) * 256]
    gi = w[:, 3072 + h:3073 + h]
    gf = w[:, 3076 + h:3077 + h]
    wc = np.concatenate([q, k, k, v, o, gi, gf], 1)
    wc = wc.reshape(8, 128, ML_WC).transpose(1, 0, 2)
    gb = inp["ml_gate_b"][j][[h, 4 + h]].reshape(1, 2)
    nwv = inp["ml_norm_w"][j][h * 256:(h + 1) * 256].reshape(1, 256)
    return {"w": np.ascontiguousarray(wc.reshape(128, 8 * ML_WC), np.float32),
            "gb": np.ascontiguousarray(gb, np.float32), "nwv": np.ascontiguousarray(nwv, np.float32),
            "cst": ml_consts()}


def ml_consts():
    c = np.zeros((128, 64 + 512 + 128), np.float32)
    c[0:64, 0:64] = np.eye(64, dtype=np.float32)
    jj = np.arange(64)[:, None]
    ii = np.arange(64)[None, :]
    mb = (jj > ii).astype(np.float32) * BIGNEG
    c[0:64, 64:576] = np.tile(mb, (1, 8))
    c[:, 576:704] = np.eye(128, dtype=np.float32)
    return c


G_WC = 12 * 128 + 8
L2_EPS = 1e-6
GDN_EPS = 1e-6
C_I64, C_MUI, C_MUS, C_MLS, C_NMLS, C_NMUS, C_I128, C_ONES = 0, 64, 128, 192, 256, 320, 384, 512
C_TOT = 640


def gdn_consts():
    c = np.zeros((128, C_TOT), np.float32)
    p = np.arange(64)[:, None]
    f = np.arange(64)[None, :]
    c[0:64, C_I64:C_I64 + 64] = (p == f)
    c[0:64, C_MUI:C_MUI + 64] = (p <= f)
    c[0:64, C_MUS:C_MUS + 64] = (p < f)
    c[0:64, C_MLS:C_MLS + 64] = (p > f)
    c[0:64, C_NMLS:C_NMLS + 64] = -1.0 * (p > f)
    c[0:64, C_NMUS:C_NMUS + 64] = -1.0 * (p < f)
    c[:, C_I128:C_I128 + 128] = np.eye(128, dtype=np.float32)
    c[:, C_ONES:C_ONES + 128] = 1.0
    return c


def emit_M_gdn(S, tag, io):
    S.push(tag)
    ainF_g, ainH_g = io["ainF_g"], io["ainH_g"]
    w_d, cw_d, hv_d, gnw_d, cst_d = io["w"], io["cw"], io["hv"], io["gnw"], io["cst"]
    ogF, ogH = io["ogF"], io["ogH"]
    c_og = 4

    w_sb = S.sb("w_sb", [128, 8, G_WC], BF16)
    S.dma("pool", w_sb[:].rearrange("p a b -> p (a b)"), w_d[:, :], writes=[w_sb])
    cst = S.sb("cst_sb", [128, C_TOT], F32)
    S.dma("sp", cst[:], cst_d[:, :], writes=[cst])
    cwg = S.sb("cwg", [128, 8, 4], F32)
    S.dma("sp", cwg[:].rearrange("p a b -> p (a b)"), cw_d[:, :], writes=[cwg])
    hv = S.sb("hv_sb", [64, 8], F32)
    S.dma("sp", hv[:], hv_d[0:1, :].partition_broadcast(64), writes=[hv])
    gnw = S.sb("gnw_sb", [128, 1], F32)
    S.dma("sp", gnw[:], gnw_d[:, :], writes=[gnw])
    ident_b = S.sb("ident_b", [128, 128], BF16)
    ones_b = S.sb("ones_b", [128, 128], BF16)
    S.op("act", lambda e: e.activation(out=ident_b[:], in_=cst[:, C_I128:C_I128 + 128], func=AF.Copy), reads=[cst], writes=[ident_b])
    S.op("act", lambda e: e.activation(out=ones_b[:], in_=cst[:, C_ONES:C_ONES + 128], func=AF.Copy), reads=[cst], writes=[ones_b])
    dg = S.sb("dg", [128, 32, 128], BF16)
    for mc in range(8):
        for k in range(4):
            S.op("dve", lambda e: e.tensor_scalar(out=dg[:, mc * 4 + k, :], in0=cst[:, C_I128:C_I128 + 128], scalar1=cwg[:, mc, k:k + 1],
                                                  scalar2=None, op0=ALU.mult), reads=[cst, cwg], writes=[dg])
    I64 = cst[0:64, C_I64:C_I64 + 64]
    MUI = cst[0:64, C_MUI:C_MUI + 64]
    MLS = cst[0:64, C_MLS:C_MLS + 64]
    NMLS = cst[0:64, C_NMLS:C_NMLS + 64]
    NMUS = cst[0:64, C_NMUS:C_NMUS + 64]
    ONES64x128 = cst[0:64, C_ONES:C_ONES + 128]

    a_sb = [S.sb("a_sb%d" % i, [128, 8, 512], BF16) for i in range(2)]
    pp = PsumPool(S, 4)
    po2 = [S.ps("po%d" % i, [128, 512]) for i in range(2)]
    pTall = S.ps("pTall", [128, 1024], BF16)
    pTon = S.ps("pTon", [128, 1024], BF16)

    def load_a(i):
        ab = a_sb[i % 2]
        if i == 0:
            S.op("pool", lambda e: e.memset(ab[:, :, 0:XPAD], 0.0), writes=[ab])
            S.dma("sp", ab[:, :, XPAD:TS], ainH_g[0:128, :].rearrange("p (c t) -> p c t", c=8), reads=[ainH_g], writes=[ab])
        else:
            r0 = ((i - 1) % 4) * 512 + ((i - 1) // 4) * 128
            S.dma("sp", ab[:].rearrange("p c t -> p (c t)"), ainF_g[r0:r0 + 128, :], reads=[ainF_g], writes=[ab])

    NH = NCH * 4
    bg = S.sb("bg", [64, NCH, 8], F32)
    load_a(0)
    for ti in range(NTS):
        if ti + 1 < NTS:
            load_a(ti + 1)
        ab = a_sb[ti % 2]
        pb = pp.get()
        for cj in range(CPT):
            for kc in range(8):
                S.op("pe", lambda e: e.matmul(pb[0:64, cj * 8:(cj + 1) * 8], ab[:, kc, cj * CH:(cj + 1) * CH], w_sb[:, kc, 1536:1544],
                                              start=(kc == 0), stop=(kc == 7)), reads=[ab, w_sb], writes=[pb])
        S.op("act", lambda e: e.activation(out=bg[:, ti * CPT:(ti + 1) * CPT, :], in_=pb[0:64, 0:64].rearrange("p (a b) -> p a b", a=CPT),
                                           func=AF.Copy), reads=[pb], writes=[bg])
    lnb = S.sb("lnb", [64, NCH, 4], F32)
    bt = S.sb("bt", [64, NCH, 4], F32)
    gt = S.sb("gt", [64, NCH, 4], F32)
    beG = S.sb("beG", [64, NCH, 4], F32)
    ekt = S.sb("ekt", [64, NCH, 4], F32)
    eGl = S.sb("eGl", [128, NCH, 4], F32)
    tmpg = S.sb("tmpg", [64, NCH, 4], F32)
    eal = S.sb("eal", [64, 4], F32)
    S.op("act", lambda e: e.activation(out=lnb[:], in_=bg[:, :, 0:4], func=AF.Exp, scale=-1.0), reads=[bg], writes=[lnb])
    S.op("act", lambda e: e.activation(out=lnb[:], in_=lnb[:], func=AF.Ln, bias=1.0, scale=1.0), reads=[lnb], writes=[lnb])
    S.op("dve", lambda e: e.tensor_scalar_mul(out=lnb[:], in0=lnb[:], scalar1=-1.0), reads=[lnb], writes=[lnb])
    S.op("act", lambda e: e.activation(out=bt[:], in_=lnb[:], func=AF.Exp), reads=[lnb], writes=[bt])
    S.op("dve", lambda e: e.tensor_tensor(out=gt[:], in0=bg[:, :, 4:8], in1=bcast_mid(hv[:, 4:8], NCH), op=ALU.add), reads=[bg, hv], writes=[gt])
    S.op("act", lambda e: e.activation(out=gt[:], in_=gt[:], func=AF.Exp), reads=[gt], writes=[gt])
    S.op("act", lambda e: e.activation(out=gt[:], in_=gt[:], func=AF.Ln, bias=1.0, scale=1.0), reads=[gt], writes=[gt])
    S.op("act", lambda e: e.activation(out=eal[:], in_=hv[:, 0:4], func=AF.Exp), reads=[hv], writes=[eal])
    S.op("dve", lambda e: e.tensor_scalar_mul(out=eal[:], in0=eal[:], scalar1=-1.0), reads=[eal], writes=[eal])
    S.op("dve", lambda e: e.tensor_tensor(out=gt[:], in0=gt[:], in1=bcast_mid(eal[:], NCH), op=ALU.mult), reads=[gt, eal], writes=[gt])
    gflat = gt[:].rearrange("p a b -> p (a b)")
    for (c0, c1) in ((0, 272), (272, NH)):
        pb = pp.get()
        S.op("pe", lambda e: e.matmul(pb[0:64, 0:c1 - c0], MUI, gflat[:, c0:c1], start=True, stop=True), reads=[cst, gt], writes=[pb])
        pl = pp.get()
        S.op("pe", lambda e: e.matmul(pl[:, 0:c1 - c0], ONES64x128, gflat[:, c0:c1], start=True, stop=True), reads=[cst, gt], writes=[pl])
        S.op("act", lambda e: e.activation(out=tmpg[:].rearrange("p a b -> p (a b)")[:, c0:c1], in_=pb[0:64, 0:c1 - c0], func=AF.Exp),
             reads=[pb], writes=[tmpg])
        S.op("act", lambda e: e.activation(out=eGl[:].rearrange("p a b -> p (a b)")[:, c0:c1], in_=pl[:, 0:c1 - c0], func=AF.Exp),
             reads=[pl], writes=[eGl])
        S.op("act", lambda e: e.activation(out=ekt[:].rearrange("p a b -> p (a b)")[:, c0:c1], in_=pb[0:64, 0:c1 - c0], func=AF.Copy),
             reads=[pb], writes=[ekt])
        S.op("dve", lambda e: e.tensor_tensor(out=ekt[:].rearrange("p a b -> p (a b)")[:, c0:c1], in0=pl[0:64, 0:c1 - c0],
                                              in1=ekt[:].rearrange("p a b -> p (a b)")[:, c0:c1], op=ALU.subtract), reads=[pl, ekt], writes=[ekt])
    S.op("act", lambda e: e.activation(out=ekt[:], in_=ekt[:], func=AF.Exp), reads=[ekt], writes=[ekt])
    S.op("dve", lambda e: e.tensor_tensor(out=beG[:], in0=bt[:], in1=tmpg[:], op=ALU.mult), reads=[bt, tmpg], writes=[beG])

    xb = [S.sb("xb%d" % i, [128, 8, 3 + TS], BF16) for i in range(2)]
    S.op("pool", lambda e: e.memset(xb[1][:, :, TS:TS + 3], 0.0), writes=[xb[1]])
    sx = [S.sb("sx%d" % i, [128, TS], F32) for i in range(2)]
    sqb = [S.sb("sqb%d" % i, [128, TS], BF16) for i in range(2)]
    rs = [S.sb("rs%d" % i, [128, TS], F32) for i in range(2)]
    qT = [S.sb("qT%d" % i, [128, 2, TS], BF16) for i in range(2)]
    kT = [S.sb("kT%d" % i, [128, 2, TS], BF16) for i in range(2)]
    svT = [S.sb("svT%d" % i, [128, 4, TS], BF16) for i in range(2)]
    zs = [S.sb("zs%d" % i, [128, 4, TS], F32) for i in range(2)]
    onT = [S.sb("onT%d" % i, [128, 4, TS], BF16) for i in range(2)]
    ogt = [S.sb("ogt0", [128, 4, TS], BF16)] * 2
    S_f = S.sb("S_f", [128, 4, 128], F32)
    S_b = S.sb("S_b", [128, 4, 128], BF16)
    S_t = S.sb("S_t", [128, 4, 128], F32)
    S.op("pool", lambda e: e.memset(S_f[:], 0.0), writes=[S_f])
    S.op("pool", lambda e: e.memset(S_b[:], 0.0), writes=[S_b])
    NR = 2
    mk = lambda nm, shp, dt: [S.sb("%s%d" % (nm, i), shp, dt) for i in range(NR)]
    kbg = mk("kbg", [64, 4, 128], BF16)
    ktm = mk("ktm", [64, 4, 128], BF16)
    vb = mk("vb", [64, 4, 128], BF16)
    rg1 = mk("rg1", [64, 4, 64], F32)
    rg2 = mk("rg2", [64, 4, 64], F32)
    rg3 = mk("rg3", [64, 4, 64], F32)
    Et = mk("Et", [64, 4, 64], F32)
    Wt_ = mk("Wt", [64, 4, 64], F32)
    W_ = mk("W", [64, 4, 64], F32)
    eGb = mk("eGb", [128, 4, 64], F32)
    KKlo = mk("KKlo", [64, 2, 64], F32)
    KKup = mk("KKup", [64, 2, 64], F32)
    KQm = mk("KQm", [64, 2, 64], F32)
    Qt = mk("Qt", [64, 4, 64], BF16)
    qdT = mk("qdT", [128, 4, 64], BF16)
    PP = [mk("PP%d" % k, [64, 8, 64], F32) for k in range(2)]
    Xt = [mk("Xt%d" % k, [64, 4, 64], F32) for k in range(2)]
    Tt = mk("Tt", [64, 4, 64], BF16)
    nwT = mk("nwT", [128, 4, 64], BF16)
    vn = mk("vn", [64, 4, 128], BF16)
    sqo = mk("sqo", [64, 4, 128], F32)
    sso = mk("sso", [64, 8], F32)
    on = mk("on", [64, 4, 128], BF16)

    def silu_from_psum(pb, W, out_ap, out_buf, idx):
        S.op("act", lambda e: e.activation(out=out_ap, in_=pb[:, :W], func=AF.Silu), reads=[pb], writes=[out_buf])

    def tile_level(ti):
        if ti + 1 < NTS:
            load_a(ti + 1)
        ab = a_sb[ti % 2]
        t0 = ti * TS
        xcur, xprev = xb[ti % 2], xb[(ti + 1) % 2]
        qTb, kTb, svb, zsb, onTb, ogb = qT[ti % 2], kT[ti % 2], svT[ti % 2], zs[ti % 2], onT[ti % 2], ogt[ti % 2]
        S.op("pool", lambda e: e.tensor_copy(out=xcur[:, :, 0:3], in_=xprev[:, :, TS:TS + 3]), reads=[xprev], writes=[xcur])
        for mc in range(12):
            pb = pp.get()
            for kc in range(8):
                S.op("pe", lambda e: e.matmul(pb[:, :], w_sb[:, kc, mc * 128:(mc + 1) * 128], ab[:, kc, :], start=(kc == 0), stop=(kc == 7)),
                     reads=[w_sb, ab], writes=[pb])
            if mc < 8:
                S.op("act", lambda e: e.activation(out=xcur[:, mc, 3:3 + TS], in_=pb[:, :], func=AF.Copy), reads=[pb], writes=[xcur])
            else:
                silu_from_psum(pb, TS, zsb[:, mc - 8, :], zsb, mc)
        for mc in range(8):
            pb = pp.get()
            for k in range(4):
                S.op("pe", lambda e: e.matmul(pb[:, :], dg[:, mc * 4 + k, :], xcur[:, mc, k:k + TS], start=(k == 0), stop=(k == 3)),
                     reads=[dg, xcur], writes=[pb])
            if mc >= 4:
                silu_from_psum(pb, TS, svb[:, mc - 4, :], svb, mc)
            else:
                sxb, sq, rsb = sx[mc % 2], sqb[mc % 2], rs[mc % 2]
                silu_from_psum(pb, TS, sxb[:, :], sxb, mc)
                S.op("act", lambda e: e.activation(out=sq[:, :], in_=sxb[:, :], func=AF.Square), reads=[sxb], writes=[sq])
                ps2 = pp.get()
                S.op("pe", lambda e: e.matmul(ps2[:, :], ones_b[:], sq[:, :], start=True, stop=True), reads=[ones_b, sq], writes=[ps2])
                rstd_from_ss(S, ps2, rsb, 1.0, L2_EPS, TS)
                dst = qTb if mc < 2 else kTb
                scl = (128.0 ** -0.5) if mc < 2 else 1.0
                S.op("dve", lambda e: e.scalar_tensor_tensor(out=dst[:, mc % 2, :], in0=sxb[:, :], scalar=scl, in1=rsb[:, :],
                                                             op0=ALU.mult, op1=ALU.mult), reads=[sxb, rsb], writes=[dst])

    def stageA(n):
        ti, cj = n // CPT, n % CPT
        qTb, kTb, svb, onTb = qT[ti % 2], kT[ti % 2], svT[ti % 2], onT[ti % 2]
        r = n % NR
        c0 = cj * CH
        for qh in range(2):
            S.op("pe", lambda e: e.transpose(pTall[0:64, qh * 128:(qh + 1) * 128], kTb[:, qh, c0:c0 + CH], ident_b[:, :]),
                 reads=[kTb, ident_b], writes=[pTall])
        for h in range(4):
            S.op("pe", lambda e: e.transpose(pTall[0:64, 256 + h * 128:256 + (h + 1) * 128], svb[:, h, c0:c0 + CH], ident_b[:, :]),
                 reads=[svb, ident_b], writes=[pTall])
        ktm_ps = pTall[0:64, 0:256].rearrange("p (a b) -> p a b", a=2)
        ktm_rep = ktm_ps.unsqueeze(2).broadcast_to([64, 2, 2, 128])
        as4 = lambda ap: ap.rearrange("p (a r) d -> p a r d", r=2)
        S.op("dve", lambda e: e.tensor_tensor(out=as4(kbg[r][:]), in0=ktm_rep, in1=as4(bcast_last(beG[:, n, :], 128)), op=ALU.mult),
             reads=[pTall, beG], writes=[kbg[r]])
        S.op("dve", lambda e: e.tensor_tensor(out=as4(ktm[r][:]), in0=ktm_rep, in1=as4(bcast_last(ekt[:, n, :], 128)), op=ALU.mult),
             reads=[pTall, ekt], writes=[ktm[r]])
        S.op("dve", lambda e: e.tensor_tensor(out=vb[r][:], in0=pTall[0:64, 256:768].rearrange("p (a b) -> p a b", a=4),
                                              in1=bcast_last(bt[:, n, :], 128), op=ALU.mult), reads=[pTall, bt], writes=[vb[r]])
        yield
        S.op("dve", lambda e: e.tensor_tensor(out=rg1[r][:], in0=bcast_mid(MUI, 4), in1=bcast_last(gt[:, n, :], 64), op=ALU.mult),
             reads=[cst, gt], writes=[rg1[r]])
        S.op("dve", lambda e: e.tensor_tensor(out=rg2[r][:], in0=bcast_mid(I64, 4), in1=bcast_last(lnb[:, n, :], 64), op=ALU.mult),
             reads=[cst, lnb], writes=[rg2[r]])
        S.op("dve", lambda e: e.tensor_tensor(out=rg2[r][:], in0=rg2[r][:], in1=rg1[r][:], op=ALU.add), reads=[rg1[r], rg2[r]], writes=[rg2[r]])
        S.op("dve", lambda e: e.tensor_tensor(out=rg3[r][:], in0=bcast_mid(MLS, 4), in1=bcast_last(gt[:, n, :], 64), op=ALU.mult),
             reads=[cst, gt], writes=[rg3[r]])
        fl = lambda b_: b_[:].rearrange("p a b -> p (a b)")
        pd1 = pp.get()
        S.op("pe", lambda e: e.matmul(pd1[0:64, 0:256], MLS, fl(rg1[r]), start=True, stop=True), reads=[cst, rg1[r]], writes=[pd1])
        S.op("pe", lambda e: e.matmul(pd1[0:64, 256:512], MLS, fl(rg2[r]), start=True, stop=True), reads=[cst, rg2[r]], writes=[pd1])
        pd2 = pp.get()
        S.op("pe", lambda e: e.matmul(pd2[0:64, 0:256], MUI, fl(rg3[r]), start=True, stop=True), reads=[cst, rg3[r]], writes=[pd2])
        pd3 = pp.get()
        S.op("pe", lambda e: e.matmul(pd3[:, 0:256], ONES64x128, fl(rg1[r]), start=True, stop=True), reads=[cst, rg1[r]], writes=[pd3])
        S.op("act", lambda e: e.activation(out=fl(Et[r]), in_=pd1[0:64, 0:256], func=AF.Exp), reads=[pd1], writes=[Et[r]])
        S.op("act", lambda e: e.activation(out=fl(Wt_[r]), in_=pd1[0:64, 256:512], func=AF.Exp), reads=[pd1], writes=[Wt_[r]])
        for h in range(4):
            S.op("act", lambda e: e.activation(out=W_[r][:, h, :], in_=pd2[0:64, h * 64:(h + 1) * 64], func=AF.Exp,
                                               bias=lnb[:, n, h:h + 1], scale=1.0), reads=[pd2, lnb], writes=[W_[r]])
        S.op("act", lambda e: e.activation(out=fl(eGb[r]), in_=pd3[:, 0:256], func=AF.Exp), reads=[pd3], writes=[eGb[r]])
        yield
        pg = pp.get()
        for qh in range(2):
            S.op("pe", lambda e: e.matmul(pg[0:64, qh * 64:(qh + 1) * 64], kTb[:, qh, c0:c0 + CH], kTb[:, qh, c0:c0 + CH], start=True, stop=True),
                 reads=[kTb], writes=[pg])
            S.op("pe", lambda e: e.matmul(pg[0:64, 128 + qh * 64:128 + (qh + 1) * 64], kTb[:, qh, c0:c0 + CH], qTb[:, qh, c0:c0 + CH],
                                          start=True, stop=True), reads=[kTb, qTb], writes=[pg])
        kkv = pg[0:64, 0:128].rearrange("p (a b) -> p a b", a=2)
        kqv = pg[0:64, 128:256].rearrange("p (a b) -> p a b", a=2)
        S.op("dve", lambda e: e.tensor_tensor(out=KKlo[r][:], in0=kkv, in1=bcast_mid(NMLS, 2), op=ALU.mult), reads=[pg, cst], writes=[KKlo[r]])
        S.op("dve", lambda e: e.tensor_tensor(out=KKup[r][:], in0=kkv, in1=bcast_mid(NMUS, 2), op=ALU.mult), reads=[pg, cst], writes=[KKup[r]])
        S.op("dve", lambda e: e.tensor_tensor(out=KQm[r][:], in0=kqv, in1=bcast_mid(MUI, 2), op=ALU.mult), reads=[pg, cst], writes=[KQm[r]])
        yield
        rep = lambda b_: b_[:].unsqueeze(2).broadcast_to([64, 2, 2, 64])
        P0 = PP[0][r]
        S.op("dve", lambda e: e.tensor_tensor(out=as4(P0[:, 0:4, :]), in0=rep(KKlo[r]), in1=as4(W_[r][:]), op=ALU.mult),
             reads=[KKlo[r], W_[r]], writes=[P0])
        S.op("dve", lambda e: e.tensor_tensor(out=as4(P0[:, 4:8, :]), in0=rep(KKup[r]), in1=as4(Wt_[r][:]), op=ALU.mult),
             reads=[KKup[r], Wt_[r]], writes=[P0])
        S.op("dve", lambda e: e.tensor_tensor(out=as4(Qt[r][:]), in0=rep(KQm[r]), in1=as4(Et[r][:]), op=ALU.mult),
             reads=[KQm[r], Et[r]], writes=[Qt[r]])
        S.op("dve", lambda e: e.tensor_tensor(out=as4(qdT[r][:]), in0=qTb[:, :, c0:c0 + CH].unsqueeze(2).broadcast_to([128, 2, 2, 64]),
                                              in1=as4(eGb[r][:]), op=ALU.mult), reads=[qTb, eGb[r]], writes=[qdT[r]])
        yield
        X = Xt[0][r]
        S.op("dve", lambda e: e.tensor_tensor(out=X[:], in0=P0[:, 4:8, :], in1=bcast_mid(I64, 4), op=ALU.add), reads=[P0, cst], writes=[X])
        for k in range(1, 6):
            Pp, Pn = PP[(k - 1) % 2][r], PP[k % 2][r]
            pq = pp.get()
            for h in range(4):
                S.op("pe", lambda e: e.matmul(pq[0:64, h * 64:(h + 1) * 64], Pp[:, 4 + h, :], Pp[:, h, :], start=True, stop=True),
                     reads=[Pp], writes=[pq])
            if k < 5:
                for h in range(4):
                    S.op("pe", lambda e: e.matmul(pq[0:64, 256 + h * 64:256 + (h + 1) * 64], Pp[:, h, :], Pp[:, 4 + h, :], start=True, stop=True),
                         reads=[Pp], writes=[pq])
            wdt = 512 if k < 5 else 256
            S.op("act", lambda e: e.activation(out=Pn[:].rearrange("p a b -> p (a b)")[:, 0:wdt], in_=pq[0:64, 0:wdt], func=AF.Copy),
                 reads=[pq], writes=[Pn])
            yield
            px = pp.get()
            Xo = Xt[(k - 1) % 2][r]
            Xn = Xt[k % 2][r]
            for h in range(4):
                S.op("pe", lambda e: e.matmul(px[0:64, h * 64:(h + 1) * 64], Pn[:, h, :], Xo[:, h, :], start=True, stop=True),
                     reads=[Pn, Xo], writes=[px])
            yield
            if k < 5:
                S.op("dve", lambda e: e.tensor_tensor(out=fl(Xn), in0=px[0:64, 0:256], in1=fl(Xo), op=ALU.add), reads=[px, Xo], writes=[Xn])
            else:
                S.op("dve", lambda e: e.tensor_tensor(out=fl(Tt[r]), in0=px[0:64, 0:256], in1=fl(Xo), op=ALU.add), reads=[px, Xo], writes=[Tt[r]])
        yield
        pw = pp.get()
        for h in range(4):
            S.op("pe", lambda e: e.matmul(pw[:, h * 64:(h + 1) * 64], kbg[r][:, h, :], Tt[r][:, h, :], start=True, stop=True),
                 reads=[kbg[r], Tt[r]], writes=[pw])
        S.op("act", lambda e: e.activation(out=fl(nwT[r]), in_=pw[:, 0:256], func=AF.Copy, scale=-1.0), reads=[pw], writes=[nwT[r]])
        yield

    def stageB(n):
        ti, cj = n // CPT, n % CPT
        qTb, kTb, svb, onTb = qT[ti % 2], kT[ti % 2], svT[ti % 2], onT[ti % 2]
        r = n % NR
        c0 = cj * CH
        pu = pp.get()
        for h in range(4):
            S.op("pe", lambda e: e.matmul(pu[0:64, h * 128:(h + 1) * 128], Tt[r][:, h, :], vb[r][:, h, :], start=True, stop=False),
                 reads=[Tt[r], vb[r]], writes=[pu])
            S.op("pe", lambda e: e.matmul(pu[0:64, h * 128:(h + 1) * 128], nwT[r][:, h, :], S_b[:, h, :], start=False, stop=True),
                 reads=[nwT[r], S_b], writes=[pu])
        S.op("act", lambda e: e.activation(out=vn[r][:].rearrange("p a b -> p (a b)"), in_=pu[0:64, :], func=AF.Copy), reads=[pu], writes=[vn[r]])
        yield
        po = po2[n % 2]
        for h in range(4):
            S.op("pe", lambda e: e.matmul(po[0:64, h * 128:(h + 1) * 128], qdT[r][:, h, :], S_b[:, h, :], start=True, stop=False),
                 reads=[qdT[r], S_b], writes=[po])
            S.op("pe", lambda e: e.matmul(po[0:64, h * 128:(h + 1) * 128], Qt[r][:, h, :], vn[r][:, h, :], start=False, stop=True),
                 reads=[Qt[r], vn[r]], writes=[po])
        yield
        pS = pp.get()
        for h in range(4):
            S.op("pe", lambda e: e.matmul(pS[:, h * 128:(h + 1) * 128], ktm[r][:, h, :], vn[r][:, h, :], start=True, stop=True),
                 reads=[ktm[r], vn[r]], writes=[pS])
        for h in range(4):
            S.op("dve", lambda e: e.scalar_tensor_tensor(out=S_f[:, h, :], in0=S_f[:, h, :], scalar=eGl[:, n, h:h + 1],
                                                         in1=pS[:, h * 128:(h + 1) * 128], op0=ALU.mult, op1=ALU.add),
                 reads=[S_f, eGl, pS], writes=[S_f])
        S.op("act", lambda e: e.activation(out=S_b[:], in_=S_f[:], func=AF.Copy), reads=[S_f], writes=[S_b])
        yield
        S.op("act", lambda e: e.activation(out=sqo[r][:].rearrange("p a b -> p (a b)"), in_=po[0:64, :], func=AF.Square), reads=[po], writes=[sqo[r]])
        S.op("dve", lambda e: e.tensor_reduce(out=sso[r][:, 0:4], in_=sqo[r][:], axis=AX.X, op=ALU.add), reads=[sqo[r]], writes=[sso[r]])
        S.op("act", lambda e: e.activation(out=sso[r][:, 4:8], in_=sso[r][:, 0:4], func=AF.Ln, scale=1.0 / 128.0, bias=GDN_EPS),
             reads=[sso[r]], writes=[sso[r]])
        S.op("act", lambda e: e.activation(out=sso[r][:, 4:8], in_=sso[r][:, 4:8], func=AF.Exp, scale=-0.5), reads=[sso[r]], writes=[sso[r]])
        S.op("dve", lambda e: e.tensor_tensor(out=on[r][:], in0=po[0:64, :].rearrange("p (a b) -> p a b", a=4),
                                              in1=bcast_last(sso[r][:, 4:8], 128), op=ALU.mult), reads=[po, sso[r]], writes=[on[r]])
        yield
        for h in range(4):
            S.op("pe", lambda e: e.transpose(pTon[:, h * 64:(h + 1) * 64], on[r][:, h, :], ident_b[0:64, 0:64]),
                 reads=[on[r], ident_b], writes=[pTon])
        S.op("act", lambda e: e.activation(out=onTb[:, :, c0:c0 + CH], in_=pTon[:, 0:256].rearrange("p (a b) -> p a b", a=4), func=AF.Copy),
             reads=[pTon], writes=[onTb])
        yield

    def tile_finish(ti):
        zsb, onTb, ogb = zs[ti % 2], onT[ti % 2], ogt[ti % 2]
        S.op("dve", lambda e: e.scalar_tensor_tensor(out=ogb[:].rearrange("p a b -> p (a b)"), in0=onTb[:].rearrange("p a b -> p (a b)"),
                                                     scalar=gnw[:, 0:1], in1=zsb[:].rearrange("p a b -> p (a b)"), op0=ALU.mult, op1=ALU.mult),
             reads=[onTb, gnw, zsb], writes=[ogb])
        if ti >= 1:
            S.dma("pool", ogF[ti - 1][:, :].rearrange("p (c t) -> p c t", c=c_og), ogb[:], reads=[ogb], writes=[ogF[ti - 1]])
        if ti % 4 == 0 and ti <= 12:
            S.dma("pool", ogH[(ti // 4) * 128:(ti // 4 + 1) * 128, :].rearrange("p (c t) -> p c t", c=c_og), ogb[:, :, TS - 64:TS],
                  reads=[ogb], writes=[ogH])
        io["after_og"](ti)

    def interleave(gens):
        gens = [g for g in gens if g is not None]
        while gens:
            for g in list(gens):
                try:
                    next(g)
                except StopIteration:
                    gens.remove(g)

    load_a(0)
    tile_level(0)
    interleave([stageA(0)])
    NCK = NTS * CPT
    for n in range(NCK):
        nxt = None
        if n + 1 < NCK:
            if (n + 1) % CPT == 0:
                tile_level((n + 1) // CPT)
            nxt = stageA(n + 1)
        interleave([stageB(n), nxt])
        if (n + 1) % CPT == 0:
            tile_finish(n // CPT)
    S.pop()


def prep_M_gdn(inp, j, hg):
    w = inp["gdn_w_in"][j]
    cols = []
    for qh in range(2):
        cols.append(np.arange(128) + 128 * (2 * hg + qh))
    for qh in range(2):
        cols.append(1024 + np.arange(128) + 128 * (2 * hg + qh))
    for h in range(4):
        cols.append(2048 + np.arange(128) + 128 * (4 * hg + h))
    conv_cols = np.concatenate(cols)
    for h in range(4):
        cols.append(4096 + np.arange(128) + 128 * (4 * hg + h))
    cols.append(6144 + 4 * hg + np.arange(4))
    cols.append(6160 + 4 * hg + np.arange(4))
    cols = np.concatenate(cols)
    wc = w[:, cols].reshape(8, 128, G_WC).transpose(1, 0, 2)
    cw = inp["gdn_conv_w"][j][:, conv_cols].reshape(4, 8, 128).transpose(2, 1, 0)
    hv = np.concatenate([inp["gdn_a_log"][j][4 * hg:4 * hg + 4], inp["gdn_dt_bias"][j][4 * hg:4 * hg + 4]]).reshape(1, 8)
    return {"w": np.ascontiguousarray(wc.reshape(128, 8 * G_WC), np.float32),
            "cw": np.ascontiguousarray(cw.reshape(128, 32), np.float32),
            "hv": np.ascontiguousarray(hv, np.float32),
            "gnw": np.ascontiguousarray(inp["gdn_norm_w"][j].reshape(128, 1), np.float32),
            "cst": gdn_consts()}


GROUPS = [[0, 1, 2, 3], [4, 5, 6, 7]]


def build_fused(nl=4):
    nc = bass.Bass("TRN2", target_bir_lowering=False)
    es = ExitStack()
    S = Sched(nc, es)
    ext = lambda n, shp, dt=F32: S.dram(n, shp, dt, kind="ExternalInput")
    hs0 = ext("hs0", [D, WIN])
    keep = ext("keep", [1, WIN])
    gidx4 = ext("gidx4", [128, 20], mybir.dt.int32)
    gidx2 = ext("gidx2", [128, 20], mybir.dt.int32) if nl > 1 else None
    nwT0 = ext("nwT_0", [128, 32])
    cst_g = ext("cst_g", [128, C_TOT])
    cst_m = ext("cst_m", [128, 64 + 512 + 128]) if nl > 1 else None
    hs_out = S.dram("hs_out", [D, WIN], F32, kind="ExternalOutput")
    hs_loc = S.dram("hs_loc", [D, WIN], F32)
    def slices(name, rows_total, cols, rows_per):
        big = S.dram(name, [rows_total, cols], BF16)
        return big, [Buf(big.t[k * rows_per:(k + 1) * rows_per, :], "%s_%d" % (name, k)) for k in range(rows_total // rows_per)]

    ainF_all, ainF = slices("ainF", 512, 4096, 128)
    ainF_g, ainF_gs = slices("ainF_g", 2048, 4096, 512)
    ainH = S.dram("ainH", [128, 512], BF16)
    ainH_g = S.dram("ainH_g", [512, 512], BF16)
    og = {}
    for c in (4, 2):
        tpc = 8 // c
        ogF_all, ogF = slices("ogF%d" % c, 2048, c * 512, 128)
        ogF_g, _ = slices("ogF%d_g" % c, 8192, c * 512, 8192)
        nq = 16 // tpc
        og[c] = dict(ogF=ogF, ogF_all=ogF_all, ogH=S.dram("ogH%d" % c, [512, c * 64], BF16), tpc=tpc,
                     ogF_g=ogF_g, ogH_g=S.dram("ogH%d_g" % c, [2048, c * 64], BF16),
                     src=[Buf(ogF_all.t[q * tpc * 128:(q + 1) * tpc * 128, :], "ogsrc%d_%d" % (c, q)) for q in range(nq)],
                     dst=[Buf(ogF_g.t[q * 4 * tpc * 128:(q + 1) * 4 * tpc * 128, :], "ogdst%d_%d" % (c, q)) for q in range(nq)])
    lay = []
    for l in range(nl):
        KO = 2048 if l % 2 == 0 else 1024
        KC = KO // 128
        d = dict(nwT=ext("nwT_l%d" % l, [128, 32]), cw=ext("cw_l%d" % l, [128, 44 * 3]), cb=ext("cb_l%d" % l, [128, 44]),
                 wout_d=ext("wout_l%d" % l, [8, 128, KC * 128]), wup_d=ext("wup_l%d" % l, [NG, 128, 8 * 256]),
                 wdn_d=ext("wdn_l%d" % l, [8, 128, NG * 128]),
                 wout_b=S.dram("wout_b%d" % l, [8, 128, KC * 128], BF16), wup_b=S.dram("wup_b%d" % l, [NG, 128, 8 * 256], BF16),
                 wdn_b=S.dram("wdn_b%d" % l, [8, 128, NG * 128], BF16))
        if l % 2 == 0:
            d["m"] = dict(w=ext("gw_l%d" % l, [128, 8 * G_WC]), cw=ext("gcw_l%d" % l, [128, 32]), hv=ext("ghv_l%d" % l, [1, 8]),
                          gnw=ext("ggnw_l%d" % l, [128, 1]), cst=cst_g)
        else:
            d["m"] = dict(w=ext("mw_l%d" % l, [128, 8 * ML_WC]), gb=ext("mgb_l%d" % l, [1, 2]), nwv=ext("mnwv_l%d" % l, [1, ML_DV]), cst=cst_m)
        lay.append(d)

    def after_ain(ti):
        if ti == 0:
            S.coll("AllGather", ainH_g, ainH, GROUPS)
        else:
            S.coll("AllGather", ainF_gs[ti - 1], ainF[ti - 1], GROUPS)

    def mk_after_og(c):
        o = og[c]
        tpc = o["tpc"]

        def after_og(ti):
            wt = ti - 1
            if ti >= 1 and (wt + 1) % tpc == 0:
                q = wt // tpc
                src = o["src"][q]
                S.coll("AllGather", o["dst"][q], src, GROUPS, extra=[o["ogF"][k] for k in range(q * tpc, (q + 1) * tpc)])
            if ti == 12:
                S.coll("AllGather", o["ogH_g"], o["ogH"], GROUPS)
        return after_og

    emit_T(S, "t0", 2048, True, False, dict(hs_src=hs0, nwT=nwT0, ainF=ainF, ainH=ainH, after_ain=after_ain))
    for l in range(nl):
        d = lay[l]
        c = 4 if l % 2 == 0 else 2
        emit_casts(S, d)
        mio = dict(d["m"], ainF_g=ainF_g, ainH_g=ainH_g, ogF=og[c]["ogF"], ogH=og[c]["ogH"], after_og=mk_after_og(c))
        if l % 2 == 0:
            emit_M_gdn(S, "g%d" % l, mio)
        else:
            emit_M_ml(S, "m%d" % l, mio)
        last = l == nl - 1
        tio = dict(d, hs_src=(hs0 if l == 0 else hs_loc), hs_dst=(hs_out if last else hs_loc), ogF_g=og[c]["ogF_g"], ogH_g=og[c]["ogH_g"],
                   keep=keep, gidx=(gidx4 if c == 4 else gidx2), ainF=ainF, ainH=ainH, after_ain=after_ain)
        emit_T(S, "t%d" % (l + 1), 512 * c, False, last, tio)
    S.finish([hs_out])
    return nc, es


def kernel(x, meta_tokens, norm_w, gdn_w_in, gdn_conv_w, gdn_a_log, gdn_dt_bias, gdn_norm_w, gdn_w_out,
           ml_w_in, ml_gate_b, ml_norm_w, ml_w_out, ffn_w_up, ffn_conv_w, ffn_conv_b, ffn_w_down, _nl=4):
    inp = dict(x=x, meta_tokens=meta_tokens, norm_w=norm_w, gdn_w_in=gdn_w_in, gdn_conv_w=gdn_conv_w, gdn_a_log=gdn_a_log,
               gdn_dt_bias=gdn_dt_bias, gdn_norm_w=gdn_norm_w, gdn_w_out=gdn_w_out, ml_w_in=ml_w_in, ml_gate_b=ml_gate_b,
               ml_norm_w=ml_norm_w, ml_w_out=ml_w_out, ffn_w_up=ffn_w_up, ffn_conv_w=ffn_conv_w, ffn_conv_b=ffn_conv_b,
               ffn_w_down=ffn_w_down)
    inp = {k: np.asarray(v, np.float32) for k, v in inp.items()}
    shared = {"cst_g": gdn_consts(), "cst_m": ml_consts(),
              "nwT_0": np.ascontiguousarray(np.stack([_cm(inp["norm_w"][0, 0])] * 4, 1).reshape(128, 32), np.float32)}
    for l in range(_nl):
        t = prep_T(inp, l)
        for k, v in t.items():
            shared["%s_l%d" % (k, l)] = v
    maps = []
    for c in range(8):
        b, r = c // 4, c % 4
        m = dict(shared)
        h = np.zeros((LP, D), np.float32)
        h[XPAD + 48:XPAD + 64] = inp["meta_tokens"]
        h[XPAD + 64:] = inp["x"][b]
        lo = XPAD + 2048 * r
        m["hs0"] = np.ascontiguousarray(h[lo:lo + WIN].T)
        k = np.ones((1, WIN), np.float32)
        if r == 0:
            k[0, :48] = 0.0
        m["keep"] = k
        p = np.arange(128)
        for cc in (4, 2):
            tpc = 8 // cc
            gi = np.zeros((128, 20), np.int32)
            for hg in range(4):
                gi[:, hg * 5] = hg * 512 + r * 128 + p
                for i in range(1, 5):
                    wt = 4 * r + i - 1
                    gi[:, hg * 5 + i] = (wt // tpc) * (4 * tpc * 128) + hg * (tpc * 128) + (wt % tpc) * 128 + p
            m["gidx%d" % cc] = gi
        for l in range(_nl):
            if l % 2 == 0:
                g = prep_M_gdn(inp, l // 2, r)
                m["gw_l%d" % l], m["gcw_l%d" % l], m["ghv_l%d" % l], m["ggnw_l%d" % l] = g["w"], g["cw"], g["hv"], g["gnw"]
            else:
                g = prep_M_ml(inp, l // 2, r)
                m["mw_l%d" % l], m["mgb_l%d" % l], m["mnwv_l%d" % l] = g["w"], g["gb"], g["nwv"]
        maps.append(m)
    if _nl == 1:
        shared.pop("cst_m")
        for m in maps:
            m.pop("cst_m", None)
            m.pop("gidx2", None)
    nc, es = build_fused(_nl)
    res = run_bass_kernel_spmd(nc, maps, core_ids=list(range(8))).results
    out = np.zeros((NB, SEQ, D), np.float32)
    for c in range(8):
        b, r = c // 4, c % 4
        out[b, 2048 * r:2048 * (r + 1)] = res[c]["hs_out"][:, 64:].T
    return out
```

```python
from contextlib import ExitStack
import numpy as np
import ml_dtypes
import concourse.bass as bass
import concourse.mybir as mybir
from concourse.bass_utils import run_bass_kernel_spmd

F32 = mybir.dt.float32
BF16 = mybir.dt.bfloat16
AF = mybir.ActivationFunctionType
ALU = mybir.AluOpType
AX = mybir.AxisListType

D = 1024
SEQ = 8192
NB = 2
LP = 8704
XPAD = LP - SEQ - 64
WIN = 64 + 2048
FFN = 2816
NG = FFN // 128
RMS_EPS = 1e-6
CH = 64
NCH = LP // CH
TS = 512
NTS = LP // TS
CPT = TS // CH


class Buf:
    __slots__ = ("t", "lw", "rd", "sem", "semv", "name")

    def __init__(self, t, name=""):
        self.t = t
        self.lw = None
        self.rd = {}
        self.sem = None
        self.semv = 0
        self.name = name

    def __getitem__(self, k):
        return self.t[k]


class Sched:
    def __init__(self, nc, es):
        self.nc = nc
        self.es = es
        self.eng = {"pe": nc.tensor, "act": nc.scalar, "dve": nc.vector, "pool": nc.gpsimd, "sp": nc.sync}
        self.sem = {k: es.enter_context(nc.semaphore("sem_" + k)) for k in self.eng}
        self.cnt = {k: 0 for k in self.eng}
        self.seen = {k: {} for k in self.eng}
        self.nsem = 0
        self.out_events = []
        self.ninst = 0
        self.scopes = []
        self.dsems = []
        self.free_dsems = []
        self.scope_bufs = []

    def push(self, tag):
        self.scopes.append((ExitStack(), tag))
        self.scope_bufs.append([])

    def _own_sem(self, own):
        if own.sem is None:
            if self.free_dsems:
                own.sem, own.semv = self.free_dsems.pop()
            else:
                own.sem = self.es.enter_context(self.nc.semaphore("dsem%d" % self.nsem))
                self.nsem += 1
            self.dsems.append(own)

    def pop(self):
        self.barrier()
        st, _ = self.scopes.pop()
        st.close()
        for b in self.scope_bufs.pop():
            if b.sem is not None:
                self.free_dsems.append((b.sem, b.semv))
                self.dsems.remove(b)
                b.sem = None

    def _scope(self):
        return self.scopes[-1] if self.scopes else (self.es, "g")

    def sb(self, name, shape, dt):
        st, tag = self._scope()
        name = tag + "_" + name
        b = Buf(st.enter_context(self.nc.sbuf_tensor(name, list(shape), dt)), name)
        if self.scope_bufs:
            self.scope_bufs[-1].append(b)
        return b

    def ps(self, name, shape, dt=F32):
        st, tag = self._scope()
        name = tag + "_" + name
        return Buf(st.enter_context(self.nc.psum_tensor(name, list(shape), dt)), name)

    def barrier(self):
        for e in self.eng:
            eng = self.eng[e]
            for k in ("pe", "act", "dve", "pool", "sp"):
                if k != e and self.cnt[k] and self.seen[e].get(k, 0) < self.cnt[k]:
                    eng.wait_ge(self.sem[k], self.cnt[k])
                    self.seen[e][k] = self.cnt[k]
            for b in self.dsems:
                key = "d_" + b.name
                if self.seen[e].get(key, 0) < b.semv:
                    eng.wait_ge(b.sem, b.semv)
                    self.seen[e][key] = b.semv

    def dram(self, name, shape, dt, kind="Internal"):
        t = self.nc.dram_tensor(name, list(shape), dt, kind=kind)
        return Buf(t.ap(), name)

    def _deps(self, reads, writes):
        deps = {}

        def add(ev):
            if ev is None:
                return
            sem, val, key = ev
            if key not in deps or deps[key][1] < val:
                deps[key] = (sem, val)

        for b in reads:
            add(b.lw)
        for b in writes:
            add(b.lw)
            for ev in b.rd.values():
                add(ev)
        return deps

    def _wait(self, e, deps):
        eng = self.eng[e]
        for key, (sem, val) in deps.items():
            if e == "pe" and key == "pe":
                continue
            if self.seen[e].get(key, 0) >= val:
                continue
            eng.wait_ge(sem, val)
            self.seen[e][key] = val

    def _record(self, ev, reads, writes):
        for b in writes:
            b.lw = ev
            b.rd = {}
        for b in reads:
            if b not in writes:
                b.rd[ev[2]] = ev

    def op(self, e, fn, reads=(), writes=()):
        self._wait(e, self._deps(reads, writes))
        ins = fn(self.eng[e])
        self.cnt[e] += 1
        ins.then_inc(self.sem[e], 1)
        self.ninst += 1
        self._record((self.sem[e], self.cnt[e], e), reads, writes)

    def dma(self, q, out, in_, reads=(), writes=(), owner=None):
        self._wait(q, self._deps(reads, writes))
        own = owner if owner is not None else (writes[0] if writes else reads[0])
        self._own_sem(own)
        own.semv += 16
        ins = self.eng[q].dma_start(out=out, in_=in_)
        ins.then_inc(own.sem, 16)
        self.ninst += 1
        ev = (own.sem, own.semv, "d_" + own.name)
        self._record(ev, reads, writes)
        return ev

    def gather(self, out_ap, table_ap, idx_ap, reads=(), writes=()):
        self._wait("pool", self._deps(reads, writes))
        own = writes[0]
        self._own_sem(own)
        own.semv += 16
        ins = self.nc.gpsimd.indirect_dma_start(out=out_ap, out_offset=None, in_=table_ap,
                                                in_offset=bass.IndirectOffsetOnAxis(ap=idx_ap, axis=0))
        ins.then_inc(own.sem, 16)
        self.ninst += 1
        self._record((own.sem, own.semv, "d_" + own.name), reads, writes)

    def coll(self, kind, out, in_, groups, extra=()):
        self._wait("pool", self._deps([in_] + list(extra), [out]))
        self._own_sem(out)
        out.semv += 1
        ins = self.nc.gpsimd.collective_compute(kind, ALU.bypass, replica_groups=groups, ins=[in_.t.opt()], outs=[out.t.opt()])
        ins.then_inc(out.sem, 1)
        self.ninst += 1
        self._record((out.sem, out.semv, "d_" + out.name), [in_], [out])

    def finish(self, bufs):
        for b in bufs:
            if b.sem is not None:
                self.eng["sp"].wait_ge(b.sem, b.semv)
        for k in ("pe", "act", "dve", "pool"):
            if self.cnt[k]:
                self.eng["sp"].wait_ge(self.sem[k], self.cnt[k])


class PsumPool:
    def __init__(self, S, n, prefix="pb"):
        self.banks = [S.ps("%s%d" % (prefix, i), [128, 512]) for i in range(n)]
        self.i = 0

    def get(self):
        b = self.banks[self.i % len(self.banks)]
        self.i += 1
        return b


def bcast_mid(ap2, n):
    return ap2.unsqueeze(1).broadcast_to([ap2.shape[0], n, ap2.shape[1]])


def bcast_last(ap2, n):
    return ap2.unsqueeze(2).broadcast_to([ap2.shape[0], ap2.shape[1], n])


def rstd_from_ss(S, ss_ps, out_sb, scale, eps, W):
    S.op("act", lambda e: e.activation(out=out_sb[:, :W], in_=ss_ps[:, :W], func=AF.Ln, scale=scale, bias=eps),
         reads=[ss_ps], writes=[out_sb])
    S.op("act", lambda e: e.activation(out=out_sb[:, :W], in_=out_sb[:, :W], func=AF.Exp, scale=-0.5),
         reads=[out_sb], writes=[out_sb])


T_TILES = [(0, 64), (64, 512), (576, 512), (1088, 512), (1600, 512)]


def emit_casts(S, io):
    for m in range(8):
        S.dma("pool", io["wout_b"][m], io["wout_d"][m], writes=[io["wout_b"]])
    for g in range(NG):
        S.dma("pool", io["wup_b"][g], io["wup_d"][g], writes=[io["wup_b"]])
    for m in range(8):
        S.dma("pool", io["wdn_b"][m], io["wdn_d"][m], writes=[io["wdn_b"]])


def emit_T(S, tag, KO, first, last, io):
    S.push(tag)
    KC = KO // 128
    c_og = KC // 4
    hs_in = io["hs_src"]
    nwT_d = io["nwT"]
    if not last:
        ainF, ainH = io["ainF"], io["ainH"]
    if not first:
        ogF_g, ogH_g = io["ogF_g"], io["ogH_g"]
        keep_d, cw_d, cb_d = io["keep"], io["cw"], io["cb"]
        wout_b, wup_b, wdn_b = io["wout_b"], io["wup_b"], io["wdn_b"]
        hs_out = io["hs_dst"]
        gidx = S.sb("gidx", [128, 20], mybir.dt.int32)
        S.dma("sp", gidx[:], io["gidx"][:, :], writes=[gidx])

    ones_f = S.sb("ones_f", [128, 128], F32)
    ones_b = S.sb("ones_b", [128, 128], BF16)
    S.op("pool", lambda e: e.memset(ones_f[:], 1.0), writes=[ones_f])
    S.op("act", lambda e: e.activation(out=ones_b[:], in_=ones_f[:], func=AF.Copy), reads=[ones_f], writes=[ones_b])
    nwT = S.sb("nwT_sb", [128, 4, 8], F32)
    S.dma("sp", nwT[:].rearrange("p a b -> p (a b)"), nwT_d[:, :], writes=[nwT])

    hs_sb = [S.sb("hs_sb%d" % i, [128, 8, 512], F32) for i in range(2)]
    sq_sb = [S.sb("sq_sb%d" % i, [128, 512], BF16) for i in range(2)]
    rstd = S.sb("rstd", [128, 512], F32)
    a_sb = S.sb("a_sb", [128, 8, 512], BF16)
    pp = PsumPool(S, 7)
    ss_ps = S.ps("ss_ps", [128, 512])
    if not first:
        og_sb = [S.sb("og_sb%d" % i, [128, KC, 512], BF16) for i in range(2)]
        ogh_sb = S.sb("ogh_sb", [128, KC, 64], BF16)
        keep_sb = S.sb("keep_sb", [128, WIN], F32)
        S.dma("sp", keep_sb[:], keep_d[0:1, :].partition_broadcast(128), writes=[keep_sb])
        cw = S.sb("cw_sb", [128, 44, 3], F32)
        cb = S.sb("cb_sb", [128, 44], F32)
        S.dma("sp", cw[:].rearrange("p a b -> p (a b)"), cw_d[:, :], writes=[cw])
        S.dma("sp", cb[:], cb_d[:, :], writes=[cb])
        mix_sb = S.sb("mix_sb", [128, 8, 512], F32)
        rk = S.sb("rk", [128, 512], F32)
        tmp_sb = [S.sb("tmp_sb%d" % i, [128, 512], F32) for i in range(2)]
        h_sb = S.sb("h_sb", [128, NG, 512], BF16)
        u_sb = [S.sb("u_sb%d" % i, [128, 2, 2 + 512], F32) for i in range(2)]
        y_sb = [S.sb("y_sb%d" % i, [128, 2, 512], F32) for i in range(2)]
        e_sb = [S.sb("e_sb%d" % i, [128, 512], F32) for i in range(2)]
        halo = S.sb("halo", [128, 44, 2], F32)
        S.op("pool", lambda e: e.memset(halo[:], 0.0), writes=[halo])
        wo_s = [S.sb("wo_s%d" % i, [128, KC, 128], BF16) for i in range(2)]
        wu_s = [S.sb("wu_s%d" % i, [128, 8, 256], BF16) for i in range(3)]
        wd_s = [S.sb("wd_s%d" % i, [128, NG, 128], BF16) for i in range(2)]

    def norm_ss(src, W, eng_sq="act"):
        for m in range(8):
            sq = sq_sb[m % 2]
            S.op(eng_sq, lambda e: e.activation(out=sq[:, :W], in_=src[:, m, :W], func=AF.Square),
                 reads=[src], writes=[sq])
            S.op("pe", lambda e: e.matmul(ss_ps[:, :W], ones_b[:], sq[:, :W], start=(m == 0), stop=(m == 7)),
                 reads=[ones_b, sq], writes=[ss_ps])

    def load_tile(i):
        t0, W = T_TILES[i]
        hb = hs_sb[i % 2]
        S.dma("sp", hb[:, :, :W], hs_in[:, t0:t0 + W].rearrange("(c p) t -> p c t", p=128), writes=[hb])
        if not first:
            ob = ogh_sb if i == 0 else og_sb[i % 2]
            tab = ogH_g if i == 0 else ogF_g
            for hg in range(4):
                S.gather(ob[:, hg * c_og:(hg + 1) * c_og, :].rearrange("p c w -> p (c w)"), tab[:, :],
                         gidx[:, hg * 5 + i:hg * 5 + i + 1], reads=[gidx], writes=[ob])

    load_tile(0)
    for ti, (t0, W) in enumerate(T_TILES):
        if ti + 1 < len(T_TILES):
            load_tile(ti + 1)
        hb = hs_sb[ti % 2]
        if not first:
            ob = ogh_sb if ti == 0 else og_sb[ti % 2]
            S.dma("sp", wo_s[0][:].rearrange("p a b -> p (a b)"), wout_b[0], reads=[wout_b], writes=[wo_s[0]])
            for m in range(8):
                if m + 1 < 8:
                    S.dma("sp", wo_s[(m + 1) % 2][:].rearrange("p a b -> p (a b)"), wout_b[m + 1],
                          reads=[wout_b], writes=[wo_s[(m + 1) % 2]])
                ws = wo_s[m % 2]
                pb = pp.get()
                for kc in range(KC):
                    S.op("pe", lambda e: e.matmul(pb[:, :W], ws[:, kc, :], ob[:, kc, :W], start=(kc == 0), stop=(kc == KC - 1)),
                         reads=[ws, ob], writes=[pb])
                S.op("act", lambda e: e.activation(out=mix_sb[:, m, :W], in_=pb[:, :W], func=AF.Copy), reads=[pb], writes=[mix_sb])
                sq = sq_sb[m % 2]
                S.op("act", lambda e: e.activation(out=sq[:, :W], in_=pb[:, :W], func=AF.Square), reads=[pb], writes=[sq])
                S.op("pe", lambda e: e.matmul(ss_ps[:, :W], ones_b[:], sq[:, :W], start=(m == 0), stop=(m == 7)),
                     reads=[ones_b, sq], writes=[ss_ps])
            rstd_from_ss(S, ss_ps, rstd, 1.0 / D, RMS_EPS, W)
            S.op("dve", lambda e: e.tensor_tensor(out=rk[:, :W], in0=rstd[:, :W], in1=keep_sb[:, t0:t0 + W], op=ALU.mult),
                 reads=[rstd, keep_sb], writes=[rk])
            for m in range(8):
                tb = tmp_sb[m % 2]
                S.op("dve", lambda e: e.tensor_tensor(out=tb[:, :W], in0=mix_sb[:, m, :W], in1=rk[:, :W], op=ALU.mult),
                     reads=[mix_sb, rk], writes=[tb])
                S.op("dve", lambda e: e.scalar_tensor_tensor(out=hb[:, m, :W], in0=tb[:, :W], scalar=nwT[:, 1, m:m + 1],
                                                             in1=hb[:, m, :W], op0=ALU.mult, op1=ALU.add),
                     reads=[tb, nwT, hb], writes=[hb])
            norm_ss(hb, W)
            rstd_from_ss(S, ss_ps, rstd, 1.0 / D, RMS_EPS, W)
            for m in range(8):
                S.op("dve", lambda e: e.scalar_tensor_tensor(out=a_sb[:, m, :W], in0=hb[:, m, :W], scalar=nwT[:, 2, m:m + 1],
                                                             in1=rstd[:, :W], op0=ALU.mult, op1=ALU.mult),
                     reads=[hb, nwT, rstd], writes=[a_sb])
            S.dma("sp", wu_s[0][:].rearrange("p a b -> p (a b)"), wup_b[0], reads=[wup_b], writes=[wu_s[0]])
            S.dma("sp", wu_s[1][:].rearrange("p a b -> p (a b)"), wup_b[1], reads=[wup_b], writes=[wu_s[1]])
            for g in range(NG):
                if g + 2 < NG:
                    S.dma("sp", wu_s[(g + 2) % 3][:].rearrange("p a b -> p (a b)"), wup_b[g + 2],
                          reads=[wup_b], writes=[wu_s[(g + 2) % 3]])
                ws = wu_s[g % 3]
                ub = u_sb[g % 2]
                yb = y_sb[g % 2]
                eb = e_sb[g % 2]
                for hf in range(2):
                    ci = g + hf * NG
                    pb = pp.get()
                    for kc in range(8):
                        S.op("pe", lambda e: e.matmul(pb[:, :W], ws[:, kc, hf * 128:(hf + 1) * 128], a_sb[:, kc, :W],
                                                      start=(kc == 0), stop=(kc == 7)),
                             reads=[ws, a_sb], writes=[pb])
                    S.op("pool", lambda e: e.tensor_copy(out=ub[:, hf, 0:2], in_=halo[:, ci, :]), reads=[halo], writes=[ub])
                    S.op("act", lambda e: e.activation(out=ub[:, hf, 2:2 + W], in_=pb[:, :W], func=AF.Copy), reads=[pb], writes=[ub])
                    S.op("pool", lambda e: e.tensor_copy(out=halo[:, ci, :], in_=ub[:, hf, W:W + 2]), reads=[ub], writes=[halo])
                    S.op("act", lambda e: e.activation(out=yb[:, hf, :W], in_=pb[:, :W], func=AF.Identity,
                                                       scale=cw[:, ci, 2:3], bias=cb[:, ci:ci + 1]),
                         reads=[pb, cw, cb], writes=[yb])
                    S.op("dve", lambda e: e.scalar_tensor_tensor(out=yb[:, hf, :W], in0=ub[:, hf, 1:1 + W], scalar=cw[:, ci, 1:2],
                                                                 in1=yb[:, hf, :W], op0=ALU.mult, op1=ALU.add),
                         reads=[ub, cw, yb], writes=[yb])
                    S.op("dve", lambda e: e.scalar_tensor_tensor(out=yb[:, hf, :W], in0=ub[:, hf, 0:W], scalar=cw[:, ci, 0:1],
                                                                 in1=yb[:, hf, :W], op0=ALU.mult, op1=ALU.add),
                         reads=[ub, cw, yb], writes=[yb])
                S.op("act", lambda e: e.activation(out=eb[:, :W], in_=yb[:, 0, :W], func=AF.Silu), reads=[yb], writes=[eb])
                S.op("dve", lambda e: e.tensor_tensor(out=h_sb[:, g, :W], in0=yb[:, 1, :W], in1=eb[:, :W], op=ALU.mult),
                     reads=[yb, eb], writes=[h_sb])
            S.dma("sp", wd_s[0][:].rearrange("p a b -> p (a b)"), wdn_b[0], reads=[wdn_b], writes=[wd_s[0]])
            for m in range(8):
                if m + 1 < 8:
                    S.dma("sp", wd_s[(m + 1) % 2][:].rearrange("p a b -> p (a b)"), wdn_b[m + 1],
                          reads=[wdn_b], writes=[wd_s[(m + 1) % 2]])
                ws = wd_s[m % 2]
                pb = pp.get()
                for kc in range(NG):
                    S.op("pe", lambda e: e.matmul(pb[:, :W], ws[:, kc, :], h_sb[:, kc, :W], start=(kc == 0), stop=(kc == NG - 1)),
                         reads=[ws, h_sb], writes=[pb])
                S.op("act", lambda e: e.activation(out=mix_sb[:, m, :W], in_=pb[:, :W], func=AF.Copy), reads=[pb], writes=[mix_sb])
                sq = sq_sb[m % 2]
                S.op("act", lambda e: e.activation(out=sq[:, :W], in_=pb[:, :W], func=AF.Square), reads=[pb], writes=[sq])
                S.op("pe", lambda e: e.matmul(ss_ps[:, :W], ones_b[:], sq[:, :W], start=(m == 0), stop=(m == 7)),
                     reads=[ones_b, sq], writes=[ss_ps])
            rstd_from_ss(S, ss_ps, rstd, 1.0 / D, RMS_EPS, W)
            S.op("dve", lambda e: e.tensor_tensor(out=rk[:, :W], in0=rstd[:, :W], in1=keep_sb[:, t0:t0 + W], op=ALU.mult),
                 reads=[rstd, keep_sb], writes=[rk])
            for m in range(8):
                tb = tmp_sb[m % 2]
                S.op("dve", lambda e: e.tensor_tensor(out=tb[:, :W], in0=mix_sb[:, m, :W], in1=rk[:, :W], op=ALU.mult),
                     reads=[mix_sb, rk], writes=[tb])
                S.op("dve", lambda e: e.scalar_tensor_tensor(out=hb[:, m, :W], in0=tb[:, :W], scalar=nwT[:, 3, m:m + 1],
                                                             in1=hb[:, m, :W], op0=ALU.mult, op1=ALU.add),
                     reads=[tb, nwT, hb], writes=[hb])
            S.dma("pool", hs_out[:, t0:t0 + W].rearrange("(c p) t -> p c t", p=128), hb[:, :, :W], reads=[hb], owner=hs_out)
        if not last:
            norm_ss(hb, W)
            rstd_from_ss(S, ss_ps, rstd, 1.0 / D, RMS_EPS, W)
            for m in range(8):
                S.op("dve", lambda e: e.scalar_tensor_tensor(out=a_sb[:, m, :W], in0=hb[:, m, :W], scalar=nwT[:, 0, m:m + 1],
                                                             in1=rstd[:, :W], op0=ALU.mult, op1=ALU.mult),
                     reads=[hb, nwT, rstd], writes=[a_sb])
            if ti == 0:
                S.dma("pool", ainH[:, :].rearrange("p (c t) -> p c t", c=8), a_sb[:, :, :W], reads=[a_sb], writes=[ainH])
            else:
                S.dma("pool", ainF[ti - 1][:, :].rearrange("p (c t) -> p c t", c=8), a_sb[:, :, :W], reads=[a_sb], writes=[ainF[ti - 1]])
            io["after_ain"](ti)
    S.pop()


def _cm(v):
    return np.ascontiguousarray(v.reshape(-1, 128).T)


def prep_T(inp, layer):
    j = layer // 2
    if layer % 2 == 0:
        w_out = inp["gdn_w_out"][j]
    else:
        w_out = inp["ml_w_out"][j]
    KO = w_out.shape[0]
    KC = KO // 128
    nw = inp["norm_w"]
    nxt = nw[layer + 1, 0] if layer + 1 < 4 else nw[layer, 0]
    nwT = np.stack([_cm(nxt), _cm(nw[layer, 1]), _cm(nw[layer, 2]), _cm(nw[layer, 3])], 1)
    wout = w_out.reshape(KC, 128, 8, 128).transpose(2, 1, 0, 3)
    wu = inp["ffn_w_up"][layer].reshape(8, 128, 2, NG, 128).transpose(3, 1, 0, 2, 4)
    wd = inp["ffn_w_down"][layer].reshape(NG, 128, 8, 128).transpose(2, 1, 0, 3)
    cw = inp["ffn_conv_w"][layer].reshape(3, 44, 128).transpose(2, 1, 0)
    cb = _cm(inp["ffn_conv_b"][layer])
    return {
        "nwT": np.ascontiguousarray(nwT.reshape(128, 32), np.float32),
        "wout": np.ascontiguousarray(wout.reshape(8, 128, KC * 128), np.float32),
        "wup": np.ascontiguousarray(wu.reshape(NG, 128, 8 * 256), np.float32),
        "wdn": np.ascontiguousarray(wd.reshape(8, 128, NG * 128), np.float32),
        "cw": np.ascontiguousarray(cw.reshape(128, 44 * 3), np.float32),
        "cb": np.ascontiguousarray(cb, np.float32),
    }


ML_DK = 128
ML_DV = 256
ML_WC = 128 + 128 + 128 + 256 + 256 + 2
BIGNEG = 30000.0


def emit_M_ml(S, tag, io):
    S.push(tag)
    ainF_g, ainH_g = io["ainF_g"], io["ainH_g"]
    w_d, gb_d, nwv_d, cst_d = io["w"], io["gb"], io["nwv"], io["cst"]
    ogF, ogH = io["ogF"], io["ogH"]
    c_og = 2

    w_sb = S.sb("w_sb", [128, 8, ML_WC], BF16)
    S.dma("pool", w_sb[:].rearrange("p a b -> p (a b)"), w_d[:, :], writes=[w_sb])
    wg_sb = S.sb("wg_sb", [128, 8, 2], BF16)
    cst = S.sb("cst_sb", [128, 64 + 512 + 128], F32)
    S.dma("sp", cst[:], cst_d[:, :], writes=[cst])
    identf = cst
    ident_b = S.sb("ident_b", [128, 128], BF16)
    S.op("act", lambda e: e.activation(out=ident_b[:], in_=cst[:, 576:704], func=AF.Copy), reads=[cst], writes=[ident_b])
    gb = S.sb("gb_sb", [1, 2], F32)
    S.dma("sp", gb[:], gb_d[:, :], writes=[gb])
    nwv = S.sb("nwv_sb", [64, ML_DV], F32)
    S.dma("sp", nwv[:], nwv_d[0:1, :].partition_broadcast(64), writes=[nwv])
    ones_row = S.sb("ones_row", [1, 128], F32)
    S.op("pool", lambda e: e.memset(ones_row[:], 1.0), writes=[ones_row])

    a_sb = [S.sb("a_sb%d" % i, [128, 8, 512], BF16) for i in range(2)]
    pp = PsumPool(S, 5)
    pia2 = [S.ps("pia%d" % i, [128, 512]) for i in range(2)]

    li_row = S.sb("li_row", [1, LP], F32)
    lf_row = S.sb("lf_row", [1, LP], F32)
    bb_row = S.sb("bb_row", [1, LP], F32)
    ones_bc = ones_row[0:1, 0:1].broadcast_to([1, LP])

    def load_a(i):
        ab = a_sb[i % 2]
        if i == 0:
            S.op("pool", lambda e: e.memset(ab[:, :, 0:XPAD], 0.0), writes=[ab])
            S.dma("sp", ab[:, :, XPAD:TS], ainH_g[0:128, :].rearrange("p (c t) -> p c t", c=8), reads=[ainH_g], writes=[ab])
        else:
            r0 = ((i - 1) % 4) * 512 + ((i - 1) // 4) * 128
            S.dma("sp", ab[:].rearrange("p c t -> p (c t)"), ainF_g[r0:r0 + 128, :], reads=[ainF_g], writes=[ab])

    load_a(0)
    for ti in range(NTS):
        if ti + 1 < NTS:
            load_a(ti + 1)
        ab = a_sb[ti % 2]
        for gi_, row in ((0, li_row), (1, lf_row)):
            pr = pp.get()
            for kc in range(8):
                S.op("pe", lambda e: e.matmul(pr[0:1, :], w_sb[:, kc, ML_WC - 2 + gi_:ML_WC - 1 + gi_], ab[:, kc, :],
                                              start=(kc == 0), stop=(kc == 7)), reads=[w_sb, ab], writes=[pr])
            S.op("act", lambda e: e.activation(out=row[:, ti * TS:(ti + 1) * TS], in_=pr[0:1, :], func=AF.Identity,
                                               bias=gb[:, gi_:gi_ + 1], scale=1.0), reads=[pr, gb], writes=[row])
    for row in (li_row, lf_row):
        S.op("act", lambda e: e.activation(out=row[:], in_=row[:], func=AF.Exp, scale=2.0 / 15.0), reads=[row], writes=[row])
        S.op("dve", lambda e: e.tensor_scalar_add(out=row[:], in0=row[:], scalar1=1.0), reads=[row], writes=[row])
        S.op("dve", lambda e: e.reciprocal(out=row[:], in_=row[:]), reads=[row], writes=[row])
        S.op("dve", lambda e: e.tensor_scalar(out=row[:], in0=row[:], scalar1=-30.0, scalar2=15.0, op0=ALU.mult, op1=ALU.add),
             reads=[row], writes=[row])
    S.op("act", lambda e: e.activation(out=lf_row[:], in_=lf_row[:], func=AF.Exp, scale=-1.0), reads=[lf_row], writes=[lf_row])
    S.op("act", lambda e: e.activation(out=lf_row[:], in_=lf_row[:], func=AF.Ln, bias=1.0, scale=1.0), reads=[lf_row], writes=[lf_row])
    S.op("dve", lambda e: e.tensor_scalar_mul(out=lf_row[:], in0=lf_row[:], scalar1=-1.0), reads=[lf_row], writes=[lf_row])
    S.op("pool", lambda e: e.memset(lf_row[:, 0:XPAD], 0.0), writes=[lf_row])
    S.op("pool", lambda e: e.memset(li_row[:, 0:XPAD], -BIGNEG), writes=[li_row])
    S.op("dve", lambda e: e.tensor_tensor_scan(out=bb_row[:], data0=ones_bc, data1=lf_row[:], initial=0.0,
                                               op0=ALU.mult, op1=ALU.add), reads=[ones_row, lf_row], writes=[bb_row])
    c_row = lf_row
    S.op("dve", lambda e: e.tensor_tensor(out=c_row[:], in0=li_row[:], in1=bb_row[:], op=ALU.subtract), reads=[li_row, bb_row], writes=[c_row])
    M_row = li_row
    S.op("dve", lambda e: e.tensor_tensor_scan(out=M_row[:], data0=ones_bc, data1=c_row[:], initial=0.0,
                                               op0=ALU.mult, op1=ALU.max), reads=[ones_row, c_row], writes=[M_row])
    en_row = bb_row
    S.op("dve", lambda e: e.tensor_tensor(out=en_row[:], in0=bb_row[:], in1=M_row[:], op=ALU.add), reads=[bb_row, M_row], writes=[en_row])
    S.op("act", lambda e: e.activation(out=en_row[:], in_=en_row[:], func=AF.Exp, scale=-1.0), reads=[en_row], writes=[en_row])

    c_tm = S.sb("c_tm", [64, NCH], F32)
    M_tm = S.sb("M_tm", [64, NCH], F32)
    en_tm = S.sb("en_tm", [64, NCH], F32)
    for row, tmb in ((c_row, c_tm), (M_row, M_tm), (en_row, en_tm)):
        pb = pp.get()
        for n in range(NCH):
            S.op("pe", lambda e: e.matmul(pb[0:64, n:n + 1], row[0:1, n * CH:(n + 1) * CH], ones_row[0:1, 0:1], start=True, stop=True),
                 reads=[row, ones_row], writes=[pb])
        S.op("act", lambda e: e.activation(out=tmb[:], in_=pb[0:64, 0:NCH], func=AF.Copy), reads=[pb], writes=[tmb])
    Mend_b = S.sb("Mend_b", [128, NCH], F32)
    Mprev_b = S.sb("Mprev_b", [128, NCH], F32)
    pb = pp.get()
    S.op("pe", lambda e: e.matmul(pb[:, 0:NCH], ones_row[0:1, :], M_row[0:1, CH - 1::CH], start=True, stop=True),
         reads=[ones_row, M_row], writes=[pb])
    S.op("act", lambda e: e.activation(out=Mend_b[:], in_=pb[:, 0:NCH], func=AF.Copy), reads=[pb], writes=[Mend_b])
    S.op("pool", lambda e: e.memset(Mprev_b[:, 0:1], 0.0), writes=[Mprev_b])
    S.op("pool", lambda e: e.tensor_copy(out=Mprev_b[:, 1:NCH], in_=Mend_b[:, 0:NCH - 1]), reads=[Mend_b], writes=[Mprev_b])
    sc_tm = S.sb("sc_tm", [64, NCH], F32)
    kws_tm = S.sb("kws_tm", [64, NCH], F32)
    dec_b = S.sb("dec_b", [128, NCH], F32)
    S.op("dve", lambda e: e.tensor_tensor(out=sc_tm[:], in0=Mprev_b[0:64, :], in1=M_tm[:], op=ALU.subtract), reads=[Mprev_b, M_tm], writes=[sc_tm])
    S.op("act", lambda e: e.activation(out=sc_tm[:], in_=sc_tm[:], func=AF.Exp), reads=[sc_tm], writes=[sc_tm])
    S.op("dve", lambda e: e.tensor_tensor(out=kws_tm[:], in0=c_tm[:], in1=Mend_b[0:64, :], op=ALU.subtract), reads=[c_tm, Mend_b], writes=[kws_tm])
    S.op("act", lambda e: e.activation(out=kws_tm[:], in_=kws_tm[:], func=AF.Exp), reads=[kws_tm], writes=[kws_tm])
    S.op("dve", lambda e: e.tensor_tensor(out=dec_b[:], in0=Mprev_b[:], in1=Mend_b[:], op=ALU.subtract), reads=[Mprev_b, Mend_b], writes=[dec_b])
    S.op("act", lambda e: e.activation(out=dec_b[:], in_=dec_b[:], func=AF.Exp), reads=[dec_b], writes=[dec_b])

    qT = [S.sb("qT%d" % i, [128, 512], BF16) for i in range(2)]
    kT = [S.sb("kT%d" % i, [128, 512], BF16) for i in range(2)]
    Wt = [S.sb("Wt%d" % i, [64, 512], F32) for i in range(2)]
    ogT = [S.sb("ogT%d" % i, [128, 2, 512], BF16) for i in range(2)]
    C_f = S.sb("C_f", [128, ML_DV + 1], F32)
    C_b = S.sb("C_b", [128, ML_DV + 1], BF16)
    S.op("pool", lambda e: e.memset(C_f[:], 0.0), writes=[C_f])
    S.op("pool", lambda e: e.memset(C_b[:], 0.0), writes=[C_b])
    NR = 3
    kw_sb = [S.sb("kw_sb%d" % i, [64, 128], BF16) for i in range(NR)]
    va_sb = [S.sb("va_sb%d" % i, [64, ML_DV + 1], BF16) for i in range(NR)]
    og_sb = [S.sb("ogs%d" % i, [64, ML_DV], F32) for i in range(NR)]
    St_sb = [S.sb("St%d" % i, [64, 64], BF16) for i in range(NR)]
    t1_sb = [S.sb("t1_%d" % i, [64, ML_DV + 1], F32) for i in range(NR)]
    sm_sb = [S.sb("sm%d" % i, [64, 8], F32) for i in range(NR)]
    junk = [S.sb("junk%d" % i, [64, ML_DV], F32) for i in range(NR)]
    hn_sb = [S.sb("hn%d" % i, [64, ML_DV], BF16) for i in range(NR)]
    for i in range(NR):
        S.op("pool", lambda e: e.memset(va_sb[i][:, ML_DV:ML_DV + 1], 1.0), writes=[va_sb[i]])
    pTall = S.ps("pTall", [128, 1024], BF16)

    def tile_level(ti):
        if ti + 1 < NTS:
            load_a(ti + 1)
        ab = a_sb[ti % 2]
        t0 = ti * TS
        qTb, kTb, Wtb, ogTb = qT[ti % 2], kT[ti % 2], Wt[ti % 2], ogT[ti % 2]
        for wi, (dst, scl) in enumerate(((qTb, ML_DK ** -0.5), (kTb, 1.0))):
            pb = pp.get()
            for kc in range(8):
                S.op("pe", lambda e: e.matmul(pb[:, :], w_sb[:, kc, wi * 128:(wi + 1) * 128], ab[:, kc, :], start=(kc == 0), stop=(kc == 7)),
                     reads=[w_sb, ab], writes=[pb])
            S.op("act", lambda e: e.activation(out=dst[:], in_=pb[:, :], func=AF.Copy, scale=scl), reads=[pb], writes=[dst])
        pb = pp.get()
        S.op("pe", lambda e: e.matmul(pb[0:64, :], ones_row[0:1, 0:64], M_row[0:1, t0:t0 + TS], start=True, stop=False),
             reads=[ones_row, M_row], writes=[pb])
        S.op("pe", lambda e: e.matmul(pb[0:64, :], cst[0:64, 0:64], cst[0:64, 64:576], start=False, stop=True),
             reads=[cst], writes=[pb])
        S.op("dve", lambda e: e.tensor_tensor(out=Wtb[:].rearrange("p (n i) -> p n i", n=CPT), in0=pb[0:64, :].rearrange("p (n i) -> p n i", n=CPT),
                                              in1=bcast_last(c_tm[:, ti * CPT:(ti + 1) * CPT], CH), op=ALU.subtract),
             reads=[pb, c_tm], writes=[Wtb])
        S.op("act", lambda e: e.activation(out=Wtb[:], in_=Wtb[:], func=AF.Exp, scale=-1.0), reads=[Wtb], writes=[Wtb])

    def stageA(n):
        ti, cj = n // CPT, n % CPT
        ab = a_sb[ti % 2]
        qTb, kTb, Wtb, ogTb = qT[ti % 2], kT[ti % 2], Wt[ti % 2], ogT[ti % 2]
        r = n % NR
        c0 = cj * CH
        pkv = pp.get()
        for kc in range(8):
            S.op("pe", lambda e: e.matmul(pkv[0:64, 0:384], ab[:, kc, c0:c0 + CH], w_sb[:, kc, 256:640], start=(kc == 0), stop=(kc == 7)),
                 reads=[w_sb, ab], writes=[pkv])
        pog = pp.get()
        for kc in range(8):
            S.op("pe", lambda e: e.matmul(pog[0:64, 0:256], ab[:, kc, c0:c0 + CH], w_sb[:, kc, 640:896], start=(kc == 0), stop=(kc == 7)),
                 reads=[w_sb, ab], writes=[pog])
        S.op("act", lambda e: e.activation(out=kw_sb[r][:], in_=pkv[0:64, 0:128], func=AF.Copy, scale=kws_tm[:, n:n + 1]),
             reads=[pkv, kws_tm], writes=[kw_sb[r]])
        S.op("act", lambda e: e.activation(out=va_sb[r][:, 0:ML_DV], in_=pkv[0:64, 128:384], func=AF.Copy), reads=[pkv], writes=[va_sb[r]])
        S.op("act", lambda e: e.activation(out=og_sb[r][:], in_=pog[0:64, 0:256], func=AF.Exp, scale=-1.0), reads=[pog], writes=[og_sb[r]])
        yield
        S.op("dve", lambda e: e.tensor_scalar_add(out=og_sb[r][:], in0=og_sb[r][:], scalar1=1.0), reads=[og_sb[r]], writes=[og_sb[r]])
        S.op("dve", lambda e: e.reciprocal(out=og_sb[r][:], in_=og_sb[r][:]), reads=[og_sb[r]], writes=[og_sb[r]])
        S.op("dve", lambda e: e.tensor_tensor(out=og_sb[r][:], in0=og_sb[r][:], in1=nwv[:], op=ALU.mult), reads=[og_sb[r], nwv], writes=[og_sb[r]])
        pq = pp.get()
        S.op("pe", lambda e: e.matmul(pq[0:64, 0:64], kTb[:, c0:c0 + CH], qTb[:, c0:c0 + CH], start=True, stop=True),
             reads=[kTb, qTb], writes=[pq])
        S.op("dve", lambda e: e.tensor_tensor(out=St_sb[r][:], in0=pq[0:64, 0:64], in1=Wtb[:, c0:c0 + CH], op=ALU.mult),
             reads=[pq, Wtb], writes=[St_sb[r]])
        yield
        pia = pia2[n % 2]
        S.op("pe", lambda e: e.matmul(pia[0:64, 0:ML_DV + 1], St_sb[r][:], va_sb[r][:], start=True, stop=True),
             reads=[St_sb[r], va_sb[r]], writes=[pia])
        yield

    def stageB(n):
        ti, cj = n // CPT, n % CPT
        ab = a_sb[ti % 2]
        qTb, kTb, Wtb, ogTb = qT[ti % 2], kT[ti % 2], Wt[ti % 2], ogT[ti % 2]
        r = n % NR
        c0 = cj * CH
        pia = pia2[n % 2]
        pint = pp.get()
        S.op("pe", lambda e: e.matmul(pint[0:64, 0:ML_DV + 1], qTb[:, c0:c0 + CH], C_b[:], start=True, stop=True),
             reads=[qTb, C_b], writes=[pint])
        pst = pp.get()
        S.op("pe", lambda e: e.matmul(pst[:, 0:ML_DV + 1], kw_sb[r][:], va_sb[r][:], start=True, stop=True),
             reads=[kw_sb[r], va_sb[r]], writes=[pst])
        S.op("dve", lambda e: e.scalar_tensor_tensor(out=C_f[:], in0=C_f[:], scalar=dec_b[:, n:n + 1], in1=pst[:, 0:ML_DV + 1],
                                                     op0=ALU.mult, op1=ALU.add), reads=[C_f, dec_b, pst], writes=[C_f])
        S.op("act", lambda e: e.activation(out=C_b[:], in_=C_f[:], func=AF.Copy), reads=[C_f], writes=[C_b])
        t1 = t1_sb[r]
        S.op("act", lambda e: e.activation(out=t1[:], in_=pint[0:64, 0:ML_DV + 1], func=AF.Copy, scale=sc_tm[:, n:n + 1]),
             reads=[pint, sc_tm], writes=[t1])
        S.op("dve", lambda e: e.tensor_tensor(out=t1[:], in0=t1[:], in1=pia[0:64, 0:ML_DV + 1], op=ALU.add), reads=[t1, pia], writes=[t1])
        yield
        sm = sm_sb[r]
        S.op("act", lambda e: e.activation(out=sm[:, 0:1], in_=t1[:, ML_DV:ML_DV + 1], func=AF.Abs), reads=[t1], writes=[sm])
        S.op("dve", lambda e: e.tensor_tensor(out=sm[:, 0:1], in0=sm[:, 0:1], in1=en_tm[:, n:n + 1], op=ALU.max),
             reads=[sm, en_tm], writes=[sm])
        S.op("dve", lambda e: e.reciprocal(out=sm[:, 1:2], in_=sm[:, 0:1]), reads=[sm], writes=[sm])
        S.op("act", lambda e: e.activation(out=junk[r][:], in_=t1[:, 0:ML_DV], func=AF.Square, scale=sm[:, 1:2], accum_out=sm[:, 2:3]),
             reads=[t1, sm], writes=[junk[r], sm])
        S.op("act", lambda e: e.activation(out=sm[:, 3:4], in_=sm[:, 2:3], func=AF.Ln, scale=1.0 / ML_DV, bias=RMS_EPS), reads=[sm], writes=[sm])
        S.op("act", lambda e: e.activation(out=sm[:, 3:4], in_=sm[:, 3:4], func=AF.Exp, scale=-0.5), reads=[sm], writes=[sm])
        S.op("dve", lambda e: e.tensor_tensor(out=sm[:, 4:5], in0=sm[:, 3:4], in1=sm[:, 1:2], op=ALU.mult), reads=[sm], writes=[sm])
        S.op("dve", lambda e: e.scalar_tensor_tensor(out=hn_sb[r][:], in0=t1[:, 0:ML_DV], scalar=sm[:, 4:5], in1=og_sb[r][:],
                                                     op0=ALU.mult, op1=ALU.mult), reads=[t1, sm, og_sb[r]], writes=[hn_sb[r]])
        yield
        ptv = pTall[:, (n % 4) * 128:(n % 4 + 1) * 128].rearrange("p (a b) -> p a b", a=2)
        for hh in range(2):
            S.op("pe", lambda e: e.transpose(ptv[:, hh, :], hn_sb[r][:, hh * 128:(hh + 1) * 128], ident_b[0:64, 0:64]),
                 reads=[hn_sb[r], ident_b], writes=[pTall])
        S.op("act", lambda e: e.activation(out=ogTb[:, :, c0:c0 + CH], in_=ptv, func=AF.Copy), reads=[pTall], writes=[ogTb])
        yield

    def tile_finish(ti):
        ogTb = ogT[ti % 2]
        if ti >= 1:
            S.dma("pool", ogF[ti - 1][:, :].rearrange("p (c t) -> p c t", c=c_og), ogTb[:], reads=[ogTb], writes=[ogF[ti - 1]])
        if ti % 4 == 0 and ti <= 12:
            S.dma("pool", ogH[(ti // 4) * 128:(ti // 4 + 1) * 128, :].rearrange("p (c t) -> p c t", c=c_og), ogTb[:, :, TS - 64:TS],
                  reads=[ogTb], writes=[ogH])
        io["after_og"](ti)

    def interleave(gens):
        gens = [g for g in gens if g is not None]
        while gens:
            for g in list(gens):
                try:
                    next(g)
                except StopIteration:
                    gens.remove(g)

    load_a(0)
    tile_level(0)
    interleave([stageA(0)])
    NCK = NTS * CPT
    for n in range(NCK):
        nxt = None
        if n + 1 < NCK:
            if (n + 1) % CPT == 0:
                tile_level((n + 1) // CPT)
            nxt = stageA(n + 1)
        interleave([stageB(n), nxt])
        if (n + 1) % CPT == 0:
            tile_finish(n // CPT)
    S.pop()


def prep_M_ml(inp, j, h):
    w = inp["ml_w_in"][j]
    q = w[:, h * 128:(h + 1) * 128]
    k = w[:, 512 + h * 128:512 + (h + 1) * 128]
    v = w[:, 1024 + h * 256:1024 + (h + 1) * 256]
    o = w[:, 2048 + h * 256:2048 + (h + 1) * 256]
    gi = w[:, 3072 + h:3073 + h]
    gf = w[:, 3076 + h:3077 + h]
    wc = np.concatenate([q, k, k, v, o, gi, gf], 1)
    wc = wc.reshape(8, 128, ML_WC).transpose(1, 0, 2)
    gb = inp["ml_gate_b"][j][[h, 4 + h]].reshape(1, 2)
    nwv = inp["ml_norm_w"][j][h * 256:(h + 1) * 256].reshape(1, 256)
    return {"w": np.ascontiguousarray(wc.reshape(128, 8 * ML_WC), np.float32),
            "gb": np.ascontiguousarray(gb, np.float32), "nwv": np.ascontiguousarray(nwv, np.float32),
            "cst": ml_consts()}


def ml_consts():
    c = np.zeros((128, 64 + 512 + 128), np.float32)
    c[0:64, 0:64] = np.eye(64, dtype=np.float32)
    jj = np.arange(64)[:, None]
    ii = np.arange(64)[None, :]
    mb = (jj > ii).astype(np.float32) * BIGNEG
    c[0:64, 64:576] = np.tile(mb, (1, 8))
    c[:, 576:704] = np.eye(128, dtype=np.float32)
    return c


G_WC = 12 * 128 + 8
L2_EPS = 1e-6
GDN_EPS = 1e-6
C_I64, C_MUI, C_MUS, C_MLS, C_NMLS, C_NMUS, C_I128, C_ONES = 0, 64, 128, 192, 256, 320, 384, 512
C_TOT = 640


def gdn_consts():
    c = np.zeros((128, C_TOT), np.float32)
    p = np.arange(64)[:, None]
    f = np.arange(64)[None, :]
    c[0:64, C_I64:C_I64 + 64] = (p == f)
    c[0:64, C_MUI:C_MUI + 64] = (p <= f)
    c[0:64, C_MUS:C_MUS + 64] = (p < f)
    c[0:64, C_MLS:C_MLS + 64] = (p > f)
    c[0:64, C_NMLS:C_NMLS + 64] = -1.0 * (p > f)
    c[0:64, C_NMUS:C_NMUS + 64] = -1.0 * (p < f)
    c[:, C_I128:C_I128 + 128] = np.eye(128, dtype=np.float32)
    c[:, C_ONES:C_ONES + 128] = 1.0
    return c


def emit_M_gdn(S, tag, io):
    S.push(tag)
    ainF_g, ainH_g = io["ainF_g"], io["ainH_g"]
    w_d, cw_d, hv_d, gnw_d, cst_d = io["w"], io["cw"], io["hv"], io["gnw"], io["cst"]
    ogF, ogH = io["ogF"], io["ogH"]
    c_og = 4

    w_sb = S.sb("w_sb", [128, 8, G_WC], BF16)
    S.dma("pool", w_sb[:].rearrange("p a b -> p (a b)"), w_d[:, :], writes=[w_sb])
    cst = S.sb("cst_sb", [128, C_TOT], F32)
    S.dma("sp", cst[:], cst_d[:, :], writes=[cst])
    cwg = S.sb("cwg", [128, 8, 4], F32)
    S.dma("sp", cwg[:].rearrange("p a b -> p (a b)"), cw_d[:, :], writes=[cwg])
    hv = S.sb("hv_sb", [64, 8], F32)
    S.dma("sp", hv[:], hv_d[0:1, :].partition_broadcast(64), writes=[hv])
    gnw = S.sb("gnw_sb", [128, 1], F32)
    S.dma("sp", gnw[:], gnw_d[:, :], writes=[gnw])
    ident_b = S.sb("ident_b", [128, 128], BF16)
    ones_b = S.sb("ones_b", [128, 128], BF16)
    S.op("act", lambda e: e.activation(out=ident_b[:], in_=cst[:, C_I128:C_I128 + 128], func=AF.Copy), reads=[cst], writes=[ident_b])
    S.op("act", lambda e: e.activation(out=ones_b[:], in_=cst[:, C_ONES:C_ONES + 128], func=AF.Copy), reads=[cst], writes=[ones_b])
    dg = S.sb("dg", [128, 32, 128], BF16)
    for mc in range(8):
        for k in range(4):
            S.op("dve", lambda e: e.tensor_scalar(out=dg[:, mc * 4 + k, :], in0=cst[:, C_I128:C_I128 + 128], scalar1=cwg[:, mc, k:k + 1],
                                                  scalar2=None, op0=ALU.mult), reads=[cst, cwg], writes=[dg])
    I64 = cst[0:64, C_I64:C_I64 + 64]
    MUI = cst[0:64, C_MUI:C_MUI + 64]
    MLS = cst[0:64, C_MLS:C_MLS + 64]
    NMLS = cst[0:64, C_NMLS:C_NMLS + 64]
    NMUS = cst[0:64, C_NMUS:C_NMUS + 64]
    ONES64x128 = cst[0:64, C_ONES:C_ONES + 128]

    a_sb = [S.sb("a_sb%d" % i, [128, 8, 512], BF16) for i in range(2)]
    pp = PsumPool(S, 4)
    po2 = [S.ps("po%d" % i, [128, 512]) for i in range(2)]
    pTall = S.ps("pTall", [128, 1024], BF16)
    pTon = S.ps("pTon", [128, 1024], BF16)

    def load_a(i):
        ab = a_sb[i % 2]
        if i == 0:
            S.op("pool", lambda e: e.memset(ab[:, :, 0:XPAD], 0.0), writes=[ab])
            S.dma("sp", ab[:, :, XPAD:TS], ainH_g[0:128, :].rearrange("p (c t) -> p c t", c=8), reads=[ainH_g], writes=[ab])
        else:
            r0 = ((i - 1) % 4) * 512 + ((i - 1) // 4) * 128
            S.dma("sp", ab[:].rearrange("p c t -> p (c t)"), ainF_g[r0:r0 + 128, :], reads=[ainF_g], writes=[ab])

    NH = NCH * 4
    bg = S.sb("bg", [64, NCH, 8], F32)
    load_a(0)
    for ti in range(NTS):
        if ti + 1 < NTS:
            load_a(ti + 1)
        ab = a_sb[ti % 2]
        pb = pp.get()
        for cj in range(CPT):
            for kc in range(8):
                S.op("pe", lambda e: e.matmul(pb[0:64, cj * 8:(cj + 1) * 8], ab[:, kc, cj * CH:(cj + 1) * CH], w_sb[:, kc, 1536:1544],
                                              start=(kc == 0), stop=(kc == 7)), reads=[ab, w_sb], writes=[pb])
        S.op("act", lambda e: e.activation(out=bg[:, ti * CPT:(ti + 1) * CPT, :], in_=pb[0:64, 0:64].rearrange("p (a b) -> p a b", a=CPT),
                                           func=AF.Copy), reads=[pb], writes=[bg])
    lnb = S.sb("lnb", [64, NCH, 4], F32)
    bt = S.sb("bt", [64, NCH, 4], F32)
    gt = S.sb("gt", [64, NCH, 4], F32)
    beG = S.sb("beG", [64, NCH, 4], F32)
    ekt = S.sb("ekt", [64, NCH, 4], F32)
    eGl = S.sb("eGl", [128, NCH, 4], F32)
    tmpg = S.sb("tmpg", [64, NCH, 4], F32)
    eal = S.sb("eal", [64, 4], F32)
    S.op("act", lambda e: e.activation(out=lnb[:], in_=bg[:, :, 0:4], func=AF.Exp, scale=-1.0), reads=[bg], writes=[lnb])
    S.op("act", lambda e: e.activation(out=lnb[:], in_=lnb[:], func=AF.Ln, bias=1.0, scale=1.0), reads=[lnb], writes=[lnb])
    S.op("dve", lambda e: e.tensor_scalar_mul(out=lnb[:], in0=lnb[:], scalar1=-1.0), reads=[lnb], writes=[lnb])
    S.op("act", lambda e: e.activation(out=bt[:], in_=lnb[:], func=AF.Exp), reads=[lnb], writes=[bt])
    S.op("dve", lambda e: e.tensor_tensor(out=gt[:], in0=bg[:, :, 4:8], in1=bcast_mid(hv[:, 4:8], NCH), op=ALU.add), reads=[bg, hv], writes=[gt])
    S.op("act", lambda e: e.activation(out=gt[:], in_=gt[:], func=AF.Exp), reads=[gt], writes=[gt])
    S.op("act", lambda e: e.activation(out=gt[:], in_=gt[:], func=AF.Ln, bias=1.0, scale=1.0), reads=[gt], writes=[gt])
    S.op("act", lambda e: e.activation(out=eal[:], in_=hv[:, 0:4], func=AF.Exp), reads=[hv], writes=[eal])
    S.op("dve", lambda e: e.tensor_scalar_mul(out=eal[:], in0=eal[:], scalar1=-1.0), reads=[eal], writes=[eal])
    S.op("dve", lambda e: e.tensor_tensor(out=gt[:], in0=gt[:], in1=bcast_mid(eal[:], NCH), op=ALU.mult), reads=[gt, eal], writes=[gt])
    gflat = gt[:].rearrange("p a b -> p (a b)")
    for (c0, c1) in ((0, 272), (272, NH)):
        pb = pp.get()
        S.op("pe", lambda e: e.matmul(pb[0:64, 0:c1 - c0], MUI, gflat[:, c0:c1], start=True, stop=True), reads=[cst, gt], writes=[pb])
        pl = pp.get()
        S.op("pe", lambda e: e.matmul(pl[:, 0:c1 - c0], ONES64x128, gflat[:, c0:c1], start=True, stop=True), reads=[cst, gt], writes=[pl])
        S.op("act", lambda e: e.activation(out=tmpg[:].rearrange("p a b -> p (a b)")[:, c0:c1], in_=pb[0:64, 0:c1 - c0], func=AF.Exp),
             reads=[pb], writes=[tmpg])
        S.op("act", lambda e: e.activation(out=eGl[:].rearrange("p a b -> p (a b)")[:, c0:c1], in_=pl[:, 0:c1 - c0], func=AF.Exp),
             reads=[pl], writes=[eGl])
        S.op("act", lambda e: e.activation(out=ekt[:].rearrange("p a b -> p (a b)")[:, c0:c1], in_=pb[0:64, 0:c1 - c0], func=AF.Copy),
             reads=[pb], writes=[ekt])
        S.op("dve", lambda e: e.tensor_tensor(out=ekt[:].rearrange("p a b -> p (a b)")[:, c0:c1], in0=pl[0:64, 0:c1 - c0],
                                              in1=ekt[:].rearrange("p a b -> p (a b)")[:, c0:c1], op=ALU.subtract), reads=[pl, ekt], writes=[ekt])
    S.op("act", lambda e: e.activation(out=ekt[:], in_=ekt[:], func=AF.Exp), reads=[ekt], writes=[ekt])
    S.op("dve", lambda e: e.tensor_tensor(out=beG[:], in0=bt[:], in1=tmpg[:], op=ALU.mult), reads=[bt, tmpg], writes=[beG])

    xb = [S.sb("xb%d" % i, [128, 8, 3 + TS], BF16) for i in range(2)]
    S.op("pool", lambda e: e.memset(xb[1][:, :, TS:TS + 3], 0.0), writes=[xb[1]])
    sx = [S.sb("sx%d" % i, [128, TS], F32) for i in range(2)]
    sqb = [S.sb("sqb%d" % i, [128, TS], BF16) for i in range(2)]
    rs = [S.sb("rs%d" % i, [128, TS], F32) for i in range(2)]
    qT = [S.sb("qT%d" % i, [128, 2, TS], BF16) for i in range(2)]
    kT = [S.sb("kT%d" % i, [128, 2, TS], BF16) for i in range(2)]
    svT = [S.sb("svT%d" % i, [128, 4, TS], BF16) for i in range(2)]
    zs = [S.sb("zs%d" % i, [128, 4, TS], F32) for i in range(2)]
    onT = [S.sb("onT%d" % i, [128, 4, TS], BF16) for i in range(2)]
    ogt = [S.sb("ogt0", [128, 4, TS], BF16)] * 2
    S_f = S.sb("S_f", [128, 4, 128], F32)
    S_b = S.sb("S_b", [128, 4, 128], BF16)
    S_t = S.sb("S_t", [128, 4, 128], F32)
    S.op("pool", lambda e: e.memset(S_f[:], 0.0), writes=[S_f])
    S.op("pool", lambda e: e.memset(S_b[:], 0.0), writes=[S_b])
    NR = 2
    mk = lambda nm, shp, dt: [S.sb("%s%d" % (nm, i), shp, dt) for i in range(NR)]
    kbg = mk("kbg", [64, 4, 128], BF16)
    ktm = mk("ktm", [64, 4, 128], BF16)
    vb = mk("vb", [64, 4, 128], BF16)
    rg1 = mk("rg1", [64, 4, 64], F32)
    rg2 = mk("rg2", [64, 4, 64], F32)
    rg3 = mk("rg3", [64, 4, 64], F32)
    Et = mk("Et", [64, 4, 64], F32)
    Wt_ = mk("Wt", [64, 4, 64], F32)
    W_ = mk("W", [64, 4, 64], F32)
    eGb = mk("eGb", [128, 4, 64], F32)
    KKlo = mk("KKlo", [64, 2, 64], F32)
    KKup = mk("KKup", [64, 2, 64], F32)
    KQm = mk("KQm", [64, 2, 64], F32)
    Qt = mk("Qt", [64, 4, 64], BF16)
    qdT = mk("qdT", [128, 4, 64], BF16)
    PP = [mk("PP%d" % k, [64, 8, 64], BF16) for k in range(2)]
    Xt = [mk("Xt%d" % k, [64, 4, 64], BF16) for k in range(2)]
    Tt = mk("Tt", [64, 4, 64], BF16)
    nwT = mk("nwT", [128, 4, 64], BF16)
    vn = mk("vn", [64, 4, 128], BF16)
    sqo = mk("sqo", [64, 4, 128], F32)
    sso = mk("sso", [64, 8], F32)
    on = mk("on", [64, 4, 128], BF16)

    def silu_from_psum(pb, W, out_ap, out_buf, idx):
        S.op("act", lambda e: e.activation(out=out_ap, in_=pb[:, :W], func=AF.Silu), reads=[pb], writes=[out_buf])

    def tile_level(ti):
        if ti + 1 < NTS:
            load_a(ti + 1)
        ab = a_sb[ti % 2]
        t0 = ti * TS
        xcur, xprev = xb[ti % 2], xb[(ti + 1) % 2]
        qTb, kTb, svb, zsb, onTb, ogb = qT[ti % 2], kT[ti % 2], svT[ti % 2], zs[ti % 2], onT[ti % 2], ogt[ti % 2]
        S.op("pool", lambda e: e.tensor_copy(out=xcur[:, :, 0:3], in_=xprev[:, :, TS:TS + 3]), reads=[xprev], writes=[xcur])
        for mc in range(12):
            pb = pp.get()
            for kc in range(8):
                S.op("pe", lambda e: e.matmul(pb[:, :], w_sb[:, kc, mc * 128:(mc + 1) * 128], ab[:, kc, :], start=(kc == 0), stop=(kc == 7)),
                     reads=[w_sb, ab], writes=[pb])
            if mc < 8:
                S.op("act", lambda e: e.activation(out=xcur[:, mc, 3:3 + TS], in_=pb[:, :], func=AF.Copy), reads=[pb], writes=[xcur])
            else:
                silu_from_psum(pb, TS, zsb[:, mc - 8, :], zsb, mc)
        for mc in range(8):
            pb = pp.get()
            for k in range(4):
                S.op("pe", lambda e: e.matmul(pb[:, :], dg[:, mc * 4 + k, :], xcur[:, mc, k:k + TS], start=(k == 0), stop=(k == 3)),
                     reads=[dg, xcur], writes=[pb])
            if mc >= 4:
                silu_from_psum(pb, TS, svb[:, mc - 4, :], svb, mc)
            else:
                sxb, sq, rsb = sx[mc % 2], sqb[mc % 2], rs[mc % 2]
                silu_from_psum(pb, TS, sxb[:, :], sxb, mc)
                S.op("act", lambda e: e.activation(out=sq[:, :], in_=sxb[:, :], func=AF.Square), reads=[sxb], writes=[sq])
                ps2 = pp.get()
                S.op("pe", lambda e: e.matmul(ps2[:, :], ones_b[:], sq[:, :], start=True, stop=True), reads=[ones_b, sq], writes=[ps2])
                rstd_from_ss(S, ps2, rsb, 1.0, L2_EPS, TS)
                dst = qTb if mc < 2 else kTb
                scl = (128.0 ** -0.5) if mc < 2 else 1.0
                S.op("dve", lambda e: e.scalar_tensor_tensor(out=dst[:, mc % 2, :], in0=sxb[:, :], scalar=scl, in1=rsb[:, :],
                                                             op0=ALU.mult, op1=ALU.mult), reads=[sxb, rsb], writes=[dst])

    def stageA(n):
        ti, cj = n // CPT, n % CPT
        qTb, kTb, svb, onTb = qT[ti % 2], kT[ti % 2], svT[ti % 2], onT[ti % 2]
        r = n % NR
        c0 = cj * CH
        for qh in range(2):
            S.op("pe", lambda e: e.transpose(pTall[0:64, qh * 128:(qh + 1) * 128], kTb[:, qh, c0:c0 + CH], ident_b[:, :]),
                 reads=[kTb, ident_b], writes=[pTall])
        for h in range(4):
            S.op("pe", lambda e: e.transpose(pTall[0:64, 256 + h * 128:256 + (h + 1) * 128], svb[:, h, c0:c0 + CH], ident_b[:, :]),
                 reads=[svb, ident_b], writes=[pTall])
        ktm_ps = pTall[0:64, 0:256].rearrange("p (a b) -> p a b", a=2)
        ktm_rep = ktm_ps.unsqueeze(2).broadcast_to([64, 2, 2, 128])
        as4 = lambda ap: ap.rearrange("p (a r) d -> p a r d", r=2)
        S.op("dve", lambda e: e.tensor_tensor(out=as4(kbg[r][:]), in0=ktm_rep, in1=as4(bcast_last(beG[:, n, :], 128)), op=ALU.mult),
             reads=[pTall, beG], writes=[kbg[r]])
        S.op("dve", lambda e: e.tensor_tensor(out=as4(ktm[r][:]), in0=ktm_rep, in1=as4(bcast_last(ekt[:, n, :], 128)), op=ALU.mult),
             reads=[pTall, ekt], writes=[ktm[r]])
        S.op("dve", lambda e: e.tensor_tensor(out=vb[r][:], in0=pTall[0:64, 256:768].rearrange("p (a b) -> p a b", a=4),
                                              in1=bcast_last(bt[:, n, :], 128), op=ALU.mult), reads=[pTall, bt], writes=[vb[r]])
        yield
        S.op("dve", lambda e: e.tensor_tensor(out=rg1[r][:], in0=bcast_mid(MUI, 4), in1=bcast_last(gt[:, n, :], 64), op=ALU.mult),
             reads=[cst, gt], writes=[rg1[r]])
        S.op("dve", lambda e: e.tensor_tensor(out=rg2[r][:], in0=bcast_mid(I64, 4), in1=bcast_last(lnb[:, n, :], 64), op=ALU.mult),
             reads=[cst, lnb], writes=[rg2[r]])
        S.op("dve", lambda e: e.tensor_tensor(out=rg2[r][:], in0=rg2[r][:], in1=rg1[r][:], op=ALU.add), reads=[rg1[r], rg2[r]], writes=[rg2[r]])
        S.op("dve", lambda e: e.tensor_tensor(out=rg3[r][:], in0=bcast_mid(MLS, 4), in1=bcast_last(gt[:, n, :], 64), op=ALU.mult),
             reads=[cst, gt], writes=[rg3[r]])
        fl = lambda b_: b_[:].rearrange("p a b -> p (a b)")
        pd1 = pp.get()
        S.op("pe", lambda e: e.matmul(pd1[0:64, 0:256], MLS, fl(rg1[r]), start=True, stop=True), reads=[cst, rg1[r]], writes=[pd1])
        S.op("pe", lambda e: e.matmul(pd1[0:64, 256:512], MLS, fl(rg2[r]), start=True, stop=True), reads=[cst, rg2[r]], writes=[pd1])
        pd2 = pp.get()
        S.op("pe", lambda e: e.matmul(pd2[0:64, 0:256], MUI, fl(rg3[r]), start=True, stop=True), reads=[cst, rg3[r]], writes=[pd2])
        pd3 = pp.get()
        S.op("pe", lambda e: e.matmul(pd3[:, 0:256], ONES64x128, fl(rg1[r]), start=True, stop=True), reads=[cst, rg1[r]], writes=[pd3])
        S.op("act", lambda e: e.activation(out=fl(Et[r]), in_=pd1[0:64, 0:256], func=AF.Exp), reads=[pd1], writes=[Et[r]])
        S.op("act", lambda e: e.activation(out=fl(Wt_[r]), in_=pd1[0:64, 256:512], func=AF.Exp), reads=[pd1], writes=[Wt_[r]])
        for h in range(4):
            S.op("act", lambda e: e.activation(out=W_[r][:, h, :], in_=pd2[0:64, h * 64:(h + 1) * 64], func=AF.Exp,
                                               bias=lnb[:, n, h:h + 1], scale=1.0), reads=[pd2, lnb], writes=[W_[r]])
        S.op("act", lambda e: e.activation(out=fl(eGb[r]), in_=pd3[:, 0:256], func=AF.Exp), reads=[pd3], writes=[eGb[r]])
        yield
        pg = pp.get()
        for qh in range(2):
            S.op("pe", lambda e: e.matmul(pg[0:64, qh * 64:(qh + 1) * 64], kTb[:, qh, c0:c0 + CH], kTb[:, qh, c0:c0 + CH], start=True, stop=True),
                 reads=[kTb], writes=[pg])
            S.op("pe", lambda e: e.matmul(pg[0:64, 128 + qh * 64:128 + (qh + 1) * 64], kTb[:, qh, c0:c0 + CH], qTb[:, qh, c0:c0 + CH],
                                          start=True, stop=True), reads=[kTb, qTb], writes=[pg])
        kkv = pg[0:64, 0:128].rearrange("p (a b) -> p a b", a=2)
        kqv = pg[0:64, 128:256].rearrange("p (a b) -> p a b", a=2)
        S.op("dve", lambda e: e.tensor_tensor(out=KKlo[r][:], in0=kkv, in1=bcast_mid(NMLS, 2), op=ALU.mult), reads=[pg, cst], writes=[KKlo[r]])
        S.op("dve", lambda e: e.tensor_tensor(out=KKup[r][:], in0=kkv, in1=bcast_mid(NMUS, 2), op=ALU.mult), reads=[pg, cst], writes=[KKup[r]])
        S.op("dve", lambda e: e.tensor_tensor(out=KQm[r][:], in0=kqv, in1=bcast_mid(MUI, 2), op=ALU.mult), reads=[pg, cst], writes=[KQm[r]])
        yield
        rep = lambda b_: b_[:].unsqueeze(2).broadcast_to([64, 2, 2, 64])
        P0 = PP[0][r]
        S.op("dve", lambda e: e.tensor_tensor(out=as4(P0[:, 0:4, :]), in0=rep(KKlo[r]), in1=as4(W_[r][:]), op=ALU.mult),
             reads=[KKlo[r], W_[r]], writes=[P0])
        S.op("dve", lambda e: e.tensor_tensor(out=as4(P0[:, 4:8, :]), in0=rep(KKup[r]), in1=as4(Wt_[r][:]), op=ALU.mult),
             reads=[KKup[r], Wt_[r]], writes=[P0])
        S.op("dve", lambda e: e.tensor_tensor(out=as4(Qt[r][:]), in0=rep(KQm[r]), in1=as4(Et[r][:]), op=ALU.mult),
             reads=[KQm[r], Et[r]], writes=[Qt[r]])
        S.op("dve", lambda e: e.tensor_tensor(out=as4(qdT[r][:]), in0=qTb[:, :, c0:c0 + CH].unsqueeze(2).broadcast_to([128, 2, 2, 64]),
                                              in1=as4(eGb[r][:]), op=ALU.mult), reads=[qTb, eGb[r]], writes=[qdT[r]])
        yield
        X = Xt[0][r]
        S.op("dve", lambda e: e.tensor_tensor(out=X[:], in0=P0[:, 4:8, :], in1=bcast_mid(I64, 4), op=ALU.add), reads=[P0, cst], writes=[X])
        for k in range(1, 6):
            Pp, Pn = PP[(k - 1) % 2][r], PP[k % 2][r]
            pq = pp.get()
            for h in range(4):
                S.op("pe", lambda e: e.matmul(pq[0:64, h * 64:(h + 1) * 64], Pp[:, 4 + h, :], Pp[:, h, :], start=True, stop=True),
                     reads=[Pp], writes=[pq])
            if k < 5:
                for h in range(4):
                    S.op("pe", lambda e: e.matmul(pq[0:64, 256 + h * 64:256 + (h + 1) * 64], Pp[:, h, :], Pp[:, 4 + h, :], start=True, stop=True),
                         reads=[Pp], writes=[pq])
            wdt = 512 if k < 5 else 256
            S.op("act", lambda e: e.activation(out=Pn[:].rearrange("p a b -> p (a b)")[:, 0:wdt], in_=pq[0:64, 0:wdt], func=AF.Copy),
                 reads=[pq], writes=[Pn])
            yield
            px = pp.get()
            Xo = Xt[(k - 1) % 2][r]
            Xn = Xt[k % 2][r]
            for h in range(4):
                S.op("pe", lambda e: e.matmul(px[0:64, h * 64:(h + 1) * 64], Pn[:, h, :], Xo[:, h, :], start=True, stop=True),
                     reads=[Pn, Xo], writes=[px])
            yield
            if k < 5:
                S.op("dve", lambda e: e.tensor_tensor(out=fl(Xn), in0=px[0:64, 0:256], in1=fl(Xo), op=ALU.add), reads=[px, Xo], writes=[Xn])
            else:
                S.op("dve", lambda e: e.tensor_tensor(out=fl(Tt[r]), in0=px[0:64, 0:256], in1=fl(Xo), op=ALU.add), reads=[px, Xo], writes=[Tt[r]])
        yield
        pw = pp.get()
        for h in range(4):
            S.op("pe", lambda e: e.matmul(pw[:, h * 64:(h + 1) * 64], kbg[r][:, h, :], Tt[r][:, h, :], start=True, stop=True),
                 reads=[kbg[r], Tt[r]], writes=[pw])
        S.op("act", lambda e: e.activation(out=fl(nwT[r]), in_=pw[:, 0:256], func=AF.Copy, scale=-1.0), reads=[pw], writes=[nwT[r]])
        yield

    def stageB(n):
        ti, cj = n // CPT, n % CPT
        qTb, kTb, svb, onTb = qT[ti % 2], kT[ti % 2], svT[ti % 2], onT[ti % 2]
        r = n % NR
        c0 = cj * CH
        pu = pp.get()
        for h in range(4):
            S.op("pe", lambda e: e.matmul(pu[0:64, h * 128:(h + 1) * 128], Tt[r][:, h, :], vb[r][:, h, :], start=True, stop=False),
                 reads=[Tt[r], vb[r]], writes=[pu])
            S.op("pe", lambda e: e.matmul(pu[0:64, h * 128:(h + 1) * 128], nwT[r][:, h, :], S_b[:, h, :], start=False, stop=True),
                 reads=[nwT[r], S_b], writes=[pu])
        S.op("act", lambda e: e.activation(out=vn[r][:].rearrange("p a b -> p (a b)"), in_=pu[0:64, :], func=AF.Copy), reads=[pu], writes=[vn[r]])
        yield
        po = po2[n % 2]
        for h in range(4):
            S.op("pe", lambda e: e.matmul(po[0:64, h * 128:(h + 1) * 128], qdT[r][:, h, :], S_b[:, h, :], start=True, stop=False),
                 reads=[qdT[r], S_b], writes=[po])
            S.op("pe", lambda e: e.matmul(po[0:64, h * 128:(h + 1) * 128], Qt[r][:, h, :], vn[r][:, h, :], start=False, stop=True),
                 reads=[Qt[r], vn[r]], writes=[po])
        yield
        pS = pp.get()
        for h in range(4):
            S.op("pe", lambda e: e.matmul(pS[:, h * 128:(h + 1) * 128], ktm[r][:, h, :], vn[r][:, h, :], start=True, stop=True),
                 reads=[ktm[r], vn[r]], writes=[pS])
        for h in range(4):
            S.op("dve", lambda e: e.scalar_tensor_tensor(out=S_f[:, h, :], in0=S_f[:, h, :], scalar=eGl[:, n, h:h + 1],
                                                         in1=pS[:, h * 128:(h + 1) * 128], op0=ALU.mult, op1=ALU.add),
                 reads=[S_f, eGl, pS], writes=[S_f])
        S.op("act", lambda e: e.activation(out=S_b[:], in_=S_f[:], func=AF.Copy), reads=[S_f], writes=[S_b])
        yield
        S.op("act", lambda e: e.activation(out=sqo[r][:].rearrange("p a b -> p (a b)"), in_=po[0:64, :], func=AF.Square), reads=[po], writes=[sqo[r]])
        S.op("dve", lambda e: e.tensor_reduce(out=sso[r][:, 0:4], in_=sqo[r][:], axis=AX.X, op=ALU.add), reads=[sqo[r]], writes=[sso[r]])
        S.op("act", lambda e: e.activation(out=sso[r][:, 4:8], in_=sso[r][:, 0:4], func=AF.Ln, scale=1.0 / 128.0, bias=GDN_EPS),
             reads=[sso[r]], writes=[sso[r]])
        S.op("act", lambda e: e.activation(out=sso[r][:, 4:8], in_=sso[r][:, 4:8], func=AF.Exp, scale=-0.5), reads=[sso[r]], writes=[sso[r]])
        S.op("dve", lambda e: e.tensor_tensor(out=on[r][:], in0=po[0:64, :].rearrange("p (a b) -> p a b", a=4),
                                              in1=bcast_last(sso[r][:, 4:8], 128), op=ALU.mult), reads=[po, sso[r]], writes=[on[r]])
        yield
        for h in range(4):
            S.op("pe", lambda e: e.transpose(pTon[:, h * 64:(h + 1) * 64], on[r][:, h, :], ident_b[0:64, 0:64]),
                 reads=[on[r], ident_b], writes=[pTon])
        S.op("act", lambda e: e.activation(out=onTb[:, :, c0:c0 + CH], in_=pTon[:, 0:256].rearrange("p (a b) -> p a b", a=4), func=AF.Copy),
             reads=[pTon], writes=[onTb])
        yield

    def tile_finish(ti):
        zsb, onTb, ogb = zs[ti % 2], onT[ti % 2], ogt[ti % 2]
        S.op("dve", lambda e: e.scalar_tensor_tensor(out=ogb[:].rearrange("p a b -> p (a b)"), in0=onTb[:].rearrange("p a b -> p (a b)"),
                                                     scalar=gnw[:, 0:1], in1=zsb[:].rearrange("p a b -> p (a b)"), op0=ALU.mult, op1=ALU.mult),
             reads=[onTb, gnw, zsb], writes=[ogb])
        if ti >= 1:
            S.dma("pool", ogF[ti - 1][:, :].rearrange("p (c t) -> p c t", c=c_og), ogb[:], reads=[ogb], writes=[ogF[ti - 1]])
        if ti % 4 == 0 and ti <= 12:
            S.dma("pool", ogH[(ti // 4) * 128:(ti // 4 + 1) * 128, :].rearrange("p (c t) -> p c t", c=c_og), ogb[:, :, TS - 64:TS],
                  reads=[ogb], writes=[ogH])
        io["after_og"](ti)

    def interleave(gens):
        gens = [g for g in gens if g is not None]
        while gens:
            for g in list(gens):
                try:
                    next(g)
                except StopIteration:
                    gens.remove(g)

    load_a(0)
    tile_level(0)
    interleave([stageA(0)])
    NCK = NTS * CPT
    for n in range(NCK):
        nxt = None
        if n + 1 < NCK:
            if (n + 1) % CPT == 0:
                tile_level((n + 1) // CPT)
            nxt = stageA(n + 1)
        interleave([stageB(n), nxt])
        if (n + 1) % CPT == 0:
            tile_finish(n // CPT)
    S.pop()


def prep_M_gdn(inp, j, hg):
    w = inp["gdn_w_in"][j]
    cols = []
    for qh in range(2):
        cols.append(np.arange(128) + 128 * (2 * hg + qh))
    for qh in range(2):
        cols.append(1024 + np.arange(128) + 128 * (2 * hg + qh))
    for h in range(4):
        cols.append(2048 + np.arange(128) + 128 * (4 * hg + h))
    conv_cols = np.concatenate(cols)
    for h in range(4):
        cols.append(4096 + np.arange(128) + 128 * (4 * hg + h))
    cols.append(6144 + 4 * hg + np.arange(4))
    cols.append(6160 + 4 * hg + np.arange(4))
    cols = np.concatenate(cols)
    wc = w[:, cols].reshape(8, 128, G_WC).transpose(1, 0, 2)
    cw = inp["gdn_conv_w"][j][:, conv_cols].reshape(4, 8, 128).transpose(2, 1, 0)
    hv = np.concatenate([inp["gdn_a_log"][j][4 * hg:4 * hg + 4], inp["gdn_dt_bias"][j][4 * hg:4 * hg + 4]]).reshape(1, 8)
    return {"w": np.ascontiguousarray(wc.reshape(128, 8 * G_WC), np.float32),
            "cw": np.ascontiguousarray(cw.reshape(128, 32), np.float32),
            "hv": np.ascontiguousarray(hv, np.float32),
            "gnw": np.ascontiguousarray(inp["gdn_norm_w"][j].reshape(128, 1), np.float32),
            "cst": gdn_consts()}


GROUPS = [[0, 1, 2, 3], [4, 5, 6, 7]]


def build_fused(nl=4):
    nc = bass.Bass("TRN2", target_bir_lowering=False)
    es = ExitStack()
    S = Sched(nc, es)
    ext = lambda n, shp, dt=F32: S.dram(n, shp, dt, kind="ExternalInput")
    hs0 = ext("hs0", [D, WIN])
    keep = ext("keep", [1, WIN])
    gidx4 = ext("gidx4", [128, 20], mybir.dt.int32)
    gidx2 = ext("gidx2", [128, 20], mybir.dt.int32) if nl > 1 else None
    nwT0 = ext("nwT_0", [128, 32])
    cst_g = ext("cst_g", [128, C_TOT])
    cst_m = ext("cst_m", [128, 64 + 512 + 128]) if nl > 1 else None
    hs_out = S.dram("hs_out", [D, WIN], F32, kind="ExternalOutput")
    hs_loc = S.dram("hs_loc", [D, WIN], F32)
    def slices(name, rows_total, cols, rows_per):
        big = S.dram(name, [rows_total, cols], BF16)
        return big, [Buf(big.t[k * rows_per:(k + 1) * rows_per, :], "%s_%d" % (name, k)) for k in range(rows_total // rows_per)]

    ainF_all, ainF = slices("ainF", 512, 4096, 128)
    ainF_g, ainF_gs = slices("ainF_g", 2048, 4096, 512)
    ainH = S.dram("ainH", [128, 512], BF16)
    ainH_g = S.dram("ainH_g", [512, 512], BF16)
    og = {}
    for c in (4, 2):
        tpc = 8 // c
        ogF_all, ogF = slices("ogF%d" % c, 2048, c * 512, 128)
        ogF_g, _ = slices("ogF%d_g" % c, 8192, c * 512, 8192)
        nq = 16 // tpc
        og[c] = dict(ogF=ogF, ogF_all=ogF_all, ogH=S.dram("ogH%d" % c, [512, c * 64], BF16), tpc=tpc,
                     ogF_g=ogF_g, ogH_g=S.dram("ogH%d_g" % c, [2048, c * 64], BF16),
                     src=[Buf(ogF_all.t[q * tpc * 128:(q + 1) * tpc * 128, :], "ogsrc%d_%d" % (c, q)) for q in range(nq)],
                     dst=[Buf(ogF_g.t[q * 4 * tpc * 128:(q + 1) * 4 * tpc * 128, :], "ogdst%d_%d" % (c, q)) for q in range(nq)])
    lay = []
    for l in range(nl):
        KO = 2048 if l % 2 == 0 else 1024
        KC = KO // 128
        d = dict(nwT=ext("nwT_l%d" % l, [128, 32]), cw=ext("cw_l%d" % l, [128, 44 * 3]), cb=ext("cb_l%d" % l, [128, 44]),
                 wout_d=ext("wout_l%d" % l, [8, 128, KC * 128]), wup_d=ext("wup_l%d" % l, [NG, 128, 8 * 256]),
                 wdn_d=ext("wdn_l%d" % l, [8, 128, NG * 128]),
                 wout_b=S.dram("wout_b%d" % l, [8, 128, KC * 128], BF16), wup_b=S.dram("wup_b%d" % l, [NG, 128, 8 * 256], BF16),
                 wdn_b=S.dram("wdn_b%d" % l, [8, 128, NG * 128], BF16))
        if l % 2 == 0:
            d["m"] = dict(w=ext("gw_l%d" % l, [128, 8 * G_WC]), cw=ext("gcw_l%d" % l, [128, 32]), hv=ext("ghv_l%d" % l, [1, 8]),
                          gnw=ext("ggnw_l%d" % l, [128, 1]), cst=cst_g)
        else:
            d["m"] = dict(w=ext("mw_l%d" % l, [128, 8 * ML_WC]), gb=ext("mgb_l%d" % l, [1, 2]), nwv=ext("mnwv_l%d" % l, [1, ML_DV]), cst=cst_m)
        lay.append(d)

    def after_ain(ti):
        if ti == 0:
            S.coll("AllGather", ainH_g, ainH, GROUPS)
        else:
            S.coll("AllGather", ainF_gs[ti - 1], ainF[ti - 1], GROUPS)

    def mk_after_og(c):
        o = og[c]
        tpc = o["tpc"]

        def after_og(ti):
            wt = ti - 1
            if ti >= 1 and (wt + 1) % tpc == 0:
                q = wt // tpc
                src = o["src"][q]
                S.coll("AllGather", o["dst"][q], src, GROUPS, extra=[o["ogF"][k] for k in range(q * tpc, (q + 1) * tpc)])
            if ti == 12:
                S.coll("AllGather", o["ogH_g"], o["ogH"], GROUPS)
        return after_og

    emit_T(S, "t0", 2048, True, False, dict(hs_src=hs0, nwT=nwT0, ainF=ainF, ainH=ainH, after_ain=after_ain))
    for l in range(nl):
        d = lay[l]
        c = 4 if l % 2 == 0 else 2
        emit_casts(S, d)
        mio = dict(d["m"], ainF_g=ainF_g, ainH_g=ainH_g, ogF=og[c]["ogF"], ogH=og[c]["ogH"], after_og=mk_after_og(c))
        if l % 2 == 0:
            emit_M_gdn(S, "g%d" % l, mio)
        else:
            emit_M_ml(S, "m%d" % l, mio)
        last = l == nl - 1
        tio = dict(d, hs_src=(hs0 if l == 0 else hs_loc), hs_dst=(hs_out if last else hs_loc), ogF_g=og[c]["ogF_g"], ogH_g=og[c]["ogH_g"],
                   keep=keep, gidx=(gidx4 if c == 4 else gidx2), ainF=ainF, ainH=ainH, after_ain=after_ain)
        emit_T(S, "t%d" % (l + 1), 512 * c, False, last, tio)
    S.finish([hs_out])
    return nc, es


def kernel(x, meta_tokens, norm_w, gdn_w_in, gdn_conv_w, gdn_a_log, gdn_dt_bias, gdn_norm_w, gdn_w_out,
           ml_w_in, ml_gate_b, ml_norm_w, ml_w_out, ffn_w_up, ffn_conv_w, ffn_conv_b, ffn_w_down, _nl=4):
    inp = dict(x=x, meta_tokens=meta_tokens, norm_w=norm_w, gdn_w_in=gdn_w_in, gdn_conv_w=gdn_conv_w, gdn_a_log=gdn_a_log,
               gdn_dt_bias=gdn_dt_bias, gdn_norm_w=gdn_norm_w, gdn_w_out=gdn_w_out, ml_w_in=ml_w_in, ml_gate_b=ml_gate_b,
               ml_norm_w=ml_norm_w, ml_w_out=ml_w_out, ffn_w_up=ffn_w_up, ffn_conv_w=ffn_conv_w, ffn_conv_b=ffn_conv_b,
               ffn_w_down=ffn_w_down)
    inp = {k: np.asarray(v, np.float32) for k, v in inp.items()}
    shared = {"cst_g": gdn_consts(), "cst_m": ml_consts(),
              "nwT_0": np.ascontiguousarray(np.stack([_cm(inp["norm_w"][0, 0])] * 4, 1).reshape(128, 32), np.float32)}
    for l in range(_nl):
        t = prep_T(inp, l)
        for k, v in t.items():
            shared["%s_l%d" % (k, l)] = v
    maps = []
    for c in range(8):
        b, r = c // 4, c % 4
        m = dict(shared)
        h = np.zeros((LP, D), np.float32)
        h[XPAD + 48:XPAD + 64] = inp["meta_tokens"]
        h[XPAD + 64:] = inp["x"][b]
        lo = XPAD + 2048 * r
        m["hs0"] = np.ascontiguousarray(h[lo:lo + WIN].T)
        k = np.ones((1, WIN), np.float32)
        if r == 0:
            k[0, :48] = 0.0
        m["keep"] = k
        p = np.arange(128)
        for cc in (4, 2):
            tpc = 8 // cc
            gi = np.zeros((128, 20), np.int32)
            for hg in range(4):
                gi[:, hg * 5] = hg * 512 + r * 128 + p
                for i in range(1, 5):
                    wt = 4 * r + i - 1
                    gi[:, hg * 5 + i] = (wt // tpc) * (4 * tpc * 128) + hg * (tpc * 128) + (wt % tpc) * 128 + p
            m["gidx%d" % cc] = gi
        for l in range(_nl):
            if l % 2 == 0:
                g = prep_M_gdn(inp, l // 2, r)
                m["gw_l%d" % l], m["gcw_l%d" % l], m["ghv_l%d" % l], m["ggnw_l%d" % l] = g["w"], g["cw"], g["hv"], g["gnw"]
            else:
                g = prep_M_ml(inp, l // 2, r)
                m["mw_l%d" % l], m["mgb_l%d" % l], m["mnwv_l%d" % l] = g["w"], g["gb"], g["nwv"]
        maps.append(m)
    if _nl == 1:
        shared.pop("cst_m")
        for m in maps:
            m.pop("cst_m", None)
            m.pop("gidx2", None)
    nc, es = build_fused(_nl)
    res = run_bass_kernel_spmd(nc, maps, core_ids=list(range(8))).results
    out = np.zeros((NB, SEQ, D), np.float32)
    for c in range(8):
        b, r = c // 4, c % 4
        out[b, 2048 * r:2048 * (r + 1)] = res[c]["hs_out"][:, 64:].T
    return out
```

```python
from contextlib import ExitStack
import numpy as np
import ml_dtypes
import concourse.bass as bass
import concourse.mybir as mybir
from concourse.bass_utils import run_bass_kernel_spmd

F32 = mybir.dt.float32
BF16 = mybir.dt.bfloat16
AF = mybir.ActivationFunctionType
ALU = mybir.AluOpType
AX = mybir.AxisListType

D = 1024
SEQ = 8192
NB = 2
LP = 8704
XPAD = LP - SEQ - 64
WIN = 64 + 2048
FFN = 2816
NG = FFN // 128
RMS_EPS = 1e-6
CH = 64
NCH = LP // CH
TS = 512
NTS = LP // TS
CPT = TS // CH


class Buf:
    __slots__ = ("t", "lw", "rd", "sem", "semv", "name")

    def __init__(self, t, name=""):
        self.t = t
        self.lw = None
        self.rd = {}
        self.sem = None
        self.semv = 0
        self.name = name

    def __getitem__(self, k):
        return self.t[k]


class Sched:
    def __init__(self, nc, es):
        self.nc = nc
        self.es = es
        self.eng = {"pe": nc.tensor, "act": nc.scalar, "dve": nc.vector, "pool": nc.gpsimd, "sp": nc.sync}
        self.sem = {k: es.enter_context(nc.semaphore("sem_" + k)) for k in self.eng}
        self.cnt = {k: 0 for k in self.eng}
        self.seen = {k: {} for k in self.eng}
        self.nsem = 0
        self.out_events = []
        self.ninst = 0
        self.scopes = []
        self.dsems = []
        self.free_dsems = []
        self.scope_bufs = []

    def push(self, tag):
        self.scopes.append((ExitStack(), tag))
        self.scope_bufs.append([])

    def _own_sem(self, own):
        if own.sem is None:
            if self.free_dsems:
                own.sem, own.semv = self.free_dsems.pop()
            else:
                own.sem = self.es.enter_context(self.nc.semaphore("dsem%d" % self.nsem))
                self.nsem += 1
            self.dsems.append(own)

    def pop(self):
        self.barrier()
        st, _ = self.scopes.pop()
        st.close()
        for b in self.scope_bufs.pop():
            if b.sem is not None:
                self.free_dsems.append((b.sem, b.semv))
                self.dsems.remove(b)
                b.sem = None

    def _scope(self):
        return self.scopes[-1] if self.scopes else (self.es, "g")

    def sb(self, name, shape, dt):
        st, tag = self._scope()
        name = tag + "_" + name
        b = Buf(st.enter_context(self.nc.sbuf_tensor(name, list(shape), dt)), name)
        if self.scope_bufs:
            self.scope_bufs[-1].append(b)
        return b

    def ps(self, name, shape, dt=F32):
        st, tag = self._scope()
        name = tag + "_" + name
        return Buf(st.enter_context(self.nc.psum_tensor(name, list(shape), dt)), name)

    def barrier(self):
        for e in self.eng:
            eng = self.eng[e]
            for k in ("pe", "act", "dve", "pool", "sp"):
                if k != e and self.cnt[k] and self.seen[e].get(k, 0) < self.cnt[k]:
                    eng.wait_ge(self.sem[k], self.cnt[k])
                    self.seen[e][k] = self.cnt[k]
            for b in self.dsems:
                key = "d_" + b.name
                if self.seen[e].get(key, 0) < b.semv:
                    eng.wait_ge(b.sem, b.semv)
                    self.seen[e][key] = b.semv

    def dram(self, name, shape, dt, kind="Internal"):
        t = self.nc.dram_tensor(name, list(shape), dt, kind=kind)
        return Buf(t.ap(), name)

    def _deps(self, reads, writes):
        deps = {}

        def add(ev):
            if ev is None:
                return
            sem, val, key = ev
            if key not in deps or deps[key][1] < val:
                deps[key] = (sem, val)

        for b in reads:
            add(b.lw)
        for b in writes:
            add(b.lw)
            for ev in b.rd.values():
                add(ev)
        return deps

    def _wait(self, e, deps):
        eng = self.eng[e]
        for key, (sem, val) in deps.items():
            if e == "pe" and key == "pe":
                continue
            if self.seen[e].get(key, 0) >= val:
                continue
            eng.wait_ge(sem, val)
            self.seen[e][key] = val

    def _record(self, ev, reads, writes):
        for b in writes:
            b.lw = ev
            b.rd = {}
        for b in reads:
            if b not in writes:
                b.rd[ev[2]] = ev

    def op(self, e, fn, reads=(), writes=()):
        self._wait(e, self._deps(reads, writes))
        ins = fn(self.eng[e])
        self.cnt[e] += 1
        ins.then_inc(self.sem[e], 1)
        self.ninst += 1
        self._record((self.sem[e], self.cnt[e], e), reads, writes)

    def dma(self, q, out, in_, reads=(), writes=(), owner=None):
        self._wait(q, self._deps(reads, writes))
        own = owner if owner is not None else (writes[0] if writes else reads[0])
        self._own_sem(own)
        own.semv += 16
        ins = self.eng[q].dma_start(out=out, in_=in_)
        ins.then_inc(own.sem, 16)
        self.ninst += 1
        ev = (own.sem, own.semv, "d_" + own.name)
        self._record(ev, reads, writes)
        return ev

    def gather(self, out_ap, table_ap, idx_ap, reads=(), writes=()):
        self._wait("pool", self._deps(reads, writes))
        own = writes[0]
        self._own_sem(own)
        own.semv += 16
        ins = self.nc.gpsimd.indirect_dma_start(out=out_ap, out_offset=None, in_=table_ap,
                                                in_offset=bass.IndirectOffsetOnAxis(ap=idx_ap, axis=0))
        ins.then_inc(own.sem, 16)
        self.ninst += 1
        self._record((own.sem, own.semv, "d_" + own.name), reads, writes)

    def coll(self, kind, out, in_, groups, extra=()):
        self._wait("pool", self._deps([in_] + list(extra), [out]))
        self._own_sem(out)
        out.semv += 1
        ins = self.nc.gpsimd.collective_compute(kind, ALU.bypass, replica_groups=groups, ins=[in_.t.opt()], outs=[out.t.opt()])
        ins.then_inc(out.sem, 1)
        self.ninst += 1
        self._record((out.sem, out.semv, "d_" + out.name), [in_], [out])

    def finish(self, bufs):
        for b in bufs:
            if b.sem is not None:
                self.eng["sp"].wait_ge(b.sem, b.semv)
        for k in ("pe", "act", "dve", "pool"):
            if self.cnt[k]:
                self.eng["sp"].wait_ge(self.sem[k], self.cnt[k])


class PsumPool:
    def __init__(self, S, n, prefix="pb"):
        self.banks = [S.ps("%s%d" % (prefix, i), [128, 512]) for i in range(n)]
        self.i = 0

    def get(self):
        b = self.banks[self.i % len(self.banks)]
        self.i += 1
        return b


def bcast_mid(ap2, n):
    return ap2.unsqueeze(1).broadcast_to([ap2.shape[0], n, ap2.shape[1]])


def bcast_last(ap2, n):
    return ap2.unsqueeze(2).broadcast_to([ap2.shape[0], ap2.shape[1], n])


def rstd_from_ss(S, ss_ps, out_sb, scale, eps, W):
    S.op("act", lambda e: e.activation(out=out_sb[:, :W], in_=ss_ps[:, :W], func=AF.Ln, scale=scale, bias=eps),
         reads=[ss_ps], writes=[out_sb])
    S.op("act", lambda e: e.activation(out=out_sb[:, :W], in_=out_sb[:, :W], func=AF.Exp, scale=-0.5),
         reads=[out_sb], writes=[out_sb])


T_TILES = [(0, 64), (64, 512), (576, 512), (1088, 512), (1600, 512)]


def emit_casts(S, io):
    for m in range(8):
        S.dma("pool", io["wout_b"][m], io["wout_d"][m], writes=[io["wout_b"]])
    for g in range(NG):
        S.dma("pool", io["wup_b"][g], io["wup_d"][g], writes=[io["wup_b"]])
    for m in range(8):
        S.dma("pool", io["wdn_b"][m], io["wdn_d"][m], writes=[io["wdn_b"]])


def emit_T(S, tag, KO, first, last, io):
    S.push(tag)
    KC = KO // 128
    c_og = KC // 4
    hs_in = io["hs_src"]
    nwT_d = io["nwT"]
    if not last:
        ainF, ainH = io["ainF"], io["ainH"]
    if not first:
        ogF_g, ogH_g = io["ogF_g"], io["ogH_g"]
        keep_d, cw_d, cb_d = io["keep"], io["cw"], io["cb"]
        wout_b, wup_b, wdn_b = io["wout_b"], io["wup_b"], io["wdn_b"]
        hs_out = io["hs_dst"]
        gidx = S.sb("gidx", [128, 20], mybir.dt.int32)
        S.dma("sp", gidx[:], io["gidx"][:, :], writes=[gidx])

    ones_f = S.sb("ones_f", [128, 128], F32)
    ones_b = S.sb("ones_b", [128, 128], BF16)
    S.op("pool", lambda e: e.memset(ones_f[:], 1.0), writes=[ones_f])
    S.op("act", lambda e: e.activation(out=ones_b[:], in_=ones_f[:], func=AF.Copy), reads=[ones_f], writes=[ones_b])
    nwT = S.sb("nwT_sb", [128, 4, 8], F32)
    S.dma("sp", nwT[:].rearrange("p a b -> p (a b)"), nwT_d[:, :], writes=[nwT])

    hs_sb = [S.sb("hs_sb%d" % i, [128, 8, 512], F32) for i in range(2)]
    sq_sb = [S.sb("sq_sb%d" % i, [128, 512], BF16) for i in range(2)]
    rstd = S.sb("rstd", [128, 512], F32)
    a_sb = S.sb("a_sb", [128, 8, 512], BF16)
    pp = PsumPool(S, 7)
    ss_ps = S.ps("ss_ps", [128, 512])
    if not first:
        og_sb = [S.sb("og_sb%d" % i, [128, KC, 512], BF16) for i in range(2)]
        ogh_sb = S.sb("ogh_sb", [128, KC, 64], BF16)
        keep_sb = S.sb("keep_sb", [128, WIN], F32)
        S.dma("sp", keep_sb[:], keep_d[0:1, :].partition_broadcast(128), writes=[keep_sb])
        cw = S.sb("cw_sb", [128, 44, 3], F32)
        cb = S.sb("cb_sb", [128, 44], F32)
        S.dma("sp", cw[:].rearrange("p a b -> p (a b)"), cw_d[:, :], writes=[cw])
        S.dma("sp", cb[:], cb_d[:, :], writes=[cb])
        mix_sb = S.sb("mix_sb", [128, 8, 512], F32)
        rk = S.sb("rk", [128, 512], F32)
        tmp_sb = [S.sb("tmp_sb%d" % i, [128, 512], F32) for i in range(2)]
        h_sb = S.sb("h_sb", [128, NG, 512], BF16)
        u_sb = [S.sb("u_sb%d" % i, [128, 2, 2 + 512], F32) for i in range(2)]
        y_sb = [S.sb("y_sb%d" % i, [128, 2, 512], F32) for i in range(2)]
        e_sb = [S.sb("e_sb%d" % i, [128, 512], F32) for i in range(2)]
        halo = S.sb("halo", [128, 44, 2], F32)
        S.op("pool", lambda e: e.memset(halo[:], 0.0), writes=[halo])
        wo_s = [S.sb("wo_s%d" % i, [128, KC, 128], BF16) for i in range(3)]
        wu_s = [S.sb("wu_s%d" % i, [128, 8, 256], BF16) for i in range(4)]
        wd_s = [S.sb("wd_s%d" % i, [128, NG, 128], BF16) for i in range(3)]

    def norm_ss(src, W, eng_sq="act"):
        for m in range(8):
            sq = sq_sb[m % 2]
            S.op(eng_sq, lambda e: e.activation(out=sq[:, :W], in_=src[:, m, :W], func=AF.Square),
                 reads=[src], writes=[sq])
            S.op("pe", lambda e: e.matmul(ss_ps[:, :W], ones_b[:], sq[:, :W], start=(m == 0), stop=(m == 7)),
                 reads=[ones_b, sq], writes=[ss_ps])

    def load_tile(i):
        t0, W = T_TILES[i]
        hb = hs_sb[i % 2]
        S.dma("sp", hb[:, :, :W], hs_in[:, t0:t0 + W].rearrange("(c p) t -> p c t", p=128), writes=[hb])
        if not first:
            ob = ogh_sb if i == 0 else og_sb[i % 2]
            tab = ogH_g if i == 0 else ogF_g
            for hg in range(4):
                S.gather(ob[:, hg * c_og:(hg + 1) * c_og, :].rearrange("p c w -> p (c w)"), tab[:, :],
                         gidx[:, hg * 5 + i:hg * 5 + i + 1], reads=[gidx], writes=[ob])

    load_tile(0)
    for ti, (t0, W) in enumerate(T_TILES):
        if ti + 1 < len(T_TILES):
            load_tile(ti + 1)
        hb = hs_sb[ti % 2]
        if not first:
            ob = ogh_sb if ti == 0 else og_sb[ti % 2]
            S.dma("sp", wo_s[0][:].rearrange("p a b -> p (a b)"), wout_b[0], reads=[wout_b], writes=[wo_s[0]])
            S.dma("sp", wo_s[1][:].rearrange("p a b -> p (a b)"), wout_b[1], reads=[wout_b], writes=[wo_s[1]])
            for m in range(8):
                if m + 2 < 8:
                    S.dma("sp", wo_s[(m + 2) % 3][:].rearrange("p a b -> p (a b)"), wout_b[m + 2],
                          reads=[wout_b], writes=[wo_s[(m + 2) % 3]])
                ws = wo_s[m % 3]
                pb = pp.get()
                for kc in range(KC):
                    S.op("pe", lambda e: e.matmul(pb[:, :W], ws[:, kc, :], ob[:, kc, :W], start=(kc == 0), stop=(kc == KC - 1)),
                         reads=[ws, ob], writes=[pb])
                S.op("act", lambda e: e.activation(out=mix_sb[:, m, :W], in_=pb[:, :W], func=AF.Copy), reads=[pb], writes=[mix_sb])
                sq = sq_sb[m % 2]
                S.op("act", lambda e: e.activation(out=sq[:, :W], in_=pb[:, :W], func=AF.Square), reads=[pb], writes=[sq])
                S.op("pe", lambda e: e.matmul(ss_ps[:, :W], ones_b[:], sq[:, :W], start=(m == 0), stop=(m == 7)),
                     reads=[ones_b, sq], writes=[ss_ps])
            rstd_from_ss(S, ss_ps, rstd, 1.0 / D, RMS_EPS, W)
            S.op("dve", lambda e: e.tensor_tensor(out=rk[:, :W], in0=rstd[:, :W], in1=keep_sb[:, t0:t0 + W], op=ALU.mult),
                 reads=[rstd, keep_sb], writes=[rk])
            for m in range(8):
                tb = tmp_sb[m % 2]
                S.op("dve", lambda e: e.tensor_tensor(out=tb[:, :W], in0=mix_sb[:, m, :W], in1=rk[:, :W], op=ALU.mult),
                     reads=[mix_sb, rk], writes=[tb])
                S.op("dve", lambda e: e.scalar_tensor_tensor(out=hb[:, m, :W], in0=tb[:, :W], scalar=nwT[:, 1, m:m + 1],
                                                             in1=hb[:, m, :W], op0=ALU.mult, op1=ALU.add),
                     reads=[tb, nwT, hb], writes=[hb])
            norm_ss(hb, W)
            rstd_from_ss(S, ss_ps, rstd, 1.0 / D, RMS_EPS, W)
            for m in range(8):
                S.op("dve", lambda e: e.scalar_tensor_tensor(out=a_sb[:, m, :W], in0=hb[:, m, :W], scalar=nwT[:, 2, m:m + 1],
                                                             in1=rstd[:, :W], op0=ALU.mult, op1=ALU.mult),
                     reads=[hb, nwT, rstd], writes=[a_sb])
            for g0 in range(3):
                S.dma("sp", wu_s[g0][:].rearrange("p a b -> p (a b)"), wup_b[g0], reads=[wup_b], writes=[wu_s[g0]])
            for g in range(NG):
                if g + 3 < NG:
                    S.dma("sp", wu_s[(g + 3) % 4][:].rearrange("p a b -> p (a b)"), wup_b[g + 3],
                          reads=[wup_b], writes=[wu_s[(g + 3) % 4]])
                ws = wu_s[g % 4]
                ub = u_sb[g % 2]
                yb = y_sb[g % 2]
                eb = e_sb[g % 2]
                for hf in range(2):
                    ci = g + hf * NG
                    pb = pp.get()
                    for kc in range(8):
                        S.op("pe", lambda e: e.matmul(pb[:, :W], ws[:, kc, hf * 128:(hf + 1) * 128], a_sb[:, kc, :W],
                                                      start=(kc == 0), stop=(kc == 7)),
                             reads=[ws, a_sb], writes=[pb])
                    S.op("pool", lambda e: e.tensor_copy(out=ub[:, hf, 0:2], in_=halo[:, ci, :]), reads=[halo], writes=[ub])
                    S.op("act", lambda e: e.activation(out=ub[:, hf, 2:2 + W], in_=pb[:, :W], func=AF.Copy), reads=[pb], writes=[ub])
                    S.op("pool", lambda e: e.tensor_copy(out=halo[:, ci, :], in_=ub[:, hf, W:W + 2]), reads=[ub], writes=[halo])
                    S.op("act", lambda e: e.activation(out=yb[:, hf, :W], in_=pb[:, :W], func=AF.Identity,
                                                       scale=cw[:, ci, 2:3], bias=cb[:, ci:ci + 1]),
                         reads=[pb, cw, cb], writes=[yb])
                    S.op("dve", lambda e: e.scalar_tensor_tensor(out=yb[:, hf, :W], in0=ub[:, hf, 1:1 + W], scalar=cw[:, ci, 1:2],
                                                                 in1=yb[:, hf, :W], op0=ALU.mult, op1=ALU.add),
                         reads=[ub, cw, yb], writes=[yb])
                    S.op("dve", lambda e: e.scalar_tensor_tensor(out=yb[:, hf, :W], in0=ub[:, hf, 0:W], scalar=cw[:, ci, 0:1],
                                                                 in1=yb[:, hf, :W], op0=ALU.mult, op1=ALU.add),
                         reads=[ub, cw, yb], writes=[yb])
                S.op("act", lambda e: e.activation(out=eb[:, :W], in_=yb[:, 0, :W], func=AF.Silu), reads=[yb], writes=[eb])
                S.op("dve", lambda e: e.tensor_tensor(out=h_sb[:, g, :W], in0=yb[:, 1, :W], in1=eb[:, :W], op=ALU.mult),
                     reads=[yb, eb], writes=[h_sb])
            S.dma("sp", wd_s[0][:].rearrange("p a b -> p (a b)"), wdn_b[0], reads=[wdn_b], writes=[wd_s[0]])
            S.dma("sp", wd_s[1][:].rearrange("p a b -> p (a b)"), wdn_b[1], reads=[wdn_b], writes=[wd_s[1]])
            for m in range(8):
                if m + 2 < 8:
                    S.dma("sp", wd_s[(m + 2) % 3][:].rearrange("p a b -> p (a b)"), wdn_b[m + 2],
                          reads=[wdn_b], writes=[wd_s[(m + 2) % 3]])
                ws = wd_s[m % 3]
                pb = pp.get()
                for kc in range(NG):
                    S.op("pe", lambda e: e.matmul(pb[:, :W], ws[:, kc, :], h_sb[:, kc, :W], start=(kc == 0), stop=(kc == NG - 1)),
                         reads=[ws, h_sb], writes=[pb])
                S.op("act", lambda e: e.activation(out=mix_sb[:, m, :W], in_=pb[:, :W], func=AF.Copy), reads=[pb], writes=[mix_sb])
                sq = sq_sb[m % 2]
                S.op("act", lambda e: e.activation(out=sq[:, :W], in_=pb[:, :W], func=AF.Square), reads=[pb], writes=[sq])
                S.op("pe", lambda e: e.matmul(ss_ps[:, :W], ones_b[:], sq[:, :W], start=(m == 0), stop=(m == 7)),
                     reads=[ones_b, sq], writes=[ss_ps])
            rstd_from_ss(S, ss_ps, rstd, 1.0 / D, RMS_EPS, W)
            S.op("dve", lambda e: e.tensor_tensor(out=rk[:, :W], in0=rstd[:, :W], in1=keep_sb[:, t0:t0 + W], op=ALU.mult),
                 reads=[rstd, keep_sb], writes=[rk])
            for m in range(8):
                tb = tmp_sb[m % 2]
                S.op("dve", lambda e: e.tensor_tensor(out=tb[:, :W], in0=mix_sb[:, m, :W], in1=rk[:, :W], op=ALU.mult),
                     reads=[mix_sb, rk], writes=[tb])
                S.op("dve", lambda e: e.scalar_tensor_tensor(out=hb[:, m, :W], in0=tb[:, :W], scalar=nwT[:, 3, m:m + 1],
                                                             in1=hb[:, m, :W], op0=ALU.mult, op1=ALU.add),
                     reads=[tb, nwT, hb], writes=[hb])
            S.dma("pool", hs_out[:, t0:t0 + W].rearrange("(c p) t -> p c t", p=128), hb[:, :, :W], reads=[hb], owner=hs_out)
        if not last:
            norm_ss(hb, W)
            rstd_from_ss(S, ss_ps, rstd, 1.0 / D, RMS_EPS, W)
            for m in range(8):
                S.op("dve", lambda e: e.scalar_tensor_tensor(out=a_sb[:, m, :W], in0=hb[:, m, :W], scalar=nwT[:, 0, m:m + 1],
                                                             in1=rstd[:, :W], op0=ALU.mult, op1=ALU.mult),
                     reads=[hb, nwT, rstd], writes=[a_sb])
            if ti == 0:
                S.dma("pool", ainH[:, :].rearrange("p (c t) -> p c t", c=8), a_sb[:, :, :W], reads=[a_sb], writes=[ainH])
            else:
                S.dma("pool", ainF[ti - 1][:, :].rearrange("p (c t) -> p c t", c=8), a_sb[:, :, :W], reads=[a_sb], writes=[ainF[ti - 1]])
            io["after_ain"](ti)
    S.pop()


def _cm(v):
    return np.ascontiguousarray(v.reshape(-1, 128).T)


def prep_T(inp, layer):
    j = layer // 2
    if layer % 2 == 0:
        w_out = inp["gdn_w_out"][j]
    else:
        w_out = inp["ml_w_out"][j]
    KO = w_out.shape[0]
    KC = KO // 128
    nw = inp["norm_w"]
    nxt = nw[layer + 1, 0] if layer + 1 < 4 else nw[layer, 0]
    nwT = np.stack([_cm(nxt), _cm(nw[layer, 1]), _cm(nw[layer, 2]), _cm(nw[layer, 3])], 1)
    wout = w_out.reshape(KC, 128, 8, 128).transpose(2, 1, 0, 3)
    wu = inp["ffn_w_up"][layer].reshape(8, 128, 2, NG, 128).transpose(3, 1, 0, 2, 4)
    wd = inp["ffn_w_down"][layer].reshape(NG, 128, 8, 128).transpose(2, 1, 0, 3)
    cw = inp["ffn_conv_w"][layer].reshape(3, 44, 128).transpose(2, 1, 0)
    cb = _cm(inp["ffn_conv_b"][layer])
    return {
        "nwT": np.ascontiguousarray(nwT.reshape(128, 32), np.float32),
        "wout": np.ascontiguousarray(wout.reshape(8, 128, KC * 128), np.float32),
        "wup": np.ascontiguousarray(wu.reshape(NG, 128, 8 * 256), np.float32),
        "wdn": np.ascontiguousarray(wd.reshape(8, 128, NG * 128), np.float32),
        "cw": np.ascontiguousarray(cw.reshape(128, 44 * 3), np.float32),
        "cb": np.ascontiguousarray(cb, np.float32),
    }


ML_DK = 128
ML_DV = 256
ML_WC = 128 + 128 + 128 + 256 + 256 + 2
BIGNEG = 30000.0


def emit_M_ml(S, tag, io):
    S.push(tag)
    ainF_g, ainH_g = io["ainF_g"], io["ainH_g"]
    w_d, gb_d, nwv_d, cst_d = io["w"], io["gb"], io["nwv"], io["cst"]
    ogF, ogH = io["ogF"], io["ogH"]
    c_og = 2

    w_sb = S.sb("w_sb", [128, 8, ML_WC], BF16)
    S.dma("pool", w_sb[:].rearrange("p a b -> p (a b)"), w_d[:, :], writes=[w_sb])
    wg_sb = S.sb("wg_sb", [128, 8, 2], BF16)
    cst = S.sb("cst_sb", [128, 64 + 512 + 128], F32)
    S.dma("sp", cst[:], cst_d[:, :], writes=[cst])
    identf = cst
    ident_b = S.sb("ident_b", [128, 128], BF16)
    S.op("act", lambda e: e.activation(out=ident_b[:], in_=cst[:, 576:704], func=AF.Copy), reads=[cst], writes=[ident_b])
    gb = S.sb("gb_sb", [1, 2], F32)
    S.dma("sp", gb[:], gb_d[:, :], writes=[gb])
    nwv = S.sb("nwv_sb", [64, ML_DV], F32)
    S.dma("sp", nwv[:], nwv_d[0:1, :].partition_broadcast(64), writes=[nwv])
    ones_row = S.sb("ones_row", [1, 128], F32)
    S.op("pool", lambda e: e.memset(ones_row[:], 1.0), writes=[ones_row])

    a_sb = [S.sb("a_sb%d" % i, [128, 8, 512], BF16) for i in range(2)]
    pp = PsumPool(S, 5)
    pia2 = [S.ps("pia%d" % i, [128, 512]) for i in range(2)]

    li_row = S.sb("li_row", [1, LP], F32)
    lf_row = S.sb("lf_row", [1, LP], F32)
    bb_row = S.sb("bb_row", [1, LP], F32)
    ones_bc = ones_row[0:1, 0:1].broadcast_to([1, LP])

    def load_a(i):
        ab = a_sb[i % 2]
        if i == 0:
            S.op("pool", lambda e: e.memset(ab[:, :, 0:XPAD], 0.0), writes=[ab])
            S.dma("sp", ab[:, :, XPAD:TS], ainH_g[0:128, :].rearrange("p (c t) -> p c t", c=8), reads=[ainH_g], writes=[ab])
        else:
            r0 = ((i - 1) % 4) * 512 + ((i - 1) // 4) * 128
            S.dma("sp", ab[:].rearrange("p c t -> p (c t)"), ainF_g[r0:r0 + 128, :], reads=[ainF_g], writes=[ab])

    load_a(0)
    for ti in range(NTS):
        if ti + 1 < NTS:
            load_a(ti + 1)
        ab = a_sb[ti % 2]
        for gi_, row in ((0, li_row), (1, lf_row)):
            pr = pp.get()
            for kc in range(8):
                S.op("pe", lambda e: e.matmul(pr[0:1, :], w_sb[:, kc, ML_WC - 2 + gi_:ML_WC - 1 + gi_], ab[:, kc, :],
                                              start=(kc == 0), stop=(kc == 7)), reads=[w_sb, ab], writes=[pr])
            S.op("act", lambda e: e.activation(out=row[:, ti * TS:(ti + 1) * TS], in_=pr[0:1, :], func=AF.Identity,
                                               bias=gb[:, gi_:gi_ + 1], scale=1.0), reads=[pr, gb], writes=[row])
    for row in (li_row, lf_row):
        S.op("act", lambda e: e.activation(out=row[:], in_=row[:], func=AF.Exp, scale=2.0 / 15.0), reads=[row], writes=[row])
        S.op("dve", lambda e: e.tensor_scalar_add(out=row[:], in0=row[:], scalar1=1.0), reads=[row], writes=[row])
        S.op("dve", lambda e: e.reciprocal(out=row[:], in_=row[:]), reads=[row], writes=[row])
        S.op("dve", lambda e: e.tensor_scalar(out=row[:], in0=row[:], scalar1=-30.0, scalar2=15.0, op0=ALU.mult, op1=ALU.add),
             reads=[row], writes=[row])
    S.op("act", lambda e: e.activation(out=lf_row[:], in_=lf_row[:], func=AF.Exp, scale=-1.0), reads=[lf_row], writes=[lf_row])
    S.op("act", lambda e: e.activation(out=lf_row[:], in_=lf_row[:], func=AF.Ln, bias=1.0, scale=1.0), reads=[lf_row], writes=[lf_row])
    S.op("dve", lambda e: e.tensor_scalar_mul(out=lf_row[:], in0=lf_row[:], scalar1=-1.0), reads=[lf_row], writes=[lf_row])
    S.op("pool", lambda e: e.memset(lf_row[:, 0:XPAD], 0.0), writes=[lf_row])
    S.op("pool", lambda e: e.memset(li_row[:, 0:XPAD], -BIGNEG), writes=[li_row])
    S.op("dve", lambda e: e.tensor_tensor_scan(out=bb_row[:], data0=ones_bc, data1=lf_row[:], initial=0.0,
                                               op0=ALU.mult, op1=ALU.add), reads=[ones_row, lf_row], writes=[bb_row])
    c_row = lf_row
    S.op("dve", lambda e: e.tensor_tensor(out=c_row[:], in0=li_row[:], in1=bb_row[:], op=ALU.subtract), reads=[li_row, bb_row], writes=[c_row])
    M_row = li_row
    S.op("dve", lambda e: e.tensor_tensor_scan(out=M_row[:], data0=ones_bc, data1=c_row[:], initial=0.0,
                                               op0=ALU.mult, op1=ALU.max), reads=[ones_row, c_row], writes=[M_row])
    en_row = bb_row
    S.op("dve", lambda e: e.tensor_tensor(out=en_row[:], in0=bb_row[:], in1=M_row[:], op=ALU.add), reads=[bb_row, M_row], writes=[en_row])
    S.op("act", lambda e: e.activation(out=en_row[:], in_=en_row[:], func=AF.Exp, scale=-1.0), reads=[en_row], writes=[en_row])

    c_tm = S.sb("c_tm", [64, NCH], F32)
    M_tm = S.sb("M_tm", [64, NCH], F32)
    en_tm = S.sb("en_tm", [64, NCH], F32)
    for row, tmb in ((c_row, c_tm), (M_row, M_tm), (en_row, en_tm)):
        pb = pp.get()
        for n in range(NCH):
            S.op("pe", lambda e: e.matmul(pb[0:64, n:n + 1], row[0:1, n * CH:(n + 1) * CH], ones_row[0:1, 0:1], start=True, stop=True),
                 reads=[row, ones_row], writes=[pb])
        S.op("act", lambda e: e.activation(out=tmb[:], in_=pb[0:64, 0:NCH], func=AF.Copy), reads=[pb], writes=[tmb])
    Mend_b = S.sb("Mend_b", [128, NCH], F32)
    Mprev_b = S.sb("Mprev_b", [128, NCH], F32)
    pb = pp.get()
    S.op("pe", lambda e: e.matmul(pb[:, 0:NCH], ones_row[0:1, :], M_row[0:1, CH - 1::CH], start=True, stop=True),
         reads=[ones_row, M_row], writes=[pb])
    S.op("act", lambda e: e.activation(out=Mend_b[:], in_=pb[:, 0:NCH], func=AF.Copy), reads=[pb], writes=[Mend_b])
    S.op("pool", lambda e: e.memset(Mprev_b[:, 0:1], 0.0), writes=[Mprev_b])
    S.op("pool", lambda e: e.tensor_copy(out=Mprev_b[:, 1:NCH], in_=Mend_b[:, 0:NCH - 1]), reads=[Mend_b], writes=[Mprev_b])
    sc_tm = S.sb("sc_tm", [64, NCH], F32)
    kws_tm = S.sb("kws_tm", [64, NCH], F32)
    dec_b = S.sb("dec_b", [128, NCH], F32)
    S.op("dve", lambda e: e.tensor_tensor(out=sc_tm[:], in0=Mprev_b[0:64, :], in1=M_tm[:], op=ALU.subtract), reads=[Mprev_b, M_tm], writes=[sc_tm])
    S.op("act", lambda e: e.activation(out=sc_tm[:], in_=sc_tm[:], func=AF.Exp), reads=[sc_tm], writes=[sc_tm])
    S.op("dve", lambda e: e.tensor_tensor(out=kws_tm[:], in0=c_tm[:], in1=Mend_b[0:64, :], op=ALU.subtract), reads=[c_tm, Mend_b], writes=[kws_tm])
    S.op("act", lambda e: e.activation(out=kws_tm[:], in_=kws_tm[:], func=AF.Exp), reads=[kws_tm], writes=[kws_tm])
    S.op("dve", lambda e: e.tensor_tensor(out=dec_b[:], in0=Mprev_b[:], in1=Mend_b[:], op=ALU.subtract), reads=[Mprev_b, Mend_b], writes=[dec_b])
    S.op("act", lambda e: e.activation(out=dec_b[:], in_=dec_b[:], func=AF.Exp), reads=[dec_b], writes=[dec_b])

    qT = [S.sb("qT%d" % i, [128, 512], BF16) for i in range(2)]
    kT = [S.sb("kT%d" % i, [128, 512], BF16) for i in range(2)]
    Wt = [S.sb("Wt%d" % i, [64, 512], F32) for i in range(2)]
    ogT = [S.sb("ogT%d" % i, [128, 2, 512], BF16) for i in range(2)]
    C_f = S.sb("C_f", [128, ML_DV + 1], F32)
    C_b = S.sb("C_b", [128, ML_DV + 1], BF16)
    S.op("pool", lambda e: e.memset(C_f[:], 0.0), writes=[C_f])
    S.op("pool", lambda e: e.memset(C_b[:], 0.0), writes=[C_b])
    NR = 3
    kw_sb = [S.sb("kw_sb%d" % i, [64, 128], BF16) for i in range(NR)]
    va_sb = [S.sb("va_sb%d" % i, [64, ML_DV + 1], BF16) for i in range(NR)]
    og_sb = [S.sb("ogs%d" % i, [64, ML_DV], F32) for i in range(NR)]
    St_sb = [S.sb("St%d" % i, [64, 64], BF16) for i in range(NR)]
    t1_sb = [S.sb("t1_%d" % i, [64, ML_DV + 1], F32) for i in range(NR)]
    sm_sb = [S.sb("sm%d" % i, [64, 8], F32) for i in range(NR)]
    junk = [S.sb("junk%d" % i, [64, ML_DV], F32) for i in range(NR)]
    hn_sb = [S.sb("hn%d" % i, [64, ML_DV], BF16) for i in range(NR)]
    for i in range(NR):
        S.op("pool", lambda e: e.memset(va_sb[i][:, ML_DV:ML_DV + 1], 1.0), writes=[va_sb[i]])
    pTall = S.ps("pTall", [128, 1024], BF16)

    def tile_level(ti):
        if ti + 1 < NTS:
            load_a(ti + 1)
        ab = a_sb[ti % 2]
        t0 = ti * TS
        qTb, kTb, Wtb, ogTb = qT[ti % 2], kT[ti % 2], Wt[ti % 2], ogT[ti % 2]
        for wi, (dst, scl) in enumerate(((qTb, ML_DK ** -0.5), (kTb, 1.0))):
            pb = pp.get()
            for kc in range(8):
                S.op("pe", lambda e: e.matmul(pb[:, :], w_sb[:, kc, wi * 128:(wi + 1) * 128], ab[:, kc, :], start=(kc == 0), stop=(kc == 7)),
                     reads=[w_sb, ab], writes=[pb])
            S.op("act", lambda e: e.activation(out=dst[:], in_=pb[:, :], func=AF.Copy, scale=scl), reads=[pb], writes=[dst])
        pb = pp.get()
        S.op("pe", lambda e: e.matmul(pb[0:64, :], ones_row[0:1, 0:64], M_row[0:1, t0:t0 + TS], start=True, stop=False),
             reads=[ones_row, M_row], writes=[pb])
        S.op("pe", lambda e: e.matmul(pb[0:64, :], cst[0:64, 0:64], cst[0:64, 64:576], start=False, stop=True),
             reads=[cst], writes=[pb])
        S.op("dve", lambda e: e.tensor_tensor(out=Wtb[:].rearrange("p (n i) -> p n i", n=CPT), in0=pb[0:64, :].rearrange("p (n i) -> p n i", n=CPT),
                                              in1=bcast_last(c_tm[:, ti * CPT:(ti + 1) * CPT], CH), op=ALU.subtract),
             reads=[pb, c_tm], writes=[Wtb])
        S.op("act", lambda e: e.activation(out=Wtb[:], in_=Wtb[:], func=AF.Exp, scale=-1.0), reads=[Wtb], writes=[Wtb])

    def stageA(n):
        ti, cj = n // CPT, n % CPT
        ab = a_sb[ti % 2]
        qTb, kTb, Wtb, ogTb = qT[ti % 2], kT[ti % 2], Wt[ti % 2], ogT[ti % 2]
        r = n % NR
        c0 = cj * CH
        pkv = pp.get()
        for kc in range(8):
            S.op("pe", lambda e: e.matmul(pkv[0:64, 0:384], ab[:, kc, c0:c0 + CH], w_sb[:, kc, 256:640], start=(kc == 0), stop=(kc == 7)),
                 reads=[w_sb, ab], writes=[pkv])
        pog = pp.get()
        for kc in range(8):
            S.op("pe", lambda e: e.matmul(pog[0:64, 0:256], ab[:, kc, c0:c0 + CH], w_sb[:, kc, 640:896], start=(kc == 0), stop=(kc == 7)),
                 reads=[w_sb, ab], writes=[pog])
        S.op("act", lambda e: e.activation(out=kw_sb[r][:], in_=pkv[0:64, 0:128], func=AF.Copy, scale=kws_tm[:, n:n + 1]),
             reads=[pkv, kws_tm], writes=[kw_sb[r]])
        S.op("act", lambda e: e.activation(out=va_sb[r][:, 0:ML_DV], in_=pkv[0:64, 128:384], func=AF.Copy), reads=[pkv], writes=[va_sb[r]])
        S.op("act", lambda e: e.activation(out=og_sb[r][:], in_=pog[0:64, 0:256], func=AF.Exp, scale=-1.0), reads=[pog], writes=[og_sb[r]])
        yield
        S.op("dve", lambda e: e.tensor_scalar_add(out=og_sb[r][:], in0=og_sb[r][:], scalar1=1.0), reads=[og_sb[r]], writes=[og_sb[r]])
        S.op("dve", lambda e: e.reciprocal(out=og_sb[r][:], in_=og_sb[r][:]), reads=[og_sb[r]], writes=[og_sb[r]])
        S.op("dve", lambda e: e.tensor_tensor(out=og_sb[r][:], in0=og_sb[r][:], in1=nwv[:], op=ALU.mult), reads=[og_sb[r], nwv], writes=[og_sb[r]])
        pq = pp.get()
        S.op("pe", lambda e: e.matmul(pq[0:64, 0:64], kTb[:, c0:c0 + CH], qTb[:, c0:c0 + CH], start=True, stop=True),
             reads=[kTb, qTb], writes=[pq])
        S.op("dve", lambda e: e.tensor_tensor(out=St_sb[r][:], in0=pq[0:64, 0:64], in1=Wtb[:, c0:c0 + CH], op=ALU.mult),
             reads=[pq, Wtb], writes=[St_sb[r]])
        yield
        pia = pia2[n % 2]
        S.op("pe", lambda e: e.matmul(pia[0:64, 0:ML_DV + 1], St_sb[r][:], va_sb[r][:], start=True, stop=True),
             reads=[St_sb[r], va_sb[r]], writes=[pia])
        yield

    def stageB(n):
        ti, cj = n // CPT, n % CPT
        ab = a_sb[ti % 2]
        qTb, kTb, Wtb, ogTb = qT[ti % 2], kT[ti % 2], Wt[ti % 2], ogT[ti % 2]
        r = n % NR
        c0 = cj * CH
        pia = pia2[n % 2]
        pint = pp.get()
        S.op("pe", lambda e: e.matmul(pint[0:64, 0:ML_DV + 1], qTb[:, c0:c0 + CH], C_b[:], start=True, stop=True),
             reads=[qTb, C_b], writes=[pint])
        pst = pp.get()
        S.op("pe", lambda e: e.matmul(pst[:, 0:ML_DV + 1], kw_sb[r][:], va_sb[r][:], start=True, stop=True),
             reads=[kw_sb[r], va_sb[r]], writes=[pst])
        S.op("dve", lambda e: e.scalar_tensor_tensor(out=C_f[:], in0=C_f[:], scalar=dec_b[:, n:n + 1], in1=pst[:, 0:ML_DV + 1],
                                                     op0=ALU.mult, op1=ALU.add), reads=[C_f, dec_b, pst], writes=[C_f])
        S.op("act", lambda e: e.activation(out=C_b[:], in_=C_f[:], func=AF.Copy), reads=[C_f], writes=[C_b])
        t1 = t1_sb[r]
        S.op("act", lambda e: e.activation(out=t1[:], in_=pint[0:64, 0:ML_DV + 1], func=AF.Copy, scale=sc_tm[:, n:n + 1]),
             reads=[pint, sc_tm], writes=[t1])
        S.op("dve", lambda e: e.tensor_tensor(out=t1[:], in0=t1[:], in1=pia[0:64, 0:ML_DV + 1], op=ALU.add), reads=[t1, pia], writes=[t1])
        yield
        sm = sm_sb[r]
        S.op("act", lambda e: e.activation(out=sm[:, 0:1], in_=t1[:, ML_DV:ML_DV + 1], func=AF.Abs), reads=[t1], writes=[sm])
        S.op("dve", lambda e: e.tensor_tensor(out=sm[:, 0:1], in0=sm[:, 0:1], in1=en_tm[:, n:n + 1], op=ALU.max),
             reads=[sm, en_tm], writes=[sm])
        S.op("dve", lambda e: e.reciprocal(out=sm[:, 1:2], in_=sm[:, 0:1]), reads=[sm], writes=[sm])
        S.op("act", lambda e: e.activation(out=junk[r][:], in_=t1[:, 0:ML_DV], func=AF.Square, scale=sm[:, 1:2], accum_out=sm[:, 2:3]),
             reads=[t1, sm], writes=[junk[r], sm])
        S.op("act", lambda e: e.activation(out=sm[:, 3:4], in_=sm[:, 2:3], func=AF.Ln, scale=1.0 / ML_DV, bias=RMS_EPS), reads=[sm], writes=[sm])
        S.op("act", lambda e: e.activation(out=sm[:, 3:4], in_=sm[:, 3:4], func=AF.Exp, scale=-0.5), reads=[sm], writes=[sm])
        S.op("dve", lambda e: e.tensor_tensor(out=sm[:, 4:5], in0=sm[:, 3:4], in1=sm[:, 1:2], op=ALU.mult), reads=[sm], writes=[sm])
        S.op("dve", lambda e: e.scalar_tensor_tensor(out=hn_sb[r][:], in0=t1[:, 0:ML_DV], scalar=sm[:, 4:5], in1=og_sb[r][:],
                                                     op0=ALU.mult, op1=ALU.mult), reads=[t1, sm, og_sb[r]], writes=[hn_sb[r]])
        yield
        ptv = pTall[:, (n % 4) * 128:(n % 4 + 1) * 128].rearrange("p (a b) -> p a b", a=2)
        for hh in range(2):
            S.op("pe", lambda e: e.transpose(ptv[:, hh, :], hn_sb[r][:, hh * 128:(hh + 1) * 128], ident_b[0:64, 0:64]),
                 reads=[hn_sb[r], ident_b], writes=[pTall])
        S.op("act", lambda e: e.activation(out=ogTb[:, :, c0:c0 + CH], in_=ptv, func=AF.Copy), reads=[pTall], writes=[ogTb])
        yield

    def tile_finish(ti):
        ogTb = ogT[ti % 2]
        if ti >= 1:
            S.dma("pool", ogF[ti - 1][:, :].rearrange("p (c t) -> p c t", c=c_og), ogTb[:], reads=[ogTb], writes=[ogF[ti - 1]])
        if ti % 4 == 0 and ti <= 12:
            S.dma("pool", ogH[(ti // 4) * 128:(ti // 4 + 1) * 128, :].rearrange("p (c t) -> p c t", c=c_og), ogTb[:, :, TS - 64:TS],
                  reads=[ogTb], writes=[ogH])
        io["after_og"](ti)

    def interleave(gens):
        gens = [g for g in gens if g is not None]
        while gens:
            for g in list(gens):
                try:
                    next(g)
                except StopIteration:
                    gens.remove(g)

    load_a(0)
    tile_level(0)
    interleave([stageA(0)])
    NCK = NTS * CPT
    for n in range(NCK):
        nxt = None
        if n + 1 < NCK:
            if (n + 1) % CPT == 0:
                tile_level((n + 1) // CPT)
            nxt = stageA(n + 1)
        interleave([stageB(n), nxt])
        if (n + 1) % CPT == 0:
            tile_finish(n // CPT)
    S.pop()


def prep_M_ml(inp, j, h):
    w = inp["ml_w_in"][j]
    q = w[:, h * 128:(h + 1) * 128]
    k = w[:, 512 + h * 128:512 + (h + 1) * 128]
    v = w[:, 1024 + h * 256:1024 + (h + 1) * 256]
    o = w[:, 2048 + h * 256:2048 + (h + 1) * 256]
    gi = w[:, 3072 + h:3073 + h]
    gf = w[:, 3076 + h:3077 + h]
    wc = np.concatenate([q, k, k, v, o, gi, gf], 1)
    wc = wc.reshape(8, 128, ML_WC).transpose(1, 0, 2)
    gb = inp["ml_gate_b"][j][[h, 4 + h]].reshape(1, 2)
    nwv = inp["ml_norm_w"][j][h * 256:(h + 1) * 256].reshape(1, 256)
    return {"w": np.ascontiguousarray(wc.reshape(128, 8 * ML_WC), np.float32),
            "gb": np.ascontiguousarray(gb, np.float32), "nwv": np.ascontiguousarray(nwv, np.float32),
            "cst": ml_consts()}


def ml_consts():
    c = np.zeros((128, 64 + 512 + 128), np.float32)
    c[0:64, 0:64] = np.eye(64, dtype=np.float32)
    jj = np.arange(64)[:, None]
    ii = np.arange(64)[None, :]
    mb = (jj > ii).astype(np.float32) * BIGNEG
    c[0:64, 64:576] = np.tile(mb, (1, 8))
    c[:, 576:704] = np.eye(128, dtype=np.float32)
    return c


G_WC = 12 * 128 + 8
L2_EPS = 1e-6
GDN_EPS = 1e-6
C_I64, C_MUI, C_MUS, C_MLS, C_NMLS, C_NMUS, C_I128, C_ONES = 0, 64, 128, 192, 256, 320, 384, 512
C_TOT = 640


def gdn_consts():
    c = np.zeros((128, C_TOT), np.float32)
    p = np.arange(64)[:, None]
    f = np.arange(64)[None, :]
    c[0:64, C_I64:C_I64 + 64] = (p == f)
    c[0:64, C_MUI:C_MUI + 64] = (p <= f)
    c[0:64, C_MUS:C_MUS + 64] = (p < f)
    c[0:64, C_MLS:C_MLS + 64] = (p > f)
    c[0:64, C_NMLS:C_NMLS + 64] = -1.0 * (p > f)
    c[0:64, C_NMUS:C_NMUS + 64] = -1.0 * (p < f)
    c[:, C_I128:C_I128 + 128] = np.eye(128, dtype=np.float32)
    c[:, C_ONES:C_ONES + 128] = 1.0
    return c


def emit_M_gdn(S, tag, io):
    S.push(tag)
    ainF_g, ainH_g = io["ainF_g"], io["ainH_g"]
    w_d, cw_d, hv_d, gnw_d, cst_d = io["w"], io["cw"], io["hv"], io["gnw"], io["cst"]
    ogF, ogH = io["ogF"], io["ogH"]
    c_og = 4

    w_sb = S.sb("w_sb", [128, 8, G_WC], BF16)
    S.dma("pool", w_sb[:].rearrange("p a b -> p (a b)"), w_d[:, :], writes=[w_sb])
    cst = S.sb("cst_sb", [128, C_TOT], F32)
    S.dma("sp", cst[:], cst_d[:, :], writes=[cst])
    cwg = S.sb("cwg", [128, 8, 4], F32)
    S.dma("sp", cwg[:].rearrange("p a b -> p (a b)"), cw_d[:, :], writes=[cwg])
    hv = S.sb("hv_sb", [64, 8], F32)
    S.dma("sp", hv[:], hv_d[0:1, :].partition_broadcast(64), writes=[hv])
    gnw = S.sb("gnw_sb", [128, 1], F32)
    S.dma("sp", gnw[:], gnw_d[:, :], writes=[gnw])
    ident_b = S.sb("ident_b", [128, 128], BF16)
    ones_b = S.sb("ones_b", [128, 128], BF16)
    S.op("act", lambda e: e.activation(out=ident_b[:], in_=cst[:, C_I128:C_I128 + 128], func=AF.Copy), reads=[cst], writes=[ident_b])
    S.op("act", lambda e: e.activation(out=ones_b[:], in_=cst[:, C_ONES:C_ONES + 128], func=AF.Copy), reads=[cst], writes=[ones_b])
    dg = S.sb("dg", [128, 32, 128], BF16)
    for mc in range(8):
        for k in range(4):
            S.op("dve", lambda e: e.tensor_scalar(out=dg[:, mc * 4 + k, :], in0=cst[:, C_I128:C_I128 + 128], scalar1=cwg[:, mc, k:k + 1],
                                                  scalar2=None, op0=ALU.mult), reads=[cst, cwg], writes=[dg])
    I64 = cst[0:64, C_I64:C_I64 + 64]
    MUI = cst[0:64, C_MUI:C_MUI + 64]
    MLS = cst[0:64, C_MLS:C_MLS + 64]
    NMLS = cst[0:64, C_NMLS:C_NMLS + 64]
    NMUS = cst[0:64, C_NMUS:C_NMUS + 64]
    ONES64x128 = cst[0:64, C_ONES:C_ONES + 128]

    a_sb = [S.sb("a_sb%d" % i, [128, 8, 512], BF16) for i in range(2)]
    pp = PsumPool(S, 4)
    po2 = [S.ps("po%d" % i, [128, 512]) for i in range(2)]
    pTall = S.ps("pTall", [128, 1024], BF16)
    pTon = S.ps("pTon", [128, 1024], BF16)

    def load_a(i):
        ab = a_sb[i % 2]
        if i == 0:
            S.op("pool", lambda e: e.memset(ab[:, :, 0:XPAD], 0.0), writes=[ab])
            S.dma("sp", ab[:, :, XPAD:TS], ainH_g[0:128, :].rearrange("p (c t) -> p c t", c=8), reads=[ainH_g], writes=[ab])
        else:
            r0 = ((i - 1) % 4) * 512 + ((i - 1) // 4) * 128
            S.dma("sp", ab[:].rearrange("p c t -> p (c t)"), ainF_g[r0:r0 + 128, :], reads=[ainF_g], writes=[ab])

    NH = NCH * 4
    bg = S.sb("bg", [64, NCH, 8], F32)
    load_a(0)
    for ti in range(NTS):
        if ti + 1 < NTS:
            load_a(ti + 1)
        ab = a_sb[ti % 2]
        pb = pp.get()
        for cj in range(CPT):
            for kc in range(8):
                S.op("pe", lambda e: e.matmul(pb[0:64, cj * 8:(cj + 1) * 8], ab[:, kc, cj * CH:(cj + 1) * CH], w_sb[:, kc, 1536:1544],
                                              start=(kc == 0), stop=(kc == 7)), reads=[ab, w_sb], writes=[pb])
        S.op("act", lambda e: e.activation(out=bg[:, ti * CPT:(ti + 1) * CPT, :], in_=pb[0:64, 0:64].rearrange("p (a b) -> p a b", a=CPT),
                                           func=AF.Copy), reads=[pb], writes=[bg])
    lnb = S.sb("lnb", [64, NCH, 4], F32)
    bt = S.sb("bt", [64, NCH, 4], F32)
    gt = S.sb("gt", [64, NCH, 4], F32)
    beG = S.sb("beG", [64, NCH, 4], F32)
    ekt = S.sb("ekt", [64, NCH, 4], F32)
    eGl = S.sb("eGl", [128, NCH, 4], F32)
    tmpg = S.sb("tmpg", [64, NCH, 4], F32)
    eal = S.sb("eal", [64, 4], F32)
    S.op("act", lambda e: e.activation(out=lnb[:], in_=bg[:, :, 0:4], func=AF.Exp, scale=-1.0), reads=[bg], writes=[lnb])
    S.op("act", lambda e: e.activation(out=lnb[:], in_=lnb[:], func=AF.Ln, bias=1.0, scale=1.0), reads=[lnb], writes=[lnb])
    S.op("dve", lambda e: e.tensor_scalar_mul(out=lnb[:], in0=lnb[:], scalar1=-1.0), reads=[lnb], writes=[lnb])
    S.op("act", lambda e: e.activation(out=bt[:], in_=lnb[:], func=AF.Exp), reads=[lnb], writes=[bt])
    S.op("dve", lambda e: e.tensor_tensor(out=gt[:], in0=bg[:, :, 4:8], in1=bcast_mid(hv[:, 4:8], NCH), op=ALU.add), reads=[bg, hv], writes=[gt])
    S.op("act", lambda e: e.activation(out=gt[:], in_=gt[:], func=AF.Exp), reads=[gt], writes=[gt])
    S.op("act", lambda e: e.activation(out=gt[:], in_=gt[:], func=AF.Ln, bias=1.0, scale=1.0), reads=[gt], writes=[gt])
    S.op("act", lambda e: e.activation(out=eal[:], in_=hv[:, 0:4], func=AF.Exp), reads=[hv], writes=[eal])
    S.op("dve", lambda e: e.tensor_scalar_mul(out=eal[:], in0=eal[:], scalar1=-1.0), reads=[eal], writes=[eal])
    S.op("dve", lambda e: e.tensor_tensor(out=gt[:], in0=gt[:], in1=bcast_mid(eal[:], NCH), op=ALU.mult), reads=[gt, eal], writes=[gt])
    gflat = gt[:].rearrange("p a b -> p (a b)")
    for (c0, c1) in ((0, 272), (272, NH)):
        pb = pp.get()
        S.op("pe", lambda e: e.matmul(pb[0:64, 0:c1 - c0], MUI, gflat[:, c0:c1], start=True, stop=True), reads=[cst, gt], writes=[pb])
        pl = pp.get()
        S.op("pe", lambda e: e.matmul(pl[:, 0:c1 - c0], ONES64x128, gflat[:, c0:c1], start=True, stop=True), reads=[cst, gt], writes=[pl])
        S.op("act", lambda e: e.activation(out=tmpg[:].rearrange("p a b -> p (a b)")[:, c0:c1], in_=pb[0:64, 0:c1 - c0], func=AF.Exp),
             reads=[pb], writes=[tmpg])
        S.op("act", lambda e: e.activation(out=eGl[:].rearrange("p a b -> p (a b)")[:, c0:c1], in_=pl[:, 0:c1 - c0], func=AF.Exp),
             reads=[pl], writes=[eGl])
        S.op("act", lambda e: e.activation(out=ekt[:].rearrange("p a b -> p (a b)")[:, c0:c1], in_=pb[0:64, 0:c1 - c0], func=AF.Copy),
             reads=[pb], writes=[ekt])
        S.op("dve", lambda e: e.tensor_tensor(out=ekt[:].rearrange("p a b -> p (a b)")[:, c0:c1], in0=pl[0:64, 0:c1 - c0],
                                              in1=ekt[:].rearrange("p a b -> p (a b)")[:, c0:c1], op=ALU.subtract), reads=[pl, ekt], writes=[ekt])
    S.op("act", lambda e: e.activation(out=ekt[:], in_=ekt[:], func=AF.Exp), reads=[ekt], writes=[ekt])
    S.op("dve", lambda e: e.tensor_tensor(out=beG[:], in0=bt[:], in1=tmpg[:], op=ALU.mult), reads=[bt, tmpg], writes=[beG])

    xb = [S.sb("xb%d" % i, [128, 8, 3 + TS], BF16) for i in range(2)]
    S.op("pool", lambda e: e.memset(xb[1][:, :, TS:TS + 3], 0.0), writes=[xb[1]])
    sx = [S.sb("sx%d" % i, [128, TS], F32) for i in range(2)]
    sqb = [S.sb("sqb%d" % i, [128, TS], BF16) for i in range(2)]
    rs = [S.sb("rs%d" % i, [128, TS], F32) for i in range(2)]
    qT = [S.sb("qT%d" % i, [128, 2, TS], BF16) for i in range(2)]
    kT = [S.sb("kT%d" % i, [128, 2, TS], BF16) for i in range(2)]
    svT = [S.sb("svT%d" % i, [128, 4, TS], BF16) for i in range(2)]
    zs = [S.sb("zs%d" % i, [128, 4, TS], F32) for i in range(2)]
    onT = [S.sb("onT%d" % i, [128, 4, TS], BF16) for i in range(2)]
    ogt = [S.sb("ogt0", [128, 4, TS], BF16)] * 2
    S_f = S.sb("S_f", [128, 4, 128], F32)
    S_b = S.sb("S_b", [128, 4, 128], BF16)
    S_t = S.sb("S_t", [128, 4, 128], F32)
    S.op("pool", lambda e: e.memset(S_f[:], 0.0), writes=[S_f])
    S.op("pool", lambda e: e.memset(S_b[:], 0.0), writes=[S_b])
    NR = 2
    mk = lambda nm, shp, dt: [S.sb("%s%d" % (nm, i), shp, dt) for i in range(NR)]
    kbg = mk("kbg", [64, 4, 128], BF16)
    ktm = mk("ktm", [64, 4, 128], BF16)
    vb = mk("vb", [64, 4, 128], BF16)
    rg1 = mk("rg1", [64, 4, 64], F32)
    rg2 = mk("rg2", [64, 4, 64], F32)
    rg3 = mk("rg3", [64, 4, 64], F32)
    Et = mk("Et", [64, 4, 64], F32)
    Wt_ = mk("Wt", [64, 4, 64], F32)
    W_ = mk("W", [64, 4, 64], F32)
    eGb = mk("eGb", [128, 4, 64], F32)
    KKlo = mk("KKlo", [64, 2, 64], F32)
    KKup = mk("KKup", [64, 2, 64], F32)
    KQm = mk("KQm", [64, 2, 64], F32)
    Qt = mk("Qt", [64, 4, 64], BF16)
    qdT = mk("qdT", [128, 4, 64], BF16)
    PP = [mk("PP%d" % k, [64, 8, 64], BF16) for k in range(2)]
    Xt = [mk("Xt%d" % k, [64, 4, 64], BF16) for k in range(2)]
    Tt = mk("Tt", [64, 4, 64], BF16)
    nwT = mk("nwT", [128, 4, 64], BF16)
    vn = mk("vn", [64, 4, 128], BF16)
    sqo = mk("sqo", [64, 4, 128], F32)
    sso = mk("sso", [64, 8], F32)
    on = mk("on", [64, 4, 128], BF16)

    def silu_from_psum(pb, W, out_ap, out_buf, idx):
        S.op("act", lambda e: e.activation(out=out_ap, in_=pb[:, :W], func=AF.Silu), reads=[pb], writes=[out_buf])

    def tile_level(ti):
        if ti + 1 < NTS:
            load_a(ti + 1)
        ab = a_sb[ti % 2]
        t0 = ti * TS
        xcur, xprev = xb[ti % 2], xb[(ti + 1) % 2]
        qTb, kTb, svb, zsb, onTb, ogb = qT[ti % 2], kT[ti % 2], svT[ti % 2], zs[ti % 2], onT[ti % 2], ogt[ti % 2]
        S.op("pool", lambda e: e.tensor_copy(out=xcur[:, :, 0:3], in_=xprev[:, :, TS:TS + 3]), reads=[xprev], writes=[xcur])
        for mc in range(12):
            pb = pp.get()
            for kc in range(8):
                S.op("pe", lambda e: e.matmul(pb[:, :], w_sb[:, kc, mc * 128:(mc + 1) * 128], ab[:, kc, :], start=(kc == 0), stop=(kc == 7)),
                     reads=[w_sb, ab], writes=[pb])
            if mc < 8:
                S.op("act", lambda e: e.activation(out=xcur[:, mc, 3:3 + TS], in_=pb[:, :], func=AF.Copy), reads=[pb], writes=[xcur])
            else:
                silu_from_psum(pb, TS, zsb[:, mc - 8, :], zsb, mc)
        for mc in range(8):
            pb = pp.get()
            for k in range(4):
                S.op("pe", lambda e: e.matmul(pb[:, :], dg[:, mc * 4 + k, :], xcur[:, mc, k:k + TS], start=(k == 0), stop=(k == 3)),
                     reads=[dg, xcur], writes=[pb])
            if mc >= 4:
                silu_from_psum(pb, TS, svb[:, mc - 4, :], svb, mc)
            else:
                sxb, sq, rsb = sx[mc % 2], sqb[mc % 2], rs[mc % 2]
                silu_from_psum(pb, TS, sxb[:, :], sxb, mc)
                S.op("act", lambda e: e.activation(out=sq[:, :], in_=sxb[:, :], func=AF.Square), reads=[sxb], writes=[sq])
                ps2 = pp.get()
                S.op("pe", lambda e: e.matmul(ps2[:, :], ones_b[:], sq[:, :], start=True, stop=True), reads=[ones_b, sq], writes=[ps2])
                rstd_from_ss(S, ps2, rsb, 1.0, L2_EPS, TS)
                dst = qTb if mc < 2 else kTb
                scl = (128.0 ** -0.5) if mc < 2 else 1.0
                S.op("dve", lambda e: e.scalar_tensor_tensor(out=dst[:, mc % 2, :], in0=sxb[:, :], scalar=scl, in1=rsb[:, :],
                                                             op0=ALU.mult, op1=ALU.mult), reads=[sxb, rsb], writes=[dst])

    def stageA(n):
        ti, cj = n // CPT, n % CPT
        qTb, kTb, svb, onTb = qT[ti % 2], kT[ti % 2], svT[ti % 2], onT[ti % 2]
        r = n % NR
        c0 = cj * CH
        for qh in range(2):
            S.op("pe", lambda e: e.transpose(pTall[0:64, qh * 128:(qh + 1) * 128], kTb[:, qh, c0:c0 + CH], ident_b[:, :]),
                 reads=[kTb, ident_b], writes=[pTall])
        for h in range(4):
            S.op("pe", lambda e: e.transpose(pTall[0:64, 256 + h * 128:256 + (h + 1) * 128], svb[:, h, c0:c0 + CH], ident_b[:, :]),
                 reads=[svb, ident_b], writes=[pTall])
        ktm_ps = pTall[0:64, 0:256].rearrange("p (a b) -> p a b", a=2)
        ktm_rep = ktm_ps.unsqueeze(2).broadcast_to([64, 2, 2, 128])
        as4 = lambda ap: ap.rearrange("p (a r) d -> p a r d", r=2)
        S.op("dve", lambda e: e.tensor_tensor(out=as4(kbg[r][:]), in0=ktm_rep, in1=as4(bcast_last(beG[:, n, :], 128)), op=ALU.mult),
             reads=[pTall, beG], writes=[kbg[r]])
        S.op("dve", lambda e: e.tensor_tensor(out=as4(ktm[r][:]), in0=ktm_rep, in1=as4(bcast_last(ekt[:, n, :], 128)), op=ALU.mult),
             reads=[pTall, ekt], writes=[ktm[r]])
        S.op("dve", lambda e: e.tensor_tensor(out=vb[r][:], in0=pTall[0:64, 256:768].rearrange("p (a b) -> p a b", a=4),
                                              in1=bcast_last(bt[:, n, :], 128), op=ALU.mult), reads=[pTall, bt], writes=[vb[r]])
        yield
        S.op("dve", lambda e: e.tensor_tensor(out=rg1[r][:], in0=bcast_mid(MUI, 4), in1=bcast_last(gt[:, n, :], 64), op=ALU.mult),
             reads=[cst, gt], writes=[rg1[r]])
        S.op("dve", lambda e: e.tensor_tensor(out=rg2[r][:], in0=bcast_mid(I64, 4), in1=bcast_last(lnb[:, n, :], 64), op=ALU.mult),
             reads=[cst, lnb], writes=[rg2[r]])
        S.op("dve", lambda e: e.tensor_tensor(out=rg2[r][:], in0=rg2[r][:], in1=rg1[r][:], op=ALU.add), reads=[rg1[r], rg2[r]], writes=[rg2[r]])
        S.op("dve", lambda e: e.tensor_tensor(out=rg3[r][:], in0=bcast_mid(MLS, 4), in1=bcast_last(gt[:, n, :], 64), op=ALU.mult),
             reads=[cst, gt], writes=[rg3[r]])
        fl = lambda b_: b_[:].rearrange("p a b -> p (a b)")
        pd1 = pp.get()
        S.op("pe", lambda e: e.matmul(pd1[0:64, 0:256], MLS, fl(rg1[r]), start=True, stop=True), reads=[cst, rg1[r]], writes=[pd1])
        S.op("pe", lambda e: e.matmul(pd1[0:64, 256:512], MLS, fl(rg2[r]), start=True, stop=True), reads=[cst, rg2[r]], writes=[pd1])
        pd2 = pp.get()
        S.op("pe", lambda e: e.matmul(pd2[0:64, 0:256], MUI, fl(rg3[r]), start=True, stop=True), reads=[cst, rg3[r]], writes=[pd2])
        pd3 = pp.get()
        S.op("pe", lambda e: e.matmul(pd3[:, 0:256], ONES64x128, fl(rg1[r]), start=True, stop=True), reads=[cst, rg1[r]], writes=[pd3])
        S.op("act", lambda e: e.activation(out=fl(Et[r]), in_=pd1[0:64, 0:256], func=AF.Exp), reads=[pd1], writes=[Et[r]])
        S.op("act", lambda e: e.activation(out=fl(Wt_[r]), in_=pd1[0:64, 256:512], func=AF.Exp), reads=[pd1], writes=[Wt_[r]])
        for h in range(4):
            S.op("act", lambda e: e.activation(out=W_[r][:, h, :], in_=pd2[0:64, h * 64:(h + 1) * 64], func=AF.Exp,
                                               bias=lnb[:, n, h:h + 1], scale=1.0), reads=[pd2, lnb], writes=[W_[r]])
        S.op("act", lambda e: e.activation(out=fl(eGb[r]), in_=pd3[:, 0:256], func=AF.Exp), reads=[pd3], writes=[eGb[r]])
        yield
        pg = pp.get()
        for qh in range(2):
            S.op("pe", lambda e: e.matmul(pg[0:64, qh * 64:(qh + 1) * 64], kTb[:, qh, c0:c0 + CH], kTb[:, qh, c0:c0 + CH], start=True, stop=True),
                 reads=[kTb], writes=[pg])
            S.op("pe", lambda e: e.matmul(pg[0:64, 128 + qh * 64:128 + (qh + 1) * 64], kTb[:, qh, c0:c0 + CH], qTb[:, qh, c0:c0 + CH],
                                          start=True, stop=True), reads=[kTb, qTb], writes=[pg])
        kkv = pg[0:64, 0:128].rearrange("p (a b) -> p a b", a=2)
        kqv = pg[0:64, 128:256].rearrange("p (a b) -> p a b", a=2)
        S.op("dve", lambda e: e.tensor_tensor(out=KKlo[r][:], in0=kkv, in1=bcast_mid(NMLS, 2), op=ALU.mult), reads=[pg, cst], writes=[KKlo[r]])
        S.op("dve", lambda e: e.tensor_tensor(out=KKup[r][:], in0=kkv, in1=bcast_mid(NMUS, 2), op=ALU.mult), reads=[pg, cst], writes=[KKup[r]])
        S.op("dve", lambda e: e.tensor_tensor(out=KQm[r][:], in0=kqv, in1=bcast_mid(MUI, 2), op=ALU.mult), reads=[pg, cst], writes=[KQm[r]])
        yield
        rep = lambda b_: b_[:].unsqueeze(2).broadcast_to([64, 2, 2, 64])
        P0 = PP[0][r]
        S.op("dve", lambda e: e.tensor_tensor(out=as4(P0[:, 0:4, :]), in0=rep(KKlo[r]), in1=as4(W_[r][:]), op=ALU.mult),
             reads=[KKlo[r], W_[r]], writes=[P0])
        S.op("dve", lambda e: e.tensor_tensor(out=as4(P0[:, 4:8, :]), in0=rep(KKup[r]), in1=as4(Wt_[r][:]), op=ALU.mult),
             reads=[KKup[r], Wt_[r]], writes=[P0])
        S.op("dve", lambda e: e.tensor_tensor(out=as4(Qt[r][:]), in0=rep(KQm[r]), in1=as4(Et[r][:]), op=ALU.mult),
             reads=[KQm[r], Et[r]], writes=[Qt[r]])
        S.op("dve", lambda e: e.tensor_tensor(out=as4(qdT[r][:]), in0=qTb[:, :, c0:c0 + CH].unsqueeze(2).broadcast_to([128, 2, 2, 64]),
                                              in1=as4(eGb[r][:]), op=ALU.mult), reads=[qTb, eGb[r]], writes=[qdT[r]])
        yield
        X = Xt[0][r]
        S.op("dve", lambda e: e.tensor_tensor(out=X[:], in0=P0[:, 4:8, :], in1=bcast_mid(I64, 4), op=ALU.add), reads=[P0, cst], writes=[X])
        for k in range(1, 6):
            Pp, Pn = PP[(k - 1) % 2][r], PP[k % 2][r]
            pq = pp.get()
            for h in range(4):
                S.op("pe", lambda e: e.matmul(pq[0:64, h * 64:(h + 1) * 64], Pp[:, 4 + h, :], Pp[:, h, :], start=True, stop=True),
                     reads=[Pp], writes=[pq])
            if k < 5:
                for h in range(4):
                    S.op("pe", lambda e: e.matmul(pq[0:64, 256 + h * 64:256 + (h + 1) * 64], Pp[:, h, :], Pp[:, 4 + h, :], start=True, stop=True),
                         reads=[Pp], writes=[pq])
            wdt = 512 if k < 5 else 256
            S.op("act", lambda e: e.activation(out=Pn[:].rearrange("p a b -> p (a b)")[:, 0:wdt], in_=pq[0:64, 0:wdt], func=AF.Copy),
                 reads=[pq], writes=[Pn])
            yield
            px = pp.get()
            Xo = Xt[(k - 1) % 2][r]
            Xn = Xt[k % 2][r]
            for h in range(4):
                S.op("pe", lambda e: e.matmul(px[0:64, h * 64:(h + 1) * 64], Pn[:, h, :], Xo[:, h, :], start=True, stop=True),
                     reads=[Pn, Xo], writes=[px])
            yield
            if k < 5:
                S.op("dve", lambda e: e.tensor_tensor(out=fl(Xn), in0=px[0:64, 0:256], in1=fl(Xo), op=ALU.add), reads=[px, Xo], writes=[Xn])
            else:
                S.op("dve", lambda e: e.tensor_tensor(out=fl(Tt[r]), in0=px[0:64, 0:256], in1=fl(Xo), op=ALU.add), reads=[px, Xo], writes=[Tt[r]])
        yield
        pw = pp.get()
        for h in range(4):
            S.op("pe", lambda e: e.matmul(pw[:, h * 64:(h + 1) * 64], kbg[r][:, h, :], Tt[r][:, h, :], start=True, stop=True),
                 reads=[kbg[r], Tt[r]], writes=[pw])
        S.op("act", lambda e: e.activation(out=fl(nwT[r]), in_=pw[:, 0:256], func=AF.Copy, scale=-1.0), reads=[pw], writes=[nwT[r]])
        yield

    def stageB(n):
        ti, cj = n // CPT, n % CPT
        qTb, kTb, svb, onTb = qT[ti % 2], kT[ti % 2], svT[ti % 2], onT[ti % 2]
        r = n % NR
        c0 = cj * CH
        pu = pp.get()
        for h in range(4):
            S.op("pe", lambda e: e.matmul(pu[0:64, h * 128:(h + 1) * 128], Tt[r][:, h, :], vb[r][:, h, :], start=True, stop=False),
                 reads=[Tt[r], vb[r]], writes=[pu])
            S.op("pe", lambda e: e.matmul(pu[0:64, h * 128:(h + 1) * 128], nwT[r][:, h, :], S_b[:, h, :], start=False, stop=True),
                 reads=[nwT[r], S_b], writes=[pu])
        S.op("act", lambda e: e.activation(out=vn[r][:].rearrange("p a b -> p (a b)"), in_=pu[0:64, :], func=AF.Copy), reads=[pu], writes=[vn[r]])
        yield
        po = po2[n % 2]
        for h in range(4):
            S.op("pe", lambda e: e.matmul(po[0:64, h * 128:(h + 1) * 128], qdT[r][:, h, :], S_b[:, h, :], start=True, stop=False),
                 reads=[qdT[r], S_b], writes=[po])
            S.op("pe", lambda e: e.matmul(po[0:64, h * 128:(h + 1) * 128], Qt[r][:, h, :], vn[r][:, h, :], start=False, stop=True),
                 reads=[Qt[r], vn[r]], writes=[po])
        yield
        pS = pp.get()
        for h in range(4):
            S.op("pe", lambda e: e.matmul(pS[:, h * 128:(h + 1) * 128], ktm[r][:, h, :], vn[r][:, h, :], start=True, stop=True),
                 reads=[ktm[r], vn[r]], writes=[pS])
        for h in range(4):
            S.op("dve", lambda e: e.scalar_tensor_tensor(out=S_f[:, h, :], in0=S_f[:, h, :], scalar=eGl[:, n, h:h + 1],
                                                         in1=pS[:, h * 128:(h + 1) * 128], op0=ALU.mult, op1=ALU.add),
                 reads=[S_f, eGl, pS], writes=[S_f])
        S.op("act", lambda e: e.activation(out=S_b[:], in_=S_f[:], func=AF.Copy), reads=[S_f], writes=[S_b])
        yield
        S.op("act", lambda e: e.activation(out=sqo[r][:].rearrange("p a b -> p (a b)"), in_=po[0:64, :], func=AF.Square), reads=[po], writes=[sqo[r]])
        S.op("dve", lambda e: e.tensor_reduce(out=sso[r][:, 0:4], in_=sqo[r][:], axis=AX.X, op=ALU.add), reads=[sqo[r]], writes=[sso[r]])
        S.op("act", lambda e: e.activation(out=sso[r][:, 4:8], in_=sso[r][:, 0:4], func=AF.Ln, scale=1.0 / 128.0, bias=GDN_EPS),
             reads=[sso[r]], writes=[sso[r]])
        S.op("act", lambda e: e.activation(out=sso[r][:, 4:8], in_=sso[r][:, 4:8], func=AF.Exp, scale=-0.5), reads=[sso[r]], writes=[sso[r]])
        S.op("dve", lambda e: e.tensor_tensor(out=on[r][:], in0=po[0:64, :].rearrange("p (a b) -> p a b", a=4),
                                              in1=bcast_last(sso[r][:, 4:8], 128), op=ALU.mult), reads=[po, sso[r]], writes=[on[r]])
        yield
        for h in range(4):
            S.op("pe", lambda e: e.transpose(pTon[:, h * 64:(h + 1) * 64], on[r][:, h, :], ident_b[0:64, 0:64]),
                 reads=[on[r], ident_b], writes=[pTon])
        S.op("act", lambda e: e.activation(out=onTb[:, :, c0:c0 + CH], in_=pTon[:, 0:256].rearrange("p (a b) -> p a b", a=4), func=AF.Copy),
             reads=[pTon], writes=[onTb])
        yield

    def tile_finish(ti):
        zsb, onTb, ogb = zs[ti % 2], onT[ti % 2], ogt[ti % 2]
        S.op("dve", lambda e: e.scalar_tensor_tensor(out=ogb[:].rearrange("p a b -> p (a b)"), in0=onTb[:].rearrange("p a b -> p (a b)"),
                                                     scalar=gnw[:, 0:1], in1=zsb[:].rearrange("p a b -> p (a b)"), op0=ALU.mult, op1=ALU.mult),
             reads=[onTb, gnw, zsb], writes=[ogb])
        if ti >= 1:
            S.dma("pool", ogF[ti - 1][:, :].rearrange("p (c t) -> p c t", c=c_og), ogb[:], reads=[ogb], writes=[ogF[ti - 1]])
        if ti % 4 == 0 and ti <= 12:
            S.dma("pool", ogH[(ti // 4) * 128:(ti // 4 + 1) * 128, :].rearrange("p (c t) -> p c t", c=c_og), ogb[:, :, TS - 64:TS],
                  reads=[ogb], writes=[ogH])
        io["after_og"](ti)

    def interleave(gens):
        gens = [g for g in gens if g is not None]
        while gens:
            for g in list(gens):
                try:
                    next(g)
                except StopIteration:
                    gens.remove(g)

    load_a(0)
    tile_level(0)
    interleave([stageA(0)])
    NCK = NTS * CPT
    for n in range(NCK):
        nxt = None
        if n + 1 < NCK:
            if (n + 1) % CPT == 0:
                tile_level((n + 1) // CPT)
            nxt = stageA(n + 1)
        interleave([stageB(n), nxt])
        if (n + 1) % CPT == 0:
            tile_finish(n // CPT)
    S.pop()


def prep_M_gdn(inp, j, hg):
    w = inp["gdn_w_in"][j]
    cols = []
    for qh in range(2):
        cols.append(np.arange(128) + 128 * (2 * hg + qh))
    for qh in range(2):
        cols.append(1024 + np.arange(128) + 128 * (2 * hg + qh))
    for h in range(4):
        cols.append(2048 + np.arange(128) + 128 * (4 * hg + h))
    conv_cols = np.concatenate(cols)
    for h in range(4):
        cols.append(4096 + np.arange(128) + 128 * (4 * hg + h))
    cols.append(6144 + 4 * hg + np.arange(4))
    cols.append(6160 + 4 * hg + np.arange(4))
    cols = np.concatenate(cols)
    wc = w[:, cols].reshape(8, 128, G_WC).transpose(1, 0, 2)
    cw = inp["gdn_conv_w"][j][:, conv_cols].reshape(4, 8, 128).transpose(2, 1, 0)
    hv = np.concatenate([inp["gdn_a_log"][j][4 * hg:4 * hg + 4], inp["gdn_dt_bias"][j][4 * hg:4 * hg + 4]]).reshape(1, 8)
    return {"w": np.ascontiguousarray(wc.reshape(128, 8 * G_WC), np.float32),
            "cw": np.ascontiguousarray(cw.reshape(128, 32), np.float32),
            "hv": np.ascontiguousarray(hv, np.float32),
            "gnw": np.ascontiguousarray(inp["gdn_norm_w"][j].reshape(128, 1), np.float32),
            "cst": gdn_consts()}


GROUPS = [[0, 1, 2, 3], [4, 5, 6, 7]]


def build_fused(nl=4):
    nc = bass.Bass("TRN2", target_bir_lowering=False)
    es = ExitStack()
    S = Sched(nc, es)
    ext = lambda n, shp, dt=F32: S.dram(n, shp, dt, kind="ExternalInput")
    hs0 = ext("hs0", [D, WIN])
    keep = ext("keep", [1, WIN])
    gidx4 = ext("gidx4", [128, 20], mybir.dt.int32)
    gidx2 = ext("gidx2", [128, 20], mybir.dt.int32) if nl > 1 else None
    nwT0 = ext("nwT_0", [128, 32])
    cst_g = ext("cst_g", [128, C_TOT])
    cst_m = ext("cst_m", [128, 64 + 512 + 128]) if nl > 1 else None
    hs_out = S.dram("hs_out", [D, WIN], F32, kind="ExternalOutput")
    hs_loc = S.dram("hs_loc", [D, WIN], F32)
    def slices(name, rows_total, cols, rows_per):
        big = S.dram(name, [rows_total, cols], BF16)
        return big, [Buf(big.t[k * rows_per:(k + 1) * rows_per, :], "%s_%d" % (name, k)) for k in range(rows_total // rows_per)]

    ainF_all, ainF = slices("ainF", 512, 4096, 128)
    ainF_g, ainF_gs = slices("ainF_g", 2048, 4096, 512)
    ainH = S.dram("ainH", [128, 512], BF16)
    ainH_g = S.dram("ainH_g", [512, 512], BF16)
    og = {}
    for c in (4, 2):
        tpc = 8 // c
        ogF_all, ogF = slices("ogF%d" % c, 2048, c * 512, 128)
        ogF_g, _ = slices("ogF%d_g" % c, 8192, c * 512, 8192)
        nq = 16 // tpc
        og[c] = dict(ogF=ogF, ogF_all=ogF_all, ogH=S.dram("ogH%d" % c, [512, c * 64], BF16), tpc=tpc,
                     ogF_g=ogF_g, ogH_g=S.dram("ogH%d_g" % c, [2048, c * 64], BF16),
                     src=[Buf(ogF_all.t[q * tpc * 128:(q + 1) * tpc * 128, :], "ogsrc%d_%d" % (c, q)) for q in range(nq)],
                     dst=[Buf(ogF_g.t[q * 4 * tpc * 128:(q + 1) * 4 * tpc * 128, :], "ogdst%d_%d" % (c, q)) for q in range(nq)])
    lay = []
    for l in range(nl):
        KO = 2048 if l % 2 == 0 else 1024
        KC = KO // 128
        d = dict(nwT=ext("nwT_l%d" % l, [128, 32]), cw=ext("cw_l%d" % l, [128, 44 * 3]), cb=ext("cb_l%d" % l, [128, 44]),
                 wout_d=ext("wout_l%d" % l, [8, 128, KC * 128]), wup_d=ext("wup_l%d" % l, [NG, 128, 8 * 256]),
                 wdn_d=ext("wdn_l%d" % l, [8, 128, NG * 128]),
                 wout_b=S.dram("wout_b%d" % l, [8, 128, KC * 128], BF16), wup_b=S.dram("wup_b%d" % l, [NG, 128, 8 * 256], BF16),
                 wdn_b=S.dram("wdn_b%d" % l, [8, 128, NG * 128], BF16))
        if l % 2 == 0:
            d["m"] = dict(w=ext("gw_l%d" % l, [128, 8 * G_WC]), cw=ext("gcw_l%d" % l, [128, 32]), hv=ext("ghv_l%d" % l, [1, 8]),
                          gnw=ext("ggnw_l%d" % l, [128, 1]), cst=cst_g)
        else:
            d["m"] = dict(w=ext("mw_l%d" % l, [128, 8 * ML_WC]), gb=ext("mgb_l%d" % l, [1, 2]), nwv=ext("mnwv_l%d" % l, [1, ML_DV]), cst=cst_m)
        lay.append(d)

    def after_ain(ti):
        if ti == 0:
            S.coll("AllGather", ainH_g, ainH, GROUPS)
        else:
            S.coll("AllGather", ainF_gs[ti - 1], ainF[ti - 1], GROUPS)

    def mk_after_og(c):
        o = og[c]
        tpc = o["tpc"]

        def after_og(ti):
            wt = ti - 1
            if ti >= 1 and (wt + 1) % tpc == 0:
                q = wt // tpc
                src = o["src"][q]
                S.coll("AllGather", o["dst"][q], src, GROUPS, extra=[o["ogF"][k] for k in range(q * tpc, (q + 1) * tpc)])
            if ti == 12:
                S.coll("AllGather", o["ogH_g"], o["ogH"], GROUPS)
        return after_og

    emit_T(S, "t0", 2048, True, False, dict(hs_src=hs0, nwT=nwT0, ainF=ainF, ainH=ainH, after_ain=after_ain))
    for l in range(nl):
        d = lay[l]
        c = 4 if l % 2 == 0 else 2
        emit_casts(S, d)
        mio = dict(d["m"], ainF_g=ainF_g, ainH_g=ainH_g, ogF=og[c]["ogF"], ogH=og[c]["ogH"], after_og=mk_after_og(c))
        if l % 2 == 0:
            emit_M_gdn(S, "g%d" % l, mio)
        else:
            emit_M_ml(S, "m%d" % l, mio)
        last = l == nl - 1
        tio = dict(d, hs_src=(hs0 if l == 0 else hs_loc), hs_dst=(hs_out if last else hs_loc), ogF_g=og[c]["ogF_g"], ogH_g=og[c]["ogH_g"],
                   keep=keep, gidx=(gidx4 if c == 4 else gidx2), ainF=ainF, ainH=ainH, after_ain=after_ain)
        emit_T(S, "t%d" % (l + 1), 512 * c, False, last, tio)
    S.finish([hs_out])
    return nc, es


def kernel(x, meta_tokens, norm_w, gdn_w_in, gdn_conv_w, gdn_a_log, gdn_dt_bias, gdn_norm_w, gdn_w_out,
           ml_w_in, ml_gate_b, ml_norm_w, ml_w_out, ffn_w_up, ffn_conv_w, ffn_conv_b, ffn_w_down, _nl=4):
    inp = dict(x=x, meta_tokens=meta_tokens, norm_w=norm_w, gdn_w_in=gdn_w_in, gdn_conv_w=gdn_conv_w, gdn_a_log=gdn_a_log,
               gdn_dt_bias=gdn_dt_bias, gdn_norm_w=gdn_norm_w, gdn_w_out=gdn_w_out, ml_w_in=ml_w_in, ml_gate_b=ml_gate_b,
               ml_norm_w=ml_norm_w, ml_w_out=ml_w_out, ffn_w_up=ffn_w_up, ffn_conv_w=ffn_conv_w, ffn_conv_b=ffn_conv_b,
               ffn_w_down=ffn_w_down)
    inp = {k: np.asarray(v, np.float32) for k, v in inp.items()}
    shared = {"cst_g": gdn_consts(), "cst_m": ml_consts(),
              "nwT_0": np.ascontiguousarray(np.stack([_cm(inp["norm_w"][0, 0])] * 4, 1).reshape(128, 32), np.float32)}
    for l in range(_nl):
        t = prep_T(inp, l)
        for k, v in t.items():
            shared["%s_l%d" % (k, l)] = v
    maps = []
    for c in range(8):
        b, r = c // 4, c % 4
        m = dict(shared)
        h = np.zeros((LP, D), np.float32)
        h[XPAD + 48:XPAD + 64] = inp["meta_tokens"]
        h[XPAD + 64:] = inp["x"][b]
        lo = XPAD + 2048 * r
        m["hs0"] = np.ascontiguousarray(h[lo:lo + WIN].T)
        k = np.ones((1, WIN), np.float32)
        if r == 0:
            k[0, :48] = 0.0
        m["keep"] = k
        p = np.arange(128)
        for cc in (4, 2):
            tpc = 8 // cc
            gi = np.zeros((128, 20), np.int32)
            for hg in range(4):
                gi[:, hg * 5] = hg * 512 + r * 128 + p
                for i in range(1, 5):
                    wt = 4 * r + i - 1
                    gi[:, hg * 5 + i] = (wt // tpc) * (4 * tpc * 128) + hg * (tpc * 128) + (wt % tpc) * 128 + p
            m["gidx%d" % cc] = gi
        for l in range(_nl):
            if l % 2 == 0:
                g = prep_M_gdn(inp, l // 2, r)
                m["gw_l%d" % l], m["gcw_l%d" % l], m["ghv_l%d" % l], m["ggnw_l%d" % l] = g["w"], g["cw"], g["hv"], g["gnw"]
            else:
                g = prep_M_ml(inp, l // 2, r)
                m["mw_l%d" % l], m["mgb_l%d" % l], m["mnwv_l%d" % l] = g["w"], g["gb"], g["nwv"]
        maps.append(m)
    if _nl == 1:
        shared.pop("cst_m")
        for m in maps:
            m.pop("cst_m", None)
            m.pop("gidx2", None)
    nc, es = build_fused(_nl)
    res = run_bass_kernel_spmd(nc, maps, core_ids=list(range(8))).results
    out = np.zeros((NB, SEQ, D), np.float32)
    for c in range(8):
        b, r = c // 4, c % 4
        out[b, 2048 * r:2048 * (r + 1)] = res[c]["hs_out"][:, 64:].T
    return out
```

```python
from contextlib import ExitStack
import numpy as np
import ml_dtypes
import concourse.bass as bass
import concourse.mybir as mybir
from concourse.bass_utils import run_bass_kernel_spmd

F32 = mybir.dt.float32
BF16 = mybir.dt.bfloat16
AF = mybir.ActivationFunctionType
ALU = mybir.AluOpType
AX = mybir.AxisListType

D = 1024
SEQ = 8192
NB = 2
LP = 8704
XPAD = LP - SEQ - 64
WIN = 64 + 2048
FFN = 2816
NG = FFN // 128
RMS_EPS = 1e-6
CH = 64
NCH = LP // CH
TS = 512
NTS = LP // TS
CPT = TS // CH


class Buf:
    __slots__ = ("t", "lw", "rd", "sem", "semv", "name")

    def __init__(self, t, name=""):
        self.t = t
        self.lw = None
        self.rd = {}
        self.sem = None
        self.semv = 0
        self.name = name

    def __getitem__(self, k):
        return self.t[k]


class Sched:
    def __init__(self, nc, es):
        self.nc = nc
        self.es = es
        self.eng = {"pe": nc.tensor, "act": nc.scalar, "dve": nc.vector, "pool": nc.gpsimd, "sp": nc.sync}
        self.sem = {k: es.enter_context(nc.semaphore("sem_" + k)) for k in self.eng}
        self.cnt = {k: 0 for k in self.eng}
        self.seen = {k: {} for k in self.eng}
        self.nsem = 0
        self.out_events = []
        self.ninst = 0
        self.scopes = []
        self.dsems = []
        self.free_dsems = []
        self.scope_bufs = []

    def push(self, tag):
        self.scopes.append((ExitStack(), tag))
        self.scope_bufs.append([])

    def _own_sem(self, own):
        if own.sem is None:
            if self.free_dsems:
                own.sem, own.semv = self.free_dsems.pop()
            else:
                own.sem = self.es.enter_context(self.nc.semaphore("dsem%d" % self.nsem))
                self.nsem += 1
            self.dsems.append(own)

    def pop(self):
        self.barrier()
        st, _ = self.scopes.pop()
        st.close()
        for b in self.scope_bufs.pop():
            if b.sem is not None:
                self.free_dsems.append((b.sem, b.semv))
                self.dsems.remove(b)
                b.sem = None

    def _scope(self):
        return self.scopes[-1] if self.scopes else (self.es, "g")

    def sb(self, name, shape, dt):
        st, tag = self._scope()
        name = tag + "_" + name
        b = Buf(st.enter_context(self.nc.sbuf_tensor(name, list(shape), dt)), name)
        if self.scope_bufs:
            self.scope_bufs[-1].append(b)
        return b

    def ps(self, name, shape, dt=F32):
        st, tag = self._scope()
        name = tag + "_" + name
        return Buf(st.enter_context(self.nc.psum_tensor(name, list(shape), dt)), name)

    def barrier(self):
        for e in self.eng:
            eng = self.eng[e]
            for k in ("pe", "act", "dve", "pool", "sp"):
                if k != e and self.cnt[k] and self.seen[e].get(k, 0) < self.cnt[k]:
                    eng.wait_ge(self.sem[k], self.cnt[k])
                    self.seen[e][k] = self.cnt[k]
            for b in self.dsems:
                key = "d_" + b.name
                if self.seen[e].get(key, 0) < b.semv:
                    eng.wait_ge(b.sem, b.semv)
                    self.seen[e][key] = b.semv

    def dram(self, name, shape, dt, kind="Internal"):
        t = self.nc.dram_tensor(name, list(shape), dt, kind=kind)
        return Buf(t.ap(), name)

    def _deps(self, reads, writes):
        deps = {}

        def add(ev):
            if ev is None:
                return
            sem, val, key = ev
            if key not in deps or deps[key][1] < val:
                deps[key] = (sem, val)

        for b in reads:
            add(b.lw)
        for b in writes:
            add(b.lw)
            for ev in b.rd.values():
                add(ev)
        return deps

    def _wait(self, e, deps):
        eng = self.eng[e]
        for key, (sem, val) in deps.items():
            if e == "pe" and key == "pe":
                continue
            if self.seen[e].get(key, 0) >= val:
                continue
            eng.wait_ge(sem, val)
            self.seen[e][key] = val

    def _record(self, ev, reads, writes):
        for b in writes:
            b.lw = ev
            b.rd = {}
        for b in reads:
            if b not in writes:
                b.rd[ev[2]] = ev

    def op(self, e, fn, reads=(), writes=()):
        self._wait(e, self._deps(reads, writes))
        ins = fn(self.eng[e])
        self.cnt[e] += 1
        ins.then_inc(self.sem[e], 1)
        self.ninst += 1
        self._record((self.sem[e], self.cnt[e], e), reads, writes)

    def dma(self, q, out, in_, reads=(), writes=(), owner=None):
        self._wait(q, self._deps(reads, writes))
        own = owner if owner is not None else (writes[0] if writes else reads[0])
        self._own_sem(own)
        own.semv += 16
        ins = self.eng[q].dma_start(out=out, in_=in_)
        ins.then_inc(own.sem, 16)
        self.ninst += 1
        ev = (own.sem, own.semv, "d_" + own.name)
        self._record(ev, reads, writes)
        return ev

    def gather(self, out_ap, table_ap, idx_ap, reads=(), writes=()):
        self._wait("pool", self._deps(reads, writes))
        own = writes[0]
        self._own_sem(own)
        own.semv += 16
        ins = self.nc.gpsimd.indirect_dma_start(out=out_ap, out_offset=None, in_=table_ap,
                                                in_offset=bass.IndirectOffsetOnAxis(ap=idx_ap, axis=0))
        ins.then_inc(own.sem, 16)
        self.ninst += 1
        self._record((own.sem, own.semv, "d_" + own.name), reads, writes)

    def coll(self, kind, out, in_, groups, extra=()):
        self._wait("pool", self._deps([in_] + list(extra), [out]))
        self._own_sem(out)
        out.semv += 1
        ins = self.nc.gpsimd.collective_compute(kind, ALU.bypass, replica_groups=groups, ins=[in_.t.opt()], outs=[out.t.opt()])
        ins.then_inc(out.sem, 1)
        self.ninst += 1
        self._record((out.sem, out.semv, "d_" + out.name), [in_], [out])

    def finish(self, bufs):
        for b in bufs:
            if b.sem is not None:
                self.eng["sp"].wait_ge(b.sem, b.semv)
        for k in ("pe", "act", "dve", "pool"):
            if self.cnt[k]:
                self.eng["sp"].wait_ge(self.sem[k], self.cnt[k])


class PsumPool:
    def __init__(self, S, n, prefix="pb"):
        self.banks = [S.ps("%s%d" % (prefix, i), [128, 512]) for i in range(n)]
        self.i = 0

    def get(self):
        b = self.banks[self.i % len(self.banks)]
        self.i += 1
        return b


def bcast_mid(ap2, n):
    return ap2.unsqueeze(1).broadcast_to([ap2.shape[0], n, ap2.shape[1]])


def bcast_last(ap2, n):
    return ap2.unsqueeze(2).broadcast_to([ap2.shape[0], ap2.shape[1], n])


def rstd_from_ss(S, ss_ps, out_sb, scale, eps, W):
    S.op("act", lambda e: e.activation(out=out_sb[:, :W], in_=ss_ps[:, :W], func=AF.Ln, scale=scale, bias=eps),
         reads=[ss_ps], writes=[out_sb])
    S.op("act", lambda e: e.activation(out=out_sb[:, :W], in_=out_sb[:, :W], func=AF.Exp, scale=-0.5),
         reads=[out_sb], writes=[out_sb])


T_TILES = [(0, 64), (64, 512), (576, 512), (1088, 512), (1600, 512)]


def emit_casts(S, io):
    for m in range(8):
        S.dma("pool", io["wout_b"][m], io["wout_d"][m], writes=[io["wout_b"]])
    for g in range(NG):
        S.dma("pool", io["wup_b"][g], io["wup_d"][g], writes=[io["wup_b"]])
    for m in range(8):
        S.dma("pool", io["wdn_b"][m], io["wdn_d"][m], writes=[io["wdn_b"]])


def emit_T(S, tag, KO, first, last, io):
    S.push(tag)
    KC = KO // 128
    c_og = KC // 4
    hs_in = io["hs_src"]
    nwT_d = io["nwT"]
    if not last:
        ainF, ainH = io["ainF"], io["ainH"]
    if not first:
        ogF_g, ogH_g = io["ogF_g"], io["ogH_g"]
        keep_d, cw_d, cb_d = io["keep"], io["cw"], io["cb"]
        wout_b, wup_b, wdn_b = io["wout_b"], io["wup_b"], io["wdn_b"]
        hs_out = io["hs_dst"]
        gidx = S.sb("gidx", [128, 20], mybir.dt.int32)
        S.dma("sp", gidx[:], io["gidx"][:, :], writes=[gidx])

    ones_f = S.sb("ones_f", [128, 128], F32)
    ones_b = S.sb("ones_b", [128, 128], BF16)
    S.op("pool", lambda e: e.memset(ones_f[:], 1.0), writes=[ones_f])
    S.op("act", lambda e: e.activation(out=ones_b[:], in_=ones_f[:], func=AF.Copy), reads=[ones_f], writes=[ones_b])
    nwT = S.sb("nwT_sb", [128, 4, 8], F32)
    S.dma("sp", nwT[:].rearrange("p a b -> p (a b)"), nwT_d[:, :], writes=[nwT])

    hs_sb = [S.sb("hs_sb%d" % i, [128, 8, 512], F32) for i in range(2)]
    sq_sb = [S.sb("sq_sb%d" % i, [128, 512], BF16) for i in range(2)]
    rstd = S.sb("rstd", [128, 512], F32)
    a_sb = S.sb("a_sb", [128, 8, 512], BF16)
    pp = PsumPool(S, 7)
    ss_ps = S.ps("ss_ps", [128, 512])
    if not first:
        og_sb = [S.sb("og_sb%d" % i, [128, KC, 512], BF16) for i in range(2)]
        ogh_sb = S.sb("ogh_sb", [128, KC, 64], BF16)
        keep_sb = S.sb("keep_sb", [128, WIN], F32)
        S.dma("sp", keep_sb[:], keep_d[0:1, :].partition_broadcast(128), writes=[keep_sb])
        cw = S.sb("cw_sb", [128, 44, 3], F32)
        cb = S.sb("cb_sb", [128, 44], F32)
        S.dma("sp", cw[:].rearrange("p a b -> p (a b)"), cw_d[:, :], writes=[cw])
        S.dma("sp", cb[:], cb_d[:, :], writes=[cb])
        mix_sb = S.sb("mix_sb", [128, 8, 512], F32)
        rk = S.sb("rk", [128, 512], F32)
        tmp_sb = [S.sb("tmp_sb%d" % i, [128, 512], F32) for i in range(2)]
        h_sb = S.sb("h_sb", [128, NG, 512], BF16)
        u_sb = [S.sb("u_sb%d" % i, [128, 2, 2 + 512], F32) for i in range(2)]
        y_sb = [S.sb("y_sb%d" % i, [128, 2, 512], F32) for i in range(2)]
        e_sb = [S.sb("e_sb%d" % i, [128, 512], F32) for i in range(2)]
        halo = S.sb("halo", [128, 44, 2], F32)
        S.op("pool", lambda e: e.memset(halo[:], 0.0), writes=[halo])
        wo_s = [S.sb("wo_s%d" % i, [128, KC, 128], BF16) for i in range(3)]
        wu_s = [S.sb("wu_s%d" % i, [128, 8, 256], BF16) for i in range(4)]
        wd_s = [S.sb("wd_s%d" % i, [128, NG, 128], BF16) for i in range(3)]

    def norm_ss(src, W, eng_sq="act"):
        for m in range(8):
            sq = sq_sb[m % 2]
            S.op(eng_sq, lambda e: e.activation(out=sq[:, :W], in_=src[:, m, :W], func=AF.Square),
                 reads=[src], writes=[sq])
            S.op("pe", lambda e: e.matmul(ss_ps[:, :W], ones_b[:], sq[:, :W], start=(m == 0), stop=(m == 7)),
                 reads=[ones_b, sq], writes=[ss_ps])

    def load_tile(i):
        t0, W = T_TILES[i]
        hb = hs_sb[i % 2]
        S.dma("sp", hb[:, :, :W], hs_in[:, t0:t0 + W].rearrange("(c p) t -> p c t", p=128), writes=[hb])
        if not first:
            ob = ogh_sb if i == 0 else og_sb[i % 2]
            tab = ogH_g if i == 0 else ogF_g
            for hg in range(4):
                S.gather(ob[:, hg * c_og:(hg + 1) * c_og, :].rearrange("p c w -> p (c w)"), tab[:, :],
                         gidx[:, hg * 5 + i:hg * 5 + i + 1], reads=[gidx], writes=[ob])

    load_tile(0)
    for ti, (t0, W) in enumerate(T_TILES):
        if ti + 1 < len(T_TILES):
            load_tile(ti + 1)
        hb = hs_sb[ti % 2]
        if not first:
            ob = ogh_sb if ti == 0 else og_sb[ti % 2]
            S.dma("sp", wo_s[0][:].rearrange("p a b -> p (a b)"), wout_b[0], reads=[wout_b], writes=[wo_s[0]])
            S.dma("sp", wo_s[1][:].rearrange("p a b -> p (a b)"), wout_b[1], reads=[wout_b], writes=[wo_s[1]])
            for m in range(8):
                if m + 2 < 8:
                    S.dma("sp", wo_s[(m + 2) % 3][:].rearrange("p a b -> p (a b)"), wout_b[m + 2],
                          reads=[wout_b], writes=[wo_s[(m + 2) % 3]])
                ws = wo_s[m % 3]
                pb = pp.get()
                for kc in range(KC):
                    S.op("pe", lambda e: e.matmul(pb[:, :W], ws[:, kc, :], ob[:, kc, :W], start=(kc == 0), stop=(kc == KC - 1)),
                         reads=[ws, ob], writes=[pb])
                S.op("act", lambda e: e.activation(out=mix_sb[:, m, :W], in_=pb[:, :W], func=AF.Copy), reads=[pb], writes=[mix_sb])
                sq = sq_sb[m % 2]
                S.op("act", lambda e: e.activation(out=sq[:, :W], in_=pb[:, :W], func=AF.Square), reads=[pb], writes=[sq])
                S.op("pe", lambda e: e.matmul(ss_ps[:, :W], ones_b[:], sq[:, :W], start=(m == 0), stop=(m == 7)),
                     reads=[ones_b, sq], writes=[ss_ps])
            rstd_from_ss(S, ss_ps, rstd, 1.0 / D, RMS_EPS, W)
            S.op("dve", lambda e: e.tensor_tensor(out=rk[:, :W], in0=rstd[:, :W], in1=keep_sb[:, t0:t0 + W], op=ALU.mult),
                 reads=[rstd, keep_sb], writes=[rk])
            for m in range(8):
                tb = tmp_sb[m % 2]
                S.op("dve", lambda e: e.tensor_tensor(out=tb[:, :W], in0=mix_sb[:, m, :W], in1=rk[:, :W], op=ALU.mult),
                     reads=[mix_sb, rk], writes=[tb])
                S.op("dve", lambda e: e.scalar_tensor_tensor(out=hb[:, m, :W], in0=tb[:, :W], scalar=nwT[:, 1, m:m + 1],
                                                             in1=hb[:, m, :W], op0=ALU.mult, op1=ALU.add),
                     reads=[tb, nwT, hb], writes=[hb])
            norm_ss(hb, W)
            rstd_from_ss(S, ss_ps, rstd, 1.0 / D, RMS_EPS, W)
            for m in range(8):
                S.op("dve", lambda e: e.scalar_tensor_tensor(out=a_sb[:, m, :W], in0=hb[:, m, :W], scalar=nwT[:, 2, m:m + 1],
                                                             in1=rstd[:, :W], op0=ALU.mult, op1=ALU.mult),
                     reads=[hb, nwT, rstd], writes=[a_sb])
            for g0 in range(3):
                S.dma("sp", wu_s[g0][:].rearrange("p a b -> p (a b)"), wup_b[g0], reads=[wup_b], writes=[wu_s[g0]])
            for g in range(NG):
                if g + 3 < NG:
                    S.dma("sp", wu_s[(g + 3) % 4][:].rearrange("p a b -> p (a b)"), wup_b[g + 3],
                          reads=[wup_b], writes=[wu_s[(g + 3) % 4]])
                ws = wu_s[g % 4]
                ub = u_sb[g % 2]
                yb = y_sb[g % 2]
                eb = e_sb[g % 2]
                for hf in range(2):
                    ci = g + hf * NG
                    pb = pp.get()
                    for kc in range(8):
                        S.op("pe", lambda e: e.matmul(pb[:, :W], ws[:, kc, hf * 128:(hf + 1) * 128], a_sb[:, kc, :W],
                                                      start=(kc == 0), stop=(kc == 7)),
                             reads=[ws, a_sb], writes=[pb])
                    S.op("pool", lambda e: e.tensor_copy(out=ub[:, hf, 0:2], in_=halo[:, ci, :]), reads=[halo], writes=[ub])
                    S.op("act", lambda e: e.activation(out=ub[:, hf, 2:2 + W], in_=pb[:, :W], func=AF.Copy), reads=[pb], writes=[ub])
                    S.op("pool", lambda e: e.tensor_copy(out=halo[:, ci, :], in_=ub[:, hf, W:W + 2]), reads=[ub], writes=[halo])
                    S.op("act", lambda e: e.activation(out=yb[:, hf, :W], in_=pb[:, :W], func=AF.Identity,
                                                       scale=cw[:, ci, 2:3], bias=cb[:, ci:ci + 1]),
                         reads=[pb, cw, cb], writes=[yb])
                    S.op("dve", lambda e: e.scalar_tensor_tensor(out=yb[:, hf, :W], in0=ub[:, hf, 1:1 + W], scalar=cw[:, ci, 1:2],
                                                                 in1=yb[:, hf, :W], op0=ALU.mult, op1=ALU.add),
                         reads=[ub, cw, yb], writes=[yb])
                    S.op("dve", lambda e: e.scalar_tensor_tensor(out=yb[:, hf, :W], in0=ub[:, hf, 0:W], scalar=cw[:, ci, 0:1],
                                                                 in1=yb[:, hf, :W], op0=ALU.mult, op1=ALU.add),
                         reads=[ub, cw, yb], writes=[yb])
                S.op("act", lambda e: e.activation(out=eb[:, :W], in_=yb[:, 0, :W], func=AF.Silu), reads=[yb], writes=[eb])
                S.op("dve", lambda e: e.tensor_tensor(out=h_sb[:, g, :W], in0=yb[:, 1, :W], in1=eb[:, :W], op=ALU.mult),
                     reads=[yb, eb], writes=[h_sb])
            S.dma("sp", wd_s[0][:].rearrange("p a b -> p (a b)"), wdn_b[0], reads=[wdn_b], writes=[wd_s[0]])
            S.dma("sp", wd_s[1][:].rearrange("p a b -> p (a b)"), wdn_b[1], reads=[wdn_b], writes=[wd_s[1]])
            for m in range(8):
                if m + 2 < 8:
                    S.dma("sp", wd_s[(m + 2) % 3][:].rearrange("p a b -> p (a b)"), wdn_b[m + 2],
                          reads=[wdn_b], writes=[wd_s[(m + 2) % 3]])
                ws = wd_s[m % 3]
                pb = pp.get()
                for kc in range(NG):
                    S.op("pe", lambda e: e.matmul(pb[:, :W], ws[:, kc, :], h_sb[:, kc, :W], start=(kc == 0), stop=(kc == NG - 1)),
                         reads=[ws, h_sb], writes=[pb])
                S.op("act", lambda e: e.activation(out=mix_sb[:, m, :W], in_=pb[:, :W], func=AF.Copy), reads=[pb], writes=[mix_sb])
                sq = sq_sb[m % 2]
                S.op("act", lambda e: e.activation(out=sq[:, :W], in_=pb[:, :W], func=AF.Square), reads=[pb], writes=[sq])
                S.op("pe", lambda e: e.matmul(ss_ps[:, :W], ones_b[:], sq[:, :W], start=(m == 0), stop=(m == 7)),
                     reads=[ones_b, sq], writes=[ss_ps])
            rstd_from_ss(S, ss_ps, rstd, 1.0 / D, RMS_EPS, W)
            S.op("dve", lambda e: e.tensor_tensor(out=rk[:, :W], in0=rstd[:, :W], in1=keep_sb[:, t0:t0 + W], op=ALU.mult),
                 reads=[rstd, keep_sb], writes=[rk])
            for m in range(8):
                tb = tmp_sb[m % 2]
                S.op("dve", lambda e: e.tensor_tensor(out=tb[:, :W], in0=mix_sb[:, m, :W], in1=rk[:, :W], op=ALU.mult),
                     reads=[mix_sb, rk], writes=[tb])
                S.op("dve", lambda e: e.scalar_tensor_tensor(out=hb[:, m, :W], in0=tb[:, :W], scalar=nwT[:, 3, m:m + 1],
                                                             in1=hb[:, m, :W], op0=ALU.mult, op1=ALU.add),
                     reads=[tb, nwT, hb], writes=[hb])
            S.dma("pool", hs_out[:, t0:t0 + W].rearrange("(c p) t -> p c t", p=128), hb[:, :, :W], reads=[hb], owner=hs_out)
        if not last:
            norm_ss(hb, W)
            rstd_from_ss(S, ss_ps, rstd, 1.0 / D, RMS_EPS, W)
            for m in range(8):
                S.op("dve", lambda e: e.scalar_tensor_tensor(out=a_sb[:, m, :W], in0=hb[:, m, :W], scalar=nwT[:, 0, m:m + 1],
                                                             in1=rstd[:, :W], op0=ALU.mult, op1=ALU.mult),
                     reads=[hb, nwT, rstd], writes=[a_sb])
            if ti == 0:
                S.dma("pool", ainH[:, :].rearrange("p (c t) -> p c t", c=8), a_sb[:, :, :W], reads=[a_sb], writes=[ainH])
            else:
                S.dma("pool", ainF[ti - 1][:, :].rearrange("p (c t) -> p c t", c=8), a_sb[:, :, :W], reads=[a_sb], writes=[ainF[ti - 1]])
            io["after_ain"](ti)
    S.pop()


def _cm(v):
    return np.ascontiguousarray(v.reshape(-1, 128).T)


def prep_T(inp, layer):
    j = layer // 2
    if layer % 2 == 0:
        w_out = inp["gdn_w_out"][j]
    else:
        w_out = inp["ml_w_out"][j]
    KO = w_out.shape[0]
    KC = KO // 128
    nw = inp["norm_w"]
    nxt = nw[layer + 1, 0] if layer + 1 < 4 else nw[layer, 0]
    nwT = np.stack([_cm(nxt), _cm(nw[layer, 1]), _cm(nw[layer, 2]), _cm(nw[layer, 3])], 1)
    wout = w_out.reshape(KC, 128, 8, 128).transpose(2, 1, 0, 3)
    wu = inp["ffn_w_up"][layer].reshape(8, 128, 2, NG, 128).transpose(3, 1, 0, 2, 4)
    wd = inp["ffn_w_down"][layer].reshape(NG, 128, 8, 128).transpose(2, 1, 0, 3)
    cw = inp["ffn_conv_w"][layer].reshape(3, 44, 128).transpose(2, 1, 0)
    cb = _cm(inp["ffn_conv_b"][layer])
    return {
        "nwT": np.ascontiguousarray(nwT.reshape(128, 32), np.float32),
        "wout": np.ascontiguousarray(wout.reshape(8, 128, KC * 128), np.float32),
        "wup": np.ascontiguousarray(wu.reshape(NG, 128, 8 * 256), np.float32),
        "wdn": np.ascontiguousarray(wd.reshape(8, 128, NG * 128), np.float32),
        "cw": np.ascontiguousarray(cw.reshape(128, 44 * 3), np.float32),
        "cb": np.ascontiguousarray(cb, np.float32),
    }


ML_DK = 128
ML_DV = 256
ML_WC = 128 + 128 + 128 + 256 + 256 + 2
BIGNEG = 30000.0


def emit_M_ml(S, tag, io):
    S.push(tag)
    ainF_g, ainH_g = io["ainF_g"], io["ainH_g"]
    w_d, gb_d, nwv_d, cst_d = io["w"], io["gb"], io["nwv"], io["cst"]
    ogF, ogH = io["ogF"], io["ogH"]
    c_og = 2

    w_sb = S.sb("w_sb", [128, 8, ML_WC], BF16)
    S.dma("pool", w_sb[:].rearrange("p a b -> p (a b)"), w_d[:, :], writes=[w_sb])
    wg_sb = S.sb("wg_sb", [128, 8, 2], BF16)
    cst = S.sb("cst_sb", [128, 64 + 512 + 128], F32)
    S.dma("sp", cst[:], cst_d[:, :], writes=[cst])
    identf = cst
    ident_b = S.sb("ident_b", [128, 128], BF16)
    S.op("act", lambda e: e.activation(out=ident_b[:], in_=cst[:, 576:704], func=AF.Copy), reads=[cst], writes=[ident_b])
    gb = S.sb("gb_sb", [1, 2], F32)
    S.dma("sp", gb[:], gb_d[:, :], writes=[gb])
    nwv = S.sb("nwv_sb", [64, ML_DV], F32)
    S.dma("sp", nwv[:], nwv_d[0:1, :].partition_broadcast(64), writes=[nwv])
    ones_row = S.sb("ones_row", [1, 128], F32)
    S.op("pool", lambda e: e.memset(ones_row[:], 1.0), writes=[ones_row])

    a_sb = [S.sb("a_sb%d" % i, [128, 8, 512], BF16) for i in range(2)]
    pp = PsumPool(S, 5)
    pia2 = [S.ps("pia%d" % i, [128, 512]) for i in range(2)]

    li_row = S.sb("li_row", [1, LP], F32)
    lf_row = S.sb("lf_row", [1, LP], F32)
    bb_row = S.sb("bb_row", [1, LP], F32)
    ones_bc = ones_row[0:1, 0:1].broadcast_to([1, LP])

    def load_a(i):
        ab = a_sb[i % 2]
        if i == 0:
            S.op("pool", lambda e: e.memset(ab[:, :, 0:XPAD], 0.0), writes=[ab])
            S.dma("sp", ab[:, :, XPAD:TS], ainH_g[0:128, :].rearrange("p (c t) -> p c t", c=8), reads=[ainH_g], writes=[ab])
        else:
            r0 = ((i - 1) % 4) * 512 + ((i - 1) // 4) * 128
            S.dma("sp", ab[:].rearrange("p c t -> p (c t)"), ainF_g[r0:r0 + 128, :], reads=[ainF_g], writes=[ab])

    load_a(0)
    for ti in range(NTS):
        if ti + 1 < NTS:
            load_a(ti + 1)
        ab = a_sb[ti % 2]
        for gi_, row in ((0, li_row), (1, lf_row)):
            pr = pp.get()
            for kc in range(8):
                S.op("pe", lambda e: e.matmul(pr[0:1, :], w_sb[:, kc, ML_WC - 2 + gi_:ML_WC - 1 + gi_], ab[:, kc, :],
                                              start=(kc == 0), stop=(kc == 7)), reads=[w_sb, ab], writes=[pr])
            S.op("act", lambda e: e.activation(out=row[:, ti * TS:(ti + 1) * TS], in_=pr[0:1, :], func=AF.Identity,
                                               bias=gb[:, gi_:gi_ + 1], scale=1.0), reads=[pr, gb], writes=[row])
    for row in (li_row, lf_row):
        S.op("act", lambda e: e.activation(out=row[:], in_=row[:], func=AF.Exp, scale=2.0 / 15.0), reads=[row], writes=[row])
        S.op("dve", lambda e: e.tensor_scalar_add(out=row[:], in0=row[:], scalar1=1.0), reads=[row], writes=[row])
        S.op("dve", lambda e: e.reciprocal(out=row[:], in_=row[:]), reads=[row], writes=[row])
        S.op("dve", lambda e: e.tensor_scalar(out=row[:], in0=row[:], scalar1=-30.0, scalar2=15.0, op0=ALU.mult, op1=ALU.add),
             reads=[row], writes=[row])
    S.op("act", lambda e: e.activation(out=lf_row[:], in_=lf_row[:], func=AF.Exp, scale=-1.0), reads=[lf_row], writes=[lf_row])
    S.op("act", lambda e: e.activation(out=lf_row[:], in_=lf_row[:], func=AF.Ln, bias=1.0, scale=1.0), reads=[lf_row], writes=[lf_row])
    S.op("dve", lambda e: e.tensor_scalar_mul(out=lf_row[:], in0=lf_row[:], scalar1=-1.0), reads=[lf_row], writes=[lf_row])
    S.op("pool", lambda e: e.memset(lf_row[:, 0:XPAD], 0.0), writes=[lf_row])
    S.op("pool", lambda e: e.memset(li_row[:, 0:XPAD], -BIGNEG), writes=[li_row])
    S.op("dve", lambda e: e.tensor_tensor_scan(out=bb_row[:], data0=ones_bc, data1=lf_row[:], initial=0.0,
                                               op0=ALU.mult, op1=ALU.add), reads=[ones_row, lf_row], writes=[bb_row])
    c_row = lf_row
    S.op("dve", lambda e: e.tensor_tensor(out=c_row[:], in0=li_row[:], in1=bb_row[:], op=ALU.subtract), reads=[li_row, bb_row], writes=[c_row])
    M_row = li_row
    S.op("dve", lambda e: e.tensor_tensor_scan(out=M_row[:], data0=ones_bc, data1=c_row[:], initial=0.0,
                                               op0=ALU.mult, op1=ALU.max), reads=[ones_row, c_row], writes=[M_row])
    en_row = bb_row
    S.op("dve", lambda e: e.tensor_tensor(out=en_row[:], in0=bb_row[:], in1=M_row[:], op=ALU.add), reads=[bb_row, M_row], writes=[en_row])
    S.op("act", lambda e: e.activation(out=en_row[:], in_=en_row[:], func=AF.Exp, scale=-1.0), reads=[en_row], writes=[en_row])

    c_tm = S.sb("c_tm", [64, NCH], F32)
    M_tm = S.sb("M_tm", [64, NCH], F32)
    en_tm = S.sb("en_tm", [64, NCH], F32)
    for row, tmb in ((c_row, c_tm), (M_row, M_tm), (en_row, en_tm)):
        pb = pp.get()
        for n in range(NCH):
            S.op("pe", lambda e: e.matmul(pb[0:64, n:n + 1], row[0:1, n * CH:(n + 1) * CH], ones_row[0:1, 0:1], start=True, stop=True),
                 reads=[row, ones_row], writes=[pb])
        S.op("act", lambda e: e.activation(out=tmb[:], in_=pb[0:64, 0:NCH], func=AF.Copy), reads=[pb], writes=[tmb])
    Mend_b = S.sb("Mend_b", [128, NCH], F32)
    Mprev_b = S.sb("Mprev_b", [128, NCH], F32)
    pb = pp.get()
    S.op("pe", lambda e: e.matmul(pb[:, 0:NCH], ones_row[0:1, :], M_row[0:1, CH - 1::CH], start=True, stop=True),
         reads=[ones_row, M_row], writes=[pb])
    S.op("act", lambda e: e.activation(out=Mend_b[:], in_=pb[:, 0:NCH], func=AF.Copy), reads=[pb], writes=[Mend_b])
    S.op("pool", lambda e: e.memset(Mprev_b[:, 0:1], 0.0), writes=[Mprev_b])
    S.op("pool", lambda e: e.tensor_copy(out=Mprev_b[:, 1:NCH], in_=Mend_b[:, 0:NCH - 1]), reads=[Mend_b], writes=[Mprev_b])
    sc_tm = S.sb("sc_tm", [64, NCH], F32)
    kws_tm = S.sb("kws_tm", [64, NCH], F32)
    dec_b = S.sb("dec_b", [128, NCH], F32)
    S.op("dve", lambda e: e.tensor_tensor(out=sc_tm[:], in0=Mprev_b[0:64, :], in1=M_tm[:], op=ALU.subtract), reads=[Mprev_b, M_tm], writes=[sc_tm])
    S.op("act", lambda e: e.activation(out=sc_tm[:], in_=sc_tm[:], func=AF.Exp), reads=[sc_tm], writes=[sc_tm])
    S.op("dve", lambda e: e.tensor_tensor(out=kws_tm[:], in0=c_tm[:], in1=Mend_b[0:64, :], op=ALU.subtract), reads=[c_tm, Mend_b], writes=[kws_tm])
    S.op("act", lambda e: e.activation(out=kws_tm[:], in_=kws_tm[:], func=AF.Exp), reads=[kws_tm], writes=[kws_tm])
    S.op("dve", lambda e: e.tensor_tensor(out=dec_b[:], in0=Mprev_b[:], in1=Mend_b[:], op=ALU.subtract), reads=[Mprev_b, Mend_b], writes=[dec_b])
    S.op("act", lambda e: e.activation(out=dec_b[:], in_=dec_b[:], func=AF.Exp), reads=[dec_b], writes=[dec_b])

    qT = [S.sb("qT%d" % i, [128, 512], BF16) for i in range(2)]
    kT = [S.sb("kT%d" % i, [128, 512], BF16) for i in range(2)]
    Wt = [S.sb("Wt%d" % i, [64, 512], F32) for i in range(2)]
    ogT = [S.sb("ogT%d" % i, [128, 2, 512], BF16) for i in range(2)]
    C_f = S.sb("C_f", [128, ML_DV + 1], F32)
    C_b = S.sb("C_b", [128, ML_DV + 1], BF16)
    S.op("pool", lambda e: e.memset(C_f[:], 0.0), writes=[C_f])
    S.op("pool", lambda e: e.memset(C_b[:], 0.0), writes=[C_b])
    NR = 3
    kw_sb = [S.sb("kw_sb%d" % i, [64, 128], BF16) for i in range(NR)]
    va_sb = [S.sb("va_sb%d" % i, [64, ML_DV + 1], BF16) for i in range(NR)]
    og_sb = [S.sb("ogs%d" % i, [64, ML_DV], F32) for i in range(NR)]
    St_sb = [S.sb("St%d" % i, [64, 64], BF16) for i in range(NR)]
    t1_sb = [S.sb("t1_%d" % i, [64, ML_DV + 1], F32) for i in range(NR)]
    sm_sb = [S.sb("sm%d" % i, [64, 8], F32) for i in range(NR)]
    junk = [S.sb("junk%d" % i, [64, ML_DV], F32) for i in range(NR)]
    hn_sb = [S.sb("hn%d" % i, [64, ML_DV], BF16) for i in range(NR)]
    for i in range(NR):
        S.op("pool", lambda e: e.memset(va_sb[i][:, ML_DV:ML_DV + 1], 1.0), writes=[va_sb[i]])
    pTall = S.ps("pTall", [128, 1024], BF16)

    def tile_level(ti):
        if ti + 1 < NTS:
            load_a(ti + 1)
        ab = a_sb[ti % 2]
        t0 = ti * TS
        qTb, kTb, Wtb, ogTb = qT[ti % 2], kT[ti % 2], Wt[ti % 2], ogT[ti % 2]
        for wi, (dst, scl) in enumerate(((qTb, ML_DK ** -0.5), (kTb, 1.0))):
            pb = pp.get()
            for kc in range(8):
                S.op("pe", lambda e: e.matmul(pb[:, :], w_sb[:, kc, wi * 128:(wi + 1) * 128], ab[:, kc, :], start=(kc == 0), stop=(kc == 7)),
                     reads=[w_sb, ab], writes=[pb])
            S.op("act", lambda e: e.activation(out=dst[:], in_=pb[:, :], func=AF.Copy, scale=scl), reads=[pb], writes=[dst])
        pb = pp.get()
        S.op("pe", lambda e: e.matmul(pb[0:64, :], ones_row[0:1, 0:64], M_row[0:1, t0:t0 + TS], start=True, stop=False),
             reads=[ones_row, M_row], writes=[pb])
        S.op("pe", lambda e: e.matmul(pb[0:64, :], cst[0:64, 0:64], cst[0:64, 64:576], start=False, stop=True),
             reads=[cst], writes=[pb])
        S.op("dve", lambda e: e.tensor_tensor(out=Wtb[:].rearrange("p (n i) -> p n i", n=CPT), in0=pb[0:64, :].rearrange("p (n i) -> p n i", n=CPT),
                                              in1=bcast_last(c_tm[:, ti * CPT:(ti + 1) * CPT], CH), op=ALU.subtract),
             reads=[pb, c_tm], writes=[Wtb])
        S.op("act", lambda e: e.activation(out=Wtb[:], in_=Wtb[:], func=AF.Exp, scale=-1.0), reads=[Wtb], writes=[Wtb])

    def stageA(n):
        ti, cj = n // CPT, n % CPT
        ab = a_sb[ti % 2]
        qTb, kTb, Wtb, ogTb = qT[ti % 2], kT[ti % 2], Wt[ti % 2], ogT[ti % 2]
        r = n % NR
        c0 = cj * CH
        pkv = pp.get()
        for kc in range(8):
            S.op("pe", lambda e: e.matmul(pkv[0:64, 0:384], ab[:, kc, c0:c0 + CH], w_sb[:, kc, 256:640], start=(kc == 0), stop=(kc == 7)),
                 reads=[w_sb, ab], writes=[pkv])
        pog = pp.get()
        for kc in range(8):
            S.op("pe", lambda e: e.matmul(pog[0:64, 0:256], ab[:, kc, c0:c0 + CH], w_sb[:, kc, 640:896], start=(kc == 0), stop=(kc == 7)),
                 reads=[w_sb, ab], writes=[pog])
        S.op("act", lambda e: e.activation(out=kw_sb[r][:], in_=pkv[0:64, 0:128], func=AF.Copy, scale=kws_tm[:, n:n + 1]),
             reads=[pkv, kws_tm], writes=[kw_sb[r]])
        S.op("act", lambda e: e.activation(out=va_sb[r][:, 0:ML_DV], in_=pkv[0:64, 128:384], func=AF.Copy), reads=[pkv], writes=[va_sb[r]])
        S.op("act", lambda e: e.activation(out=og_sb[r][:], in_=pog[0:64, 0:256], func=AF.Exp, scale=-1.0), reads=[pog], writes=[og_sb[r]])
        yield
        S.op("dve", lambda e: e.tensor_scalar_add(out=og_sb[r][:], in0=og_sb[r][:], scalar1=1.0), reads=[og_sb[r]], writes=[og_sb[r]])
        S.op("dve", lambda e: e.reciprocal(out=og_sb[r][:], in_=og_sb[r][:]), reads=[og_sb[r]], writes=[og_sb[r]])
        S.op("dve", lambda e: e.tensor_tensor(out=og_sb[r][:], in0=og_sb[r][:], in1=nwv[:], op=ALU.mult), reads=[og_sb[r], nwv], writes=[og_sb[r]])
        pq = pp.get()
        S.op("pe", lambda e: e.matmul(pq[0:64, 0:64], kTb[:, c0:c0 + CH], qTb[:, c0:c0 + CH], start=True, stop=True),
             reads=[kTb, qTb], writes=[pq])
        S.op("dve", lambda e: e.tensor_tensor(out=St_sb[r][:], in0=pq[0:64, 0:64], in1=Wtb[:, c0:c0 + CH], op=ALU.mult),
             reads=[pq, Wtb], writes=[St_sb[r]])
        yield
        pia = pia2[n % 2]
        S.op("pe", lambda e: e.matmul(pia[0:64, 0:ML_DV + 1], St_sb[r][:], va_sb[r][:], start=True, stop=True),
             reads=[St_sb[r], va_sb[r]], writes=[pia])
        yield

    def stageB(n):
        ti, cj = n // CPT, n % CPT
        ab = a_sb[ti % 2]
        qTb, kTb, Wtb, ogTb = qT[ti % 2], kT[ti % 2], Wt[ti % 2], ogT[ti % 2]
        r = n % NR
        c0 = cj * CH
        pia = pia2[n % 2]
        pint = pp.get()
        S.op("pe", lambda e: e.matmul(pint[0:64, 0:ML_DV + 1], qTb[:, c0:c0 + CH], C_b[:], start=True, stop=True),
             reads=[qTb, C_b], writes=[pint])
        pst = pp.get()
        S.op("pe", lambda e: e.matmul(pst[:, 0:ML_DV + 1], kw_sb[r][:], va_sb[r][:], start=True, stop=True),
             reads=[kw_sb[r], va_sb[r]], writes=[pst])
        S.op("dve", lambda e: e.scalar_tensor_tensor(out=C_f[:], in0=C_f[:], scalar=dec_b[:, n:n + 1], in1=pst[:, 0:ML_DV + 1],
                                                     op0=ALU.mult, op1=ALU.add), reads=[C_f, dec_b, pst], writes=[C_f])
        S.op("act", lambda e: e.activation(out=C_b[:], in_=C_f[:], func=AF.Copy), reads=[C_f], writes=[C_b])
        t1 = t1_sb[r]
        S.op("act", lambda e: e.activation(out=t1[:], in_=pint[0:64, 0:ML_DV + 1], func=AF.Copy, scale=sc_tm[:, n:n + 1]),
             reads=[pint, sc_tm], writes=[t1])
        S.op("dve", lambda e: e.tensor_tensor(out=t1[:], in0=t1[:], in1=pia[0:64, 0:ML_DV + 1], op=ALU.add), reads=[t1, pia], writes=[t1])
        yield
        sm = sm_sb[r]
        S.op("act", lambda e: e.activation(out=sm[:, 0:1], in_=t1[:, ML_DV:ML_DV + 1], func=AF.Abs), reads=[t1], writes=[sm])
        S.op("dve", lambda e: e.tensor_tensor(out=sm[:, 0:1], in0=sm[:, 0:1], in1=en_tm[:, n:n + 1], op=ALU.max),
             reads=[sm, en_tm], writes=[sm])
        S.op("dve", lambda e: e.reciprocal(out=sm[:, 1:2], in_=sm[:, 0:1]), reads=[sm], writes=[sm])
        S.op("act", lambda e: e.activation(out=junk[r][:], in_=t1[:, 0:ML_DV], func=AF.Square, scale=sm[:, 1:2], accum_out=sm[:, 2:3]),
             reads=[t1, sm], writes=[junk[r], sm])
        S.op("act", lambda e: e.activation(out=sm[:, 3:4], in_=sm[:, 2:3], func=AF.Ln, scale=1.0 / ML_DV, bias=RMS_EPS), reads=[sm], writes=[sm])
        S.op("act", lambda e: e.activation(out=sm[:, 3:4], in_=sm[:, 3:4], func=AF.Exp, scale=-0.5), reads=[sm], writes=[sm])
        S.op("dve", lambda e: e.tensor_tensor(out=sm[:, 4:5], in0=sm[:, 3:4], in1=sm[:, 1:2], op=ALU.mult), reads=[sm], writes=[sm])
        S.op("dve", lambda e: e.scalar_tensor_tensor(out=hn_sb[r][:], in0=t1[:, 0:ML_DV], scalar=sm[:, 4:5], in1=og_sb[r][:],
                                                     op0=ALU.mult, op1=ALU.mult), reads=[t1, sm, og_sb[r]], writes=[hn_sb[r]])
        yield
        ptv = pTall[:, (n % 4) * 128:(n % 4 + 1) * 128].rearrange("p (a b) -> p a b", a=2)
        for hh in range(2):
            S.op("pe", lambda e: e.transpose(ptv[:, hh, :], hn_sb[r][:, hh * 128:(hh + 1) * 128], ident_b[0:64, 0:64]),
                 reads=[hn_sb[r], ident_b], writes=[pTall])
        S.op("act", lambda e: e.activation(out=ogTb[:, :, c0:c0 + CH], in_=ptv, func=AF.Copy), reads=[pTall], writes=[ogTb])
        yield

    def tile_finish(ti):
        ogTb = ogT[ti % 2]
        if ti >= 1:
            S.dma("pool", ogF[ti - 1][:, :].rearrange("p (c t) -> p c t", c=c_og), ogTb[:], reads=[ogTb], writes=[ogF[ti - 1]])
        if ti % 4 == 0 and ti <= 12:
            S.dma("pool", ogH[(ti // 4) * 128:(ti // 4 + 1) * 128, :].rearrange("p (c t) -> p c t", c=c_og), ogTb[:, :, TS - 64:TS],
                  reads=[ogTb], writes=[ogH])
        io["after_og"](ti)

    def interleave(gens):
        gens = [g for g in gens if g is not None]
        while gens:
            for g in list(gens):
                try:
                    next(g)
                except StopIteration:
                    gens.remove(g)

    load_a(0)
    tile_level(0)
    interleave([stageA(0)])
    NCK = NTS * CPT
    for n in range(NCK):
        nxt = None
        if n + 1 < NCK:
            if (n + 1) % CPT == 0:
                tile_level((n + 1) // CPT)
            nxt = stageA(n + 1)
        interleave([stageB(n), nxt])
        if (n + 1) % CPT == 0:
            tile_finish(n // CPT)
    S.pop()


def prep_M_ml(inp, j, h):
    w = inp["ml_w_in"][j]
    q = w[:, h * 128:(h + 1) * 128]
    k = w[:, 512 + h * 128:512 + (h + 1) * 128]
    v = w[:, 1024 + h * 256:1024 + (h + 1) * 256]
    o = w[:, 2048 + h * 256:2048 + (h + 1) * 256]
    gi = w[:, 3072 + h:3073 + h]
    gf = w[:, 3076 + h:3077 + h]
    wc = np.concatenate([q, k, k, v, o, gi, gf], 1)
    wc = wc.reshape(8, 128, ML_WC).transpose(1, 0, 2)
    gb = inp["ml_gate_b"][j][[h, 4 + h]].reshape(1, 2)
    nwv = inp["ml_norm_w"][j][h * 256:(h + 1) * 256].reshape(1, 256)
    return {"w": np.ascontiguousarray(wc.reshape(128, 8 * ML_WC), np.float32),
            "gb": np.ascontiguousarray(gb, np.float32), "nwv": np.ascontiguousarray(nwv, np.float32),
            "cst": ml_consts()}


def ml_consts():
    c = np.zeros((128, 64 + 512 + 128), np.float32)
    c[0:64, 0:64] = np.eye(64, dtype=np.float32)
    jj = np.arange(64)[:, None]
    ii = np.arange(64)[None, :]
    mb = (jj > ii).astype(np.float32) * BIGNEG
    c[0:64, 64:576] = np.tile(mb, (1, 8))
    c[:, 576:704] = np.eye(128, dtype=np.float32)
    return c


G_WC = 12 * 128 + 8
L2_EPS = 1e-6
GDN_EPS = 1e-6
C_I64, C_MUI, C_MUS, C_MLS, C_NMLS, C_NMUS, C_I128, C_ONES = 0, 64, 128, 192, 256, 320, 384, 512
C_TOT = 640


def gdn_consts():
    c = np.zeros((128, C_TOT), np.float32)
    p = np.arange(64)[:, None]
    f = np.arange(64)[None, :]
    c[0:64, C_I64:C_I64 + 64] = (p == f)
    c[0:64, C_MUI:C_MUI + 64] = (p <= f)
    c[0:64, C_MUS:C_MUS + 64] = (p < f)
    c[0:64, C_MLS:C_MLS + 64] = (p > f)
    c[0:64, C_NMLS:C_NMLS + 64] = -1.0 * (p > f)
    c[0:64, C_NMUS:C_NMUS + 64] = -1.0 * (p < f)
    c[:, C_I128:C_I128 + 128] = np.eye(128, dtype=np.float32)
    c[:, C_ONES:C_ONES + 128] = 1.0
    return c


def emit_M_gdn(S, tag, io):
    S.push(tag)
    ainF_g, ainH_g = io["ainF_g"], io["ainH_g"]
    w_d, cw_d, hv_d, gnw_d, cst_d = io["w"], io["cw"], io["hv"], io["gnw"], io["cst"]
    ogF, ogH = io["ogF"], io["ogH"]
    c_og = 4

    w_sb = S.sb("w_sb", [128, 8, G_WC], BF16)
    S.dma("pool", w_sb[:].rearrange("p a b -> p (a b)"), w_d[:, :], writes=[w_sb])
    cst = S.sb("cst_sb", [128, C_TOT], F32)
    S.dma("sp", cst[:], cst_d[:, :], writes=[cst])
    cwg = S.sb("cwg", [128, 8, 4], F32)
    S.dma("sp", cwg[:].rearrange("p a b -> p (a b)"), cw_d[:, :], writes=[cwg])
    hv = S.sb("hv_sb", [64, 8], F32)
    S.dma("sp", hv[:], hv_d[0:1, :].partition_broadcast(64), writes=[hv])
    gnw = S.sb("gnw_sb", [128, 1], F32)
    S.dma("sp", gnw[:], gnw_d[:, :], writes=[gnw])
    ident_b = S.sb("ident_b", [128, 128], BF16)
    ones_b = S.sb("ones_b", [128, 128], BF16)
    S.op("act", lambda e: e.activation(out=ident_b[:], in_=cst[:, C_I128:C_I128 + 128], func=AF.Copy), reads=[cst], writes=[ident_b])
    S.op("act", lambda e: e.activation(out=ones_b[:], in_=cst[:, C_ONES:C_ONES + 128], func=AF.Copy), reads=[cst], writes=[ones_b])
    dg = S.sb("dg", [128, 32, 128], BF16)
    for mc in range(8):
        for k in range(4):
            S.op("dve", lambda e: e.tensor_scalar(out=dg[:, mc * 4 + k, :], in0=cst[:, C_I128:C_I128 + 128], scalar1=cwg[:, mc, k:k + 1],
                                                  scalar2=None, op0=ALU.mult), reads=[cst, cwg], writes=[dg])
    I64 = cst[0:64, C_I64:C_I64 + 64]
    MUI = cst[0:64, C_MUI:C_MUI + 64]
    MLS = cst[0:64, C_MLS:C_MLS + 64]
    NMLS = cst[0:64, C_NMLS:C_NMLS + 64]
    NMUS = cst[0:64, C_NMUS:C_NMUS + 64]
    ONES64x128 = cst[0:64, C_ONES:C_ONES + 128]

    a_sb = [S.sb("a_sb%d" % i, [128, 8, 512], BF16) for i in range(2)]
    pp = PsumPool(S, 4)
    po2 = [S.ps("po%d" % i, [128, 512]) for i in range(2)]
    pTall = S.ps("pTall", [128, 1024], BF16)
    pTon = S.ps("pTon", [128, 1024], BF16)

    def load_a(i):
        ab = a_sb[i % 2]
        if i == 0:
            S.op("pool", lambda e: e.memset(ab[:, :, 0:XPAD], 0.0), writes=[ab])
            S.dma("sp", ab[:, :, XPAD:TS], ainH_g[0:128, :].rearrange("p (c t) -> p c t", c=8), reads=[ainH_g], writes=[ab])
        else:
            r0 = ((i - 1) % 4) * 512 + ((i - 1) // 4) * 128
            S.dma("sp", ab[:].rearrange("p c t -> p (c t)"), ainF_g[r0:r0 + 128, :], reads=[ainF_g], writes=[ab])

    NH = NCH * 4
    bg = S.sb("bg", [64, NCH, 8], F32)
    load_a(0)
    for ti in range(NTS):
        if ti + 1 < NTS:
            load_a(ti + 1)
        ab = a_sb[ti % 2]
        pb = pp.get()
        for cj in range(CPT):
            for kc in range(8):
                S.op("pe", lambda e: e.matmul(pb[0:64, cj * 8:(cj + 1) * 8], ab[:, kc, cj * CH:(cj + 1) * CH], w_sb[:, kc, 1536:1544],
                                              start=(kc == 0), stop=(kc == 7)), reads=[ab, w_sb], writes=[pb])
        S.op("act", lambda e: e.activation(out=bg[:, ti * CPT:(ti + 1) * CPT, :], in_=pb[0:64, 0:64].rearrange("p (a b) -> p a b", a=CPT),
                                           func=AF.Copy), reads=[pb], writes=[bg])
    lnb = S.sb("lnb", [64, NCH, 4], F32)
    bt = S.sb("bt", [64, NCH, 4], F32)
    gt = S.sb("gt", [64, NCH, 4], F32)
    beG = S.sb("beG", [64, NCH, 4], F32)
    ekt = S.sb("ekt", [64, NCH, 4], F32)
    eGl = S.sb("eGl", [128, NCH, 4], F32)
    tmpg = S.sb("tmpg", [64, NCH, 4], F32)
    eal = S.sb("eal", [64, 4], F32)
    S.op("act", lambda e: e.activation(out=lnb[:], in_=bg[:, :, 0:4], func=AF.Exp, scale=-1.0), reads=[bg], writes=[lnb])
    S.op("act", lambda e: e.activation(out=lnb[:], in_=lnb[:], func=AF.Ln, bias=1.0, scale=1.0), reads=[lnb], writes=[lnb])
    S.op("dve", lambda e: e.tensor_scalar_mul(out=lnb[:], in0=lnb[:], scalar1=-1.0), reads=[lnb], writes=[lnb])
    S.op("act", lambda e: e.activation(out=bt[:], in_=lnb[:], func=AF.Exp), reads=[lnb], writes=[bt])
    S.op("dve", lambda e: e.tensor_tensor(out=gt[:], in0=bg[:, :, 4:8], in1=bcast_mid(hv[:, 4:8], NCH), op=ALU.add), reads=[bg, hv], writes=[gt])
    S.op("act", lambda e: e.activation(out=gt[:], in_=gt[:], func=AF.Exp), reads=[gt], writes=[gt])
    S.op("act", lambda e: e.activation(out=gt[:], in_=gt[:], func=AF.Ln, bias=1.0, scale=1.0), reads=[gt], writes=[gt])
    S.op("act", lambda e: e.activation(out=eal[:], in_=hv[:, 0:4], func=AF.Exp), reads=[hv], writes=[eal])
    S.op("dve", lambda e: e.tensor_scalar_mul(out=eal[:], in0=eal[:], scalar1=-1.0), reads=[eal], writes=[eal])
    S.op("dve", lambda e: e.tensor_tensor(out=gt[:], in0=gt[:], in1=bcast_mid(eal[:], NCH), op=ALU.mult), reads=[gt, eal], writes=[gt])
    gflat = gt[:].rearrange("p a b -> p (a b)")
    for (c0, c1) in ((0, 272), (272, NH)):
        pb = pp.get()
        S.op("pe", lambda e: e.matmul(pb[0:64, 0:c1 - c0], MUI, gflat[:, c0:c1], start=True, stop=True), reads=[cst, gt], writes=[pb])
        pl = pp.get()
        S.op("pe", lambda e: e.matmul(pl[:, 0:c1 - c0], ONES64x128, gflat[:, c0:c1], start=True, stop=True), reads=[cst, gt], writes=[pl])
        S.op("act", lambda e: e.activation(out=tmpg[:].rearrange("p a b -> p (a b)")[:, c0:c1], in_=pb[0:64, 0:c1 - c0], func=AF.Exp),
             reads=[pb], writes=[tmpg])
        S.op("act", lambda e: e.activation(out=eGl[:].rearrange("p a b -> p (a b)")[:, c0:c1], in_=pl[:, 0:c1 - c0], func=AF.Exp),
             reads=[pl], writes=[eGl])
        S.op("act", lambda e: e.activation(out=ekt[:].rearrange("p a b -> p (a b)")[:, c0:c1], in_=pb[0:64, 0:c1 - c0], func=AF.Copy),
             reads=[pb], writes=[ekt])
        S.op("dve", lambda e: e.tensor_tensor(out=ekt[:].rearrange("p a b -> p (a b)")[:, c0:c1], in0=pl[0:64, 0:c1 - c0],
                                              in1=ekt[:].rearrange("p a b -> p (a b)")[:, c0:c1], op=ALU.subtract), reads=[pl, ekt], writes=[ekt])
    S.op("act", lambda e: e.activation(out=ekt[:], in_=ekt[:], func=AF.Exp), reads=[ekt], writes=[ekt])
    S.op("dve", lambda e: e.tensor_tensor(out=beG[:], in0=bt[:], in1=tmpg[:], op=ALU.mult), reads=[bt, tmpg], writes=[beG])

    xb = [S.sb("xb%d" % i, [128, 8, 3 + TS], BF16) for i in range(2)]
    S.op("pool", lambda e: e.memset(xb[1][:, :, TS:TS + 3], 0.0), writes=[xb[1]])
    sx = [S.sb("sx%d" % i, [128, TS], F32) for i in range(2)]
    sqb = [S.sb("sqb%d" % i, [128, TS], BF16) for i in range(2)]
    rs = [S.sb("rs%d" % i, [128, TS], F32) for i in range(2)]
    qT = [S.sb("qT%d" % i, [128, 2, TS], BF16) for i in range(2)]
    kT = [S.sb("kT%d" % i, [128, 2, TS], BF16) for i in range(2)]
    svT = [S.sb("svT%d" % i, [128, 4, TS], BF16) for i in range(2)]
    zs = [S.sb("zs%d" % i, [128, 4, TS], F32) for i in range(2)]
    onT = [S.sb("onT%d" % i, [128, 4, TS], BF16) for i in range(2)]
    ogt = [S.sb("ogt0", [128, 4, TS], BF16)] * 2
    S_f = S.sb("S_f", [128, 4, 128], F32)
    S_b = S.sb("S_b", [128, 4, 128], BF16)
    S_t = S.sb("S_t", [128, 4, 128], F32)
    S.op("pool", lambda e: e.memset(S_f[:], 0.0), writes=[S_f])
    S.op("pool", lambda e: e.memset(S_b[:], 0.0), writes=[S_b])
    NR = 2
    mk = lambda nm, shp, dt: [S.sb("%s%d" % (nm, i), shp, dt) for i in range(NR)]
    kbg = mk("kbg", [64, 4, 128], BF16)
    ktm = mk("ktm", [64, 4, 128], BF16)
    vb = mk("vb", [64, 4, 128], BF16)
    rg1 = mk("rg1", [64, 4, 64], F32)
    rg2 = mk("rg2", [64, 4, 64], F32)
    rg3 = mk("rg3", [64, 4, 64], F32)
    Et = mk("Et", [64, 4, 64], F32)
    Wt_ = mk("Wt", [64, 4, 64], F32)
    W_ = mk("W", [64, 4, 64], F32)
    eGb = mk("eGb", [128, 4, 64], F32)
    KKlo = mk("KKlo", [64, 2, 64], F32)
    KKup = mk("KKup", [64, 2, 64], F32)
    KQm = mk("KQm", [64, 2, 64], F32)
    Qt = mk("Qt", [64, 4, 64], BF16)
    qdT = mk("qdT", [128, 4, 64], BF16)
    PP = [mk("PP%d" % k, [64, 8, 64], BF16) for k in range(2)]
    Xt = [mk("Xt%d" % k, [64, 4, 64], BF16) for k in range(2)]
    Tt = mk("Tt", [64, 4, 64], BF16)
    nwT = mk("nwT", [128, 4, 64], BF16)
    vn = mk("vn", [64, 4, 128], BF16)
    sqo = mk("sqo", [64, 4, 128], F32)
    sso = mk("sso", [64, 8], F32)
    on = mk("on", [64, 4, 128], BF16)

    def silu_from_psum(pb, W, out_ap, out_buf, idx):
        S.op("act", lambda e: e.activation(out=out_ap, in_=pb[:, :W], func=AF.Silu), reads=[pb], writes=[out_buf])

    def tile_level(ti):
        if ti + 1 < NTS:
            load_a(ti + 1)
        ab = a_sb[ti % 2]
        t0 = ti * TS
        xcur, xprev = xb[ti % 2], xb[(ti + 1) % 2]
        qTb, kTb, svb, zsb, onTb, ogb = qT[ti % 2], kT[ti % 2], svT[ti % 2], zs[ti % 2], onT[ti % 2], ogt[ti % 2]
        S.op("pool", lambda e: e.tensor_copy(out=xcur[:, :, 0:3], in_=xprev[:, :, TS:TS + 3]), reads=[xprev], writes=[xcur])
        for mc in range(12):
            pb = pp.get()
            for kc in range(8):
                S.op("pe", lambda e: e.matmul(pb[:, :], w_sb[:, kc, mc * 128:(mc + 1) * 128], ab[:, kc, :], start=(kc == 0), stop=(kc == 7)),
                     reads=[w_sb, ab], writes=[pb])
            if mc < 8:
                S.op("act", lambda e: e.activation(out=xcur[:, mc, 3:3 + TS], in_=pb[:, :], func=AF.Copy), reads=[pb], writes=[xcur])
            else:
                silu_from_psum(pb, TS, zsb[:, mc - 8, :], zsb, mc)
        for mc in range(8):
            pb = pp.get()
            for k in range(4):
                S.op("pe", lambda e: e.matmul(pb[:, :], dg[:, mc * 4 + k, :], xcur[:, mc, k:k + TS], start=(k == 0), stop=(k == 3)),
                     reads=[dg, xcur], writes=[pb])
            if mc >= 4:
                silu_from_psum(pb, TS, svb[:, mc - 4, :], svb, mc)
            else:
                sxb, sq, rsb = sx[mc % 2], sqb[mc % 2], rs[mc % 2]
                silu_from_psum(pb, TS, sxb[:, :], sxb, mc)
                S.op("act", lambda e: e.activation(out=sq[:, :], in_=sxb[:, :], func=AF.Square), reads=[sxb], writes=[sq])
                ps2 = pp.get()
                S.op("pe", lambda e: e.matmul(ps2[:, :], ones_b[:], sq[:, :], start=True, stop=True), reads=[ones_b, sq], writes=[ps2])
                rstd_from_ss(S, ps2, rsb, 1.0, L2_EPS, TS)
                dst = qTb if mc < 2 else kTb
                scl = (128.0 ** -0.5) if mc < 2 else 1.0
                S.op("dve", lambda e: e.scalar_tensor_tensor(out=dst[:, mc % 2, :], in0=sxb[:, :], scalar=scl, in1=rsb[:, :],
                                                             op0=ALU.mult, op1=ALU.mult), reads=[sxb, rsb], writes=[dst])

    def stageA(n):
        ti, cj = n // CPT, n % CPT
        qTb, kTb, svb, onTb = qT[ti % 2], kT[ti % 2], svT[ti % 2], onT[ti % 2]
        r = n % NR
        c0 = cj * CH
        for qh in range(2):
            S.op("pe", lambda e: e.transpose(pTall[0:64, qh * 128:(qh + 1) * 128], kTb[:, qh, c0:c0 + CH], ident_b[:, :]),
                 reads=[kTb, ident_b], writes=[pTall])
        for h in range(4):
            S.op("pe", lambda e: e.transpose(pTall[0:64, 256 + h * 128:256 + (h + 1) * 128], svb[:, h, c0:c0 + CH], ident_b[:, :]),
                 reads=[svb, ident_b], writes=[pTall])
        ktm_ps = pTall[0:64, 0:256].rearrange("p (a b) -> p a b", a=2)
        ktm_rep = ktm_ps.unsqueeze(2).broadcast_to([64, 2, 2, 128])
        as4 = lambda ap: ap.rearrange("p (a r) d -> p a r d", r=2)
        S.op("dve", lambda e: e.tensor_tensor(out=as4(kbg[r][:]), in0=ktm_rep, in1=as4(bcast_last(beG[:, n, :], 128)), op=ALU.mult),
             reads=[pTall, beG], writes=[kbg[r]])
        S.op("dve", lambda e: e.tensor_tensor(out=as4(ktm[r][:]), in0=ktm_rep, in1=as4(bcast_last(ekt[:, n, :], 128)), op=ALU.mult),
             reads=[pTall, ekt], writes=[ktm[r]])
        S.op("dve", lambda e: e.tensor_tensor(out=vb[r][:], in0=pTall[0:64, 256:768].rearrange("p (a b) -> p a b", a=4),
                                              in1=bcast_last(bt[:, n, :], 128), op=ALU.mult), reads=[pTall, bt], writes=[vb[r]])
        yield
        S.op("dve", lambda e: e.tensor_tensor(out=rg1[r][:], in0=bcast_mid(MUI, 4), in1=bcast_last(gt[:, n, :], 64), op=ALU.mult),
             reads=[cst, gt], writes=[rg1[r]])
        S.op("dve", lambda e: e.tensor_tensor(out=rg2[r][:], in0=bcast_mid(I64, 4), in1=bcast_last(lnb[:, n, :], 64), op=ALU.mult),
             reads=[cst, lnb], writes=[rg2[r]])
        S.op("dve", lambda e: e.tensor_tensor(out=rg2[r][:], in0=rg2[r][:], in1=rg1[r][:], op=ALU.add), reads=[rg1[r], rg2[r]], writes=[rg2[r]])
        S.op("dve", lambda e: e.tensor_tensor(out=rg3[r][:], in0=bcast_mid(MLS, 4), in1=bcast_last(gt[:, n, :], 64), op=ALU.mult),
             reads=[cst, gt], writes=[rg3[r]])
        fl = lambda b_: b_[:].rearrange("p a b -> p (a b)")
        pd1 = pp.get()
        S.op("pe", lambda e: e.matmul(pd1[0:64, 0:256], MLS, fl(rg1[r]), start=True, stop=True), reads=[cst, rg1[r]], writes=[pd1])
        S.op("pe", lambda e: e.matmul(pd1[0:64, 256:512], MLS, fl(rg2[r]), start=True, stop=True), reads=[cst, rg2[r]], writes=[pd1])
        pd2 = pp.get()
        S.op("pe", lambda e: e.matmul(pd2[0:64, 0:256], MUI, fl(rg3[r]), start=True, stop=True), reads=[cst, rg3[r]], writes=[pd2])
        pd3 = pp.get()
        S.op("pe", lambda e: e.matmul(pd3[:, 0:256], ONES64x128, fl(rg1[r]), start=True, stop=True), reads=[cst, rg1[r]], writes=[pd3])
        S.op("act", lambda e: e.activation(out=fl(Et[r]), in_=pd1[0:64, 0:256], func=AF.Exp), reads=[pd1], writes=[Et[r]])
        S.op("act", lambda e: e.activation(out=fl(Wt_[r]), in_=pd1[0:64, 256:512], func=AF.Exp), reads=[pd1], writes=[Wt_[r]])
        S.op("dve", lambda e: e.tensor_tensor(out=W_[r][:], in0=pd2[0:64, 0:256].rearrange("p (a b) -> p a b", a=4),
                                              in1=bcast_last(lnb[:, n, :], 64), op=ALU.add), reads=[pd2, lnb], writes=[W_[r]])
        S.op("act", lambda e: e.activation(out=fl(W_[r]), in_=fl(W_[r]), func=AF.Exp), reads=[W_[r]], writes=[W_[r]])
        S.op("act", lambda e: e.activation(out=fl(eGb[r]), in_=pd3[:, 0:256], func=AF.Exp), reads=[pd3], writes=[eGb[r]])
        yield
        pg = pp.get()
        for qh in range(2):
            S.op("pe", lambda e: e.matmul(pg[0:64, qh * 64:(qh + 1) * 64], kTb[:, qh, c0:c0 + CH], kTb[:, qh, c0:c0 + CH], start=True, stop=True),
                 reads=[kTb], writes=[pg])
            S.op("pe", lambda e: e.matmul(pg[0:64, 128 + qh * 64:128 + (qh + 1) * 64], kTb[:, qh, c0:c0 + CH], qTb[:, qh, c0:c0 + CH],
                                          start=True, stop=True), reads=[kTb, qTb], writes=[pg])
        kkv = pg[0:64, 0:128].rearrange("p (a b) -> p a b", a=2)
        kqv = pg[0:64, 128:256].rearrange("p (a b) -> p a b", a=2)
        S.op("dve", lambda e: e.tensor_tensor(out=KKlo[r][:], in0=kkv, in1=bcast_mid(NMLS, 2), op=ALU.mult), reads=[pg, cst], writes=[KKlo[r]])
        S.op("dve", lambda e: e.tensor_tensor(out=KKup[r][:], in0=kkv, in1=bcast_mid(NMUS, 2), op=ALU.mult), reads=[pg, cst], writes=[KKup[r]])
        S.op("dve", lambda e: e.tensor_tensor(out=KQm[r][:], in0=kqv, in1=bcast_mid(MUI, 2), op=ALU.mult), reads=[pg, cst], writes=[KQm[r]])
        yield
        rep = lambda b_: b_[:].unsqueeze(2).broadcast_to([64, 2, 2, 64])
        P0 = PP[0][r]
        S.op("dve", lambda e: e.tensor_tensor(out=as4(P0[:, 0:4, :]), in0=rep(KKlo[r]), in1=as4(W_[r][:]), op=ALU.mult),
             reads=[KKlo[r], W_[r]], writes=[P0])
        S.op("dve", lambda e: e.tensor_tensor(out=as4(P0[:, 4:8, :]), in0=rep(KKup[r]), in1=as4(Wt_[r][:]), op=ALU.mult),
             reads=[KKup[r], Wt_[r]], writes=[P0])
        S.op("dve", lambda e: e.tensor_tensor(out=as4(Qt[r][:]), in0=rep(KQm[r]), in1=as4(Et[r][:]), op=ALU.mult),
             reads=[KQm[r], Et[r]], writes=[Qt[r]])
        S.op("dve", lambda e: e.tensor_tensor(out=as4(qdT[r][:]), in0=qTb[:, :, c0:c0 + CH].unsqueeze(2).broadcast_to([128, 2, 2, 64]),
                                              in1=as4(eGb[r][:]), op=ALU.mult), reads=[qTb, eGb[r]], writes=[qdT[r]])
        yield
        X = Xt[0][r]
        S.op("dve", lambda e: e.tensor_tensor(out=X[:], in0=P0[:, 4:8, :], in1=bcast_mid(I64, 4), op=ALU.add), reads=[P0, cst], writes=[X])
        for k in range(1, 6):
            Pp, Pn = PP[(k - 1) % 2][r], PP[k % 2][r]
            pq = pp.get()
            for h in range(4):
                S.op("pe", lambda e: e.matmul(pq[0:64, h * 64:(h + 1) * 64], Pp[:, 4 + h, :], Pp[:, h, :], start=True, stop=True),
                     reads=[Pp], writes=[pq])
            if k < 5:
                for h in range(4):
                    S.op("pe", lambda e: e.matmul(pq[0:64, 256 + h * 64:256 + (h + 1) * 64], Pp[:, h, :], Pp[:, 4 + h, :], start=True, stop=True),
                         reads=[Pp], writes=[pq])
            wdt = 512 if k < 5 else 256
            S.op("act", lambda e: e.activation(out=Pn[:].rearrange("p a b -> p (a b)")[:, 0:wdt], in_=pq[0:64, 0:wdt], func=AF.Copy),
                 reads=[pq], writes=[Pn])
            yield
            px = pp.get()
            Xo = Xt[(k - 1) % 2][r]
            Xn = Xt[k % 2][r]
            for h in range(4):
                S.op("pe", lambda e: e.matmul(px[0:64, h * 64:(h + 1) * 64], Pn[:, h, :], Xo[:, h, :], start=True, stop=True),
                     reads=[Pn, Xo], writes=[px])
            yield
            if k < 5:
                S.op("dve", lambda e: e.tensor_tensor(out=fl(Xn), in0=px[0:64, 0:256], in1=fl(Xo), op=ALU.add), reads=[px, Xo], writes=[Xn])
            else:
                S.op("dve", lambda e: e.tensor_tensor(out=fl(Tt[r]), in0=px[0:64, 0:256], in1=fl(Xo), op=ALU.add), reads=[px, Xo], writes=[Tt[r]])
        yield
        pw = pp.get()
        for h in range(4):
            S.op("pe", lambda e: e.matmul(pw[:, h * 64:(h + 1) * 64], kbg[r][:, h, :], Tt[r][:, h, :], start=True, stop=True),
                 reads=[kbg[r], Tt[r]], writes=[pw])
        S.op("act", lambda e: e.activation(out=fl(nwT[r]), in_=pw[:, 0:256], func=AF.Copy, scale=-1.0), reads=[pw], writes=[nwT[r]])
        yield

    def stageB(n):
        ti, cj = n // CPT, n % CPT
        qTb, kTb, svb, onTb = qT[ti % 2], kT[ti % 2], svT[ti % 2], onT[ti % 2]
        r = n % NR
        c0 = cj * CH
        pu = pp.get()
        for h in range(4):
            S.op("pe", lambda e: e.matmul(pu[0:64, h * 128:(h + 1) * 128], Tt[r][:, h, :], vb[r][:, h, :], start=True, stop=False),
                 reads=[Tt[r], vb[r]], writes=[pu])
            S.op("pe", lambda e: e.matmul(pu[0:64, h * 128:(h + 1) * 128], nwT[r][:, h, :], S_b[:, h, :], start=False, stop=True),
                 reads=[nwT[r], S_b], writes=[pu])
        S.op("act", lambda e: e.activation(out=vn[r][:].rearrange("p a b -> p (a b)"), in_=pu[0:64, :], func=AF.Copy), reads=[pu], writes=[vn[r]])
        yield
        po = po2[n % 2]
        for h in range(4):
            S.op("pe", lambda e: e.matmul(po[0:64, h * 128:(h + 1) * 128], qdT[r][:, h, :], S_b[:, h, :], start=True, stop=False),
                 reads=[qdT[r], S_b], writes=[po])
            S.op("pe", lambda e: e.matmul(po[0:64, h * 128:(h + 1) * 128], Qt[r][:, h, :], vn[r][:, h, :], start=False, stop=True),
                 reads=[Qt[r], vn[r]], writes=[po])
        yield
        pS = pp.get()
        for h in range(4):
            S.op("pe", lambda e: e.matmul(pS[:, h * 128:(h + 1) * 128], ktm[r][:, h, :], vn[r][:, h, :], start=True, stop=True),
                 reads=[ktm[r], vn[r]], writes=[pS])
        for h in range(4):
            S.op("dve", lambda e: e.scalar_tensor_tensor(out=S_f[:, h, :], in0=S_f[:, h, :], scalar=eGl[:, n, h:h + 1],
                                                         in1=pS[:, h * 128:(h + 1) * 128], op0=ALU.mult, op1=ALU.add),
                 reads=[S_f, eGl, pS], writes=[S_f])
        S.op("act", lambda e: e.activation(out=S_b[:], in_=S_f[:], func=AF.Copy), reads=[S_f], writes=[S_b])
        yield
        S.op("act", lambda e: e.activation(out=sqo[r][:].rearrange("p a b -> p (a b)"), in_=po[0:64, :], func=AF.Square), reads=[po], writes=[sqo[r]])
        S.op("dve", lambda e: e.tensor_reduce(out=sso[r][:, 0:4], in_=sqo[r][:], axis=AX.X, op=ALU.add), reads=[sqo[r]], writes=[sso[r]])
        S.op("act", lambda e: e.activation(out=sso[r][:, 4:8], in_=sso[r][:, 0:4], func=AF.Ln, scale=1.0 / 128.0, bias=GDN_EPS),
             reads=[sso[r]], writes=[sso[r]])
        S.op("act", lambda e: e.activation(out=sso[r][:, 4:8], in_=sso[r][:, 4:8], func=AF.Exp, scale=-0.5), reads=[sso[r]], writes=[sso[r]])
        S.op("dve", lambda e: e.tensor_tensor(out=on[r][:], in0=po[0:64, :].rearrange("p (a b) -> p a b", a=4),
                                              in1=bcast_last(sso[r][:, 4:8], 128), op=ALU.mult), reads=[po, sso[r]], writes=[on[r]])
        yield
        for h in range(4):
            S.op("pe", lambda e: e.transpose(pTon[:, h * 64:(h + 1) * 64], on[r][:, h, :], ident_b[0:64, 0:64]),
                 reads=[on[r], ident_b], writes=[pTon])
        S.op("act", lambda e: e.activation(out=onTb[:, :, c0:c0 + CH], in_=pTon[:, 0:256].rearrange("p (a b) -> p a b", a=4), func=AF.Copy),
             reads=[pTon], writes=[onTb])
        yield

    def tile_finish(ti):
        zsb, onTb, ogb = zs[ti % 2], onT[ti % 2], ogt[ti % 2]
        S.op("dve", lambda e: e.scalar_tensor_tensor(out=ogb[:].rearrange("p a b -> p (a b)"), in0=onTb[:].rearrange("p a b -> p (a b)"),
                                                     scalar=gnw[:, 0:1], in1=zsb[:].rearrange("p a b -> p (a b)"), op0=ALU.mult, op1=ALU.mult),
             reads=[onTb, gnw, zsb], writes=[ogb])
        if ti >= 1:
            S.dma("pool", ogF[ti - 1][:, :].rearrange("p (c t) -> p c t", c=c_og), ogb[:], reads=[ogb], writes=[ogF[ti - 1]])
        if ti % 4 == 0 and ti <= 12:
            S.dma("pool", ogH[(ti // 4) * 128:(ti // 4 + 1) * 128, :].rearrange("p (c t) -> p c t", c=c_og), ogb[:, :, TS - 64:TS],
                  reads=[ogb], writes=[ogH])
        io["after_og"](ti)

    def interleave(gens):
        gens = [g for g in gens if g is not None]
        while gens:
            for g in list(gens):
                try:
                    next(g)
                except StopIteration:
                    gens.remove(g)

    load_a(0)
    tile_level(0)
    interleave([stageA(0)])
    NCK = NTS * CPT
    for n in range(NCK):
        nxt = None
        if n + 1 < NCK:
            if (n + 1) % CPT == 0:
                tile_level((n + 1) // CPT)
            nxt = stageA(n + 1)
        interleave([stageB(n), nxt])
        if (n + 1) % CPT == 0:
            tile_finish(n // CPT)
    S.pop()


def prep_M_gdn(inp, j, hg):
    w = inp["gdn_w_in"][j]
    cols = []
    for qh in range(2):
        cols.append(np.arange(128) + 128 * (2 * hg + qh))
    for qh in range(2):
        cols.append(1024 + np.arange(128) + 128 * (2 * hg + qh))
    for h in range(4):
        cols.append(2048 + np.arange(128) + 128 * (4 * hg + h))
    conv_cols = np.concatenate(cols)
    for h in range(4):
        cols.append(4096 + np.arange(128) + 128 * (4 * hg + h))
    cols.append(6144 + 4 * hg + np.arange(4))
    cols.append(6160 + 4 * hg + np.arange(4))
    cols = np.concatenate(cols)
    wc = w[:, cols].reshape(8, 128, G_WC).transpose(1, 0, 2)
    cw = inp["gdn_conv_w"][j][:, conv_cols].reshape(4, 8, 128).transpose(2, 1, 0)
    hv = np.concatenate([inp["gdn_a_log"][j][4 * hg:4 * hg + 4], inp["gdn_dt_bias"][j][4 * hg:4 * hg + 4]]).reshape(1, 8)
    return {"w": np.ascontiguousarray(wc.reshape(128, 8 * G_WC), np.float32),
            "cw": np.ascontiguousarray(cw.reshape(128, 32), np.float32),
            "hv": np.ascontiguousarray(hv, np.float32),
            "gnw": np.ascontiguousarray(inp["gdn_norm_w"][j].reshape(128, 1), np.float32),
            "cst": gdn_consts()}


GROUPS = [[0, 1, 2, 3], [4, 5, 6, 7]]


def build_fused(nl=4):
    nc = bass.Bass("TRN2", target_bir_lowering=False)
    es = ExitStack()
    S = Sched(nc, es)
    ext = lambda n, shp, dt=F32: S.dram(n, shp, dt, kind="ExternalInput")
    hs0 = ext("hs0", [D, WIN])
    keep = ext("keep", [1, WIN])
    gidx4 = ext("gidx4", [128, 20], mybir.dt.int32)
    gidx2 = ext("gidx2", [128, 20], mybir.dt.int32) if nl > 1 else None
    nwT0 = ext("nwT_0", [128, 32])
    cst_g = ext("cst_g", [128, C_TOT])
    cst_m = ext("cst_m", [128, 64 + 512 + 128]) if nl > 1 else None
    hs_out = S.dram("hs_out", [D, WIN], F32, kind="ExternalOutput")
    hs_loc = S.dram("hs_loc", [D, WIN], F32)
    def slices(name, rows_total, cols, rows_per):
        big = S.dram(name, [rows_total, cols], BF16)
        return big, [Buf(big.t[k * rows_per:(k + 1) * rows_per, :], "%s_%d" % (name, k)) for k in range(rows_total // rows_per)]

    ainF_all, ainF = slices("ainF", 512, 4096, 128)
    ainF_g, ainF_gs = slices("ainF_g", 2048, 4096, 512)
    ainH = S.dram("ainH", [128, 512], BF16)
    ainH_g = S.dram("ainH_g", [512, 512], BF16)
    og = {}
    for c in (4, 2):
        tpc = 8 // c
        ogF_all, ogF = slices("ogF%d" % c, 2048, c * 512, 128)
        ogF_g, _ = slices("ogF%d_g" % c, 8192, c * 512, 8192)
        nq = 16 // tpc
        og[c] = dict(ogF=ogF, ogF_all=ogF_all, ogH=S.dram("ogH%d" % c, [512, c * 64], BF16), tpc=tpc,
                     ogF_g=ogF_g, ogH_g=S.dram("ogH%d_g" % c, [2048, c * 64], BF16),
                     src=[Buf(ogF_all.t[q * tpc * 128:(q + 1) * tpc * 128, :], "ogsrc%d_%d" % (c, q)) for q in range(nq)],
                     dst=[Buf(ogF_g.t[q * 4 * tpc * 128:(q + 1) * 4 * tpc * 128, :], "ogdst%d_%d" % (c, q)) for q in range(nq)])
    lay = []
    for l in range(nl):
        KO = 2048 if l % 2 == 0 else 1024
        KC = KO // 128
        d = dict(nwT=ext("nwT_l%d" % l, [128, 32]), cw=ext("cw_l%d" % l, [128, 44 * 3]), cb=ext("cb_l%d" % l, [128, 44]),
                 wout_d=ext("wout_l%d" % l, [8, 128, KC * 128]), wup_d=ext("wup_l%d" % l, [NG, 128, 8 * 256]),
                 wdn_d=ext("wdn_l%d" % l, [8, 128, NG * 128]),
                 wout_b=S.dram("wout_b%d" % l, [8, 128, KC * 128], BF16), wup_b=S.dram("wup_b%d" % l, [NG, 128, 8 * 256], BF16),
                 wdn_b=S.dram("wdn_b%d" % l, [8, 128, NG * 128], BF16))
        if l % 2 == 0:
            d["m"] = dict(w=ext("gw_l%d" % l, [128, 8 * G_WC]), cw=ext("gcw_l%d" % l, [128, 32]), hv=ext("ghv_l%d" % l, [1, 8]),
                          gnw=ext("ggnw_l%d" % l, [128, 1]), cst=cst_g)
        else:
            d["m"] = dict(w=ext("mw_l%d" % l, [128, 8 * ML_WC]), gb=ext("mgb_l%d" % l, [1, 2]), nwv=ext("mnwv_l%d" % l, [1, ML_DV]), cst=cst_m)
        lay.append(d)

    def after_ain(ti):
        if ti == 0:
            S.coll("AllGather", ainH_g, ainH, GROUPS)
        else:
            S.coll("AllGather", ainF_gs[ti - 1], ainF[ti - 1], GROUPS)

    def mk_after_og(c):
        o = og[c]
        tpc = o["tpc"]

        def after_og(ti):
            wt = ti - 1
            if ti >= 1 and (wt + 1) % tpc == 0:
                q = wt // tpc
                src = o["src"][q]
                S.coll("AllGather", o["dst"][q], src, GROUPS, extra=[o["ogF"][k] for k in range(q * tpc, (q + 1) * tpc)])
            if ti == 12:
                S.coll("AllGather", o["ogH_g"], o["ogH"], GROUPS)
        return after_og

    emit_T(S, "t0", 2048, True, False, dict(hs_src=hs0, nwT=nwT0, ainF=ainF, ainH=ainH, after_ain=after_ain))
    for l in range(nl):
        d = lay[l]
        c = 4 if l % 2 == 0 else 2
        emit_casts(S, d)
        mio = dict(d["m"], ainF_g=ainF_g, ainH_g=ainH_g, ogF=og[c]["ogF"], ogH=og[c]["ogH"], after_og=mk_after_og(c))
        if l % 2 == 0:
            emit_M_gdn(S, "g%d" % l, mio)
        else:
            emit_M_ml(S, "m%d" % l, mio)
        last = l == nl - 1
        tio = dict(d, hs_src=(hs0 if l == 0 else hs_loc), hs_dst=(hs_out if last else hs_loc), ogF_g=og[c]["ogF_g"], ogH_g=og[c]["ogH_g"],
                   keep=keep, gidx=(gidx4 if c == 4 else gidx2), ainF=ainF, ainH=ainH, after_ain=after_ain)
        emit_T(S, "t%d" % (l + 1), 512 * c, False, last, tio)
    S.finish([hs_out])
    return nc, es


def kernel(x, meta_tokens, norm_w, gdn_w_in, gdn_conv_w, gdn_a_log, gdn_dt_bias, gdn_norm_w, gdn_w_out,
           ml_w_in, ml_gate_b, ml_norm_w, ml_w_out, ffn_w_up, ffn_conv_w, ffn_conv_b, ffn_w_down, _nl=4):
    inp = dict(x=x, meta_tokens=meta_tokens, norm_w=norm_w, gdn_w_in=gdn_w_in, gdn_conv_w=gdn_conv_w, gdn_a_log=gdn_a_log,
               gdn_dt_bias=gdn_dt_bias, gdn_norm_w=gdn_norm_w, gdn_w_out=gdn_w_out, ml_w_in=ml_w_in, ml_gate_b=ml_gate_b,
               ml_norm_w=ml_norm_w, ml_w_out=ml_w_out, ffn_w_up=ffn_w_up, ffn_conv_w=ffn_conv_w, ffn_conv_b=ffn_conv_b,
               ffn_w_down=ffn_w_down)
    inp = {k: np.asarray(v, np.float32) for k, v in inp.items()}
    shared = {"cst_g": gdn_consts(), "cst_m": ml_consts(),
              "nwT_0": np.ascontiguousarray(np.stack([_cm(inp["norm_w"][0, 0])] * 4, 1).reshape(128, 32), np.float32)}
    for l in range(_nl):
        t = prep_T(inp, l)
        for k, v in t.items():
            shared["%s_l%d" % (k, l)] = v
    maps = []
    for c in range(8):
        b, r = c // 4, c % 4
        m = dict(shared)
        h = np.zeros((LP, D), np.float32)
        h[XPAD + 48:XPAD + 64] = inp["meta_tokens"]
        h[XPAD + 64:] = inp["x"][b]
        lo = XPAD + 2048 * r
        m["hs0"] = np.ascontiguousarray(h[lo:lo + WIN].T)
        k = np.ones((1, WIN), np.float32)
        if r == 0:
            k[0, :48] = 0.0
        m["keep"] = k
        p = np.arange(128)
        for cc in (4, 2):
            tpc = 8 // cc
            gi = np.zeros((128, 20), np.int32)
            for hg in range(4):
                gi[:, hg * 5] = hg * 512 + r * 128 + p
                for i in range(1, 5):
                    wt = 4 * r + i - 1
                    gi[:, hg * 5 + i] = (wt // tpc) * (4 * tpc * 128) + hg * (tpc * 128) + (wt % tpc) * 128 + p
            m["gidx%d" % cc] = gi
        for l in range(_nl):
            if l % 2 == 0:
                g = prep_M_gdn(inp, l // 2, r)
                m["gw_l%d" % l], m["gcw_l%d" % l], m["ghv_l%d" % l], m["ggnw_l%d" % l] = g["w"], g["cw"], g["hv"], g["gnw"]
            else:
                g = prep_M_ml(inp, l // 2, r)
                m["mw_l%d" % l], m["mgb_l%d" % l], m["mnwv_l%d" % l] = g["w"], g["gb"], g["nwv"]
        maps.append(m)
    if _nl == 1:
        shared.pop("cst_m")
        for m in maps:
            m.pop("cst_m", None)
            m.pop("gidx2", None)
    nc, es = build_fused(_nl)
    res = run_bass_kernel_spmd(nc, maps, core_ids=list(range(8))).results
    out = np.zeros((NB, SEQ, D), np.float32)
    for c in range(8):
        b, r = c // 4, c % 4
        out[b, 2048 * r:2048 * (r + 1)] = res[c]["hs_out"][:, 64:].T
    return out
```
